# Optimizing a Trainium2 kernel written in Bass

```python
import math
import jax
import jax.numpy as jnp
from jax import lax
import numpy as np

D_MODEL = 1024
BATCH = 8
SEQ = 4096
DEPTH = 2

HEAD_DIM = 64
MIX_WIDTH = D_MODEL
N_MIXERS = 4
GROUP_WIDTH = MIX_WIDTH // N_MIXERS
DIL_HEADS = GROUP_WIDTH // HEAD_DIM
DIL_PATTERNS = ((128, 1), (512, 4), (2048, 16))
DIL_MAX_WIN = 2048
Q_BLOCK = 128
NSA_HEADS = GROUP_WIDTH // HEAD_DIM
NSA_CMP_LEN = 32
NSA_CMP_STRIDE = 16
NSA_CMP_HIDDEN = 256
NSA_SEL_LEN = 64
NSA_TOPN = 16
NSA_WIN = 512
NSA_BRANCHES = 3
FORCE_BONUS = 1.0e4
GLA_HEADS = 4
GLA_DV = GROUP_WIDTH // GLA_HEADS
GLA_DK = GLA_DV // 2
GLA_RANK = 16
GLA_TAU = 16.0
GLA_CHUNK = 16
S5_GROUP = 16
S5_GROUPS = GROUP_WIDTH // S5_GROUP
S5_STATE = 64
D_FF = 2752
N_EXPERTS = 8
TOP_K = 2
D_FF_EXPERT = 3584
N_SOFTMAX_HEADS = DIL_HEADS + NSA_HEADS
EPS = 1e-6
NEG_INF = -1e30

PROJ_LAYOUT = (
    ("dil_q", GROUP_WIDTH), ("dil_k", GROUP_WIDTH), ("dil_v", GROUP_WIDTH),
    ("nsa_q", GROUP_WIDTH),
    ("nsa_k_cmp", HEAD_DIM), ("nsa_v_cmp", HEAD_DIM),
    ("nsa_k_slc", HEAD_DIM), ("nsa_v_slc", HEAD_DIM),
    ("nsa_k_win", HEAD_DIM), ("nsa_v_win", HEAD_DIM),
    ("nsa_gate", NSA_HEADS * NSA_BRANCHES),
    ("gla_q", GLA_HEADS * GLA_DK), ("gla_k", GLA_HEADS * GLA_DK), ("gla_v", GROUP_WIDTH),
    ("gla_a", GLA_RANK), ("gla_r", GROUP_WIDTH),
    ("s5_u", GROUP_WIDTH),
)
PROJ_WIDTH = sum(w for _, w in PROJ_LAYOUT)

kernel_name = "hybrid_parallel_mixer_block"


def rms_norm(x, g):
    xf = x.astype(jnp.float32)
    y = xf * lax.rsqrt(jnp.mean(xf * xf, axis=-1, keepdims=True) + EPS)
    return (y * g.astype(jnp.float32)).astype(x.dtype)


def to_heads(t, n):
    b, s, _ = t.shape
    return t.reshape(b, s, n, -1).transpose(0, 2, 1, 3)


def from_heads(t):
    b, h, s, d = t.shape
    return t.transpose(0, 2, 1, 3).reshape(b, s, h * d)


def split_proj(p):
    out = []
    o = 0
    for _, w in PROJ_LAYOUT:
        out.append(p[..., o:o + w])
        o += w
    return out


def alibi_slopes(n):
    return 2.0 ** (-8.0 * jnp.arange(1, n + 1, dtype=jnp.float32) / n)


def dilated_attention(q, k, v, slopes):
    f32 = jnp.float32
    B, H, S, Dh = q.shape
    scale = Dh ** -0.5
    q = q.astype(f32)
    kp = jnp.pad(k.astype(f32), ((0, 0), (0, 0), (DIL_MAX_WIN, 0), (0, 0)))
    vp = jnp.pad(v.astype(f32), ((0, 0), (0, 0), (DIL_MAX_WIN, 0), (0, 0)))
    nb = S // Q_BLOCK
    q_blk = q.reshape(B, H, nb, Q_BLOCK, Dh).transpose(2, 0, 1, 3, 4)
    slope = slopes[None, :, None, None]

    def block(args):
        qb, t0 = args
        tq = t0 + jnp.arange(Q_BLOCK)
        outs, lses = [], []
        for win, dil in DIL_PATTERNS:
            n_keys = win // dil + 1
            j = jnp.arange(n_keys)
            pos = tq[:, None] - j[None, :] * dil
            idx = (pos + DIL_MAX_WIN).reshape(-1)
            kg = jnp.take(kp, idx, axis=2).reshape(B, H, Q_BLOCK, n_keys, Dh)
            vg = jnp.take(vp, idx, axis=2).reshape(B, H, Q_BLOCK, n_keys, Dh)
            s = jnp.einsum('bhqd,bhqkd->bhqk', qb, kg) * scale - slope * (j * dil).astype(f32)
            s = jnp.where((pos >= 0)[None, None], s, -jnp.inf)
            m = jnp.max(s, axis=-1, keepdims=True)
            p = jnp.exp(s - m)
            den = jnp.sum(p, axis=-1, keepdims=True)
            outs.append(jnp.einsum('bhqk,bhqkd->bhqd', p, vg) / den)
            lses.append(m + jnp.log(den))
        w = jax.nn.softmax(jnp.stack(lses, 0), axis=0)
        return jnp.sum(w * jnp.stack(outs, 0), axis=0)

    out = lax.map(block, (q_blk, jnp.arange(nb) * Q_BLOCK))
    return out.transpose(1, 2, 0, 3, 4).reshape(B, H, S, Dh)


def compress_blocks(kv, pos_emb, w1, w2):
    B, S, Dh = kv.shape
    n_cmp = (S - NSA_CMP_LEN) // NSA_CMP_STRIDE + 1
    idx = jnp.arange(n_cmp)[:, None] * NSA_CMP_STRIDE + jnp.arange(NSA_CMP_LEN)[None, :]
    blocks = kv[:, idx] + pos_emb
    h = jax.nn.gelu(blocks.reshape(B, n_cmp, NSA_CMP_LEN * Dh) @ w1)
    return h @ w2


def nsa_attention(q, k_cmp, v_cmp, k_slc, v_slc, k_win, v_win, gate_logits,
                  qk_g, cmp_pos, cmp_w1, cmp_w2, slopes):
    f32 = jnp.float32
    B, S, _ = q.shape
    scale = HEAD_DIM ** -0.5
    slope = slopes[None, :, None, None]
    qh = rms_norm(to_heads(q, NSA_HEADS), qk_g[0]).astype(f32)
    kc = rms_norm(compress_blocks(k_cmp, cmp_pos[0], cmp_w1[0], cmp_w2[0]), qk_g[1]).astype(f32)
    vc = compress_blocks(v_cmp, cmp_pos[1], cmp_w1[1], cmp_w2[1]).astype(f32)
    ks = rms_norm(k_slc, qk_g[2]).astype(f32)
    vs = v_slc.astype(f32)
    kw = rms_norm(k_win, qk_g[3]).astype(f32)
    vw = v_win.astype(f32)
    t = jnp.arange(S)

    n_cmp = kc.shape[1]
    cmp_start = jnp.arange(n_cmp) * NSA_CMP_STRIDE
    dist_c = t[:, None] - (cmp_start + NSA_CMP_LEN - 1)[None, :]
    valid_c = dist_c >= 0
    s_c = jnp.einsum('bhtd,bcd->bhtc', qh, kc) * scale - slope * dist_c.astype(f32)
    p_c = jax.nn.softmax(jnp.where(valid_c, s_c, NEG_INF), axis=-1) * valid_c
    o_cmp = jnp.einsum('bhtc,bcd->bhtd', p_c, vc)

    n_sel_blocks = S // NSA_SEL_LEN
    n_top = min(NSA_TOPN, n_sel_blocks)
    sel_start = jnp.arange(n_sel_blocks) * NSA_SEL_LEN
    cover = ((cmp_start[:, None] < sel_start[None, :] + NSA_SEL_LEN)
             & (cmp_start[:, None] + NSA_CMP_LEN > sel_start[None, :])).astype(f32)
    imp = jnp.einsum('bhtc,cj->btj', p_c, cover)
    cur = t // NSA_SEL_LEN
    j = jnp.arange(n_sel_blocks)
    forced = (j[None, :] == 0) | (j[None, :] == cur[:, None]) | (j[None, :] == cur[:, None] - 1)
    imp = jnp.where(j[None, :] <= cur[:, None], imp + jnp.where(forced, FORCE_BONUS, 0.0), NEG_INF)
    _, sel_idx = lax.top_k(imp, n_top)

    nb = S // Q_BLOCK
    ks_blocks = ks.reshape(B, n_sel_blocks, NSA_SEL_LEN, HEAD_DIM)
    vs_blocks = vs.reshape(B, n_sel_blocks, NSA_SEL_LEN, HEAD_DIM)
    kw_pad = jnp.pad(kw, ((0, 0), (NSA_WIN, 0), (0, 0)))
    vw_pad = jnp.pad(vw, ((0, 0), (NSA_WIN, 0), (0, 0)))
    q_blk = qh.reshape(B, NSA_HEADS, nb, Q_BLOCK, HEAD_DIM).transpose(2, 0, 1, 3, 4)
    idx_blk = sel_idx.reshape(B, nb, Q_BLOCK, n_top).transpose(1, 0, 2, 3)
    offs = jnp.arange(NSA_SEL_LEN)
    gather = jax.vmap(lambda kb, i: kb[i])

    def block(args):
        qb, ib, t0 = args
        tq = t0 + jnp.arange(Q_BLOCK)
        kg = gather(ks_blocks, ib).reshape(B, Q_BLOCK, n_top * NSA_SEL_LEN, HEAD_DIM)
        vg = gather(vs_blocks, ib).reshape(B, Q_BLOCK, n_top * NSA_SEL_LEN, HEAD_DIM)
        pos = (ib[..., None] * NSA_SEL_LEN + offs).reshape(B, Q_BLOCK, n_top * NSA_SEL_LEN)
        dist = tq[None, :, None] - pos
        s = jnp.einsum('bhqd,bqkd->bhqk', qb, kg) * scale - slope * dist[:, None].astype(f32)
        p = jax.nn.softmax(jnp.where((dist >= 0)[:, None], s, NEG_INF), axis=-1)
        o_s = jnp.einsum('bhqk,bqkd->bhqd', p, vg)
        kwb = lax.dynamic_slice_in_dim(kw_pad, t0, Q_BLOCK + NSA_WIN, axis=1)
        vwb = lax.dynamic_slice_in_dim(vw_pad, t0, Q_BLOCK + NSA_WIN, axis=1)
        posw = t0 - NSA_WIN + jnp.arange(Q_BLOCK + NSA_WIN)
        dw = tq[:, None] - posw[None, :]
        mask_w = (dw >= 0) & (dw < NSA_WIN) & (posw[None, :] >= 0)
        sw = jnp.einsum('bhqd,bkd->bhqk', qb, kwb) * scale - slope * dw.astype(f32)
        pw = jax.nn.softmax(jnp.where(mask_w, sw, NEG_INF), axis=-1)
        o_w = jnp.einsum('bhqk,bkd->bhqd', pw, vwb)
        return o_s, o_w

    o_s, o_w = lax.map(block, (q_blk, idx_blk, jnp.arange(nb) * Q_BLOCK))
    o_s = o_s.transpose(1, 2, 0, 3, 4).reshape(B, NSA_HEADS, S, HEAD_DIM)
    o_w = o_w.transpose(1, 2, 0, 3, 4).reshape(B, NSA_HEADS, S, HEAD_DIM)
    g = jax.nn.sigmoid(gate_logits.astype(f32)).reshape(B, S, NSA_HEADS, NSA_BRANCHES).transpose(0, 2, 1, 3)
    o = g[..., 0:1] * o_cmp + g[..., 1:2] * o_s + g[..., 2:3] * o_w
    return from_heads(o)


def gla_attention(q, k, v, a_lr, r, wa2, ba, norm_g):
    f32 = jnp.float32
    B, S, _ = q.shape
    H, dk, dv, C = GLA_HEADS, GLA_DK, GLA_DV, GLA_CHUNK
    n = S // C

    def chunks(t, d):
        return t.astype(f32).reshape(B, n, C, H, d).transpose(0, 3, 1, 2, 4)

    log_a = jax.nn.log_sigmoid((a_lr @ wa2 + ba).astype(f32)) / GLA_TAU
    qc = chunks(q, dk) * dk ** -0.5
    kc = chunks(k, dk)
    vc = chunks(v, dv)
    b = jnp.cumsum(chunks(log_a, dk), axis=3)
    causal = jnp.tril(jnp.ones((C, C), dtype=bool))
    decay = jnp.exp(jnp.where(causal[:, :, None], b[..., :, None, :] - b[..., None, :, :], -jnp.inf))
    attn = jnp.einsum('bhnik,bhnjk,bhnijk->bhnij', qc, kc, decay)
    o_intra = jnp.einsum('bhnij,bhnjv->bhniv', attn, vc)
    b_last = b[..., -1, :]
    kv_chunk = jnp.einsum('bhnjk,bhnjv->bhnkv', kc * jnp.exp(b_last[..., None, :] - b), vc)

    def step(state, inp):
        dl, kvn = inp
        return dl[..., None] * state + kvn, state

    _, prev = lax.scan(step, jnp.zeros((B, H, dk, dv), f32),
                       (jnp.moveaxis(jnp.exp(b_last), 2, 0), jnp.moveaxis(kv_chunk, 2, 0)))
    prev = jnp.moveaxis(prev, 0, 2)
    o_inter = jnp.einsum('bhnik,bhnkv->bhniv', qc * jnp.exp(b), prev)
    o = (o_intra + o_inter).transpose(0, 2, 3, 1, 4).reshape(B, S, H, dv)
    o = rms_norm(o, norm_g)
    return o.reshape(B, S, H * dv) * jax.nn.silu(r.astype(f32))


def _complex_linear_combine(e1, e2):
    a1r, a1i, x1r, x1i = e1
    a2r, a2i, x2r, x2i = e2
    return (a2r * a1r - a2i * a1i, a2r * a1i + a2i * a1r,
            a2r * x1r - a2i * x1i + x2r, a2r * x1i + a2i * x1r + x2i)


def s5_layer(u, a_re, a_im, b_re, b_im, c_re, c_im, d, log_dt, glu_w, glu_b):
    f32 = jnp.float32
    B, S, W = u.shape
    uf = u.astype(f32)
    a_re = a_re.astype(f32)
    a_im = a_im.astype(f32)
    dt = jnp.exp(log_dt.astype(f32))[:, None]
    mag = jnp.exp(dt * a_re)
    abar_re = mag * jnp.cos(dt * a_im)
    abar_im = mag * jnp.sin(dt * a_im)
    den = a_re * a_re + a_im * a_im
    f_re = ((abar_re - 1.0) * a_re + abar_im * a_im) / den
    f_im = (abar_im * a_re - (abar_re - 1.0) * a_im) / den
    b_re = b_re.astype(f32)
    b_im = b_im.astype(f32)
    bb_re = f_re[..., None] * b_re - f_im[..., None] * b_im
    bb_im = f_re[..., None] * b_im + f_im[..., None] * b_re
    ug = uf.reshape(B, S, S5_GROUPS, S5_GROUP)
    bu_re = jnp.einsum('bsgc,gnc->bsgn', ug, bb_re)
    bu_im = jnp.einsum('bsgc,gnc->bsgn', ug, bb_im)
    a_re_t = jnp.broadcast_to(abar_re, bu_re.shape)
    a_im_t = jnp.broadcast_to(abar_im, bu_re.shape)
    _, _, x_re, x_im = lax.associative_scan(_complex_linear_combine, (a_re_t, a_im_t, bu_re, bu_im), axis=1)
    y = (jnp.einsum('bsgn,gcn->bsgc', x_re, c_re.astype(f32))
         - jnp.einsum('bsgn,gcn->bsgc', x_im, c_im.astype(f32)))
    y = y.reshape(B, S, W) + d.astype(f32) * uf
    hg = jax.nn.gelu(y)
    return hg * jax.nn.sigmoid(hg @ glu_w.astype(f32) + glu_b.astype(f32))


def swiglu(x, wg, wu, wd):
    return (jax.nn.silu(x @ wg) * (x @ wu)) @ wd


def moe_swiglu(h, router_w, router_b, wg, wu, wd):
    f32 = jnp.float32
    B, S, D = h.shape
    xt = h.reshape(B * S, D)
    logits = (xt @ router_w).astype(f32) + router_b.astype(f32)
    top_v, top_i = lax.top_k(logits, TOP_K)
    top_w = jax.nn.softmax(top_v, axis=-1)
    combine = jnp.einsum('tk,tke->te', top_w, jax.nn.one_hot(top_i, N_EXPERTS, dtype=f32))
    out = jnp.zeros((B * S, D), f32)
    for e in range(N_EXPERTS):
        out = out + combine[:, e:e + 1] * swiglu(xt, wg[e], wu[e], wd[e]).astype(f32)
    return out.reshape(B, S, D).astype(h.dtype)


def setup_inputs(seed: int = 0) -> dict:
    key = jax.random.key(seed)
    ks = iter(jax.random.split(key, 64))
    f32 = jnp.float32
    L = DEPTH
    nd = (DEPTH + 1) // 2
    nm = DEPTH // 2
    G, N, C = S5_GROUPS, S5_STATE, S5_GROUP

    def nrm(shape, scale):
        return jax.random.normal(next(ks), shape, f32) * scale

    def gain(shape):
        return 1.0 + nrm(shape, 0.02)

    return {
        "x": nrm((BATCH, SEQ, D_MODEL), 1.0),
        "norm1_g": gain((L, D_MODEL)),
        "w_in": nrm((L, D_MODEL, PROJ_WIDTH), D_MODEL ** -0.5),
        "dil_qk_g": gain((L, 2, HEAD_DIM)),
        "nsa_qk_g": gain((L, 4, HEAD_DIM)),
        "nsa_cmp_pos": nrm((L, 2, NSA_CMP_LEN, HEAD_DIM), 0.02),
        "nsa_cmp_w1": nrm((L, 2, NSA_CMP_LEN * HEAD_DIM, NSA_CMP_HIDDEN), (NSA_CMP_LEN * HEAD_DIM) ** -0.5),
        "nsa_cmp_w2": nrm((L, 2, NSA_CMP_HIDDEN, HEAD_DIM), NSA_CMP_HIDDEN ** -0.5),
        "gla_wa2": nrm((L, GLA_RANK, GLA_HEADS * GLA_DK), GLA_RANK ** -0.5),
        "gla_ba": nrm((L, GLA_HEADS * GLA_DK), 0.1),
        "gla_norm_g": gain((L, GLA_DV)),
        "s5_a_re": -0.5 + nrm((L, G, N), 0.01),
        "s5_a_im": math.pi * jnp.arange(N, dtype=f32) + nrm((L, G, N), 0.01),
        "s5_b_re": nrm((L, G, N, C), (2 * C) ** -0.5),
        "s5_b_im": nrm((L, G, N, C), (2 * C) ** -0.5),
        "s5_c_re": nrm((L, G, C, N), (2 * N) ** -0.5),
        "s5_c_im": nrm((L, G, C, N), (2 * N) ** -0.5),
        "s5_d": nrm((L, GROUP_WIDTH), 1.0),
        "s5_log_dt": jax.random.uniform(next(ks), (L, G), f32, math.log(1e-3), math.log(1e-1)),
        "s5_glu_w": nrm((L, GROUP_WIDTH, GROUP_WIDTH), GROUP_WIDTH ** -0.5),
        "s5_glu_b": nrm((L, GROUP_WIDTH), 0.02),
        "out_norm_g": gain((L, MIX_WIDTH)),
        "w_out": nrm((L, MIX_WIDTH, D_MODEL), MIX_WIDTH ** -0.5),
        "norm2_g": gain((L, D_MODEL)),
        "ffn_w_gate": nrm((nd, D_MODEL, D_FF), D_MODEL ** -0.5),
        "ffn_w_up": nrm((nd, D_MODEL, D_FF), D_MODEL ** -0.5),
        "ffn_w_down": nrm((nd, D_FF, D_MODEL), D_FF ** -0.5),
        "moe_router_w": nrm((nm, D_MODEL, N_EXPERTS), D_MODEL ** -0.5),
        "moe_router_b": nrm((nm, N_EXPERTS), 0.01),
        "moe_w_gate": nrm((nm, N_EXPERTS, D_MODEL, D_FF_EXPERT), D_MODEL ** -0.5),
        "moe_w_up": nrm((nm, N_EXPERTS, D_MODEL, D_FF_EXPERT), D_MODEL ** -0.5),
        "moe_w_down": nrm((nm, N_EXPERTS, D_FF_EXPERT, D_MODEL), D_FF_EXPERT ** -0.5),
    }


def reference(x, norm1_g, w_in, dil_qk_g, nsa_qk_g, nsa_cmp_pos, nsa_cmp_w1, nsa_cmp_w2,
              gla_wa2, gla_ba, gla_norm_g, s5_a_re, s5_a_im, s5_b_re, s5_b_im, s5_c_re, s5_c_im,
              s5_d, s5_log_dt, s5_glu_w, s5_glu_b, out_norm_g, w_out, norm2_g,
              ffn_w_gate, ffn_w_up, ffn_w_down, moe_router_w, moe_router_b,
              moe_w_gate, moe_w_up, moe_w_down):
    B, S, _ = x.shape
    slopes = alibi_slopes(N_SOFTMAX_HEADS)
    dil_slopes = slopes[1::2]
    nsa_slopes = slopes[0::2]
    for l in range(DEPTH):
        h = rms_norm(x, norm1_g[l])
        (dq, dk, dv, nq, nkc, nvc, nks, nvs, nkw, nvw, ng,
         gq, gk, gv, ga, gr, su) = split_proj(h @ w_in[l])
        y_dil = from_heads(dilated_attention(
            rms_norm(to_heads(dq, DIL_HEADS), dil_qk_g[l, 0]),
            rms_norm(to_heads(dk, DIL_HEADS), dil_qk_g[l, 1]),
            to_heads(dv, DIL_HEADS), dil_slopes))
        y_nsa = nsa_attention(nq, nkc, nvc, nks, nvs, nkw, nvw, ng, nsa_qk_g[l],
                              nsa_cmp_pos[l], nsa_cmp_w1[l], nsa_cmp_w2[l], nsa_slopes)
        y_gla = gla_attention(gq, gk, gv, ga, gr, gla_wa2[l], gla_ba[l], gla_norm_g[l])
        y_s5 = s5_layer(su, s5_a_re[l], s5_a_im[l], s5_b_re[l], s5_b_im[l], s5_c_re[l], s5_c_im[l],
                        s5_d[l], s5_log_dt[l], s5_glu_w[l], s5_glu_b[l])
        mix = jnp.stack([y_dil.astype(x.dtype), y_nsa.astype(x.dtype),
                         y_gla.astype(x.dtype), y_s5.astype(x.dtype)], axis=-2)
        mix = rms_norm(mix, out_norm_g[l].reshape(N_MIXERS, GROUP_WIDTH)).reshape(B, S, MIX_WIDTH)
        x = x + mix @ w_out[l]
        h = rms_norm(x, norm2_g[l])
        if l % 2 == 0:
            i = l // 2
            x = x + swiglu(h, ffn_w_gate[i], ffn_w_up[i], ffn_w_down[i])
        else:
            i = l // 2
            x = x + moe_swiglu(h, moe_router_w[i], moe_router_b[i], moe_w_gate[i], moe_w_up[i], moe_w_down[i])
    return x
```

```python
from contextlib import ExitStack, contextmanager
import math
import numpy as np
import ml_dtypes
import concourse.bass as bass
import concourse.mybir as mybir
from concourse.bass_utils import run_bass_kernel_spmd

F32 = mybir.dt.float32
BF16 = mybir.dt.bfloat16
ALU = mybir.AluOpType
AF = mybir.ActivationFunctionType
AX = mybir.AxisListType

S = 4096
D = 1024
NT = S // 128
PW = 2460
EPS = 1e-6
D_FF = 2752
N_EXP = 8
D_FFE = 3584
OFF = {}
_o = 0
for _n, _w in (("dil_q", 256), ("dil_k", 256), ("dil_v", 256), ("nsa_q", 256),
               ("nsa_k_cmp", 64), ("nsa_v_cmp", 64), ("nsa_k_slc", 64), ("nsa_v_slc", 64),
               ("nsa_k_win", 64), ("nsa_v_win", 64), ("nsa_gate", 12),
               ("gla_q", 128), ("gla_k", 128), ("gla_v", 256), ("gla_a", 16), ("gla_r", 256),
               ("s5_u", 256)):
    OFF[_n] = _o
    _o += _w
assert _o == PW

NDMA = 24
SEM_LIMIT = 24000

FM_RANGES = [(0, 512), (768, 1216), (1280, 1344), (1420, 1676), (1932, 1948), (2204, 2460)]
TM_RANGES = [(512, 256), (1216, 64), (1344, 76), (1676, 256), (1948, 256)]
TM_STORES = [(512, 256), (1216, 64), (1344, 76), (1676, 256), (1948, 256)]
FM_TILES = []
for _a, _b in FM_RANGES:
    _c = _a
    while _c < _b:
        FM_TILES.append((_c, min(128, _b - _c)))
        _c += 128


class Buf:
    __slots__ = ("name", "w", "r")

    def __init__(self, name=""):
        self.name = name
        self.w = {}
        self.r = {}


class Ctx:
    ENG = ("pe", "act", "dve", "pool", "sp")

    def __init__(self, nc):
        self.nc = nc
        self.eng = {"pe": nc.tensor, "act": nc.scalar, "dve": nc.vector,
                    "pool": nc.gpsimd, "sp": nc.sync}
        self.gen = {e: 0 for e in self.ENG}
        self.sem = {e: nc.alloc_semaphore("s_%s_0" % e) for e in self.ENG}
        self.tick = {e: 0 for e in self.ENG}
        self.seen = {e: {} for e in self.ENG}
        self.dsem = [nc.alloc_semaphore("s_dma_%d" % i) for i in range(NDMA)]
        self.dval = [0] * NDMA
        self.dn = 0
        self.dgen = 0
        self.strict = {"act", "dve", "pool"}
        self.n_ops = 0
        self.rr = 0
        self.last_pool = None

    def _need(self, e, waits, key, val, same_ok):
        if key[0] == "e" and key[1] == e and not same_ok:
            return
        if self.seen[e].get(key, 0) >= val:
            return
        if waits.get(key, 0) < val:
            waits[key] = val

    def _ekey(self, e):
        return ("e", e, self.gen[e])

    def _emit_waits(self, e, waits):
        eng = self.eng[e]
        for key, val in waits.items():
            if key[0] == "d":
                if key[2] != self.dgen:
                    continue
                s = self.dsem[key[1]]
            else:
                if key[2] != self.gen[key[1]]:
                    continue
                s = self.sem[key[1]]
            eng.wait_ge(s, val)
            self.seen[e][key] = val

    def op(self, e, fn, reads=(), writes=()):
        waits = {}
        st = e in self.strict
        for b in reads:
            for key, val in b.w.items():
                self._need(e, waits, key, val, st)
        for b in writes:
            for key, val in b.w.items():
                self._need(e, waits, key, val, st)
            for key, val in b.r.items():
                self._need(e, waits, key, val, False)
        self._emit_waits(e, waits)
        ins = fn()
        self.tick[e] += 1
        ins.then_inc(self.sem[e], 1)
        key = self._ekey(e)
        val = self.tick[e]
        for b in reads:
            b.r[key] = val
        for b in writes:
            b.w[key] = val
            b.r = {}
        self.n_ops += 1
        return ins

    def dma(self, out, in_, reads=(), writes=(), q="sp", **kw):
        e = q
        waits = {}
        for b in reads:
            for key, val in b.w.items():
                self._need(e, waits, key, val, True)
        for b in writes:
            for key, val in b.w.items():
                if key[0] == "d":
                    continue
                self._need(e, waits, key, val, True)
            for key, val in b.r.items():
                self._need(e, waits, key, val, True)
        i = self.dn % NDMA
        self.dn += 1
        dkey = ("d", i, self.dgen)
        if self.dval[i] > 0:
            self._need(e, waits, dkey, self.dval[i], True)
        if e == "pool" and self.last_pool is not None:
            self._need(e, waits, self.last_pool[0], self.last_pool[1], True)
        self._emit_waits(e, waits)
        ins = self.eng[e].dma_start(out=out, in_=in_, **kw)
        self.dval[i] += 16
        ins.then_inc(self.dsem[i], 16)
        if e == "pool":
            self.last_pool = (dkey, self.dval[i])
        for b in reads:
            b.r[dkey] = self.dval[i]
        for b in writes:
            b.w[dkey] = self.dval[i]
        return ins

    def barrier(self):
        for e in self.ENG:
            waits = {}
            for o in self.ENG:
                if o != e and self.tick[o] > 0:
                    self._need(e, waits, self._ekey(o), self.tick[o], True)
            for i in range(NDMA):
                if self.dval[i] > 0:
                    self._need(e, waits, ("d", i, self.dgen), self.dval[i], True)
            self._emit_waits(e, waits)
        for e in self.ENG:
            if self.tick[e] > SEM_LIMIT:
                self.gen[e] += 1
                self.sem[e] = self.nc.alloc_semaphore("s_%s_%d" % (e, self.gen[e]))
                self.tick[e] = 0
        if any(v > SEM_LIMIT for v in self.dval):
            self.dgen += 1
            self.dsem = [self.nc.alloc_semaphore("s_dma_%d_g%d" % (i, self.dgen)) for i in range(NDMA)]
            self.dval = [0] * NDMA

    def evac_engine(self):
        self.rr += 1
        return "act" if (self.rr & 1) else "dve"

    def copy(self, out, in_, reads, writes, e=None):
        nc = self.nc
        e = e or self.evac_engine()
        if e == "act":
            return self.op("act", lambda: nc.scalar.copy(out=out, in_=in_), reads, writes)
        if e == "pool":
            return self.op("pool", lambda: nc.gpsimd.tensor_copy(out=out, in_=in_), reads, writes)
        return self.op("dve", lambda: nc.vector.tensor_copy(out=out, in_=in_), reads, writes)

    def mm(self, out, lhsT, rhs, start, stop, reads, writes, **kw):
        nc = self.nc
        return self.op("pe", lambda: nc.tensor.matmul(out, lhsT=lhsT, rhs=rhs, start=start, stop=stop, **kw),
                       reads, writes)


@contextmanager
def scope(c):
    with ExitStack() as es:
        yield es
        c.barrier()


_UID = [0]


def uniq(name):
    _UID[0] += 1
    return "%s_u%d" % (name, _UID[0])


class Rot:
    def __init__(self, nc, es, name, shape, dtype, n, psum=False):
        self.t = []
        self.b = []
        name = uniq(name)
        for i in range(n):
            nm = "%s_%d" % (name, i)
            if psum:
                t = es.enter_context(nc.psum_tensor(nm, shape, dtype))
            else:
                t = es.enter_context(nc.sbuf_tensor(nm, shape, dtype))
            self.t.append(t)
            self.b.append(Buf(nm))
        self.i = 0

    def next(self):
        k = self.i % len(self.t)
        self.i += 1
        return self.t[k], self.b[k]


def sb(nc, es, name, shape, dtype):
    name = uniq(name)
    return es.enter_context(nc.sbuf_tensor(name, shape, dtype)), Buf(name)


def ps(nc, es, name, shape, dtype):
    name = uniq(name)
    return es.enter_context(nc.psum_tensor(name, shape, dtype)), Buf(name)


def norm_transpose(c, es, x_d, Bx_d, gB, BgB, ident, Bid, hT, BhT, tile0, ntiles, pfx,
                   keep_x=None):
    nc = c.nc
    xt = Rot(nc, es, pfx + "xt", [128, D], F32, 2)
    junk = Rot(nc, es, pfx + "jk", [128, D], BF16, 2)
    hb = Rot(nc, es, pfx + "hb", [128, D], BF16, 2)
    ss = Rot(nc, es, pfx + "ss", [128, 2], F32, 4)
    tp = Rot(nc, es, pfx + "tp", [128, 1024], BF16, 2, psum=True)
    for i in range(ntiles):
        t = tile0 + i
        if keep_x is not None:
            x_t = keep_x[0][:, i, :]
            Bx = keep_x[1]
        else:
            xx, Bx = xt.next()
            x_t = xx[:]
        c.dma(x_t, x_d[t * 128:(t + 1) * 128, :], reads=[Bx_d], writes=[Bx])
        jk, Bjk = junk.next()
        s_, Bs = ss.next()
        c.op("act", lambda: nc.scalar.activation(out=jk[:], in_=x_t, func=AF.Square,
                                                 accum_out=s_[:, 0:1]), [Bx], [Bjk, Bs])
        c.op("act", lambda: nc.scalar.activation(out=s_[:, 1:2], in_=s_[:, 0:1], func=AF.Sqrt,
                                                 bias=gB[:, D:D + 1], scale=1.0 / D), [Bs, BgB], [Bs])
        c.op("dve", lambda: nc.vector.reciprocal(out=s_[:, 1:2], in_=s_[:, 1:2]), [Bs], [Bs])
        h_, Bh = hb.next()
        c.op("dve", lambda: nc.vector.scalar_tensor_tensor(out=h_[:], in0=x_t, scalar=s_[:, 1:2],
                                                           in1=gB[:, 0:D], op0=ALU.mult, op1=ALU.mult),
             [Bx, Bs, BgB], [Bh])
        for g in range(2):
            p_, Bp = tp.next()
            for j in range(4):
                kc = g * 4 + j
                c.op("pe", lambda: nc.tensor.transpose(out=p_[:, j * 128:(j + 1) * 128],
                                                       in_=h_[:, kc * 128:(kc + 1) * 128],
                                                       identity=ident[:]), [Bh, Bid], [Bp])
            c.copy(hT[:, g * 4:(g + 1) * 4, i * 128:(i + 1) * 128],
                   p_[:, 0:512].rearrange("p (j t) -> p j t", j=4), [Bp], [BhT])


def load_gB(c, es, g_row, Bg_d, name):
    nc = c.nc
    gB, BgB = sb(nc, es, name, [128, D + 1], F32)
    c.dma(gB[:, 0:D], g_row.partition_broadcast(128), reads=[Bg_d], writes=[BgB])
    c.op("dve", lambda: nc.vector.memset(gB[:, D:D + 1], EPS), [], [BgB])
    return gB, BgB


def phase_proj(c, T, l):
    nc = c.nc
    with scope(c) as es:
        W, BW = sb(nc, es, "pj_W", [128, 8, PW], BF16)
        ident, Bid = sb(nc, es, "pj_id", [128, 128], BF16)
        c.dma(ident[:], T["ident"][:, :], reads=[T.B["ident"]], writes=[Bid])
        gB, BgB = load_gB(c, es, T["norm1_g"][l], T.B["norm1_g"], "pj_g")
        wv = T["w_in"][l].rearrange("(kc p) n -> p kc n", p=128)
        for kc in range(8):
            c.dma(W[:, kc, :], wv[:, kc, :], reads=[T.B["w_in"]], writes=[BW], q="pool")
        hTs = [sb(nc, es, "pj_hT%d" % i, [128, 8, 2048], BF16) for i in range(2)]
        stf = Rot(nc, es, "pj_stf", [128, 2048], BF16, 2)
        stt = Rot(nc, es, "pj_stt", [128, PW], BF16, 2)
        acc = Rot(nc, es, "pj_acc", [128, 512], F32, 4, psum=True)
        x_d = T["xin%d" % l]
        for half in range(2):
            hT, BhT = hTs[half]
            with scope(c) as es2:
                norm_transpose(c, es2, x_d, T.B["xin%d" % l], gB, BgB, ident, Bid, hT, BhT,
                               half * 16, 16, "pj%d" % half)
            c.barrier()
            for (c0, M) in FM_TILES:
                st, Bst = stf.next()
                for tt in range(4):
                    a_, Ba = acc.next()
                    for k in range(8):
                        c.mm(a_[0:M, :], W[:, k, c0:c0 + M], hT[:, k, tt * 512:(tt + 1) * 512],
                             k == 0, k == 7, [BW, BhT], [Ba])
                    c.copy(st[0:M, tt * 512:(tt + 1) * 512], a_[0:M, :], [Ba], [Bst])
                c.dma(T["projT"][c0:c0 + M, half * 2048:(half + 1) * 2048], st[0:M, :],
                      reads=[Bst], writes=[T.B["projT"]])
            for i in range(16):
                st, Bst = stt.next()
                t = half * 16 + i
                for (c0, N) in TM_RANGES:
                    a_, Ba = acc.next()
                    for k in range(8):
                        c.mm(a_[:, 0:N], hT[:, k, i * 128:(i + 1) * 128], W[:, k, c0:c0 + N],
                             k == 0, k == 7, [BW, BhT], [Ba])
                    c.copy(st[:, c0:c0 + N], a_[:, 0:N], [Ba], [Bst])
                for (c0, N) in TM_STORES:
                    c.dma(T["proj"][t * 128:(t + 1) * 128, c0:c0 + N], st[:, c0:c0 + N], reads=[Bst],
                          writes=[T.B["proj"]])
    c.barrier()


DIL_SLOPES = [2.0 ** -2, 2.0 ** -4, 2.0 ** -6, 2.0 ** -8]
NSA_SLOPES = [2.0 ** -1, 2.0 ** -3, 2.0 ** -5, 2.0 ** -7]
DIL_PATTERNS = ((128, 1), (512, 4), (2048, 16))


def ssl(start, count, step):
    return slice(start, start + step * (count - 1) + 1, step)


def load_col(c, es, name, vec_ap, Bsrc, n, mul=None):
    nc = c.nc
    t, B = sb(nc, es, name, [n, 1], F32)
    c.dma(t[:, 0:1], vec_ap.rearrange("(p o) -> p o", o=1), reads=[Bsrc], writes=[B])
    if mul is not None:
        c.op("act", lambda: nc.scalar.mul(out=t[:], in_=t[:], mul=mul), [B], [B])
    return t, B


def prep_qk(c, es, T, row0, H, gcol, Bg, ones64, Bones, epsc, Beps, dst, Bdst, pfx):
    nc = c.nc
    raw = Rot(nc, es, pfx + "raw", [64, S], BF16, 2)
    sq = Rot(nc, es, pfx + "sq", [64, 512], BF16, 2)
    rs = Rot(nc, es, pfx + "rs", [64, 512], F32, 2)
    pp = Rot(nc, es, pfx + "pp", [64, 512], F32, 2, psum=True)
    for h in range(H):
        r_, Br = raw.next()
        c.dma(r_[:], T["projT"][row0 + h * 64:row0 + (h + 1) * 64, :], reads=[T.B["projT"]], writes=[Br])
        for tt in range(8):
            sl = slice(tt * 512, (tt + 1) * 512)
            q_, Bq = sq.next()
            c.op("act", lambda: nc.scalar.activation(out=q_[:], in_=r_[:, sl], func=AF.Square), [Br], [Bq])
            p_, Bp = pp.next()
            c.mm(p_[:], ones64[:], q_[:], True, True, [Bq, Bones], [Bp])
            s_, Bs = rs.next()
            c.op("act", lambda: nc.scalar.activation(out=s_[:], in_=p_[:], func=AF.Sqrt, bias=epsc[0:64, 0:1],
                                                     scale=1.0 / 64), [Bp, Beps], [Bs])
            c.op("dve", lambda: nc.vector.reciprocal(out=s_[:], in_=s_[:]), [Bs], [Bs])
            c.op("dve", lambda: nc.vector.scalar_tensor_tensor(out=dst[0:64, h, sl], in0=r_[:, sl],
                                                               scalar=gcol[:, 0:1], in1=s_[:],
                                                               op0=ALU.mult, op1=ALU.mult),
                 [Br, Bs, Bg], [Bdst])


def load_consts_small(c, es, T, pfx):
    nc = c.nc
    ones64, Bones = sb(nc, es, pfx + "ones64", [64, 64], BF16)
    c.op("dve", lambda: nc.vector.memset(ones64[:], 1.0), [], [Bones])
    epsc, Beps = sb(nc, es, pfx + "epsc", [128, 1], F32)
    c.op("dve", lambda: nc.vector.memset(epsc[:], EPS), [], [Beps])
    return ones64, Bones, epsc, Beps


def phase_dil(c, T, l):
    nc = c.nc
    with scope(c) as es:
        ones64, Bones, epsc, Beps = load_consts_small(c, es, T, "dl_")
        qT, BqT = sb(nc, es, "dl_qT", [64, 4, S], BF16)
        kT, BkT = sb(nc, es, "dl_kT", [64, 4, S], BF16)
        KB, BKB = sb(nc, es, "dl_KB", [4, 36, 128], BF16)
        c.dma(KB[:], T["c_KBd"][:, :, :], reads=[T.B["c_KBd"]], writes=[BKB])
        QB, BQB = sb(nc, es, "dl_QB", [4, 3, 4, 128], BF16)
        c.dma(QB[:], T["c_QBdil"][:, :, :, :], reads=[T.B["c_QBdil"]], writes=[BQB])
        msk, Bmsk = sb(nc, es, "dl_msk", [128, 3, 128], BF16)
        c.dma(msk[:], T["c_masks"][:, :, :], reads=[T.B["c_masks"]], writes=[Bmsk])
        with scope(c) as es2:
            gq, Bgq = load_col(c, es2, "dl_gq", T["dil_qk_g"][l, 0], T.B["dil_qk_g"], 64, mul=0.125)
            gk, Bgk = load_col(c, es2, "dl_gk", T["dil_qk_g"][l, 1], T.B["dil_qk_g"], 64)
            with scope(c) as es3:
                prep_qk(c, es3, T, OFF["dil_q"], 4, gq, Bgq, ones64, Bones, epsc, Beps, qT, BqT, "dlq")
            c.barrier()
            with scope(c) as es3:
                prep_qk(c, es3, T, OFF["dil_k"], 4, gk, Bgk, ones64, Bones, epsc, Beps, kT, BkT, "dlk")
        T.dbg(c, "dbg_qT", qT[:], (64, 4, S), BF16, BqT)
        T.dbg(c, "dbg_kT", kT[:], (64, 4, S), BF16, BkT)
        Vp = []
        for pi in range(3):
            v_, Bv = sb(nc, es, "dl_V%d" % pi, [128, 32, 4, 66], BF16)
            c.op("dve", lambda: nc.vector.memset(v_[:], 1.0), [], [Bv])
            Vp.append((v_, Bv))
        with scope(c) as es2:
            stg = Rot(nc, es2, "dl_vst", [128, 8, 256], BF16, 2)
            for pi, (win, d) in enumerate(DIL_PATTERNS):
                v_, Bv = Vp[pi]
                bpc = 32 // d
                for r in range(d):
                    for b0 in range(0, bpc, 8):
                        nb = min(8, bpc - b0)
                        s_, Bs = stg.next()
                        src = T["proj"][ssl(r + d * 128 * b0, 128 * nb, d), OFF["dil_v"]:OFF["dil_v"] + 256]
                        c.dma(s_[:, 0:nb, :], src.rearrange("(b k) c -> k b c", k=128),
                              reads=[T.B["proj"]], writes=[Bs])
                        blk0 = r * bpc + b0
                        c.copy(v_[:, blk0:blk0 + nb, :, 0:64],
                               s_[:, 0:nb, :].rearrange("k b (h e) -> k b h e", h=4), [Bs], [Bv])
        c.barrier()
        for pi in range(3):
            T.dbg(c, "dbg_V%d" % pi, Vp[pi][0][:], (128, 32, 4, 66), BF16, Vp[pi][1])
        Sps = Rot(nc, es, "dl_S", [128, 512], F32, 3, psum=True)
        Ops = Rot(nc, es, "dl_O", [128, 512], F32, 2, psum=True)
        Pt = Rot(nc, es, "dl_P", [128, 512], BF16, 5)
        Mt = Rot(nc, es, "dl_M", [128, 512], F32, 3)
        Osb = Rot(nc, es, "dl_Osb", [128, 264], F32, 3)
        SKEW = 2
        tasks = []
        for pi, (win, d) in enumerate(DIL_PATTERNS):
            bpc = 32 // d
            for r in range(d):
                for bi in range(bpc):
                    kbs = [bi - 1, bi] if bi >= 1 else [bi]
                    for i_, kbi in enumerate(kbs):
                        tasks.append((pi, d, bpc, r, bi, kbi, i_ == 0, i_ == len(kbs) - 1))
        st = {}
        cur_o = [None]

        def stage_a(ti):
            pi, d, bpc, r, bi, kbi, is_first, is_last = tasks[ti]
            qsl = ssl(r + d * 128 * bi, 128, d)
            ksl = ssl(r + d * 128 * kbi, 128, d)
            s_, Bs = Sps.next()
            for h in range(4):
                c.mm(s_[:, h * 128:(h + 1) * 128], kT[:, h, ksl], qT[:, h, qsl], True, False,
                     [BkT, BqT], [Bs])
                c.mm(s_[:, h * 128:(h + 1) * 128], KB[:, 32 + kbi - bi, :],
                     QB[:, pi, h, :], False, True, [BKB, BQB], [Bs])
            p_, Bp = Pt.next()
            mi = 0 if kbi == bi else 1
            m_, Bm = Mt.next()
            c.op("dve", lambda: nc.vector.tensor_tensor(
                out=m_[:].rearrange("k (h q) -> k h q", h=4),
                in0=s_[:].rearrange("k (h q) -> k h q", h=4),
                in1=msk[:, mi:mi + 1, :].to_broadcast([128, 4, 128]), op=ALU.add), [Bs, Bmsk], [Bm])
            c.op("act", lambda: nc.scalar.activation(out=p_[:], in_=m_[:], func=AF.Exp), [Bm], [Bp])
            st[ti] = (p_, Bp)

        def stage_c(ti):
            pi, d, bpc, r, bi, kbi, is_first, is_last = tasks[ti]
            v_, Bv = Vp[pi]
            kblk = r * bpc + kbi
            p_, Bp = st.pop(ti)
            if is_first:
                cur_o[0] = Ops.next()
            o_, Bo = cur_o[0]
            for h in range(4):
                c.mm(o_[:, h * 66:(h + 1) * 66], p_[:, h * 128:(h + 1) * 128], v_[:, kblk, h, :],
                     is_first and h == 0, False, [Bp, Bv], [Bo], skip_group_check=True)
            if is_last:
                ob, Bob = Osb.next()
                c.copy(ob[:], o_[:, 0:264], [Bo], [Bob])
                dst = T["dacc"][pi, ssl(r + d * 128 * bi, 128, d), :]
                c.dma(dst, ob[:], reads=[Bob], writes=[T.B["dacc"]])

        for ti in range(len(tasks) + SKEW):
            if ti < len(tasks):
                stage_a(ti)
            if ti - SKEW >= 0:
                stage_c(ti - SKEW)
    c.barrier()
    with scope(c) as es:
        a3 = Rot(nc, es, "dc_a", [128, 3, 264], F32, 3)
        rc = Rot(nc, es, "dc_r", [128, 4], F32, 3)
        yo = Rot(nc, es, "dc_y", [128, 256], F32, 3)
        for t in range(NT):
            a_, Ba = a3.next()
            c.dma(a_[:], T["dacc"][:, t * 128:(t + 1) * 128, :].rearrange("p t c -> t p c"),
                  reads=[T.B["dacc"]], writes=[Ba])
            c.op("dve", lambda: nc.vector.tensor_tensor(out=a_[:, 0, :], in0=a_[:, 0, :], in1=a_[:, 1, :], op=ALU.add),
                 [Ba], [Ba])
            c.op("dve", lambda: nc.vector.tensor_tensor(out=a_[:, 0, :], in0=a_[:, 0, :], in1=a_[:, 2, :], op=ALU.add),
                 [Ba], [Ba])
            av = a_[:, 0, :].rearrange("t (h e) -> t h e", h=4)
            r_, Br = rc.next()
            c.op("dve", lambda: nc.vector.reciprocal(out=r_[:].rearrange("t (h o) -> t h o", o=1), in_=av[:, :, 64:65]),
                 [Ba], [Br])
            y_, By = yo.next()
            c.op("dve", lambda: nc.vector.tensor_tensor(
                out=y_[:].rearrange("t (h e) -> t h e", h=4), in0=av[:, :, 0:64],
                in1=r_[:].rearrange("t (h o) -> t h o", o=1).to_broadcast([128, 4, 64]), op=ALU.mult),
                [Ba, Br], [By])
            c.dma(T["mix"][t * 128:(t + 1) * 128, 0:256], y_[:], reads=[By], writes=[T.B["mix"]])
    c.barrier()


NSA_STAGE = 99


def phase_nsa(c, T, l):
    _phase_nsa(c, T, l)
    c.barrier()


def _phase_nsa(c, T, l):
    nc = c.nc
    with scope(c) as es:
        ones64, Bones, epsc, Beps = load_consts_small(c, es, T, "ns_")
        ident, Bid = sb(nc, es, "ns_id", [128, 128], BF16)
        c.dma(ident[:], T["ident"][:, :], reads=[T.B["ident"]], writes=[Bid])
        qT, BqT = sb(nc, es, "ns_Q2s", [128, 4, S], BF16)
        Q2w, BQ2w = sb(nc, es, "ns_Q2w", [128, 4, S], BF16)
        c.dma(Q2w[64:128, :, :], T["c_QW"][:, :, :], reads=[T.B["c_QW"]], writes=[BQ2w])
        ksT, BksT = sb(nc, es, "ns_Ks2", [128, 1, S], BF16)
        c.dma(ksT[64:128, 0, :], T["c_K2sel"][:, :], reads=[T.B["c_K2sel"]], writes=[BksT])
        kwT, BkwT = sb(nc, es, "ns_Kw2", [128, 1, S], BF16)
        c.dma(kwT[64:128, 0, :], T["c_K2win"][:, :], reads=[T.B["c_K2win"]], writes=[BkwT])
        Asel, BAsel = sb(nc, es, "ns_Asel", [128, 32, 4], F32)
        c.dma(Asel[:], T["c_Asel"][:, :, :], reads=[T.B["c_Asel"]], writes=[BAsel])
        Vs, BVs = sb(nc, es, "ns_Vsh", [128, 32, 4, 66], BF16)
        Vw, BVw = sb(nc, es, "ns_Vwh", [128, 32, 4, 66], BF16)
        with scope(c) as es2:
            Ev, BEv = sb(nc, es2, "ns_Ev", [128, 2, 4], F32)
            c.dma(Ev[:], T["c_Ev"][:, :, :], reads=[T.B["c_Ev"]], writes=[BEv])
            for vi, (vh_, Bvh, nm) in enumerate(((Vs, BVs, "nsa_v_slc"), (Vw, BVw, "nsa_v_win"))):
                v_, Bv = sb(nc, es2, "ns_Vst%d" % vi, [128, 32, 66], BF16)
                c.op("dve", lambda: nc.vector.memset(v_[:], 1.0), [], [Bv])
                c.dma(v_[:, :, 0:64], T["proj"][:, OFF[nm]:OFF[nm] + 64].rearrange("(b k) e -> k b e", k=128),
                      reads=[T.B["proj"]], writes=[Bv])
                for h in range(4):
                    c.op("dve", lambda: nc.vector.tensor_scalar(out=vh_[:, :, h, :], in0=v_[:], scalar1=Ev[:, vi, h:h + 1],
                                                                scalar2=None, op0=ALU.mult), [Bv, BEv], [Bvh])
        kcT, BkcT = sb(nc, es, "ns_kcT", [64, 256], BF16)
        Vc, BVc = sb(nc, es, "ns_Vc", [128, 2, 130], BF16)
        c.op("dve", lambda: nc.vector.memset(Vc[:], 0.0), [], [BVc])
        c.op("dve", lambda: nc.vector.memset(Vc[:, :, 128:130], 1.0), [], [BVc])
        c.dma(Vc[:, :, 64:128], T["c_cover"][:, :, :], reads=[T.B["c_cover"]], writes=[BVc])
        with scope(c) as es2:
            g0, Bg0 = load_col(c, es2, "ns_g0", T["nsa_qk_g"][l, 0], T.B["nsa_qk_g"], 64, mul=0.125)
            g2, Bg2 = load_col(c, es2, "ns_g2", T["nsa_qk_g"][l, 2], T.B["nsa_qk_g"], 64)
            g3, Bg3 = load_col(c, es2, "ns_g3", T["nsa_qk_g"][l, 3], T.B["nsa_qk_g"], 64)
            with scope(c) as es3:
                prep_qk(c, es3, T, OFF["nsa_q"], 4, g0, Bg0, ones64, Bones, epsc, Beps, qT, BqT, "nsq")
            c.barrier()
            for h in range(4):
                c.op("act", lambda: nc.scalar.copy(out=Q2w[0:64, h, :], in_=qT[0:64, h, :]), [BqT], [BQ2w])
            with scope(c) as es3:
                prep_qk(c, es3, T, OFF["nsa_k_slc"], 1, g2, Bg2, ones64, Bones, epsc, Beps, ksT, BksT, "nss")
            with scope(c) as es3:
                prep_qk(c, es3, T, OFF["nsa_k_win"], 1, g3, Bg3, ones64, Bones, epsc, Beps, kwT, BkwT, "nsw")
            c.barrier()
        c.barrier()
        if NSA_STAGE <= 1:
            return
        with scope(c) as es2:
            g1, Bg1 = load_col(c, es2, "ns_g1", T["nsa_qk_g"][l, 1], T.B["nsa_qk_g"], 64)
            raw, Braw = sb(nc, es2, "ns_raw", [64, 2, S], BF16)
            c.dma(raw[:], T["projT"][OFF["nsa_k_cmp"]:OFF["nsa_k_cmp"] + 128, :].rearrange("(i d) t -> d i t", i=2),
                  reads=[T.B["projT"]], writes=[Braw])
            w1, Bw1 = sb(nc, es2, "ns_w1", [64, 2, 32, 256], BF16)
            w2, Bw2 = sb(nc, es2, "ns_w2", [128, 2, 2, 64], BF16)
            posT, BposT = sb(nc, es2, "ns_posT", [64, 2, 32], BF16)
            for i in range(2):
                c.dma(w1[:, i, :, :], T["nsa_cmp_w1"][l, i].rearrange("(l d) h -> d l h", d=64),
                      reads=[T.B["nsa_cmp_w1"]], writes=[Bw1], q="pool")
                c.dma(w2[:, i, :, :], T["nsa_cmp_w2"][l, i].rearrange("(hc p) e -> p hc e", p=128),
                      reads=[T.B["nsa_cmp_w2"]], writes=[Bw2], q="pool")
                c.dma(posT[:, i, :], T["nsa_cmp_pos"][l, i].rearrange("l d -> d l"),
                      reads=[T.B["nsa_cmp_pos"]], writes=[BposT], q="pool", allow_slow_non_contiguous=True)
            Hps = Rot(nc, es2, "ns_Hps", [128, 512], F32, 2, psum=True)
            bps = Rot(nc, es2, "ns_bps", [128, 512], F32, 2, psum=True)
            b1 = Rot(nc, es2, "ns_b1", [128, 2], F32, 2)
            zt = Rot(nc, es2, "ns_z", [128, 256], F32, 2)
            ut = Rot(nc, es2, "ns_u", [128, 256], F32, 2)
            G, BG = sb(nc, es2, "ns_G", [128, 2, 2, 256], BF16)
            c.op("dve", lambda: nc.vector.memset(G[:], 0.0), [], [BG])
            for i in range(2):
                for hc in range(2):
                    h_, Bh = Hps.next()
                    for ll in range(32):
                        c.mm(h_[:, 0:255], w1[:, i, ll, hc * 128:(hc + 1) * 128], raw[:, i, ssl(ll, 255, 16)],
                             ll == 0, ll == 31, [Bw1, Braw], [Bh])
                    bp, Bbp = bps.next()
                    for ll in range(32):
                        c.mm(bp[:, 0:2], w1[:, i, ll, hc * 128:(hc + 1) * 128], posT[:, i, ll:ll + 1].to_broadcast([64, 2]),
                             ll == 0, ll == 31, [Bw1, BposT], [Bbp])
                    b_, Bb = b1.next()
                    c.op("dve", lambda: nc.vector.tensor_copy(out=b_[:], in_=bp[:, 0:2]), [Bbp], [Bb])
                    z_, Bz = zt.next()
                    c.op("act", lambda: nc.scalar.activation(out=z_[:, 0:255], in_=h_[:, 0:255], func=AF.Identity,
                                                             bias=b_[:, 0:1]), [Bh, Bb], [Bz])
                    u_, Bu = ut.next()
                    c.op("dve", lambda: nc.vector.tensor_tensor(out=u_[:, 0:255], in0=z_[:, 0:255], in1=z_[:, 0:255],
                                                                op=ALU.mult), [Bz], [Bu])
                    c.op("dve", lambda: nc.vector.tensor_scalar(out=u_[:, 0:255], in0=u_[:, 0:255], scalar1=0.044715,
                                                                scalar2=1.0, op0=ALU.mult, op1=ALU.add), [Bu], [Bu])
                    c.op("dve", lambda: nc.vector.tensor_tensor(out=u_[:, 0:255], in0=u_[:, 0:255], in1=z_[:, 0:255],
                                                                op=ALU.mult), [Bu, Bz], [Bu])
                    c.op("act", lambda: nc.scalar.activation(out=u_[:, 0:255], in_=u_[:, 0:255], func=AF.Sigmoid,
                                                             scale=1.5957691216057308), [Bu], [Bu])
                    c.op("dve", lambda: nc.vector.tensor_tensor(out=G[:, i, hc, 0:255], in0=u_[:, 0:255],
                                                                in1=z_[:, 0:255], op=ALU.mult), [Bu, Bz], [BG])
            T.dbg(c, "dbg_w1", w1[:], (64, 2, 32, 256), BF16, Bw1)
            T.dbg(c, "dbg_posT", posT[:], (64, 2, 32), BF16, BposT)
            T.dbg(c, "dbg_G", G[:], (128, 2, 2, 256), BF16, BG)
            T.dbg(c, "dbg_w2", w2[:], (128, 2, 2, 64), BF16, Bw2)
            kp, Bkp = Hps.next()
            for hc in range(2):
                c.mm(kp[0:64, 0:256], w2[:, 0, hc, :], G[:, 0, hc, :], hc == 0, hc == 1, [Bw2, BG], [Bkp])
            kraw, Bkraw = sb(nc, es2, "ns_kraw", [64, 256], F32)
            c.op("dve", lambda: nc.vector.tensor_copy(out=kraw[:], in_=kp[0:64, 0:256]), [Bkp], [Bkraw])
            ksq, Bksq = sb(nc, es2, "ns_ksq", [64, 256], BF16)
            c.op("act", lambda: nc.scalar.activation(out=ksq[:], in_=kraw[:], func=AF.Square), [Bkraw], [Bksq])
            sp_, Bsp = bps.next()
            c.mm(sp_[0:64, 0:256], ones64[:], ksq[:], True, True, [Bksq, Bones], [Bsp])
            krs, Bkrs = sb(nc, es2, "ns_krs", [64, 256], F32)
            c.op("act", lambda: nc.scalar.activation(out=krs[:], in_=sp_[0:64, 0:256], func=AF.Sqrt,
                                                     bias=epsc[0:64, 0:1], scale=1.0 / 64), [Bsp, Beps], [Bkrs])
            c.op("dve", lambda: nc.vector.reciprocal(out=krs[:], in_=krs[:]), [Bkrs], [Bkrs])
            c.op("dve", lambda: nc.vector.scalar_tensor_tensor(out=kcT[:], in0=kraw[:], scalar=g1[:, 0:1], in1=krs[:],
                                                               op0=ALU.mult, op1=ALU.mult), [Bkraw, Bkrs, Bg1], [BkcT])
            for ct in range(2):
                rows = 128 if ct == 0 else 127
                vp_, Bvp = Hps.next()
                for hc in range(2):
                    c.mm(vp_[0:rows, 0:64], G[:, 1, hc, ct * 128:ct * 128 + rows], w2[:, 1, hc, :], hc == 0, hc == 1,
                         [BG, Bw2], [Bvp])
                c.op("dve", lambda: nc.vector.tensor_copy(out=Vc[0:rows, ct, 0:64], in_=vp_[0:rows, 0:64]), [Bvp], [BVc])
        c.barrier()
        KBc, BKBc = sb(nc, es, "ns_KBc", [4, 32, 128], BF16)
        c.dma(KBc[:], T["c_KBc"][:, :, :], reads=[T.B["c_KBc"]], writes=[BKBc])
        QBc, BQBc = sb(nc, es, "ns_QBc", [4, 4, 128], BF16)
        c.dma(QBc[:], T["c_QBc"][:, :, :], reads=[T.B["c_QBc"]], writes=[BQBc])
        cmask, Bcmask = sb(nc, es, "ns_cmask", [128, 17, 128], BF16)
        c.dma(cmask[:], T["c_cmask"][:, :, :], reads=[T.B["c_cmask"]], writes=[Bcmask])
        msk, Bmsk = sb(nc, es, "ns_msk", [128, 3, 128], BF16)
        c.dma(msk[:], T["c_masks"][:, :, :], reads=[T.B["c_masks"]], writes=[Bmsk])
        adj, Badj = sb(nc, es, "ns_adj", [128, 32, 64], F32)
        c.dma(adj[:], T["c_adj"][:, :, :], reads=[T.B["c_adj"]], writes=[Badj])
        gates, Bgates = sb(nc, es, "ns_gates", [128, 32, 12], F32)
        yacc, Byacc = sb(nc, es, "ns_yacc", [128, 32, 256], F32)
        with scope(c) as es2:
            gst, Bgst = sb(nc, es2, "ns_gst", [128, 32, 12], BF16)
            c.dma(gst[:], T["proj"][:, OFF["nsa_gate"]:OFF["nsa_gate"] + 12].rearrange("(b k) e -> k b e", k=128),
                  reads=[T.B["proj"]], writes=[Bgst])
            c.op("act", lambda: nc.scalar.activation(out=gates[:], in_=gst[:], func=AF.Sigmoid), [Bgst], [Bgates])
        c.barrier()
        T.dbg(c, "dbg_kcT", kcT[:], (64, 256), BF16, BkcT)
        T.dbg(c, "dbg_Vc", Vc[:], (128, 2, 130), BF16, BVc)
        if NSA_STAGE <= 2:
            return
        Sps = Rot(nc, es, "ns_S", [128, 512], F32, 3, psum=True)
        Ops = Rot(nc, es, "ns_O", [128, 512], F32, 2, psum=True)
        Tps = Rot(nc, es, "ns_T", [128, 256], BF16, 1, psum=True)
        Pt = Rot(nc, es, "ns_P", [128, 512], BF16, 5)
        Mt = Rot(nc, es, "ns_M", [128, 512], F32, 2)
        Ocs = Rot(nc, es, "ns_Oc", [128, 4, 130], F32, 2)
        Osb = Rot(nc, es, "ns_Osb", [128, 264], F32, 2)
        sm = Rot(nc, es, "ns_sm", [128, 16], F32, 3)
        impt = Rot(nc, es, "ns_imp", [128, 64], F32, 2)
        imp2 = Rot(nc, es, "ns_imp2", [128, 64], F32, 2)
        mx = Rot(nc, es, "ns_mx", [128, 16], F32, 2)
        selb = Rot(nc, es, "ns_selb", [128, 128], BF16, 2)
        for _i in range(2):
            c.op("dve", lambda: nc.vector.memset(selb.t[_i][:], 0.0), [], [selb.b[_i]])
        tmpy = Rot(nc, es, "ns_tmpy", [128, 256], F32, 2)

        def softmax_tile(s_, Bs, mask_ap, Bmask):
            p_, Bp = Pt.next()
            if mask_ap is not None:
                m_, Bm = Mt.next()
                c.op("dve", lambda: nc.vector.tensor_tensor(
                    out=m_[:].rearrange("k (h q) -> k h q", h=4), in0=s_[:].rearrange("k (h q) -> k h q", h=4),
                    in1=mask_ap.to_broadcast([128, 4, 128]), op=ALU.add), [Bs, Bmask], [Bm])
                c.op("act", lambda: nc.scalar.activation(out=p_[:], in_=m_[:], func=AF.Exp), [Bm], [Bp])
            else:
                c.op("act", lambda: nc.scalar.activation(out=p_[:], in_=s_[:], func=AF.Exp), [Bs], [Bp])
            return p_, Bp

        def finish_branch(o_ap, Bo, bq, br, first):
            num, den = o_ap
            s_, Bs_ = sm.next()
            c.op("dve", lambda: nc.vector.tensor_scalar(out=s_[:, 0:4].rearrange("t (h o) -> t h o", o=1), in0=den,
                                                        scalar1=1e-30, scalar2=None, op0=ALU.max), [Bo], [Bs_])
            c.op("dve", lambda: nc.vector.reciprocal(out=s_[:, 4:8], in_=s_[:, 0:4]), [Bs_], [Bs_])
            c.op("dve", lambda: nc.vector.tensor_tensor(out=s_[:, 8:12], in0=s_[:, 4:8], in1=gates[:, bq, ssl(br, 4, 3)],
                                                        op=ALU.mult), [Bs_, Bgates], [Bs_])
            wb = s_[:, 8:12].rearrange("t (h o) -> t h o", o=1).to_broadcast([128, 4, 64])
            yv = yacc[:, bq, :].rearrange("t (h e) -> t h e", h=4)
            if first:
                c.op("dve", lambda: nc.vector.tensor_tensor(out=yv, in0=num, in1=wb, op=ALU.mult), [Bo, Bs_], [Byacc])
            else:
                t_, Bt = tmpy.next()
                tv = t_[:].rearrange("t (h e) -> t h e", h=4)
                c.op("dve", lambda: nc.vector.tensor_tensor(out=tv, in0=num, in1=wb, op=ALU.mult), [Bo, Bs_], [Bt])
                c.op("dve", lambda: nc.vector.tensor_tensor(out=yacc[:, bq, :], in0=yacc[:, bq, :], in1=t_[:],
                                                            op=ALU.add), [Bt, Byacc], [Byacc])
            return s_, Bs_

        for bq in range(NT):
            cts = [0] if bq < 16 else [0, 1]
            oA, BoA = Ops.next()
            oB, BoB = Ops.next()
            first = True
            for ct in cts:
                m = bq - 16 * ct
                s_, Bs = Sps.next()
                s4 = s_[:].rearrange("k (h q) -> k h q", h=4)
                c.mm(s4, kcT[:, ct * 128:(ct + 1) * 128], qT[0:64, :, bq * 128:(bq + 1) * 128],
                     True, False, [BkcT, BqT], [Bs])
                c.mm(s4, KBc[:, m, :], QBc[:, :, :], False, True, [BKBc, BQBc], [Bs])
                p_, Bp = softmax_tile(s_, Bs, cmask[:, m:m + 1, :] if m <= 16 else None, Bcmask)
                for h in range(4):
                    o_, Bo = (oA, BoA) if h < 2 else (oB, BoB)
                    hh = h % 2
                    c.mm(o_[:, hh * 130:(hh + 1) * 130], p_[:, h * 128:(h + 1) * 128], Vc[:, ct, :],
                         first and hh == 0, False, [Bp, BVc], [Bo], skip_group_check=True)
                first = False
            oc, Boc = Ocs.next()
            c.copy(oc[:, 0:2, :], oA[:, 0:260].rearrange("t (h e) -> t h e", h=2), [BoA], [Boc])
            c.copy(oc[:, 2:4, :], oB[:, 0:260].rearrange("t (h e) -> t h e", h=2), [BoB], [Boc])
            s_, Bs_ = finish_branch((oc[:, :, 0:64], oc[:, :, 128:129]), Boc, bq, 0, True)
            im, Bim = impt.next()
            c.op("dve", lambda: nc.vector.scalar_tensor_tensor(out=im[:], in0=oc[:, 0, 64:128], scalar=s_[:, 4:5],
                                                               in1=adj[:, bq, :], op0=ALU.mult, op1=ALU.add),
                 [Boc, Bs_, Badj], [Bim])
            for h in range(1, 4):
                c.op("dve", lambda: nc.vector.scalar_tensor_tensor(out=im[:], in0=oc[:, h, 64:128], scalar=s_[:, 4 + h:5 + h],
                                                                   in1=im[:], op0=ALU.mult, op1=ALU.add),
                     [Boc, Bs_, Bim], [Bim])
            T.dbg(c, "dbg_imp%d" % bq, im[:], (128, 64), F32, Bim)
            mx_, Bmx = mx.next()
            c.op("dve", lambda: nc.vector.max(out=mx_[:, 0:8], in_=im[:]), [Bim], [Bmx])
            i2, Bi2 = imp2.next()
            c.op("dve", lambda: nc.vector.match_replace(out=i2[:], in_to_replace=mx_[:, 0:8], in_values=im[:],
                                                        imm_value=-3.0e38), [Bim, Bmx], [Bi2])
            c.op("dve", lambda: nc.vector.max(out=mx_[:, 8:16], in_=i2[:]), [Bi2], [Bmx])
            sb_, Bsb = selb.next()
            c.op("dve", lambda: nc.vector.tensor_scalar(out=sb_[:, 64:128], in0=im[:], scalar1=mx_[:, 15:16], scalar2=None,
                                                        op0=ALU.is_ge), [Bim, Bmx], [Bsb])
            tp_, Btp = Tps.next()
            c.op("pe", lambda: nc.tensor.transpose(out=tp_[:, 0:128], in_=sb_[:], identity=ident[:]), [Bsb, Bid], [Btp])
            for h in range(4):
                c.op("dve", lambda: nc.vector.tensor_scalar(
                    out=qT[64:128, h, bq * 128:(bq + 1) * 128], in0=tp_[64:128, 0:128], scalar1=Asel[64:128, bq, h:h + 1],
                    scalar2=-30000.0, op0=ALU.mult, op1=ALU.add), [Btp, BAsel], [BqT])
        T.dbg(c, "dbg_ycmp", yacc[:], (128, 32, 256), F32, Byacc)
        if NSA_STAGE <= 3:
            return
        SKEW = 2
        tasks = []
        for bq in range(NT):
            for br in (1, 2):
                kbs = list(range(0, bq + 1)) if br == 1 else list(range(max(0, bq - 4), bq + 1))
                for i_, kb in enumerate(kbs):
                    tasks.append((bq, br, kb, i_ == 0, i_ == len(kbs) - 1))
        opnd = {1: (ksT, BksT, Vs, BVs, qT, BqT), 2: (kwT, BkwT, Vw, BVw, Q2w, BQ2w)}
        st = {}
        cur_o = [None]

        def stage_a(ti):
            bq, br, kb, is_first, is_last = tasks[ti]
            kT_, BkT_, V_, BV_, Q_, BQ_ = opnd[br]
            s_, Bs = Sps.next()
            s4 = s_[:].rearrange("k (h q) -> k h q", h=4)
            c.mm(s4, kT_[:, 0, kb * 128:(kb + 1) * 128], Q_[:, :, bq * 128:(bq + 1) * 128], True, True, [BkT_, BQ_], [Bs])
            mask_ap = None
            if kb == bq:
                mask_ap = msk[:, 0:1, :]
            elif br == 2 and kb == bq - 4:
                mask_ap = msk[:, 2:3, :]
            st[ti] = softmax_tile(s_, Bs, mask_ap, Bmsk)

        def stage_c(ti):
            bq, br, kb, is_first, is_last = tasks[ti]
            kT_, BkT_, V_, BV_, Q_, BQ_ = opnd[br]
            p_, Bp = st.pop(ti)
            if is_first:
                cur_o[0] = Ops.next()
            o_, Bo = cur_o[0]
            for h in range(4):
                c.mm(o_[:, h * 66:(h + 1) * 66], p_[:, h * 128:(h + 1) * 128], V_[:, kb, h, :],
                     is_first and h == 0, False, [Bp, BV_], [Bo], skip_group_check=True)
            if is_last:
                ob, Bob = Osb.next()
                c.copy(ob[:], o_[:, 0:264], [Bo], [Bob])
                ov = ob[:].rearrange("t (h e) -> t h e", h=4)
                finish_branch((ov[:, :, 0:64], ov[:, :, 64:65]), Bob, bq, br, False)

        for ti in range(len(tasks) + SKEW):
            if ti < len(tasks):
                stage_a(ti)
            if ti - SKEW >= 0:
                stage_c(ti - SKEW)
        c.dma(T["mix"][:, 256:512].rearrange("(b q) e -> q b e", q=128), yacc[:], reads=[Byacc], writes=[T.B["mix"]])
    c.barrier()


GLA_STAGE = 99


def phase_gla(c, T, l):
    _phase_gla(c, T, l)
    c.barrier()


def _phase_gla(c, T, l):
    nc = c.nc
    with scope(c) as es:
        ident, Bid = sb(nc, es, "gl_id", [128, 128], BF16)
        c.dma(ident[:], T["ident"][:, :], reads=[T.B["ident"]], writes=[Bid])
        m01, Bm01 = sb(nc, es, "gl_m01", [128, 128], BF16)
        c.dma(m01[:], T["c_mask01"][:, :], reads=[T.B["c_mask01"]], writes=[Bm01])
        epsc, Beps = sb(nc, es, "gl_eps", [128, 1], F32)
        c.op("dve", lambda: nc.vector.memset(epsc[:], EPS), [], [Beps])
        gnB, BgnB = sb(nc, es, "gl_gnB", [128, 64], F32)
        c.dma(gnB[:], T["gla_norm_g"][l].partition_broadcast(128), reads=[T.B["gla_norm_g"]], writes=[BgnB])
        qt, Bqt = sb(nc, es, "gl_qt", [32, 4, S], BF16)
        kt, Bkt = sb(nc, es, "gl_kt", [32, 4, S], BF16)
        ktm, Bktm = sb(nc, es, "gl_ktm", [128, NT, 128], BF16)
        ebl, Bebl = sb(nc, es, "gl_ebl", [32, 4, NT], F32)
        with scope(c) as es2:
            wa, Bwa = sb(nc, es2, "gl_wa", [16, 128], BF16)
            c.dma(wa[:], T["gla_wa2"][l], reads=[T.B["gla_wa2"]], writes=[Bwa], q="pool")
            bac, Bbac = sb(nc, es2, "gl_ba", [32, 4], F32)
            c.dma(bac[:], T["gla_ba"][l].rearrange("(g p) -> p g", g=4), reads=[T.B["gla_ba"]], writes=[Bbac],
                  allow_slow_non_contiguous=True)
            aT, BaT = sb(nc, es2, "gl_aT", [16, S], BF16)
            c.dma(aT[:], T["projT"][OFF["gla_a"]:OFF["gla_a"] + 16, :], reads=[T.B["projT"]], writes=[BaT])
            seg, Bseg = sb(nc, es2, "gl_seg", [32, S], BF16)
            c.op("dve", lambda: nc.vector.memset(seg[:], 1.0), [], [Bseg])
            c.op("dve", lambda: nc.vector.memset(seg[:, ssl(0, NT, 128)], 0.0), [], [Bseg])
            zp = Rot(nc, es2, "gl_zp", [128, 512], F32, 2, psum=True)
            qkr = Rot(nc, es2, "gl_qk", [32, 2, S], BF16, 2)
            lar = Rot(nc, es2, "gl_la", [32, S], F32, 2)
            bbr = Rot(nc, es2, "gl_bb", [32, S], F32, 2)
            qkv = T["projT"][OFF["gla_q"]:OFF["gla_q"] + 256, :].rearrange("(i p) t -> p i t", i=8)
            for g in range(4):
                qk, Bqk = qkr.next()
                c.dma(qk[:, 0, :], qkv[:, g, :], reads=[T.B["projT"]], writes=[Bqk])
                c.dma(qk[:, 1, :], qkv[:, 4 + g, :], reads=[T.B["projT"]], writes=[Bqk])
                la, Bla = lar.next()
                bb, Bbb = bbr.next()
                for tt in range(8):
                    sl = slice(tt * 512, (tt + 1) * 512)
                    z_, Bz = zp.next()
                    c.mm(z_[0:32, :], wa[:, g * 32:(g + 1) * 32], aT[:, sl], True, True, [Bwa, BaT], [Bz])
                    c.op("act", lambda: nc.scalar.activation(out=la[:, sl], in_=z_[0:32, :], func=AF.Sigmoid,
                                                             bias=bac[:, g:g + 1]), [Bz, Bbac], [Bla])
                c.op("act", lambda: nc.scalar.activation(out=la[:], in_=la[:], func=AF.Ln), [Bla], [Bla])
                c.op("act", lambda: nc.scalar.mul(out=la[:], in_=la[:], mul=1.0 / 16.0), [Bla], [Bla])
                c.op("dve", lambda: nc.vector.tensor_tensor_scan(out=bb[:], data0=seg[:], data1=la[:],
                                                                 initial=0.0, op0=ALU.mult, op1=ALU.add),
                     [Bseg, Bla], [Bbb])
                c.op("act", lambda: nc.scalar.activation(out=la[:], in_=bb[:], func=AF.Exp), [Bbb], [Bla])
                c.op("dve", lambda: nc.vector.tensor_copy(out=ebl[:, g, :], in_=la[:, ssl(127, NT, 128)]), [Bla], [Bebl])
                c.op("dve", lambda: nc.vector.scalar_tensor_tensor(out=qt[:, g, :], in0=qk[:, 0, :], scalar=32.0 ** -0.5,
                                                                   in1=la[:], op0=ALU.mult, op1=ALU.mult),
                     [Bqk, Bla], [Bqt])
                c.op("act", lambda: nc.scalar.activation(out=bb[:], in_=bb[:], func=AF.Exp, scale=-1.0),
                     [Bbb], [Bbb])
                c.op("dve", lambda: nc.vector.tensor_tensor(out=kt[:, g, :], in0=qk[:, 1, :], in1=bb[:],
                                                            op=ALU.mult), [Bqk, Bbb], [Bkt])
            tp = Rot(nc, es2, "gl_tp", [128, 1024], BF16, 2, psum=True)
            for n in range(NT):
                p_, Bp = tp.next()
                for g in range(4):
                    c.op("pe", lambda: nc.tensor.transpose(out=p_[:, g * 32:(g + 1) * 32],
                                                           in_=kt[:, g, n * 128:(n + 1) * 128],
                                                           identity=ident[0:32, 0:32]), [Bkt, Bid], [Bp])
                c.copy(ktm[:, n, :], p_[:, 0:128], [Bp], [Bktm])
        v, Bv = sb(nc, es, "gl_v", [128, NT, 256], BF16)
        c.dma(v[:], T["proj"][:, OFF["gla_v"]:OFF["gla_v"] + 256].rearrange("(n j) e -> j n e", j=128),
              reads=[T.B["proj"]], writes=[Bv])
        oall, Boall = sb(nc, es, "gl_oall", [128, NT, 256], F32)
        Sps = Rot(nc, es, "gl_S", [128, 512], F32, 2, psum=True)
        Ops = Rot(nc, es, "gl_O", [128, 512], F32, 2, psum=True)
        Pps = Rot(nc, es, "gl_P", [128, 512], F32, 2, psum=True)
        At = Rot(nc, es, "gl_A", [128, 512], BF16, 3)
        Sf, BSf = sb(nc, es, "gl_Sf", [32, 4, 64], F32)
        Sf2, BSf2 = sb(nc, es, "gl_Sf2", [32, 4, 64], F32)
        Sf3, BSf3 = sb(nc, es, "gl_Sf3", [32, 4, 64], F32)
        Sst, BSst = sb(nc, es, "gl_Sst", [32, 4, 64], BF16)
        c.op("dve", lambda: nc.vector.memset(Sf[:], 0.0), [], [BSf])
        c.op("dve", lambda: nc.vector.memset(Sst[:], 0.0), [], [BSst])
        for n in range(NT):
            sl = slice(n * 128, (n + 1) * 128)
            s_, Bs = Sps.next()
            for h in range(4):
                c.mm(s_[:, h * 128:(h + 1) * 128], kt[:, h, sl], qt[:, h, sl], True, True, [Bkt, Bqt], [Bs])
            pp, Bpp = Pps.next()
            for h in range(4):
                c.mm(pp[0:32, h * 64:(h + 1) * 64], ktm[:, n, 32 * h:32 * h + 32], v[:, n, 64 * h:64 * h + 64],
                     True, True, [Bktm, Bv], [Bpp])
            a_, Ba = At.next()
            c.op("dve", lambda: nc.vector.tensor_tensor(
                out=a_[:].rearrange("k (h q) -> k h q", h=4), in0=s_[:].rearrange("k (h q) -> k h q", h=4),
                in1=m01[:].rearrange("k (o q) -> k o q", o=1).to_broadcast([128, 4, 128]), op=ALU.mult), [Bs, Bm01], [Ba])
            o_, Bo = Ops.next()
            for h in range(4):
                c.mm(o_[:, 64 * h:64 * h + 64], a_[:, h * 128:(h + 1) * 128], v[:, n, 64 * h:64 * h + 64],
                     True, False, [Ba, Bv], [Bo])
                c.mm(o_[:, 64 * h:64 * h + 64], qt[:, h, sl], Sst[:, h, :], False, True, [Bqt, BSst], [Bo])
            c.copy(oall[:, n, :], o_[:, 0:256], [Bo], [Boall])
            eb = ebl[:, :, n:n + 1].to_broadcast([32, 4, 64])
            c.op("dve", lambda: nc.vector.tensor_tensor(out=Sf2[:], in0=Sf[:], in1=eb, op=ALU.mult), [BSf, Bebl], [BSf2])
            c.op("dve", lambda: nc.vector.tensor_tensor(out=Sf3[:], in0=pp[0:32, 0:256].rearrange("p (h e) -> p h e", h=4),
                                                        in1=eb, op=ALU.mult), [Bpp, Bebl], [BSf3])
            c.op("dve", lambda: nc.vector.tensor_tensor(out=Sf[:], in0=Sf2[:], in1=Sf3[:], op=ALU.add), [BSf2, BSf3], [BSf])
            c.op("dve", lambda: nc.vector.tensor_copy(out=Sst[:], in_=Sf[:]), [BSf], [BSst])
        if GLA_STAGE <= 5:
            return
        with scope(c) as es2:
            r_, Br = sb(nc, es2, "gl_r", [128, NT, 256], BF16)
            c.dma(r_[:], T["proj"][:, OFF["gla_r"]:OFF["gla_r"] + 256].rearrange("(n j) e -> j n e", j=128),
                  reads=[T.B["proj"]], writes=[Br])
            sq, Bsq = sb(nc, es2, "gl_sq", [128, NT, 256], F32)
            ss, Bss = sb(nc, es2, "gl_ss", [128, NT * 4], F32)
            c.op("act", lambda: nc.scalar.activation(out=sq[:], in_=oall[:], func=AF.Square), [Boall], [Bsq])
            c.op("dve", lambda: nc.vector.tensor_reduce(out=ss[:], in_=sq[:].rearrange("t n (h e) -> t (n h) e", h=4),
                                                        axis=AX.X, op=ALU.add), [Bsq], [Bss])
            c.op("act", lambda: nc.scalar.activation(out=ss[:], in_=ss[:], func=AF.Sqrt, bias=epsc[:, 0:1], scale=1.0 / 64),
                 [Bss, Beps], [Bss])
            c.op("dve", lambda: nc.vector.reciprocal(out=ss[:], in_=ss[:]), [Bss], [Bss])
            ov = oall[:].rearrange("t n (h e) -> t (n h) e", h=4)
            c.op("dve", lambda: nc.vector.tensor_tensor(
                out=ov, in0=ov, in1=ss[:].rearrange("t (m o) -> t m o", o=1).to_broadcast([128, NT * 4, 64]), op=ALU.mult),
                [Boall, Bss], [Boall])
            c.op("dve", lambda: nc.vector.tensor_tensor(
                out=ov, in0=ov, in1=gnB[:].rearrange("t (o e) -> t o e", o=1).to_broadcast([128, NT * 4, 64]), op=ALU.mult),
                [Boall, BgnB], [Boall])
            c.op("act", lambda: nc.scalar.activation(out=sq[:], in_=r_[:], func=AF.Silu), [Br], [Bsq])
            c.op("dve", lambda: nc.vector.tensor_tensor(out=oall[:], in0=oall[:], in1=sq[:], op=ALU.mult),
                 [Boall, Bsq], [Boall])
            c.dma(T["mix"][:, 512:768].rearrange("(n j) e -> j n e", j=128), oall[:], reads=[Boall], writes=[T.B["mix"]])


S5_L = 256
S5_STAGE = 99


def phase_s5(c, T, l):
    _phase_s5(c, T, l)
    c.barrier()


def _phase_s5(c, T, l):
    nc = c.nc
    L = S5_L
    NCH = S // L
    uoff = OFF["s5_u"]

    def dv(fn, r, w):
        return c.op("dve", fn, r, w)

    def tt(out, a, b, op, r, w):
        return c.op("dve", lambda: nc.vector.tensor_tensor(out=out, in0=a, in1=b, op=op), r, w)

    with scope(c) as es:
        ident, Bid = sb(nc, es, "s5_id", [128, 128], BF16)
        c.dma(ident[:], T["ident"][:, :], reads=[T.B["ident"]], writes=[Bid])
        identf, Bidf = sb(nc, es, "s5_idf", [128, 128], F32)
        c.dma(identf[:], T["identf"][:, :], reads=[T.B["identf"]], writes=[Bidf])
        mask, Bmask = sb(nc, es, "s5_mask", [128, 8, 128], F32)
        c.dma(mask[:], T["c_s5mask"][:, :, :], reads=[T.B["c_s5mask"]], writes=[Bmask])
        P, BP = sb(nc, es, "s5_P", [128, 40, 8], F32)
        hp, Bhp = sb(nc, es, "s5_hp", [128, 1], F32)
        dv(lambda: nc.vector.memset(hp[:], math.pi / 2.0), [], [Bhp])
        (ARE, AIM, LDT, DT, MAG, TH, CS, SN, C2, S2, TA, TB_, ABR, ABI, DEN, AM1, FRE, FIM, PR, PI_, PR2, PI2,
         WLR, WLI, X1, X2) = range(26)

        def col(i):
            return P[:, i, :]

        for gg in range(2):
            ps_ = slice(gg * 64, (gg + 1) * 64)
            c.dma(P[ps_, ARE, :], T["s5_a_re"][l].rearrange("(m gg) n -> gg n m", gg=2)[gg], reads=[T.B["s5_a_re"]],
                  writes=[BP], allow_slow_non_contiguous=True)
            c.dma(P[ps_, AIM, :], T["s5_a_im"][l].rearrange("(m gg) n -> gg n m", gg=2)[gg], reads=[T.B["s5_a_im"]],
                  writes=[BP], allow_slow_non_contiguous=True)
            c.dma(P[ps_, LDT, :], T["s5_log_dt"][l].rearrange("(m gg) -> gg m", gg=2)[gg].partition_broadcast(64),
                  reads=[T.B["s5_log_dt"]], writes=[BP], allow_slow_non_contiguous=True)
        c.op("act", lambda: nc.scalar.activation(out=col(DT), in_=col(LDT), func=AF.Exp), [BP], [BP])
        tt(col(TA), col(DT), col(ARE), ALU.mult, [BP], [BP])
        c.op("act", lambda: nc.scalar.activation(out=col(MAG), in_=col(TA), func=AF.Exp), [BP], [BP])
        tt(col(TH), col(DT), col(AIM), ALU.mult, [BP], [BP])
        c.op("act", lambda: nc.scalar.activation(out=col(SN), in_=col(TH), func=AF.Sin, scale=1.0 / 16.0), [BP], [BP])
        c.op("act", lambda: nc.scalar.activation(out=col(CS), in_=col(TH), func=AF.Sin, scale=1.0 / 16.0,
                                                 bias=hp[:, 0:1]), [BP, Bhp], [BP])

        def csq(cr, ci, orr, oi):
            tt(col(TA), col(cr), col(cr), ALU.mult, [BP], [BP])
            tt(col(TB_), col(ci), col(ci), ALU.mult, [BP], [BP])
            dv(lambda: nc.vector.scalar_tensor_tensor(out=col(oi), in0=col(cr), scalar=2.0, in1=col(ci),
                                                      op0=ALU.mult, op1=ALU.mult), [BP], [BP])
            tt(col(orr), col(TA), col(TB_), ALU.subtract, [BP], [BP])

        csq(CS, SN, C2, S2)
        csq(C2, S2, CS, SN)
        csq(CS, SN, C2, S2)
        csq(C2, S2, CS, SN)
        tt(col(ABR), col(MAG), col(CS), ALU.mult, [BP], [BP])
        tt(col(ABI), col(MAG), col(SN), ALU.mult, [BP], [BP])
        tt(col(TA), col(ARE), col(ARE), ALU.mult, [BP], [BP])
        tt(col(TB_), col(AIM), col(AIM), ALU.mult, [BP], [BP])
        tt(col(DEN), col(TA), col(TB_), ALU.add, [BP], [BP])
        dv(lambda: nc.vector.reciprocal(out=col(DEN), in_=col(DEN)), [BP], [BP])
        dv(lambda: nc.vector.tensor_scalar(out=col(AM1), in0=col(ABR), scalar1=-1.0, scalar2=None, op0=ALU.add), [BP], [BP])
        tt(col(TA), col(AM1), col(ARE), ALU.mult, [BP], [BP])
        tt(col(TB_), col(ABI), col(AIM), ALU.mult, [BP], [BP])
        tt(col(FRE), col(TA), col(TB_), ALU.add, [BP], [BP])
        tt(col(FRE), col(FRE), col(DEN), ALU.mult, [BP], [BP])
        tt(col(TA), col(ABI), col(ARE), ALU.mult, [BP], [BP])
        tt(col(TB_), col(AM1), col(AIM), ALU.mult, [BP], [BP])
        tt(col(FIM), col(TA), col(TB_), ALU.subtract, [BP], [BP])
        tt(col(FIM), col(FIM), col(DEN), ALU.mult, [BP], [BP])
        BT, BBT = sb(nc, es, "s5_BT", [128, 2, 8, 128], BF16)
        Cm, BCm = sb(nc, es, "s5_Cm", [128, 2, 8, 128], BF16)
        with scope(c) as es2:
            braw, Bbraw = sb(nc, es2, "s5_braw", [128, 2, 8, 16], F32)
            craw, Bcraw = sb(nc, es2, "s5_craw", [128, 2, 8, 16], F32)
            for gg in range(2):
                ps_ = slice(gg * 64, (gg + 1) * 64)
                for ri, nm in enumerate(("s5_b_re", "s5_b_im")):
                    c.dma(braw[ps_, ri, :, :], T[nm][l].rearrange("(m gg) n c -> gg n m c", gg=2)[gg], reads=[T.B[nm]],
                          writes=[Bbraw], allow_slow_non_contiguous=True)
                for ri, nm in enumerate(("s5_c_re", "s5_c_im")):
                    for m in range(8):
                        c.dma(craw[ps_, ri, m, :], T[nm][l, 2 * m + gg].rearrange("c n -> n c"), reads=[T.B[nm]],
                              writes=[Bcraw], allow_slow_non_contiguous=True)
            bbc, Bbbc = sb(nc, es2, "s5_bbc", [128, 2, 8, 16], F32)
            t1, Bt1 = sb(nc, es2, "s5_t1", [128, 8, 16], F32)
            t2, Bt2 = sb(nc, es2, "s5_t2", [128, 8, 16], F32)
            fre_b = P[:, FRE, :].rearrange("p (m o) -> p m o", o=1).to_broadcast([128, 8, 16])
            fim_b = P[:, FIM, :].rearrange("p (m o) -> p m o", o=1).to_broadcast([128, 8, 16])
            tt(t1[:], braw[:, 0, :, :], fre_b, ALU.mult, [Bbraw, BP], [Bt1])
            tt(t2[:], braw[:, 1, :, :], fim_b, ALU.mult, [Bbraw, BP], [Bt2])
            tt(bbc[:, 0, :, :], t1[:], t2[:], ALU.subtract, [Bt1, Bt2], [Bbbc])
            tt(t1[:], braw[:, 1, :, :], fre_b, ALU.mult, [Bbraw, BP], [Bt1])
            tt(t2[:], braw[:, 0, :, :], fim_b, ALU.mult, [Bbraw, BP], [Bt2])
            tt(bbc[:, 1, :, :], t1[:], t2[:], ALU.add, [Bt1, Bt2], [Bbbc])
            Bd, BBd = sb(nc, es2, "s5_Bd", [128, 2, 8, 128], BF16)
            mask4 = mask[:].rearrange("p m (j c) -> p m j c", c=16)
            for ri in range(2):
                tt(Bd[:, ri, :, :].rearrange("p m (j c) -> p m j c", c=16), mask4,
                   bbc[:, ri, :, :].rearrange("p m (o c) -> p m o c", o=1).to_broadcast([128, 8, 8, 16]), ALU.mult,
                   [Bmask, Bbbc], [BBd])
            tt(Cm[:, 0, :, :].rearrange("p m (j c) -> p m j c", c=16), mask4,
               craw[:, 0, :, :].rearrange("p m (o c) -> p m o c", o=1).to_broadcast([128, 8, 8, 16]), ALU.mult,
               [Bmask, Bcraw], [BCm])
            dv(lambda: nc.vector.tensor_scalar(out=craw[:, 1, :, :], in0=craw[:, 1, :, :], scalar1=-1.0, scalar2=None,
                                               op0=ALU.mult), [Bcraw], [Bcraw])
            tt(Cm[:, 1, :, :].rearrange("p m (j c) -> p m j c", c=16), mask4,
               craw[:, 1, :, :].rearrange("p m (o c) -> p m o c", o=1).to_broadcast([128, 8, 8, 16]), ALU.mult,
               [Bmask, Bcraw], [BCm])
            tpb = Rot(nc, es2, "s5_tpb", [128, 1024], BF16, 2, psum=True)
            for ri in range(2):
                for g2 in range(2):
                    p_, Bp = tpb.next()
                    for j in range(4):
                        m = g2 * 4 + j
                        c.op("pe", lambda: nc.tensor.transpose(out=p_[:, j * 128:(j + 1) * 128], in_=Bd[:, ri, m, :],
                                                               identity=ident[:]), [BBd, Bid], [Bp])
                    c.copy(BT[:, ri, g2 * 4:(g2 + 1) * 4, :], p_[:, 0:512].rearrange("p (j t) -> p j t", j=4), [Bp], [BBT])
        c.barrier()
        T.dbg(c, "dbg_s5P", P[:], (128, 40, 8), F32, BP)
        if S5_STAGE <= 1:
            return
        w, Bw = sb(nc, es, "s5_w", [128, 2, 8, L], F32)
        Rb, BRb = sb(nc, es, "s5_Rb", [128, 8, L], F32)
        tA, BtA = sb(nc, es, "s5_tA", [128, 8, L], F32)
        tB, BtB = sb(nc, es, "s5_tB", [128, 8, L], F32)
        dv(lambda: nc.vector.tensor_copy(out=Rb[:], in_=P[:, MAG, :].rearrange("p (m o) -> p m o", o=1)
                                         .to_broadcast([128, 8, L])), [BP], [BRb])
        dv(lambda: nc.vector.memset(w[:, 0, :, 0:1], 1.0), [], [Bw])
        dv(lambda: nc.vector.memset(w[:, 1, :, 0:1], 0.0), [], [Bw])
        dv(lambda: nc.vector.tensor_copy(out=col(PR), in_=col(CS)), [BP], [BP])
        dv(lambda: nc.vector.tensor_copy(out=col(PI_), in_=col(SN)), [BP], [BP])
        k = 1
        cur = (PR, PI_)
        oth = (PR2, PI2)
        while k < L:
            prb = P[:, cur[0], :].rearrange("p (m o) -> p m o", o=1).to_broadcast([128, 8, k])
            pib = P[:, cur[1], :].rearrange("p (m o) -> p m o", o=1).to_broadcast([128, 8, k])
            wr0 = w[:, 0, :, 0:k]
            wi0 = w[:, 1, :, 0:k]
            tt(tA[:, :, 0:k], wr0, prb, ALU.mult, [Bw, BP], [BtA])
            tt(tB[:, :, 0:k], wi0, pib, ALU.mult, [Bw, BP], [BtB])
            tt(w[:, 0, :, k:2 * k], tA[:, :, 0:k], tB[:, :, 0:k], ALU.subtract, [BtA, BtB], [Bw])
            tt(tA[:, :, 0:k], wr0, pib, ALU.mult, [Bw, BP], [BtA])
            tt(tB[:, :, 0:k], wi0, prb, ALU.mult, [Bw, BP], [BtB])
            tt(w[:, 1, :, k:2 * k], tA[:, :, 0:k], tB[:, :, 0:k], ALU.add, [BtA, BtB], [Bw])
            csq(cur[0], cur[1], oth[0], oth[1])
            cur, oth = oth, cur
            k *= 2
        dv(lambda: nc.vector.tensor_copy(out=col(WLR), in_=col(cur[0])), [BP], [BP])
        dv(lambda: nc.vector.tensor_copy(out=col(WLI), in_=col(cur[1])), [BP], [BP])
        T.dbg(c, "dbg_s5w", w[:], (128, 2, 8, L), F32, Bw)
        gw, Bgw = sb(nc, es, "s5_gwt", [128, 2, 256], BF16)
        c.dma(gw[:], T["s5_glu_w"][l].rearrange("(kt p) n -> p kt n", p=128), reads=[T.B["s5_glu_w"]], writes=[Bgw], q="pool")
        gbc, Bgbc = sb(nc, es, "s5_gbt", [128, 2], F32)
        c.dma(gbc[:], T["s5_glu_b"][l].rearrange("(h p) -> p h", p=128), reads=[T.B["s5_glu_b"]], writes=[Bgbc],
              allow_slow_non_contiguous=True)
        dcl, Bdcl = sb(nc, es, "s5_dcol", [128, 2], F32)
        c.dma(dcl[:], T["s5_d"][l].rearrange("(h p) -> p h", p=128), reads=[T.B["s5_d"]], writes=[Bdcl],
              allow_slow_non_contiguous=True)
        if S5_STAGE <= 2:
            return
        uTr = Rot(nc, es, "s5_uT", [128, 2, L], BF16, 2)
        BUps = Rot(nc, es, "s5_BU", [128, 512], F32, 2, psum=True)
        Yps = Rot(nc, es, "s5_Y", [128, 512], F32, 2, psum=True)
        Tpf = Rot(nc, es, "s5_Tp", [128, 512], F32, 2, psum=True)
        d1 = Rot(nc, es, "s5_d1", [128, L], F32, 3)
        d2 = Rot(nc, es, "s5_d2", [128, L], F32, 3)
        zb = Rot(nc, es, "s5_zb", [128, 2, L], F32, 3)
        Z, BZ = sb(nc, es, "s5_Z", [128, 2, 8, L], F32)
        X, BX = sb(nc, es, "s5_X", [128, 2, 8, L], BF16)
        init = Rot(nc, es, "s5_init", [128, 2, 8], F32, 2)
        zt, Bzt = sb(nc, es, "s5_zt", [128, 2, L], F32)
        u2, Bu2 = sb(nc, es, "s5_u2", [128, 2, L], F32)
        hg, Bhg = sb(nc, es, "s5_hg", [128, 2, L], F32)
        hgb, Bhgb = sb(nc, es, "s5_hgb", [128, 2, L], BF16)
        sgt, Bsgt = sb(nc, es, "s5_sg", [128, 2, L], F32)
        ot, Bot = sb(nc, es, "s5_ot", [128, 2, L], F32)
        otm = Rot(nc, es, "s5_otm", [128, L // 128, 256], F32, 2)
        in_, Bin = init.next()
        dv(lambda: nc.vector.memset(in_[:], 0.0), [], [Bin])
        for ci in range(NCH):
            t0 = ci * L
            u_, Bu = uTr.next()
            c.dma(u_[:], T["projT"][uoff:uoff + 256, t0:t0 + L].rearrange("(h p) t -> p h t", p=128),
                  reads=[T.B["projT"]], writes=[Bu])
            for m in range(8):
                bk, Bbk = BUps.next()
                c.mm(bk[:, 0:L], BT[:, 0, m, :], u_[:, m // 4, :], True, True, [BBT, Bu], [Bbk])
                c.mm(bk[:, L:2 * L], BT[:, 1, m, :], u_[:, m // 4, :], True, True, [BBT, Bu], [Bbk])
                z_, Bz = zb.next()
                a1, Ba1 = d1.next()
                a2, Ba2 = d2.next()
                tt(a1[:], bk[:, 0:L], w[:, 0, m, :], ALU.mult, [Bbk, Bw], [Ba1])
                tt(a2[:], bk[:, L:2 * L], w[:, 1, m, :], ALU.mult, [Bbk, Bw], [Ba2])
                tt(z_[:, 0, :], a1[:], a2[:], ALU.add, [Ba1, Ba2], [Bz])
                a1, Ba1 = d1.next()
                a2, Ba2 = d2.next()
                tt(a1[:], bk[:, L:2 * L], w[:, 0, m, :], ALU.mult, [Bbk, Bw], [Ba1])
                tt(a2[:], bk[:, 0:L], w[:, 1, m, :], ALU.mult, [Bbk, Bw], [Ba2])
                tt(z_[:, 1, :], a1[:], a2[:], ALU.subtract, [Ba1, Ba2], [Bz])
                for ri in range(2):
                    dv(lambda: nc.vector.tensor_tensor_scan(out=Z[:, ri, m, :], data0=Rb[:, m, :], data1=z_[:, ri, :],
                                                            initial=in_[:, ri, m:m + 1], op0=ALU.mult, op1=ALU.add),
                       [BRb, Bz, Bin], [BZ])
            nx, Bnx = init.next()
            zr = Z[:, 0, :, L - 1]
            zi = Z[:, 1, :, L - 1]
            tt(col(X1), zr, col(WLR), ALU.mult, [BZ, BP], [BP])
            tt(col(X2), zi, col(WLI), ALU.mult, [BZ, BP], [BP])
            tt(nx[:, 0, :], col(X1), col(X2), ALU.subtract, [BP], [Bnx])
            tt(col(X1), zr, col(WLI), ALU.mult, [BZ, BP], [BP])
            tt(col(X2), zi, col(WLR), ALU.mult, [BZ, BP], [BP])
            tt(nx[:, 1, :], col(X1), col(X2), ALU.add, [BP], [Bnx])
            in_, Bin = nx, Bnx
            def pt(out, a, b, op, r, w_):
                return c.op("dve", lambda: nc.vector.tensor_tensor(out=out, in0=a, in1=b, op=op), r, w_)
            pt(tA[:], Z[:, 0, :, :], w[:, 0, :, :], ALU.mult, [BZ, Bw], [BtA])
            pt(tB[:], Z[:, 1, :, :], w[:, 1, :, :], ALU.mult, [BZ, Bw], [BtB])
            pt(X[:, 0, :, :], tA[:], tB[:], ALU.subtract, [BtA, BtB], [BX])
            pt(tA[:], Z[:, 0, :, :], w[:, 1, :, :], ALU.mult, [BZ, Bw], [BtA])
            pt(tB[:], Z[:, 1, :, :], w[:, 0, :, :], ALU.mult, [BZ, Bw], [BtB])
            pt(X[:, 1, :, :], tA[:], tB[:], ALU.add, [BtA, BtB], [BX])
            if ci == 0:
                T.dbg(c, "dbg_s5X", X[:], (128, 2, 8, L), BF16, BX)
            for half in range(2):
                yp, Byp = Yps.next()
                for mm_ in range(4):
                    m = half * 4 + mm_
                    c.mm(yp[:, 0:L], Cm[:, 0, m, :], X[:, 0, m, :], mm_ == 0, False, [BCm, BX], [Byp])
                    c.mm(yp[:, 0:L], Cm[:, 1, m, :], X[:, 1, m, :], False, mm_ == 3, [BCm, BX], [Byp])
                dv(lambda: nc.vector.scalar_tensor_tensor(out=zt[:, half, :], in0=u_[:, half, :], scalar=dcl[:, half:half + 1],
                                                          in1=yp[:, 0:L], op0=ALU.mult, op1=ALU.add),
                   [Bu, Bdcl, Byp], [Bzt])
            tt(u2[:], zt[:], zt[:], ALU.mult, [Bzt], [Bu2])
            dv(lambda: nc.vector.tensor_scalar(out=u2[:], in0=u2[:], scalar1=0.044715, scalar2=1.0, op0=ALU.mult,
                                               op1=ALU.add), [Bu2], [Bu2])
            tt(u2[:], u2[:], zt[:], ALU.mult, [Bu2, Bzt], [Bu2])
            c.op("act", lambda: nc.scalar.activation(out=u2[:], in_=u2[:], func=AF.Sigmoid, scale=1.5957691216057308),
                 [Bu2], [Bu2])
            tt(hg[:], u2[:], zt[:], ALU.mult, [Bu2, Bzt], [Bhg])
            c.op("act", lambda: nc.scalar.copy(out=hgb[:], in_=hg[:]), [Bhg], [Bhgb])
            for h2 in range(2):
                gp, Bgp = Yps.next()
                for kt_ in range(2):
                    c.mm(gp[:, 0:L], gw[:, kt_, h2 * 128:(h2 + 1) * 128], hgb[:, kt_, :], kt_ == 0, kt_ == 1, [Bgw, Bhgb], [Bgp])
                c.op("act", lambda: nc.scalar.activation(out=sgt[:, h2, :], in_=gp[:, 0:L], func=AF.Sigmoid,
                                                         bias=gbc[:, h2:h2 + 1]), [Bgp, Bgbc], [Bsgt])
            tt(ot[:], hg[:], sgt[:], ALU.mult, [Bhg, Bsgt], [Bot])
            tp_, Btp = Tpf.next()
            for ts_ in range(L // 128):
                for h2 in range(2):
                    c.op("pe", lambda: nc.tensor.transpose(out=tp_[:, ts_ * 256 + h2 * 128:ts_ * 256 + (h2 + 1) * 128],
                                                           in_=ot[:, h2, ts_ * 128:(ts_ + 1) * 128], identity=identf[:]),
                         [Bot, Bidf], [Btp])
            om, Bom = otm.next()
            c.copy(om[:], tp_[:, 0:(L // 128) * 256].rearrange("p (a e) -> p a e", e=256), [Btp], [Bom])
            c.dma(T["mix"][t0:t0 + L, 768:1024].rearrange("(a p) e -> p a e", p=128), om[:], reads=[Bom],
                  writes=[T.B["mix"]])


def norm_tile(c, x_t, Bx, gB, BgB, h_, Bh, s_, Bs, jk, Bjk, ngroups=1):
    nc = c.nc
    Wd = D // ngroups
    for g in range(ngroups):
        gs = slice(g * Wd, (g + 1) * Wd)
        c.op("act", lambda: nc.scalar.activation(out=jk[:, gs], in_=x_t[:, gs], func=AF.Square,
                                                 accum_out=s_[:, g:g + 1]), [Bx], [Bjk, Bs])
    c.op("act", lambda: nc.scalar.activation(out=s_[:, 4:4 + ngroups], in_=s_[:, 0:ngroups], func=AF.Sqrt,
                                             bias=gB[:, D:D + 1], scale=1.0 / Wd), [Bs, BgB], [Bs])
    c.op("dve", lambda: nc.vector.reciprocal(out=s_[:, 4:4 + ngroups], in_=s_[:, 4:4 + ngroups]), [Bs], [Bs])
    for g in range(ngroups):
        gs = slice(g * Wd, (g + 1) * Wd)
        c.op("dve", lambda: nc.vector.scalar_tensor_tensor(out=h_[:, gs], in0=x_t[:, gs], scalar=s_[:, 4 + g:5 + g],
                                                           in1=gB[:, gs], op0=ALU.mult, op1=ALU.mult),
             [Bx, Bs, BgB], [Bh])


def phase_out(c, T, l):
    nc = c.nc
    x_d = T["xin%d" % l]
    Bx_d = T.B["xin%d" % l]
    with scope(c) as es:
        W, BW = sb(nc, es, "po_W", [128, 8, D], BF16)
        ident, Bid = sb(nc, es, "po_id", [128, 128], BF16)
        c.dma(ident[:], T["ident"][:, :], reads=[T.B["ident"]], writes=[Bid])
        gB, BgB = load_gB(c, es, T["out_norm_g"][l], T.B["out_norm_g"], "po_g")
        wv = T["w_out"][l].rearrange("(kc p) n -> p kc n", p=128)
        for kc in range(8):
            c.dma(W[:, kc, :], wv[:, kc, :], reads=[T.B["w_out"]], writes=[BW], q="pool")
        mt = Rot(nc, es, "po_mt", [128, D], F32, 2)
        xt = Rot(nc, es, "po_xt", [128, D], F32, 2)
        jkr = Rot(nc, es, "po_jk", [128, D], BF16, 2)
        hb = Rot(nc, es, "po_hb", [128, D], BF16, 2)
        ss = Rot(nc, es, "po_ss", [128, 8], F32, 4)
        tp = Rot(nc, es, "po_tp", [128, 1024], BF16, 2, psum=True)
        hTr = Rot(nc, es, "po_hT", [128, 8, 128], BF16, 2)
        acc = Rot(nc, es, "po_acc", [128, 512], F32, 2, psum=True)
        xo = Rot(nc, es, "po_xo", [128, D], F32, 2)
        for t in range(NT):
            rows = slice(t * 128, (t + 1) * 128)
            m_, Bm = mt.next()
            c.dma(m_[:], T["mix"][rows, :], reads=[T.B["mix"]], writes=[Bm])
            x_, Bx = xt.next()
            c.dma(x_[:], x_d[rows, :], reads=[Bx_d], writes=[Bx])
            jk, Bjk = jkr.next()
            s_, Bs = ss.next()
            h_, Bh = hb.next()
            norm_tile(c, m_, Bm, gB, BgB, h_, Bh, s_, Bs, jk, Bjk, ngroups=4)
            hT, BhT = hTr.next()
            for g in range(2):
                p_, Bp = tp.next()
                for j in range(4):
                    kc = g * 4 + j
                    c.op("pe", lambda: nc.tensor.transpose(out=p_[:, j * 128:(j + 1) * 128],
                                                           in_=h_[:, kc * 128:(kc + 1) * 128],
                                                           identity=ident[:]), [Bh, Bid], [Bp])
                c.copy(hT[:, g * 4:(g + 1) * 4, :], p_[:, 0:512].rearrange("p (j t) -> p j t", j=4), [Bp], [BhT])
            o_, Bo = xo.next()
            for dc in range(2):
                ds_ = slice(dc * 512, (dc + 1) * 512)
                a_, Ba = acc.next()
                for k in range(8):
                    c.mm(a_[:, :], hT[:, k, :], W[:, k, ds_], k == 0, k == 7, [BW, BhT], [Ba])
                c.op("dve", lambda: nc.vector.tensor_tensor(out=o_[:, ds_], in0=a_[:, :], in1=x_[:, ds_], op=ALU.add),
                     [Ba, Bx], [Bo])
            c.dma(T["xmid"][rows, :], o_[:], reads=[Bo], writes=[T.B["xmid"]])
    c.barrier()


FFN_TB = 1024


def phase_ffn(c, T, l, out_name):
    nc = c.nc
    moe = (l % 2 == 1)
    i = l // 2
    if moe:
        F = D_FFE
        experts = [(T["moe_w_gate"][i, e], T["moe_w_up"][i, e], T["moe_w_down"][i, e]) for e in range(N_EXP)]
        BWs = (T.B["moe_w_gate"], T.B["moe_w_up"], T.B["moe_w_down"])
    else:
        F = D_FF
        experts = [(T["ffn_w_gate"][i], T["ffn_w_up"][i], T["ffn_w_down"][i])]
        BWs = (T.B["ffn_w_gate"], T.B["ffn_w_up"], T.B["ffn_w_down"])
    TB = FFN_TB
    NTB = TB // 128
    FC = 512
    chunks = [(f0, min(FC, F - f0)) for f0 in range(0, F, FC)]
    with scope(c) as es:
        ident, Bid = sb(nc, es, "ff_id", [128, 128], BF16)
        c.dma(ident[:], T["ident"][:, :], reads=[T.B["ident"]], writes=[Bid])
        gB, BgB = load_gB(c, es, T["norm2_g"][l], T.B["norm2_g"], "ff_g")
        if moe:
            Wr, BWr = sb(nc, es, "ff_Wr", [128, 8, N_EXP], BF16)
            c.dma(Wr[:], T["moe_router_w"][i].rearrange("(kc p) e -> p kc e", p=128), reads=[T.B["moe_router_w"]],
                  writes=[BWr], q="pool")
            rb, Brb = sb(nc, es, "ff_rb", [128, N_EXP], F32)
            c.dma(rb[:], T["moe_router_b"][i].partition_broadcast(128), reads=[T.B["moe_router_b"]], writes=[Brb])
            comb, Bcomb = sb(nc, es, "ff_comb", [128, NTB, N_EXP], F32)
            lg = Rot(nc, es, "ff_lg", [128, N_EXP], F32, 2)
            mx = Rot(nc, es, "ff_mx", [128, 8], F32, 2)
            ex = Rot(nc, es, "ff_ex", [128, 2 * N_EXP], F32, 2)
        wg = Rot(nc, es, "ff_wg", [128, 8, FC], BF16, 2)
        wu = Rot(nc, es, "ff_wu", [128, 8, FC], BF16, 2)
        wd = Rot(nc, es, "ff_wd", [128, 4, D], BF16, 2)
        Gp = Rot(nc, es, "ff_G", [128, 512], F32, 2, psum=True)
        Up = Rot(nc, es, "ff_U", [128, 512], F32, 2, psum=True)
        Dp = Rot(nc, es, "ff_D", [128, 512], F32, 2, psum=True)
        sg = Rot(nc, es, "ff_sg", [128, 512], F32, 2)
        act = Rot(nc, es, "ff_act", [128, 4, TB], BF16, 2)
        hT, BhT = sb(nc, es, "ff_hT", [128, 8, TB], BF16)
        acc, Bacc = sb(nc, es, "ff_acc", [128, NTB, D], F32)
        for tb in range(S // TB):
            with scope(c) as es2:
                norm_transpose(c, es2, T["xmid"], T.B["xmid"], gB, BgB, ident, Bid, hT, BhT, tb * NTB, NTB,
                               "ff%d_" % tb, keep_x=(acc, Bacc))
            if moe:
                for it in range(NTB):
                    l_, Bl = lg.next()
                    p_, Bp = Dp.next()
                    for k in range(8):
                        c.mm(p_[:, 0:N_EXP], hT[:, k, it * 128:(it + 1) * 128], Wr[:, k, :], k == 0, k == 7,
                             [BhT, BWr], [Bp])
                    c.op("dve", lambda: nc.vector.tensor_tensor(out=l_[:], in0=p_[:, 0:N_EXP], in1=rb[:], op=ALU.add),
                         [Bp, Brb], [Bl])
                    m_, Bm = mx.next()
                    c.op("dve", lambda: nc.vector.max(out=m_[:, 0:8], in_=l_[:]), [Bl], [Bm])
                    e_, Be = ex.next()
                    c.op("dve", lambda: nc.vector.tensor_scalar(out=e_[:, 0:8], in0=l_[:], scalar1=m_[:, 0:1], scalar2=None,
                                                                op0=ALU.subtract), [Bl, Bm], [Be])
                    c.op("act", lambda: nc.scalar.activation(out=e_[:, 0:8], in_=e_[:, 0:8], func=AF.Exp), [Be], [Be])
                    c.op("dve", lambda: nc.vector.scalar_tensor_tensor(out=e_[:, 8:16], in0=l_[:], scalar=m_[:, 1:2],
                                                                       in1=e_[:, 0:8], op0=ALU.is_ge, op1=ALU.mult),
                         [Bl, Bm, Be], [Be])
                    c.op("dve", lambda: nc.vector.tensor_reduce(out=m_[:, 2:3], in_=e_[:, 8:16], axis=AX.X, op=ALU.add),
                         [Be], [Bm])
                    c.op("dve", lambda: nc.vector.reciprocal(out=m_[:, 3:4], in_=m_[:, 2:3]), [Bm], [Bm])
                    c.op("dve", lambda: nc.vector.tensor_scalar(out=comb[:, it, :], in0=e_[:, 8:16], scalar1=m_[:, 3:4],
                                                                scalar2=None, op0=ALU.mult), [Be, Bm], [Bcomb])
            for e, (wg_d, wu_d, wd_d) in enumerate(experts):
                wgv = wg_d.rearrange("(kc p) f -> p kc f", p=128)
                wuv = wu_d.rearrange("(kc p) f -> p kc f", p=128)
                for (f0, fw) in chunks:
                    nfull = fw // 128
                    rem = fw - nfull * 128
                    nft = nfull + (1 if rem else 0)
                    g_, Bg = wg.next()
                    c.dma(g_[:, :, 0:fw], wgv[:, :, f0:f0 + fw], reads=[BWs[0]], writes=[Bg], q="pool")
                    u_, Bu = wu.next()
                    c.dma(u_[:, :, 0:fw], wuv[:, :, f0:f0 + fw], reads=[BWs[1]], writes=[Bu], q="pool")
                    d_, Bd = wd.next()
                    if nfull:
                        c.dma(d_[:, 0:nfull, :], wd_d[f0:f0 + nfull * 128, :].rearrange("(ft p) d -> p ft d", p=128),
                              reads=[BWs[2]], writes=[Bd], q="pool")
                    if rem:
                        c.dma(d_[0:rem, nfull, :], wd_d[f0 + nfull * 128:f0 + fw, :], reads=[BWs[2]], writes=[Bd], q="pool")
                    a_, Ba = act.next()
                    for ft in range(nft):
                        M = min(128, fw - ft * 128)
                        fs = slice(ft * 128, ft * 128 + M)
                        for tt in range(TB // 512):
                            ts_ = slice(tt * 512, (tt + 1) * 512)
                            G, BG = Gp.next()
                            for k in range(8):
                                c.mm(G[0:M, :], g_[:, k, fs], hT[:, k, ts_], k == 0, k == 7, [Bg, BhT], [BG])
                            U, BU = Up.next()
                            for k in range(8):
                                c.mm(U[0:M, :], u_[:, k, fs], hT[:, k, ts_], k == 0, k == 7, [Bu, BhT], [BU])
                            s_, Bs = sg.next()
                            c.op("act", lambda: nc.scalar.activation(out=s_[0:M, :], in_=G[0:M, :], func=AF.Silu), [BG], [Bs])
                            c.op("dve", lambda: nc.vector.tensor_tensor(out=a_[0:M, ft, ts_], in0=s_[0:M, :], in1=U[0:M, :],
                                                                        op=ALU.mult), [Bs, BU], [Ba])
                    for it in range(NTB):
                        for dc in range(2):
                            ds_ = slice(dc * 512, (dc + 1) * 512)
                            Dd, BD = Dp.next()
                            for ft in range(nft):
                                M = min(128, fw - ft * 128)
                                c.mm(Dd[:, :], a_[0:M, ft, it * 128:(it + 1) * 128], d_[0:M, ft, ds_], ft == 0, ft == nft - 1,
                                     [Ba, Bd], [BD])
                            if moe:
                                c.op("dve", lambda: nc.vector.scalar_tensor_tensor(
                                    out=acc[:, it, ds_], in0=Dd[:, :], scalar=comb[:, it, e:e + 1], in1=acc[:, it, ds_],
                                    op0=ALU.mult, op1=ALU.add), [BD, Bcomb, Bacc], [Bacc])
                            else:
                                c.op("dve", lambda: nc.vector.tensor_tensor(out=acc[:, it, ds_], in0=Dd[:, :],
                                                                            in1=acc[:, it, ds_], op=ALU.add),
                                     [BD, Bacc], [Bacc])
            c.dma(T[out_name][tb * TB:(tb + 1) * TB, :].rearrange("(i p) d -> p i d", p=128), acc[:], reads=[Bacc],
                  writes=[T.B[out_name]])
            c.barrier()
    c.barrier()


class Tensors:
    def __init__(self, nc, ext_in=(), ext_out=()):
        self.nc = nc
        self.t = {}
        self.B = {}
        self.ext_in = set(ext_in)
        self.ext_out = set(ext_out)
        self.in_names = []
        self.out_names = []

    def add(self, name, shape, dtype, kind=None):
        if kind is None:
            if name in self.ext_in:
                kind = "ExternalInput"
            elif name in self.ext_out:
                kind = "ExternalOutput"
            else:
                kind = "Internal"
        if kind == "ExternalInput":
            self.in_names.append(name)
        if kind == "ExternalOutput":
            self.out_names.append(name)
        self.t[name] = self.nc.dram_tensor(name, list(shape), dtype, kind=kind).ap()
        self.B[name] = Buf(name)
        return self.t[name]

    def __getitem__(self, k):
        return self.t[k]

    def dbg(self, c, name, ap, shape, dtype, Bsrc):
        if name not in self.ext_out:
            return
        d = self.add(name, shape, dtype, kind="ExternalOutput")
        c.dma(d, ap, reads=[Bsrc], writes=[self.B[name]])


PARAM_SHAPES = {
    "norm1_g": (2, 1024), "w_in": (2, 1024, 2460), "dil_qk_g": (2, 2, 64), "nsa_qk_g": (2, 4, 64),
    "nsa_cmp_pos": (2, 2, 32, 64), "nsa_cmp_w1": (2, 2, 2048, 256), "nsa_cmp_w2": (2, 2, 256, 64),
    "gla_wa2": (2, 16, 128), "gla_ba": (2, 128), "gla_norm_g": (2, 64),
    "s5_a_re": (2, 16, 64), "s5_a_im": (2, 16, 64), "s5_b_re": (2, 16, 64, 16), "s5_b_im": (2, 16, 64, 16),
    "s5_c_re": (2, 16, 16, 64), "s5_c_im": (2, 16, 16, 64), "s5_d": (2, 256), "s5_log_dt": (2, 16),
    "s5_glu_w": (2, 256, 256), "s5_glu_b": (2, 256), "out_norm_g": (2, 1024), "w_out": (2, 1024, 1024),
    "norm2_g": (2, 1024), "ffn_w_gate": (1, 1024, 2752), "ffn_w_up": (1, 1024, 2752),
    "ffn_w_down": (1, 2752, 1024), "moe_router_w": (1, 1024, 8), "moe_router_b": (1, 8),
    "moe_w_gate": (1, 8, 1024, 3584), "moe_w_up": (1, 8, 1024, 3584), "moe_w_down": (1, 8, 3584, 1024),
}


def host_consts():
    cst = {}
    bf = ml_dtypes.bfloat16
    cst["ident"] = np.eye(128, dtype=np.float32).astype(bf)
    p = np.arange(S)
    loc = np.arange(128).astype(np.float32)
    KBd = np.zeros((4, 36, 128), np.float32)
    for di in range(36):
        KBd[0, di] = 128.0 * (di - 32)
        KBd[1, di] = loc
        KBd[2, di] = 1.0
        KBd[3, di] = 1.0
    cst["c_KBd"] = KBd.astype(bf)
    QB = np.zeros((4, 3, 4, 128), np.float32)
    for pi, (win, d) in enumerate(DIL_PATTERNS):
        for h in range(4):
            a = DIL_SLOPES[h] * d
            QB[0, pi, h] = a
            QB[1, pi, h] = a
            QB[2, pi, h] = 0.0
            QB[3, pi, h] = -a * loc
    cst["c_QBdil"] = QB.astype(bf)
    kl = np.arange(128)[:, None]
    ql = np.arange(128)[None, :]
    masks = np.stack([(kl <= ql), (kl >= ql), (kl > ql)], axis=1).astype(np.float32)
    cst["c_masks"] = ((masks - 1.0) * 30000.0).astype(bf)
    cst["c_mask01"] = (kl <= ql).astype(np.float32).astype(bf)
    KBS = np.zeros((68, S), np.float32)
    KBS[p // 64, p] = 32768.0
    KBS[64] = 128.0 * (p // 128)
    KBS[65] = p % 128
    KBS[66] = 1.0
    KBS[67] = 1.0
    cst["c_KBS"] = KBS.astype(bf)
    QBa = np.zeros((4, 4, S), np.float32)
    for h in range(4):
        a = NSA_SLOPES[h]
        QBa[0, h] = a
        QBa[1, h] = a
        QBa[2, h] = -a * 128.0 * (p // 128)
        QBa[3, h] = -a * (p % 128)
    cst["c_QBabs"] = QBa.astype(bf)
    KBc = np.zeros((4, 32, 128), np.float32)
    for m in range(32):
        KBc[0, m] = -128.0 * m
        KBc[1, m] = 16.0 * loc
        KBc[2, m] = 31.0
        KBc[3, m] = 1.0
    cst["c_KBc"] = KBc.astype(bf)
    QBc = np.zeros((4, 4, 128), np.float32)
    for h in range(4):
        a = NSA_SLOPES[h]
        QBc[0, h] = a
        QBc[1, h] = a
        QBc[2, h] = a
        QBc[3, h] = -a * loc
    cst["c_QBc"] = QBc.astype(bf)
    cm = np.zeros((128, 17, 128), np.float32)
    for m in range(17):
        valid = (ql - 16 * kl) >= (31 - 128 * m)
        cm[:, m, :] = np.where(valid, 0.0, -30000.0)
    cst["c_cmask"] = cm.astype(bf)
    cc = np.arange(256)
    jj = np.arange(64)
    cover = ((16 * cc[:, None] < 64 * jj[None, :] + 64) & (16 * cc[:, None] + 32 > 64 * jj[None, :])).astype(np.float32)
    cover[255] = 0.0
    cst["c_cover"] = np.ascontiguousarray(cover.reshape(2, 128, 64).transpose(1, 0, 2)).astype(bf)
    tt = (128 * np.arange(32)[None, :, None] + np.arange(128)[:, None, None])
    cur = tt // 64
    j3 = jj[None, None, :]
    forced = (j3 == 0) | (j3 == cur) | (j3 == cur - 1)
    adjv = np.where(j3 <= cur, np.where(forced, 1.0e4, 0.0), -1.0e30).astype(np.float32)
    cst["c_adj"] = np.ascontiguousarray(adjv)
    cst["identf"] = np.eye(128, dtype=np.float32)
    K2s = np.zeros((64, S), np.float32)
    K2s[p // 64, p] = 1.0
    cst["c_K2sel"] = K2s.astype(bf)
    K2w = np.zeros((64, S), np.float32)
    K2w[p // 128, p] = 1.0
    cst["c_K2win"] = K2w.astype(bf)
    QW = np.zeros((64, 4, S), np.float32)
    for h in range(4):
        QW[0:32, h, :] = NSA_SLOPES[h] * 128.0 * (np.arange(32)[:, None] - (p // 128)[None, :])
    cst["c_QW"] = QW.astype(bf)
    As = np.zeros((128, 32, 4), np.float32)
    for h in range(4):
        As[64:128, :, h] = NSA_SLOPES[h] * 64.0 * (np.arange(64)[:, None] - 2 * np.arange(32)[None, :] - 1) + 30000.0
    cst["c_Asel"] = As
    Ev = np.zeros((128, 2, 4), np.float64)
    for h in range(4):
        Ev[:, 0, h] = np.exp(NSA_SLOPES[h] * (np.arange(128) % 64))
        Ev[:, 1, h] = np.exp(NSA_SLOPES[h] * np.arange(128))
    cst["c_Ev"] = Ev.astype(np.float32)
    pp_ = np.arange(128)[:, None, None]
    mm_ = np.arange(8)[None, :, None]
    ch_ = np.arange(128)[None, None, :]
    cst["c_s5mask"] = ((ch_ // 16) == (2 * (mm_ % 4) + pp_ // 64)).astype(np.float32)
    return cst


CONST_SPECS = {"ident": ((128, 128), BF16), "c_KBd": ((4, 36, 128), BF16), "c_QBdil": ((4, 3, 4, 128), BF16),
               "c_masks": ((128, 3, 128), BF16), "c_KBS": ((68, S), BF16), "c_QBabs": ((4, 4, S), BF16),
               "c_KBc": ((4, 32, 128), BF16), "c_QBc": ((4, 4, 128), BF16), "c_cmask": ((128, 17, 128), BF16),
               "c_cover": ((128, 2, 64), BF16), "c_mask01": ((128, 128), BF16), "c_adj": ((128, 32, 64), F32),
               "identf": ((128, 128), F32), "c_s5mask": ((128, 8, 128), F32),
               "c_K2sel": ((64, S), BF16), "c_K2win": ((64, S), BF16), "c_QW": ((64, 4, S), BF16),
               "c_Asel": ((128, 32, 4), F32), "c_Ev": ((128, 2, 4), F32)}


def build(phases=("proj",), layers=(0, 1), ext_in=(), ext_out=()):
    nc = bass.Bass("TRN2", target_bir_lowering=False)
    _UID[0] = 0
    T = Tensors(nc, ext_in, ext_out)
    T.add("xin0", (S, D), F32, kind="ExternalInput")
    for n, shp in PARAM_SHAPES.items():
        T.add(n, shp, F32, kind="ExternalInput")
    for n, (shp, dt_) in CONST_SPECS.items():
        T.add(n, shp, dt_, kind="ExternalInput")
    T.add("projT", (PW, S), BF16)
    T.add("proj", (S, PW), BF16)
    T.add("xin1", (S, D), F32)
    T.add("dacc", (3, S, 264), F32)
    T.add("mix", (S, D), F32)
    T.add("xmid", (S, D), F32)
    T.add("xin2", (S, D), F32, kind="ExternalOutput")
    c = Ctx(nc)
    for l in layers:
        if "proj" in phases:
            phase_proj(c, T, l)
        if "dil" in phases:
            phase_dil(c, T, l)
        if "nsa" in phases:
            phase_nsa(c, T, l)
        if "gla" in phases:
            phase_gla(c, T, l)
        if "s5" in phases:
            phase_s5(c, T, l)
        if "out" in phases:
            phase_out(c, T, l)
        if "ffn" in phases:
            phase_ffn(c, T, l, "xin%d" % (l + 1))
    c.barrier()
    return nc, T


ALL_PHASES = ("proj", "dil", "nsa", "gla", "s5", "out", "ffn")


def kernel(**inputs):
    nc, T = build(phases=ALL_PHASES, layers=(0, 1))
    cst = host_consts()
    x = np.asarray(inputs["x"], dtype=np.float32)
    shared = {}
    for n in T.in_names:
        if n == "xin0":
            continue
        if n in cst:
            shared[n] = cst[n]
        else:
            shared[n] = np.ascontiguousarray(np.asarray(inputs[n], dtype=np.float32))
    maps = []
    for b in range(8):
        m = dict(shared)
        m["xin0"] = np.ascontiguousarray(x[b])
        maps.append(m)
    res = run_bass_kernel_spmd(nc, maps, core_ids=list(range(8)))
    return np.stack([np.asarray(r["xin2"], dtype=np.float32) for r in res.results], axis=0)
```

```python
from contextlib import ExitStack, contextmanager
import math
import numpy as np
import ml_dtypes
import concourse.bass as bass
import concourse.mybir as mybir
from concourse.bass_utils import run_bass_kernel_spmd

F32 = mybir.dt.float32
BF16 = mybir.dt.bfloat16
ALU = mybir.AluOpType
AF = mybir.ActivationFunctionType
AX = mybir.AxisListType

S = 4096
D = 1024
NT = S // 128
PW = 2460
EPS = 1e-6
D_FF = 2752
N_EXP = 8
D_FFE = 3584
OFF = {}
_o = 0
for _n, _w in (("dil_q", 256), ("dil_k", 256), ("dil_v", 256), ("nsa_q", 256),
               ("nsa_k_cmp", 64), ("nsa_v_cmp", 64), ("nsa_k_slc", 64), ("nsa_v_slc", 64),
               ("nsa_k_win", 64), ("nsa_v_win", 64), ("nsa_gate", 12),
               ("gla_q", 128), ("gla_k", 128), ("gla_v", 256), ("gla_a", 16), ("gla_r", 256),
               ("s5_u", 256)):
    OFF[_n] = _o
    _o += _w
assert _o == PW

NDMA = 24
SEM_LIMIT = 24000

FM_RANGES = [(0, 512), (768, 1216), (1280, 1344), (1420, 1676), (1932, 1948), (2204, 2460)]
TM_RANGES = [(512, 256), (1216, 64), (1344, 76), (1676, 256), (1948, 256)]
TM_STORES = [(512, 256), (1216, 64), (1344, 76), (1676, 256), (1948, 256)]
FM_TILES = []
for _a, _b in FM_RANGES:
    _c = _a
    while _c < _b:
        FM_TILES.append((_c, min(128, _b - _c)))
        _c += 128


class Buf:
    __slots__ = ("name", "w", "r")

    def __init__(self, name=""):
        self.name = name
        self.w = {}
        self.r = {}


class Ctx:
    ENG = ("pe", "act", "dve", "pool", "sp")

    def __init__(self, nc):
        self.nc = nc
        self.eng = {"pe": nc.tensor, "act": nc.scalar, "dve": nc.vector,
                    "pool": nc.gpsimd, "sp": nc.sync}
        self.gen = {e: 0 for e in self.ENG}
        self.sem = {e: nc.alloc_semaphore("s_%s_0" % e) for e in self.ENG}
        self.tick = {e: 0 for e in self.ENG}
        self.seen = {e: {} for e in self.ENG}
        self.dsem = [nc.alloc_semaphore("s_dma_%d" % i) for i in range(NDMA)]
        self.dval = [0] * NDMA
        self.dn = 0
        self.dgen = 0
        self.strict = {"act", "dve", "pool"}
        self.n_ops = 0
        self.rr = 0
        self.last_pool = None

    def _need(self, e, waits, key, val, same_ok):
        if key[0] == "e" and key[1] == e and not same_ok:
            return
        if self.seen[e].get(key, 0) >= val:
            return
        if waits.get(key, 0) < val:
            waits[key] = val

    def _ekey(self, e):
        return ("e", e, self.gen[e])

    def _emit_waits(self, e, waits):
        eng = self.eng[e]
        for key, val in waits.items():
            if key[0] == "d":
                if key[2] != self.dgen:
                    continue
                s = self.dsem[key[1]]
            else:
                if key[2] != self.gen[key[1]]:
                    continue
                s = self.sem[key[1]]
            eng.wait_ge(s, val)
            self.seen[e][key] = val

    def op(self, e, fn, reads=(), writes=()):
        waits = {}
        st = e in self.strict
        for b in reads:
            for key, val in b.w.items():
                self._need(e, waits, key, val, st)
        for b in writes:
            for key, val in b.w.items():
                self._need(e, waits, key, val, st)
            for key, val in b.r.items():
                self._need(e, waits, key, val, False)
        self._emit_waits(e, waits)
        ins = fn()
        self.tick[e] += 1
        ins.then_inc(self.sem[e], 1)
        key = self._ekey(e)
        val = self.tick[e]
        for b in reads:
            b.r[key] = val
        for b in writes:
            b.w[key] = val
            b.r = {}
        self.n_ops += 1
        return ins

    def dma(self, out, in_, reads=(), writes=(), q="sp", **kw):
        e = q
        waits = {}
        for b in reads:
            for key, val in b.w.items():
                self._need(e, waits, key, val, True)
        for b in writes:
            for key, val in b.w.items():
                if key[0] == "d":
                    continue
                self._need(e, waits, key, val, True)
            for key, val in b.r.items():
                self._need(e, waits, key, val, True)
        i = self.dn % NDMA
        self.dn += 1
        dkey = ("d", i, self.dgen)
        if self.dval[i] > 0:
            self._need(e, waits, dkey, self.dval[i], True)
        if e == "pool" and self.last_pool is not None:
            self._need(e, waits, self.last_pool[0], self.last_pool[1], True)
        self._emit_waits(e, waits)
        ins = self.eng[e].dma_start(out=out, in_=in_, **kw)
        self.dval[i] += 16
        ins.then_inc(self.dsem[i], 16)
        if e == "pool":
            self.last_pool = (dkey, self.dval[i])
        for b in reads:
            b.r[dkey] = self.dval[i]
        for b in writes:
            b.w[dkey] = self.dval[i]
        return ins

    def barrier(self):
        for e in self.ENG:
            waits = {}
            for o in self.ENG:
                if o != e and self.tick[o] > 0:
                    self._need(e, waits, self._ekey(o), self.tick[o], True)
            for i in range(NDMA):
                if self.dval[i] > 0:
                    self._need(e, waits, ("d", i, self.dgen), self.dval[i], True)
            self._emit_waits(e, waits)
        for e in self.ENG:
            if self.tick[e] > SEM_LIMIT:
                self.gen[e] += 1
                self.sem[e] = self.nc.alloc_semaphore("s_%s_%d" % (e, self.gen[e]))
                self.tick[e] = 0
        if any(v > SEM_LIMIT for v in self.dval):
            self.dgen += 1
            self.dsem = [self.nc.alloc_semaphore("s_dma_%d_g%d" % (i, self.dgen)) for i in range(NDMA)]
            self.dval = [0] * NDMA

    def evac_engine(self):
        self.rr += 1
        return "act" if (self.rr & 1) else "dve"

    def copy(self, out, in_, reads, writes, e=None):
        nc = self.nc
        e = e or self.evac_engine()
        if e == "act":
            return self.op("act", lambda: nc.scalar.copy(out=out, in_=in_), reads, writes)
        if e == "pool":
            return self.op("pool", lambda: nc.gpsimd.tensor_copy(out=out, in_=in_), reads, writes)
        return self.op("dve", lambda: nc.vector.tensor_copy(out=out, in_=in_), reads, writes)

    def mm(self, out, lhsT, rhs, start, stop, reads, writes, **kw):
        nc = self.nc
        return self.op("pe", lambda: nc.tensor.matmul(out, lhsT=lhsT, rhs=rhs, start=start, stop=stop, **kw),
                       reads, writes)


@contextmanager
def scope(c):
    with ExitStack() as es:
        yield es
        c.barrier()


_UID = [0]


def uniq(name):
    _UID[0] += 1
    return "%s_u%d" % (name, _UID[0])


class Rot:
    def __init__(self, nc, es, name, shape, dtype, n, psum=False):
        self.t = []
        self.b = []
        name = uniq(name)
        for i in range(n):
            nm = "%s_%d" % (name, i)
            if psum:
                t = es.enter_context(nc.psum_tensor(nm, shape, dtype))
            else:
                t = es.enter_context(nc.sbuf_tensor(nm, shape, dtype))
            self.t.append(t)
            self.b.append(Buf(nm))
        self.i = 0

    def next(self):
        k = self.i % len(self.t)
        self.i += 1
        return self.t[k], self.b[k]


def sb(nc, es, name, shape, dtype):
    name = uniq(name)
    return es.enter_context(nc.sbuf_tensor(name, shape, dtype)), Buf(name)


def ps(nc, es, name, shape, dtype):
    name = uniq(name)
    return es.enter_context(nc.psum_tensor(name, shape, dtype)), Buf(name)


def norm_pools(nc, es, pfx, need_xt=True):
    xt = Rot(nc, es, pfx + "xt", [128, D], F32, 2) if need_xt else None
    junk = Rot(nc, es, pfx + "jk", [128, D], BF16, 2)
    hb = Rot(nc, es, pfx + "hb", [128, D], BF16, 2)
    ss = Rot(nc, es, pfx + "ss", [128, 2], F32, 4)
    tp = Rot(nc, es, pfx + "tp", [128, 1024], BF16, 2, psum=True)
    return xt, junk, hb, ss, tp


def norm_transpose_steps(c, pools, x_d, Bx_d, gB, BgB, ident, Bid, hT, BhT, tile0, ntiles, keep_x=None):
    nc = c.nc
    xt, junk, hb, ss, tp = pools
    held = {}

    def make_n(i):
        def n_step():
            t = tile0 + i
            if keep_x is not None:
                x_t = keep_x[0][:, i, :]
                Bx = keep_x[1]
            else:
                xx, Bx = xt.next()
                x_t = xx[:]
            c.dma(x_t, x_d[t * 128:(t + 1) * 128, :], reads=[Bx_d], writes=[Bx])
            jk, Bjk = junk.next()
            s_, Bs = ss.next()
            c.op("act", lambda: nc.scalar.activation(out=jk[:], in_=x_t, func=AF.Square,
                                                     accum_out=s_[:, 0:1]), [Bx], [Bjk, Bs])
            c.op("act", lambda: nc.scalar.activation(out=s_[:, 1:2], in_=s_[:, 0:1], func=AF.Sqrt,
                                                     bias=gB[:, D:D + 1], scale=1.0 / D), [Bs, BgB], [Bs])
            c.op("dve", lambda: nc.vector.reciprocal(out=s_[:, 1:2], in_=s_[:, 1:2]), [Bs], [Bs])
            h_, Bh = hb.next()
            c.op("dve", lambda: nc.vector.scalar_tensor_tensor(out=h_[:], in0=x_t, scalar=s_[:, 1:2],
                                                               in1=gB[:, 0:D], op0=ALU.mult, op1=ALU.mult),
                 [Bx, Bs, BgB], [Bh])
            held[i] = (h_, Bh)
        return n_step

    def make_t(i):
        def t_step():
            h_, Bh = held.pop(i)
            for g in range(2):
                p_, Bp = tp.next()
                for j in range(4):
                    kc = g * 4 + j
                    c.op("pe", lambda: nc.tensor.transpose(out=p_[:, j * 128:(j + 1) * 128],
                                                           in_=h_[:, kc * 128:(kc + 1) * 128],
                                                           identity=ident[:]), [Bh, Bid], [Bp])
                c.copy(hT[:, g * 4:(g + 1) * 4, i * 128:(i + 1) * 128],
                       p_[:, 0:512].rearrange("p (j t) -> p j t", j=4), [Bp], [BhT])
        return t_step

    steps = []
    for i in range(ntiles):
        steps.append(make_n(i))
        if i >= 1:
            steps.append(make_t(i - 1))
    steps.append(make_t(ntiles - 1))
    return steps


def norm_transpose(c, es, x_d, Bx_d, gB, BgB, ident, Bid, hT, BhT, tile0, ntiles, pfx,
                   keep_x=None, pools=None):
    nc = c.nc
    if pools is None:
        pools = norm_pools(nc, es, pfx, keep_x is None)
    for st_ in norm_transpose_steps(c, pools, x_d, Bx_d, gB, BgB, ident, Bid, hT, BhT, tile0, ntiles, keep_x):
        st_()


def load_gB(c, es, g_row, Bg_d, name):
    nc = c.nc
    gB, BgB = sb(nc, es, name, [128, D + 1], F32)
    c.dma(gB[:, 0:D], g_row.partition_broadcast(128), reads=[Bg_d], writes=[BgB])
    c.op("dve", lambda: nc.vector.memset(gB[:, D:D + 1], EPS), [], [BgB])
    return gB, BgB


def phase_proj(c, T, l):
    nc = c.nc
    with scope(c) as es:
        W, BW = sb(nc, es, "pj_W", [128, 8, PW], BF16)
        ident, Bid = sb(nc, es, "pj_id", [128, 128], BF16)
        c.dma(ident[:], T["ident"][:, :], reads=[T.B["ident"]], writes=[Bid])
        gB, BgB = load_gB(c, es, T["norm1_g"][l], T.B["norm1_g"], "pj_g")
        wv = T["w_in"][l].rearrange("(kc p) n -> p kc n", p=128)
        for kc in range(8):
            c.dma(W[:, kc, :], wv[:, kc, :], reads=[T.B["w_in"]], writes=[BW], q="pool")
        hTs = [sb(nc, es, "pj_hT%d" % i, [128, 8, 2048], BF16) for i in range(2)]
        stf = Rot(nc, es, "pj_stf", [128, 2048], BF16, 2)
        stt = Rot(nc, es, "pj_stt", [128, PW], BF16, 2)
        acc = Rot(nc, es, "pj_acc", [128, 512], F32, 4, psum=True)
        x_d = T["xin%d" % l]
        for half in range(2):
            hT, BhT = hTs[half]
            with scope(c) as es2:
                norm_transpose(c, es2, x_d, T.B["xin%d" % l], gB, BgB, ident, Bid, hT, BhT,
                               half * 16, 16, "pj%d" % half)
            c.barrier()
            for (c0, M) in FM_TILES:
                st, Bst = stf.next()
                for tt in range(4):
                    a_, Ba = acc.next()
                    for k in range(8):
                        c.mm(a_[0:M, :], W[:, k, c0:c0 + M], hT[:, k, tt * 512:(tt + 1) * 512],
                             k == 0, k == 7, [BW, BhT], [Ba])
                    c.copy(st[0:M, tt * 512:(tt + 1) * 512], a_[0:M, :], [Ba], [Bst])
                c.dma(T["projT"][c0:c0 + M, half * 2048:(half + 1) * 2048], st[0:M, :],
                      reads=[Bst], writes=[T.B["projT"]])
            for i in range(16):
                st, Bst = stt.next()
                t = half * 16 + i
                for (c0, N) in TM_RANGES:
                    a_, Ba = acc.next()
                    for k in range(8):
                        c.mm(a_[:, 0:N], hT[:, k, i * 128:(i + 1) * 128], W[:, k, c0:c0 + N],
                             k == 0, k == 7, [BW, BhT], [Ba])
                    c.copy(st[:, c0:c0 + N], a_[:, 0:N], [Ba], [Bst])
                for (c0, N) in TM_STORES:
                    c.dma(T["proj"][t * 128:(t + 1) * 128, c0:c0 + N], st[:, c0:c0 + N], reads=[Bst],
                          writes=[T.B["proj"]])
    c.barrier()


DIL_SLOPES = [2.0 ** -2, 2.0 ** -4, 2.0 ** -6, 2.0 ** -8]
NSA_SLOPES = [2.0 ** -1, 2.0 ** -3, 2.0 ** -5, 2.0 ** -7]
DIL_PATTERNS = ((128, 1), (512, 4), (2048, 16))


def ssl(start, count, step):
    return slice(start, start + step * (count - 1) + 1, step)


def load_col(c, es, name, vec_ap, Bsrc, n, mul=None):
    nc = c.nc
    t, B = sb(nc, es, name, [n, 1], F32)
    c.dma(t[:, 0:1], vec_ap.rearrange("(p o) -> p o", o=1), reads=[Bsrc], writes=[B])
    if mul is not None:
        c.op("act", lambda: nc.scalar.mul(out=t[:], in_=t[:], mul=mul), [B], [B])
    return t, B


def prep_qk(c, es, T, row0, H, gcol, Bg, ones64, Bones, epsc, Beps, dst, Bdst, pfx):
    nc = c.nc
    raw = Rot(nc, es, pfx + "raw", [64, S], BF16, 2)
    sq = Rot(nc, es, pfx + "sq", [64, 512], BF16, 2)
    rs = Rot(nc, es, pfx + "rs", [64, 512], F32, 2)
    pp = Rot(nc, es, pfx + "pp", [64, 512], F32, 2, psum=True)
    for h in range(H):
        r_, Br = raw.next()
        c.dma(r_[:], T["projT"][row0 + h * 64:row0 + (h + 1) * 64, :], reads=[T.B["projT"]], writes=[Br])
        for tt in range(8):
            sl = slice(tt * 512, (tt + 1) * 512)
            q_, Bq = sq.next()
            c.op("act", lambda: nc.scalar.activation(out=q_[:], in_=r_[:, sl], func=AF.Square), [Br], [Bq])
            p_, Bp = pp.next()
            c.mm(p_[:], ones64[:], q_[:], True, True, [Bq, Bones], [Bp])
            s_, Bs = rs.next()
            c.op("act", lambda: nc.scalar.activation(out=s_[:], in_=p_[:], func=AF.Sqrt, bias=epsc[0:64, 0:1],
                                                     scale=1.0 / 64), [Bp, Beps], [Bs])
            c.op("dve", lambda: nc.vector.reciprocal(out=s_[:], in_=s_[:]), [Bs], [Bs])
            c.op("dve", lambda: nc.vector.scalar_tensor_tensor(out=dst[0:64, h, sl], in0=r_[:, sl],
                                                               scalar=gcol[:, 0:1], in1=s_[:],
                                                               op0=ALU.mult, op1=ALU.mult),
                 [Br, Bs, Bg], [Bdst])


def load_consts_small(c, es, T, pfx):
    nc = c.nc
    ones64, Bones = sb(nc, es, pfx + "ones64", [64, 64], BF16)
    c.op("dve", lambda: nc.vector.memset(ones64[:], 1.0), [], [Bones])
    epsc, Beps = sb(nc, es, pfx + "epsc", [128, 1], F32)
    c.op("dve", lambda: nc.vector.memset(epsc[:], EPS), [], [Beps])
    return ones64, Bones, epsc, Beps


def phase_dil(c, T, l):
    nc = c.nc
    with scope(c) as es:
        ones64, Bones, epsc, Beps = load_consts_small(c, es, T, "dl_")
        qT, BqT = sb(nc, es, "dl_qT", [64, 4, S], BF16)
        kT, BkT = sb(nc, es, "dl_kT", [64, 4, S], BF16)
        KB, BKB = sb(nc, es, "dl_KB", [4, 36, 128], BF16)
        c.dma(KB[:], T["c_KBd"][:, :, :], reads=[T.B["c_KBd"]], writes=[BKB])
        QB, BQB = sb(nc, es, "dl_QB", [4, 3, 4, 128], BF16)
        c.dma(QB[:], T["c_QBdil"][:, :, :, :], reads=[T.B["c_QBdil"]], writes=[BQB])
        msk, Bmsk = sb(nc, es, "dl_msk", [128, 3, 128], BF16)
        c.dma(msk[:], T["c_masks"][:, :, :], reads=[T.B["c_masks"]], writes=[Bmsk])
        with scope(c) as es2:
            gq, Bgq = load_col(c, es2, "dl_gq", T["dil_qk_g"][l, 0], T.B["dil_qk_g"], 64, mul=0.125)
            gk, Bgk = load_col(c, es2, "dl_gk", T["dil_qk_g"][l, 1], T.B["dil_qk_g"], 64)
            with scope(c) as es3:
                prep_qk(c, es3, T, OFF["dil_q"], 4, gq, Bgq, ones64, Bones, epsc, Beps, qT, BqT, "dlq")
            c.barrier()
            with scope(c) as es3:
                prep_qk(c, es3, T, OFF["dil_k"], 4, gk, Bgk, ones64, Bones, epsc, Beps, kT, BkT, "dlk")
        T.dbg(c, "dbg_qT", qT[:], (64, 4, S), BF16, BqT)
        T.dbg(c, "dbg_kT", kT[:], (64, 4, S), BF16, BkT)
        Vp = []
        for pi in range(3):
            v_, Bv = sb(nc, es, "dl_V%d" % pi, [128, 32, 4, 66], BF16)
            c.op("dve", lambda: nc.vector.memset(v_[:], 1.0), [], [Bv])
            Vp.append((v_, Bv))
        with scope(c) as es2:
            stg = Rot(nc, es2, "dl_vst", [128, 8, 256], BF16, 2)
            for pi, (win, d) in enumerate(DIL_PATTERNS):
                v_, Bv = Vp[pi]
                bpc = 32 // d
                for r in range(d):
                    for b0 in range(0, bpc, 8):
                        nb = min(8, bpc - b0)
                        s_, Bs = stg.next()
                        src = T["proj"][ssl(r + d * 128 * b0, 128 * nb, d), OFF["dil_v"]:OFF["dil_v"] + 256]
                        c.dma(s_[:, 0:nb, :], src.rearrange("(b k) c -> k b c", k=128),
                              reads=[T.B["proj"]], writes=[Bs])
                        blk0 = r * bpc + b0
                        c.copy(v_[:, blk0:blk0 + nb, :, 0:64],
                               s_[:, 0:nb, :].rearrange("k b (h e) -> k b h e", h=4), [Bs], [Bv])
        c.barrier()
        for pi in range(3):
            T.dbg(c, "dbg_V%d" % pi, Vp[pi][0][:], (128, 32, 4, 66), BF16, Vp[pi][1])
        Sps = Rot(nc, es, "dl_S", [128, 512], F32, 3, psum=True)
        Ops = Rot(nc, es, "dl_O", [128, 512], F32, 2, psum=True)
        Pt = Rot(nc, es, "dl_P", [128, 512], BF16, 5)
        Mt = Rot(nc, es, "dl_M", [128, 512], F32, 3)
        Osb = Rot(nc, es, "dl_Osb", [128, 264], F32, 3)
        SKEW = 2
        tasks = []
        for pi, (win, d) in enumerate(DIL_PATTERNS):
            bpc = 32 // d
            for r in range(d):
                for bi in range(bpc):
                    kbs = [bi - 1, bi] if bi >= 1 else [bi]
                    for i_, kbi in enumerate(kbs):
                        tasks.append((pi, d, bpc, r, bi, kbi, i_ == 0, i_ == len(kbs) - 1))
        st = {}
        cur_o = [None]

        def stage_a(ti):
            pi, d, bpc, r, bi, kbi, is_first, is_last = tasks[ti]
            qsl = ssl(r + d * 128 * bi, 128, d)
            ksl = ssl(r + d * 128 * kbi, 128, d)
            s_, Bs = Sps.next()
            for h in range(4):
                c.mm(s_[:, h * 128:(h + 1) * 128], kT[:, h, ksl], qT[:, h, qsl], True, False,
                     [BkT, BqT], [Bs])
                c.mm(s_[:, h * 128:(h + 1) * 128], KB[:, 32 + kbi - bi, :],
                     QB[:, pi, h, :], False, True, [BKB, BQB], [Bs])
            p_, Bp = Pt.next()
            mi = 0 if kbi == bi else 1
            m_, Bm = Mt.next()
            c.op("dve", lambda: nc.vector.tensor_tensor(
                out=m_[:].rearrange("k (h q) -> k h q", h=4),
                in0=s_[:].rearrange("k (h q) -> k h q", h=4),
                in1=msk[:, mi:mi + 1, :].to_broadcast([128, 4, 128]), op=ALU.add), [Bs, Bmsk], [Bm])
            c.op("act", lambda: nc.scalar.activation(out=p_[:], in_=m_[:], func=AF.Exp), [Bm], [Bp])
            st[ti] = (p_, Bp)

        def stage_c(ti):
            pi, d, bpc, r, bi, kbi, is_first, is_last = tasks[ti]
            v_, Bv = Vp[pi]
            kblk = r * bpc + kbi
            p_, Bp = st.pop(ti)
            if is_first:
                cur_o[0] = Ops.next()
            o_, Bo = cur_o[0]
            for h in range(4):
                c.mm(o_[:, h * 66:(h + 1) * 66], p_[:, h * 128:(h + 1) * 128], v_[:, kblk, h, :],
                     is_first and h == 0, False, [Bp, Bv], [Bo], skip_group_check=True)
            if is_last:
                ob, Bob = Osb.next()
                c.copy(ob[:], o_[:, 0:264], [Bo], [Bob])
                dst = T["dacc"][pi, ssl(r + d * 128 * bi, 128, d), :]
                c.dma(dst, ob[:], reads=[Bob], writes=[T.B["dacc"]])

        for ti in range(len(tasks) + SKEW):
            if ti < len(tasks):
                stage_a(ti)
            if ti - SKEW >= 0:
                stage_c(ti - SKEW)
    c.barrier()
    with scope(c) as es:
        a3 = Rot(nc, es, "dc_a", [128, 3, 264], F32, 3)
        rc = Rot(nc, es, "dc_r", [128, 4], F32, 3)
        yo = Rot(nc, es, "dc_y", [128, 256], F32, 3)
        for t in range(NT):
            a_, Ba = a3.next()
            c.dma(a_[:], T["dacc"][:, t * 128:(t + 1) * 128, :].rearrange("p t c -> t p c"),
                  reads=[T.B["dacc"]], writes=[Ba])
            c.op("dve", lambda: nc.vector.tensor_tensor(out=a_[:, 0, :], in0=a_[:, 0, :], in1=a_[:, 1, :], op=ALU.add),
                 [Ba], [Ba])
            c.op("dve", lambda: nc.vector.tensor_tensor(out=a_[:, 0, :], in0=a_[:, 0, :], in1=a_[:, 2, :], op=ALU.add),
                 [Ba], [Ba])
            av = a_[:, 0, :].rearrange("t (h e) -> t h e", h=4)
            r_, Br = rc.next()
            c.op("dve", lambda: nc.vector.reciprocal(out=r_[:].rearrange("t (h o) -> t h o", o=1), in_=av[:, :, 64:65]),
                 [Ba], [Br])
            y_, By = yo.next()
            c.op("dve", lambda: nc.vector.tensor_tensor(
                out=y_[:].rearrange("t (h e) -> t h e", h=4), in0=av[:, :, 0:64],
                in1=r_[:].rearrange("t (h o) -> t h o", o=1).to_broadcast([128, 4, 64]), op=ALU.mult),
                [Ba, Br], [By])
            c.dma(T["mix"][t * 128:(t + 1) * 128, 0:256], y_[:], reads=[By], writes=[T.B["mix"]])
    c.barrier()


NSA_STAGE = 99


def phase_nsa(c, T, l):
    _phase_nsa(c, T, l)
    c.barrier()


def _phase_nsa(c, T, l):
    nc = c.nc
    with scope(c) as es:
        ones64, Bones, epsc, Beps = load_consts_small(c, es, T, "ns_")
        ident, Bid = sb(nc, es, "ns_id", [128, 128], BF16)
        c.dma(ident[:], T["ident"][:, :], reads=[T.B["ident"]], writes=[Bid])
        qT, BqT = sb(nc, es, "ns_Q2s", [128, 4, S], BF16)
        Q2w, BQ2w = sb(nc, es, "ns_Q2w", [128, 4, S], BF16)
        c.dma(Q2w[64:128, :, :], T["c_QW"][:, :, :], reads=[T.B["c_QW"]], writes=[BQ2w])
        ksT, BksT = sb(nc, es, "ns_Ks2", [128, 1, S], BF16)
        c.dma(ksT[64:128, 0, :], T["c_K2sel"][:, :], reads=[T.B["c_K2sel"]], writes=[BksT])
        kwT, BkwT = sb(nc, es, "ns_Kw2", [128, 1, S], BF16)
        c.dma(kwT[64:128, 0, :], T["c_K2win"][:, :], reads=[T.B["c_K2win"]], writes=[BkwT])
        Asel, BAsel = sb(nc, es, "ns_Asel", [128, 32, 4], F32)
        c.dma(Asel[:], T["c_Asel"][:, :, :], reads=[T.B["c_Asel"]], writes=[BAsel])
        Vs, BVs = sb(nc, es, "ns_Vsh", [128, 32, 4, 66], BF16)
        Vw, BVw = sb(nc, es, "ns_Vwh", [128, 32, 4, 66], BF16)
        with scope(c) as es2:
            Ev, BEv = sb(nc, es2, "ns_Ev", [128, 2, 4], F32)
            c.dma(Ev[:], T["c_Ev"][:, :, :], reads=[T.B["c_Ev"]], writes=[BEv])
            for vi, (vh_, Bvh, nm) in enumerate(((Vs, BVs, "nsa_v_slc"), (Vw, BVw, "nsa_v_win"))):
                v_, Bv = sb(nc, es2, "ns_Vst%d" % vi, [128, 32, 66], BF16)
                c.op("dve", lambda: nc.vector.memset(v_[:], 1.0), [], [Bv])
                c.dma(v_[:, :, 0:64], T["proj"][:, OFF[nm]:OFF[nm] + 64].rearrange("(b k) e -> k b e", k=128),
                      reads=[T.B["proj"]], writes=[Bv])
                for h in range(4):
                    c.op("dve", lambda: nc.vector.tensor_scalar(out=vh_[:, :, h, :], in0=v_[:], scalar1=Ev[:, vi, h:h + 1],
                                                                scalar2=None, op0=ALU.mult), [Bv, BEv], [Bvh])
        kcT, BkcT = sb(nc, es, "ns_kcT", [64, 256], BF16)
        Vc, BVc = sb(nc, es, "ns_Vc", [128, 2, 130], BF16)
        c.op("dve", lambda: nc.vector.memset(Vc[:], 0.0), [], [BVc])
        c.op("dve", lambda: nc.vector.memset(Vc[:, :, 128:130], 1.0), [], [BVc])
        c.dma(Vc[:, :, 64:128], T["c_cover"][:, :, :], reads=[T.B["c_cover"]], writes=[BVc])
        with scope(c) as es2:
            g0, Bg0 = load_col(c, es2, "ns_g0", T["nsa_qk_g"][l, 0], T.B["nsa_qk_g"], 64, mul=0.125)
            g2, Bg2 = load_col(c, es2, "ns_g2", T["nsa_qk_g"][l, 2], T.B["nsa_qk_g"], 64)
            g3, Bg3 = load_col(c, es2, "ns_g3", T["nsa_qk_g"][l, 3], T.B["nsa_qk_g"], 64)
            with scope(c) as es3:
                prep_qk(c, es3, T, OFF["nsa_q"], 4, g0, Bg0, ones64, Bones, epsc, Beps, qT, BqT, "nsq")
            c.barrier()
            for h in range(4):
                c.op("act", lambda: nc.scalar.copy(out=Q2w[0:64, h, :], in_=qT[0:64, h, :]), [BqT], [BQ2w])
            with scope(c) as es3:
                prep_qk(c, es3, T, OFF["nsa_k_slc"], 1, g2, Bg2, ones64, Bones, epsc, Beps, ksT, BksT, "nss")
            with scope(c) as es3:
                prep_qk(c, es3, T, OFF["nsa_k_win"], 1, g3, Bg3, ones64, Bones, epsc, Beps, kwT, BkwT, "nsw")
            c.barrier()
        c.barrier()
        if NSA_STAGE <= 1:
            return
        with scope(c) as es2:
            g1, Bg1 = load_col(c, es2, "ns_g1", T["nsa_qk_g"][l, 1], T.B["nsa_qk_g"], 64)
            raw, Braw = sb(nc, es2, "ns_raw", [64, 2, S], BF16)
            c.dma(raw[:], T["projT"][OFF["nsa_k_cmp"]:OFF["nsa_k_cmp"] + 128, :].rearrange("(i d) t -> d i t", i=2),
                  reads=[T.B["projT"]], writes=[Braw])
            w1, Bw1 = sb(nc, es2, "ns_w1", [64, 2, 32, 256], BF16)
            w2, Bw2 = sb(nc, es2, "ns_w2", [128, 2, 2, 64], BF16)
            posT, BposT = sb(nc, es2, "ns_posT", [64, 2, 32], BF16)
            for i in range(2):
                c.dma(w1[:, i, :, :], T["nsa_cmp_w1"][l, i].rearrange("(l d) h -> d l h", d=64),
                      reads=[T.B["nsa_cmp_w1"]], writes=[Bw1], q="pool")
                c.dma(w2[:, i, :, :], T["nsa_cmp_w2"][l, i].rearrange("(hc p) e -> p hc e", p=128),
                      reads=[T.B["nsa_cmp_w2"]], writes=[Bw2], q="pool")
                c.dma(posT[:, i, :], T["nsa_cmp_pos"][l, i].rearrange("l d -> d l"),
                      reads=[T.B["nsa_cmp_pos"]], writes=[BposT], q="pool", allow_slow_non_contiguous=True)
            Hps = Rot(nc, es2, "ns_Hps", [128, 512], F32, 2, psum=True)
            bps = Rot(nc, es2, "ns_bps", [128, 512], F32, 2, psum=True)
            b1 = Rot(nc, es2, "ns_b1", [128, 2], F32, 2)
            zt = Rot(nc, es2, "ns_z", [128, 256], F32, 2)
            ut = Rot(nc, es2, "ns_u", [128, 256], F32, 2)
            G, BG = sb(nc, es2, "ns_G", [128, 2, 2, 256], BF16)
            c.op("dve", lambda: nc.vector.memset(G[:], 0.0), [], [BG])
            for i in range(2):
                for hc in range(2):
                    h_, Bh = Hps.next()
                    for ll in range(32):
                        c.mm(h_[:, 0:255], w1[:, i, ll, hc * 128:(hc + 1) * 128], raw[:, i, ssl(ll, 255, 16)],
                             ll == 0, ll == 31, [Bw1, Braw], [Bh])
                    bp, Bbp = bps.next()
                    for ll in range(32):
                        c.mm(bp[:, 0:2], w1[:, i, ll, hc * 128:(hc + 1) * 128], posT[:, i, ll:ll + 1].to_broadcast([64, 2]),
                             ll == 0, ll == 31, [Bw1, BposT], [Bbp])
                    b_, Bb = b1.next()
                    c.op("dve", lambda: nc.vector.tensor_copy(out=b_[:], in_=bp[:, 0:2]), [Bbp], [Bb])
                    z_, Bz = zt.next()
                    c.op("act", lambda: nc.scalar.activation(out=z_[:, 0:255], in_=h_[:, 0:255], func=AF.Identity,
                                                             bias=b_[:, 0:1]), [Bh, Bb], [Bz])
                    u_, Bu = ut.next()
                    c.op("dve", lambda: nc.vector.tensor_tensor(out=u_[:, 0:255], in0=z_[:, 0:255], in1=z_[:, 0:255],
                                                                op=ALU.mult), [Bz], [Bu])
                    c.op("dve", lambda: nc.vector.tensor_scalar(out=u_[:, 0:255], in0=u_[:, 0:255], scalar1=0.044715,
                                                                scalar2=1.0, op0=ALU.mult, op1=ALU.add), [Bu], [Bu])
                    c.op("dve", lambda: nc.vector.tensor_tensor(out=u_[:, 0:255], in0=u_[:, 0:255], in1=z_[:, 0:255],
                                                                op=ALU.mult), [Bu, Bz], [Bu])
                    c.op("act", lambda: nc.scalar.activation(out=u_[:, 0:255], in_=u_[:, 0:255], func=AF.Sigmoid,
                                                             scale=1.5957691216057308), [Bu], [Bu])
                    c.op("dve", lambda: nc.vector.tensor_tensor(out=G[:, i, hc, 0:255], in0=u_[:, 0:255],
                                                                in1=z_[:, 0:255], op=ALU.mult), [Bu, Bz], [BG])
            T.dbg(c, "dbg_w1", w1[:], (64, 2, 32, 256), BF16, Bw1)
            T.dbg(c, "dbg_posT", posT[:], (64, 2, 32), BF16, BposT)
            T.dbg(c, "dbg_G", G[:], (128, 2, 2, 256), BF16, BG)
            T.dbg(c, "dbg_w2", w2[:], (128, 2, 2, 64), BF16, Bw2)
            kp, Bkp = Hps.next()
            for hc in range(2):
                c.mm(kp[0:64, 0:256], w2[:, 0, hc, :], G[:, 0, hc, :], hc == 0, hc == 1, [Bw2, BG], [Bkp])
            kraw, Bkraw = sb(nc, es2, "ns_kraw", [64, 256], F32)
            c.op("dve", lambda: nc.vector.tensor_copy(out=kraw[:], in_=kp[0:64, 0:256]), [Bkp], [Bkraw])
            ksq, Bksq = sb(nc, es2, "ns_ksq", [64, 256], BF16)
            c.op("act", lambda: nc.scalar.activation(out=ksq[:], in_=kraw[:], func=AF.Square), [Bkraw], [Bksq])
            sp_, Bsp = bps.next()
            c.mm(sp_[0:64, 0:256], ones64[:], ksq[:], True, True, [Bksq, Bones], [Bsp])
            krs, Bkrs = sb(nc, es2, "ns_krs", [64, 256], F32)
            c.op("act", lambda: nc.scalar.activation(out=krs[:], in_=sp_[0:64, 0:256], func=AF.Sqrt,
                                                     bias=epsc[0:64, 0:1], scale=1.0 / 64), [Bsp, Beps], [Bkrs])
            c.op("dve", lambda: nc.vector.reciprocal(out=krs[:], in_=krs[:]), [Bkrs], [Bkrs])
            c.op("dve", lambda: nc.vector.scalar_tensor_tensor(out=kcT[:], in0=kraw[:], scalar=g1[:, 0:1], in1=krs[:],
                                                               op0=ALU.mult, op1=ALU.mult), [Bkraw, Bkrs, Bg1], [BkcT])
            for ct in range(2):
                rows = 128 if ct == 0 else 127
                vp_, Bvp = Hps.next()
                for hc in range(2):
                    c.mm(vp_[0:rows, 0:64], G[:, 1, hc, ct * 128:ct * 128 + rows], w2[:, 1, hc, :], hc == 0, hc == 1,
                         [BG, Bw2], [Bvp])
                c.op("dve", lambda: nc.vector.tensor_copy(out=Vc[0:rows, ct, 0:64], in_=vp_[0:rows, 0:64]), [Bvp], [BVc])
        c.barrier()
        KBc, BKBc = sb(nc, es, "ns_KBc", [4, 32, 128], BF16)
        c.dma(KBc[:], T["c_KBc"][:, :, :], reads=[T.B["c_KBc"]], writes=[BKBc])
        QBc, BQBc = sb(nc, es, "ns_QBc", [4, 4, 128], BF16)
        c.dma(QBc[:], T["c_QBc"][:, :, :], reads=[T.B["c_QBc"]], writes=[BQBc])
        cmask, Bcmask = sb(nc, es, "ns_cmask", [128, 17, 128], BF16)
        c.dma(cmask[:], T["c_cmask"][:, :, :], reads=[T.B["c_cmask"]], writes=[Bcmask])
        msk, Bmsk = sb(nc, es, "ns_msk", [128, 3, 128], BF16)
        c.dma(msk[:], T["c_masks"][:, :, :], reads=[T.B["c_masks"]], writes=[Bmsk])
        adj, Badj = sb(nc, es, "ns_adj", [128, 32, 64], F32)
        c.dma(adj[:], T["c_adj"][:, :, :], reads=[T.B["c_adj"]], writes=[Badj])
        gates, Bgates = sb(nc, es, "ns_gates", [128, 32, 12], F32)
        yacc, Byacc = sb(nc, es, "ns_yacc", [128, 32, 256], F32)
        with scope(c) as es2:
            gst, Bgst = sb(nc, es2, "ns_gst", [128, 32, 12], BF16)
            c.dma(gst[:], T["proj"][:, OFF["nsa_gate"]:OFF["nsa_gate"] + 12].rearrange("(b k) e -> k b e", k=128),
                  reads=[T.B["proj"]], writes=[Bgst])
            c.op("act", lambda: nc.scalar.activation(out=gates[:], in_=gst[:], func=AF.Sigmoid), [Bgst], [Bgates])
        c.barrier()
        T.dbg(c, "dbg_kcT", kcT[:], (64, 256), BF16, BkcT)
        T.dbg(c, "dbg_Vc", Vc[:], (128, 2, 130), BF16, BVc)
        if NSA_STAGE <= 2:
            return
        Sps = Rot(nc, es, "ns_S", [128, 512], F32, 3, psum=True)
        Ops = Rot(nc, es, "ns_O", [128, 512], F32, 2, psum=True)
        Tps = Rot(nc, es, "ns_T", [128, 256], BF16, 1, psum=True)
        Pt = Rot(nc, es, "ns_P", [128, 512], BF16, 5)
        Mt = Rot(nc, es, "ns_M", [128, 512], F32, 2)
        Ocs = Rot(nc, es, "ns_Oc", [128, 4, 130], F32, 2)
        Osb = Rot(nc, es, "ns_Osb", [128, 264], F32, 2)
        sm = Rot(nc, es, "ns_sm", [128, 16], F32, 3)
        impt = Rot(nc, es, "ns_imp", [128, 64], F32, 2)
        imp2 = Rot(nc, es, "ns_imp2", [128, 64], F32, 2)
        mx = Rot(nc, es, "ns_mx", [128, 16], F32, 2)
        selb = Rot(nc, es, "ns_selb", [128, 128], BF16, 2)
        for _i in range(2):
            c.op("dve", lambda: nc.vector.memset(selb.t[_i][:], 0.0), [], [selb.b[_i]])
        tmpy = Rot(nc, es, "ns_tmpy", [128, 256], F32, 2)

        def softmax_tile(s_, Bs, mask_ap, Bmask):
            p_, Bp = Pt.next()
            if mask_ap is not None:
                m_, Bm = Mt.next()
                c.op("dve", lambda: nc.vector.tensor_tensor(
                    out=m_[:].rearrange("k (h q) -> k h q", h=4), in0=s_[:].rearrange("k (h q) -> k h q", h=4),
                    in1=mask_ap.to_broadcast([128, 4, 128]), op=ALU.add), [Bs, Bmask], [Bm])
                c.op("act", lambda: nc.scalar.activation(out=p_[:], in_=m_[:], func=AF.Exp), [Bm], [Bp])
            else:
                c.op("act", lambda: nc.scalar.activation(out=p_[:], in_=s_[:], func=AF.Exp), [Bs], [Bp])
            return p_, Bp

        def finish_branch(o_ap, Bo, bq, br, first):
            num, den = o_ap
            s_, Bs_ = sm.next()
            c.op("dve", lambda: nc.vector.tensor_scalar(out=s_[:, 0:4].rearrange("t (h o) -> t h o", o=1), in0=den,
                                                        scalar1=1e-30, scalar2=None, op0=ALU.max), [Bo], [Bs_])
            c.op("dve", lambda: nc.vector.reciprocal(out=s_[:, 4:8], in_=s_[:, 0:4]), [Bs_], [Bs_])
            c.op("dve", lambda: nc.vector.tensor_tensor(out=s_[:, 8:12], in0=s_[:, 4:8], in1=gates[:, bq, ssl(br, 4, 3)],
                                                        op=ALU.mult), [Bs_, Bgates], [Bs_])
            wb = s_[:, 8:12].rearrange("t (h o) -> t h o", o=1).to_broadcast([128, 4, 64])
            yv = yacc[:, bq, :].rearrange("t (h e) -> t h e", h=4)
            if first:
                c.op("dve", lambda: nc.vector.tensor_tensor(out=yv, in0=num, in1=wb, op=ALU.mult), [Bo, Bs_], [Byacc])
            else:
                t_, Bt = tmpy.next()
                tv = t_[:].rearrange("t (h e) -> t h e", h=4)
                c.op("dve", lambda: nc.vector.tensor_tensor(out=tv, in0=num, in1=wb, op=ALU.mult), [Bo, Bs_], [Bt])
                c.op("dve", lambda: nc.vector.tensor_tensor(out=yacc[:, bq, :], in0=yacc[:, bq, :], in1=t_[:],
                                                            op=ALU.add), [Bt, Byacc], [Byacc])
            return s_, Bs_

        for bq in range(NT):
            cts = [0] if bq < 16 else [0, 1]
            oA, BoA = Ops.next()
            oB, BoB = Ops.next()
            first = True
            for ct in cts:
                m = bq - 16 * ct
                s_, Bs = Sps.next()
                s4 = s_[:].rearrange("k (h q) -> k h q", h=4)
                c.mm(s4, kcT[:, ct * 128:(ct + 1) * 128], qT[0:64, :, bq * 128:(bq + 1) * 128],
                     True, False, [BkcT, BqT], [Bs])
                c.mm(s4, KBc[:, m, :], QBc[:, :, :], False, True, [BKBc, BQBc], [Bs])
                p_, Bp = softmax_tile(s_, Bs, cmask[:, m:m + 1, :] if m <= 16 else None, Bcmask)
                for h in range(4):
                    o_, Bo = (oA, BoA) if h < 2 else (oB, BoB)
                    hh = h % 2
                    c.mm(o_[:, hh * 130:(hh + 1) * 130], p_[:, h * 128:(h + 1) * 128], Vc[:, ct, :],
                         first and hh == 0, False, [Bp, BVc], [Bo], skip_group_check=True)
                first = False
            oc, Boc = Ocs.next()
            c.copy(oc[:, 0:2, :], oA[:, 0:260].rearrange("t (h e) -> t h e", h=2), [BoA], [Boc])
            c.copy(oc[:, 2:4, :], oB[:, 0:260].rearrange("t (h e) -> t h e", h=2), [BoB], [Boc])
            s_, Bs_ = finish_branch((oc[:, :, 0:64], oc[:, :, 128:129]), Boc, bq, 0, True)
            im, Bim = impt.next()
            c.op("dve", lambda: nc.vector.scalar_tensor_tensor(out=im[:], in0=oc[:, 0, 64:128], scalar=s_[:, 4:5],
                                                               in1=adj[:, bq, :], op0=ALU.mult, op1=ALU.add),
                 [Boc, Bs_, Badj], [Bim])
            for h in range(1, 4):
                c.op("dve", lambda: nc.vector.scalar_tensor_tensor(out=im[:], in0=oc[:, h, 64:128], scalar=s_[:, 4 + h:5 + h],
                                                                   in1=im[:], op0=ALU.mult, op1=ALU.add),
                     [Boc, Bs_, Bim], [Bim])
            T.dbg(c, "dbg_imp%d" % bq, im[:], (128, 64), F32, Bim)
            mx_, Bmx = mx.next()
            c.op("dve", lambda: nc.vector.max(out=mx_[:, 0:8], in_=im[:]), [Bim], [Bmx])
            i2, Bi2 = imp2.next()
            c.op("dve", lambda: nc.vector.match_replace(out=i2[:], in_to_replace=mx_[:, 0:8], in_values=im[:],
                                                        imm_value=-3.0e38), [Bim, Bmx], [Bi2])
            c.op("dve", lambda: nc.vector.max(out=mx_[:, 8:16], in_=i2[:]), [Bi2], [Bmx])
            sb_, Bsb = selb.next()
            c.op("dve", lambda: nc.vector.tensor_scalar(out=sb_[:, 64:128], in0=im[:], scalar1=mx_[:, 15:16], scalar2=None,
                                                        op0=ALU.is_ge), [Bim, Bmx], [Bsb])
            tp_, Btp = Tps.next()
            c.op("pe", lambda: nc.tensor.transpose(out=tp_[:, 0:128], in_=sb_[:], identity=ident[:]), [Bsb, Bid], [Btp])
            for h in range(4):
                c.op("dve", lambda: nc.vector.tensor_scalar(
                    out=qT[64:128, h, bq * 128:(bq + 1) * 128], in0=tp_[64:128, 0:128], scalar1=Asel[64:128, bq, h:h + 1],
                    scalar2=-30000.0, op0=ALU.mult, op1=ALU.add), [Btp, BAsel], [BqT])
        T.dbg(c, "dbg_ycmp", yacc[:], (128, 32, 256), F32, Byacc)
        if NSA_STAGE <= 3:
            return
        SKEW = 2
        tasks = []
        for bq in range(NT):
            for br in (1, 2):
                kbs = list(range(0, bq + 1)) if br == 1 else list(range(max(0, bq - 4), bq + 1))
                for i_, kb in enumerate(kbs):
                    tasks.append((bq, br, kb, i_ == 0, i_ == len(kbs) - 1))
        opnd = {1: (ksT, BksT, Vs, BVs, qT, BqT), 2: (kwT, BkwT, Vw, BVw, Q2w, BQ2w)}
        st = {}
        cur_o = [None]

        def stage_a(ti):
            bq, br, kb, is_first, is_last = tasks[ti]
            kT_, BkT_, V_, BV_, Q_, BQ_ = opnd[br]
            s_, Bs = Sps.next()
            s4 = s_[:].rearrange("k (h q) -> k h q", h=4)
            c.mm(s4, kT_[:, 0, kb * 128:(kb + 1) * 128], Q_[:, :, bq * 128:(bq + 1) * 128], True, True, [BkT_, BQ_], [Bs])
            mask_ap = None
            if kb == bq:
                mask_ap = msk[:, 0:1, :]
            elif br == 2 and kb == bq - 4:
                mask_ap = msk[:, 2:3, :]
            st[ti] = softmax_tile(s_, Bs, mask_ap, Bmsk)

        def stage_c(ti):
            bq, br, kb, is_first, is_last = tasks[ti]
            kT_, BkT_, V_, BV_, Q_, BQ_ = opnd[br]
            p_, Bp = st.pop(ti)
            if is_first:
                cur_o[0] = Ops.next()
            o_, Bo = cur_o[0]
            for h in range(4):
                c.mm(o_[:, h * 66:(h + 1) * 66], p_[:, h * 128:(h + 1) * 128], V_[:, kb, h, :],
                     is_first and h == 0, False, [Bp, BV_], [Bo], skip_group_check=True)
            if is_last:
                ob, Bob = Osb.next()
                c.copy(ob[:], o_[:, 0:264], [Bo], [Bob])
                ov = ob[:].rearrange("t (h e) -> t h e", h=4)
                finish_branch((ov[:, :, 0:64], ov[:, :, 64:65]), Bob, bq, br, False)

        for ti in range(len(tasks) + SKEW):
            if ti < len(tasks):
                stage_a(ti)
            if ti - SKEW >= 0:
                stage_c(ti - SKEW)
        c.dma(T["mix"][:, 256:512].rearrange("(b q) e -> q b e", q=128), yacc[:], reads=[Byacc], writes=[T.B["mix"]])
    c.barrier()


GLA_STAGE = 99


def phase_gla(c, T, l):
    _phase_gla(c, T, l)
    c.barrier()


def _phase_gla(c, T, l):
    nc = c.nc
    with scope(c) as es:
        ident, Bid = sb(nc, es, "gl_id", [128, 128], BF16)
        c.dma(ident[:], T["ident"][:, :], reads=[T.B["ident"]], writes=[Bid])
        m01, Bm01 = sb(nc, es, "gl_m01", [128, 128], BF16)
        c.dma(m01[:], T["c_mask01"][:, :], reads=[T.B["c_mask01"]], writes=[Bm01])
        epsc, Beps = sb(nc, es, "gl_eps", [128, 1], F32)
        c.op("dve", lambda: nc.vector.memset(epsc[:], EPS), [], [Beps])
        gnB, BgnB = sb(nc, es, "gl_gnB", [128, 64], F32)
        c.dma(gnB[:], T["gla_norm_g"][l].partition_broadcast(128), reads=[T.B["gla_norm_g"]], writes=[BgnB])
        qt, Bqt = sb(nc, es, "gl_qt", [32, 4, S], BF16)
        kt, Bkt = sb(nc, es, "gl_kt", [32, 4, S], BF16)
        ktm, Bktm = sb(nc, es, "gl_ktm", [128, NT, 128], BF16)
        ebl, Bebl = sb(nc, es, "gl_ebl", [32, 4, NT], F32)
        with scope(c) as es2:
            wa, Bwa = sb(nc, es2, "gl_wa", [16, 128], BF16)
            c.dma(wa[:], T["gla_wa2"][l], reads=[T.B["gla_wa2"]], writes=[Bwa], q="pool")
            bac, Bbac = sb(nc, es2, "gl_ba", [32, 4], F32)
            c.dma(bac[:], T["gla_ba"][l].rearrange("(g p) -> p g", g=4), reads=[T.B["gla_ba"]], writes=[Bbac],
                  allow_slow_non_contiguous=True)
            aT, BaT = sb(nc, es2, "gl_aT", [16, S], BF16)
            c.dma(aT[:], T["projT"][OFF["gla_a"]:OFF["gla_a"] + 16, :], reads=[T.B["projT"]], writes=[BaT])
            seg, Bseg = sb(nc, es2, "gl_seg", [32, S], BF16)
            c.op("dve", lambda: nc.vector.memset(seg[:], 1.0), [], [Bseg])
            c.op("dve", lambda: nc.vector.memset(seg[:, ssl(0, NT, 128)], 0.0), [], [Bseg])
            zp = Rot(nc, es2, "gl_zp", [128, 512], F32, 2, psum=True)
            qkr = Rot(nc, es2, "gl_qk", [32, 2, S], BF16, 2)
            lar = Rot(nc, es2, "gl_la", [32, S], F32, 2)
            bbr = Rot(nc, es2, "gl_bb", [32, S], F32, 2)
            qkv = T["projT"][OFF["gla_q"]:OFF["gla_q"] + 256, :].rearrange("(i p) t -> p i t", i=8)
            for g in range(4):
                qk, Bqk = qkr.next()
                c.dma(qk[:, 0, :], qkv[:, g, :], reads=[T.B["projT"]], writes=[Bqk])
                c.dma(qk[:, 1, :], qkv[:, 4 + g, :], reads=[T.B["projT"]], writes=[Bqk])
                la, Bla = lar.next()
                bb, Bbb = bbr.next()
                for tt in range(8):
                    sl = slice(tt * 512, (tt + 1) * 512)
                    z_, Bz = zp.next()
                    c.mm(z_[0:32, :], wa[:, g * 32:(g + 1) * 32], aT[:, sl], True, True, [Bwa, BaT], [Bz])
                    c.op("act", lambda: nc.scalar.activation(out=la[:, sl], in_=z_[0:32, :], func=AF.Sigmoid,
                                                             bias=bac[:, g:g + 1]), [Bz, Bbac], [Bla])
                c.op("act", lambda: nc.scalar.activation(out=la[:], in_=la[:], func=AF.Ln), [Bla], [Bla])
                c.op("act", lambda: nc.scalar.mul(out=la[:], in_=la[:], mul=1.0 / 16.0), [Bla], [Bla])
                c.op("dve", lambda: nc.vector.tensor_tensor_scan(out=bb[:], data0=seg[:], data1=la[:],
                                                                 initial=0.0, op0=ALU.mult, op1=ALU.add),
                     [Bseg, Bla], [Bbb])
                c.op("act", lambda: nc.scalar.activation(out=la[:], in_=bb[:], func=AF.Exp), [Bbb], [Bla])
                c.op("dve", lambda: nc.vector.tensor_copy(out=ebl[:, g, :], in_=la[:, ssl(127, NT, 128)]), [Bla], [Bebl])
                c.op("dve", lambda: nc.vector.scalar_tensor_tensor(out=qt[:, g, :], in0=qk[:, 0, :], scalar=32.0 ** -0.5,
                                                                   in1=la[:], op0=ALU.mult, op1=ALU.mult),
                     [Bqk, Bla], [Bqt])
                c.op("act", lambda: nc.scalar.activation(out=bb[:], in_=bb[:], func=AF.Exp, scale=-1.0),
                     [Bbb], [Bbb])
                c.op("dve", lambda: nc.vector.tensor_tensor(out=kt[:, g, :], in0=qk[:, 1, :], in1=bb[:],
                                                            op=ALU.mult), [Bqk, Bbb], [Bkt])
            tp = Rot(nc, es2, "gl_tp", [128, 1024], BF16, 2, psum=True)
            for n in range(NT):
                p_, Bp = tp.next()
                for g in range(4):
                    c.op("pe", lambda: nc.tensor.transpose(out=p_[:, g * 32:(g + 1) * 32],
                                                           in_=kt[:, g, n * 128:(n + 1) * 128],
                                                           identity=ident[0:32, 0:32]), [Bkt, Bid], [Bp])
                c.copy(ktm[:, n, :], p_[:, 0:128], [Bp], [Bktm])
        v, Bv = sb(nc, es, "gl_v", [128, NT, 256], BF16)
        c.dma(v[:], T["proj"][:, OFF["gla_v"]:OFF["gla_v"] + 256].rearrange("(n j) e -> j n e", j=128),
              reads=[T.B["proj"]], writes=[Bv])
        oall, Boall = sb(nc, es, "gl_oall", [128, NT, 256], F32)
        Sps = Rot(nc, es, "gl_S", [128, 512], F32, 2, psum=True)
        Ops = Rot(nc, es, "gl_O", [128, 512], F32, 2, psum=True)
        Pps = Rot(nc, es, "gl_P", [128, 512], F32, 2, psum=True)
        At = Rot(nc, es, "gl_A", [128, 512], BF16, 3)
        Sf, BSf = sb(nc, es, "gl_Sf", [32, 4, 64], F32)
        Sf2, BSf2 = sb(nc, es, "gl_Sf2", [32, 4, 64], F32)
        Sf3, BSf3 = sb(nc, es, "gl_Sf3", [32, 4, 64], F32)
        Sst, BSst = sb(nc, es, "gl_Sst", [32, 4, 64], BF16)
        c.op("dve", lambda: nc.vector.memset(Sf[:], 0.0), [], [BSf])
        c.op("dve", lambda: nc.vector.memset(Sst[:], 0.0), [], [BSst])
        for n in range(NT):
            sl = slice(n * 128, (n + 1) * 128)
            s_, Bs = Sps.next()
            for h in range(4):
                c.mm(s_[:, h * 128:(h + 1) * 128], kt[:, h, sl], qt[:, h, sl], True, True, [Bkt, Bqt], [Bs])
            pp, Bpp = Pps.next()
            for h in range(4):
                c.mm(pp[0:32, h * 64:(h + 1) * 64], ktm[:, n, 32 * h:32 * h + 32], v[:, n, 64 * h:64 * h + 64],
                     True, True, [Bktm, Bv], [Bpp])
            a_, Ba = At.next()
            c.op("dve", lambda: nc.vector.tensor_tensor(
                out=a_[:].rearrange("k (h q) -> k h q", h=4), in0=s_[:].rearrange("k (h q) -> k h q", h=4),
                in1=m01[:].rearrange("k (o q) -> k o q", o=1).to_broadcast([128, 4, 128]), op=ALU.mult), [Bs, Bm01], [Ba])
            o_, Bo = Ops.next()
            for h in range(4):
                c.mm(o_[:, 64 * h:64 * h + 64], a_[:, h * 128:(h + 1) * 128], v[:, n, 64 * h:64 * h + 64],
                     True, False, [Ba, Bv], [Bo])
                c.mm(o_[:, 64 * h:64 * h + 64], qt[:, h, sl], Sst[:, h, :], False, True, [Bqt, BSst], [Bo])
            c.copy(oall[:, n, :], o_[:, 0:256], [Bo], [Boall])
            eb = ebl[:, :, n:n + 1].to_broadcast([32, 4, 64])
            c.op("dve", lambda: nc.vector.tensor_tensor(out=Sf2[:], in0=Sf[:], in1=eb, op=ALU.mult), [BSf, Bebl], [BSf2])
            c.op("dve", lambda: nc.vector.tensor_tensor(out=Sf3[:], in0=pp[0:32, 0:256].rearrange("p (h e) -> p h e", h=4),
                                                        in1=eb, op=ALU.mult), [Bpp, Bebl], [BSf3])
            c.op("dve", lambda: nc.vector.tensor_tensor(out=Sf[:], in0=Sf2[:], in1=Sf3[:], op=ALU.add), [BSf2, BSf3], [BSf])
            c.op("dve", lambda: nc.vector.tensor_copy(out=Sst[:], in_=Sf[:]), [BSf], [BSst])
        if GLA_STAGE <= 5:
            return
        with scope(c) as es2:
            r_, Br = sb(nc, es2, "gl_r", [128, NT, 256], BF16)
            c.dma(r_[:], T["proj"][:, OFF["gla_r"]:OFF["gla_r"] + 256].rearrange("(n j) e -> j n e", j=128),
                  reads=[T.B["proj"]], writes=[Br])
            sq, Bsq = sb(nc, es2, "gl_sq", [128, NT, 256], F32)
            ss, Bss = sb(nc, es2, "gl_ss", [128, NT * 4], F32)
            c.op("act", lambda: nc.scalar.activation(out=sq[:], in_=oall[:], func=AF.Square), [Boall], [Bsq])
            c.op("dve", lambda: nc.vector.tensor_reduce(out=ss[:], in_=sq[:].rearrange("t n (h e) -> t (n h) e", h=4),
                                                        axis=AX.X, op=ALU.add), [Bsq], [Bss])
            c.op("act", lambda: nc.scalar.activation(out=ss[:], in_=ss[:], func=AF.Sqrt, bias=epsc[:, 0:1], scale=1.0 / 64),
                 [Bss, Beps], [Bss])
            c.op("dve", lambda: nc.vector.reciprocal(out=ss[:], in_=ss[:]), [Bss], [Bss])
            ov = oall[:].rearrange("t n (h e) -> t (n h) e", h=4)
            c.op("dve", lambda: nc.vector.tensor_tensor(
                out=ov, in0=ov, in1=ss[:].rearrange("t (m o) -> t m o", o=1).to_broadcast([128, NT * 4, 64]), op=ALU.mult),
                [Boall, Bss], [Boall])
            c.op("dve", lambda: nc.vector.tensor_tensor(
                out=ov, in0=ov, in1=gnB[:].rearrange("t (o e) -> t o e", o=1).to_broadcast([128, NT * 4, 64]), op=ALU.mult),
                [Boall, BgnB], [Boall])
            c.op("act", lambda: nc.scalar.activation(out=sq[:], in_=r_[:], func=AF.Silu), [Br], [Bsq])
            c.op("dve", lambda: nc.vector.tensor_tensor(out=oall[:], in0=oall[:], in1=sq[:], op=ALU.mult),
                 [Boall, Bsq], [Boall])
            c.dma(T["mix"][:, 512:768].rearrange("(n j) e -> j n e", j=128), oall[:], reads=[Boall], writes=[T.B["mix"]])


S5_L = 256
S5_STAGE = 99


def phase_s5(c, T, l):
    _phase_s5(c, T, l)
    c.barrier()


def _phase_s5(c, T, l):
    nc = c.nc
    L = S5_L
    NCH = S // L
    uoff = OFF["s5_u"]

    def dv(fn, r, w):
        return c.op("dve", fn, r, w)

    def tt(out, a, b, op, r, w):
        return c.op("dve", lambda: nc.vector.tensor_tensor(out=out, in0=a, in1=b, op=op), r, w)

    with scope(c) as es:
        ident, Bid = sb(nc, es, "s5_id", [128, 128], BF16)
        c.dma(ident[:], T["ident"][:, :], reads=[T.B["ident"]], writes=[Bid])
        identf, Bidf = sb(nc, es, "s5_idf", [128, 128], F32)
        c.dma(identf[:], T["identf"][:, :], reads=[T.B["identf"]], writes=[Bidf])
        mask, Bmask = sb(nc, es, "s5_mask", [128, 8, 128], F32)
        c.dma(mask[:], T["c_s5mask"][:, :, :], reads=[T.B["c_s5mask"]], writes=[Bmask])
        P, BP = sb(nc, es, "s5_P", [128, 40, 8], F32)
        hp, Bhp = sb(nc, es, "s5_hp", [128, 1], F32)
        dv(lambda: nc.vector.memset(hp[:], math.pi / 2.0), [], [Bhp])
        (ARE, AIM, LDT, DT, MAG, TH, CS, SN, C2, S2, TA, TB_, ABR, ABI, DEN, AM1, FRE, FIM, PR, PI_, PR2, PI2,
         WLR, WLI, X1, X2) = range(26)

        def col(i):
            return P[:, i, :]

        for gg in range(2):
            ps_ = slice(gg * 64, (gg + 1) * 64)
            c.dma(P[ps_, ARE, :], T["s5_a_re"][l].rearrange("(m gg) n -> gg n m", gg=2)[gg], reads=[T.B["s5_a_re"]],
                  writes=[BP], allow_slow_non_contiguous=True)
            c.dma(P[ps_, AIM, :], T["s5_a_im"][l].rearrange("(m gg) n -> gg n m", gg=2)[gg], reads=[T.B["s5_a_im"]],
                  writes=[BP], allow_slow_non_contiguous=True)
            c.dma(P[ps_, LDT, :], T["s5_log_dt"][l].rearrange("(m gg) -> gg m", gg=2)[gg].partition_broadcast(64),
                  reads=[T.B["s5_log_dt"]], writes=[BP], allow_slow_non_contiguous=True)
        c.op("act", lambda: nc.scalar.activation(out=col(DT), in_=col(LDT), func=AF.Exp), [BP], [BP])
        tt(col(TA), col(DT), col(ARE), ALU.mult, [BP], [BP])
        c.op("act", lambda: nc.scalar.activation(out=col(MAG), in_=col(TA), func=AF.Exp), [BP], [BP])
        tt(col(TH), col(DT), col(AIM), ALU.mult, [BP], [BP])
        c.op("act", lambda: nc.scalar.activation(out=col(SN), in_=col(TH), func=AF.Sin, scale=1.0 / 16.0), [BP], [BP])
        c.op("act", lambda: nc.scalar.activation(out=col(CS), in_=col(TH), func=AF.Sin, scale=1.0 / 16.0,
                                                 bias=hp[:, 0:1]), [BP, Bhp], [BP])

        def csq(cr, ci, orr, oi):
            tt(col(TA), col(cr), col(cr), ALU.mult, [BP], [BP])
            tt(col(TB_), col(ci), col(ci), ALU.mult, [BP], [BP])
            dv(lambda: nc.vector.scalar_tensor_tensor(out=col(oi), in0=col(cr), scalar=2.0, in1=col(ci),
                                                      op0=ALU.mult, op1=ALU.mult), [BP], [BP])
            tt(col(orr), col(TA), col(TB_), ALU.subtract, [BP], [BP])

        csq(CS, SN, C2, S2)
        csq(C2, S2, CS, SN)
        csq(CS, SN, C2, S2)
        csq(C2, S2, CS, SN)
        tt(col(ABR), col(MAG), col(CS), ALU.mult, [BP], [BP])
        tt(col(ABI), col(MAG), col(SN), ALU.mult, [BP], [BP])
        tt(col(TA), col(ARE), col(ARE), ALU.mult, [BP], [BP])
        tt(col(TB_), col(AIM), col(AIM), ALU.mult, [BP], [BP])
        tt(col(DEN), col(TA), col(TB_), ALU.add, [BP], [BP])
        dv(lambda: nc.vector.reciprocal(out=col(DEN), in_=col(DEN)), [BP], [BP])
        dv(lambda: nc.vector.tensor_scalar(out=col(AM1), in0=col(ABR), scalar1=-1.0, scalar2=None, op0=ALU.add), [BP], [BP])
        tt(col(TA), col(AM1), col(ARE), ALU.mult, [BP], [BP])
        tt(col(TB_), col(ABI), col(AIM), ALU.mult, [BP], [BP])
        tt(col(FRE), col(TA), col(TB_), ALU.add, [BP], [BP])
        tt(col(FRE), col(FRE), col(DEN), ALU.mult, [BP], [BP])
        tt(col(TA), col(ABI), col(ARE), ALU.mult, [BP], [BP])
        tt(col(TB_), col(AM1), col(AIM), ALU.mult, [BP], [BP])
        tt(col(FIM), col(TA), col(TB_), ALU.subtract, [BP], [BP])
        tt(col(FIM), col(FIM), col(DEN), ALU.mult, [BP], [BP])
        BT, BBT = sb(nc, es, "s5_BT", [128, 2, 8, 128], BF16)
        Cm, BCm = sb(nc, es, "s5_Cm", [128, 2, 8, 128], BF16)
        with scope(c) as es2:
            braw, Bbraw = sb(nc, es2, "s5_braw", [128, 2, 8, 16], F32)
            craw, Bcraw = sb(nc, es2, "s5_craw", [128, 2, 8, 16], F32)
            for gg in range(2):
                ps_ = slice(gg * 64, (gg + 1) * 64)
                for ri, nm in enumerate(("s5_b_re", "s5_b_im")):
                    c.dma(braw[ps_, ri, :, :], T[nm][l].rearrange("(m gg) n c -> gg n m c", gg=2)[gg], reads=[T.B[nm]],
                          writes=[Bbraw], allow_slow_non_contiguous=True)
                for ri, nm in enumerate(("s5_c_re", "s5_c_im")):
                    for m in range(8):
                        c.dma(craw[ps_, ri, m, :], T[nm][l, 2 * m + gg].rearrange("c n -> n c"), reads=[T.B[nm]],
                              writes=[Bcraw], allow_slow_non_contiguous=True)
            bbc, Bbbc = sb(nc, es2, "s5_bbc", [128, 2, 8, 16], F32)
            t1, Bt1 = sb(nc, es2, "s5_t1", [128, 8, 16], F32)
            t2, Bt2 = sb(nc, es2, "s5_t2", [128, 8, 16], F32)
            fre_b = P[:, FRE, :].rearrange("p (m o) -> p m o", o=1).to_broadcast([128, 8, 16])
            fim_b = P[:, FIM, :].rearrange("p (m o) -> p m o", o=1).to_broadcast([128, 8, 16])
            tt(t1[:], braw[:, 0, :, :], fre_b, ALU.mult, [Bbraw, BP], [Bt1])
            tt(t2[:], braw[:, 1, :, :], fim_b, ALU.mult, [Bbraw, BP], [Bt2])
            tt(bbc[:, 0, :, :], t1[:], t2[:], ALU.subtract, [Bt1, Bt2], [Bbbc])
            tt(t1[:], braw[:, 1, :, :], fre_b, ALU.mult, [Bbraw, BP], [Bt1])
            tt(t2[:], braw[:, 0, :, :], fim_b, ALU.mult, [Bbraw, BP], [Bt2])
            tt(bbc[:, 1, :, :], t1[:], t2[:], ALU.add, [Bt1, Bt2], [Bbbc])
            Bd, BBd = sb(nc, es2, "s5_Bd", [128, 2, 8, 128], BF16)
            mask4 = mask[:].rearrange("p m (j c) -> p m j c", c=16)
            for ri in range(2):
                tt(Bd[:, ri, :, :].rearrange("p m (j c) -> p m j c", c=16), mask4,
                   bbc[:, ri, :, :].rearrange("p m (o c) -> p m o c", o=1).to_broadcast([128, 8, 8, 16]), ALU.mult,
                   [Bmask, Bbbc], [BBd])
            tt(Cm[:, 0, :, :].rearrange("p m (j c) -> p m j c", c=16), mask4,
               craw[:, 0, :, :].rearrange("p m (o c) -> p m o c", o=1).to_broadcast([128, 8, 8, 16]), ALU.mult,
               [Bmask, Bcraw], [BCm])
            dv(lambda: nc.vector.tensor_scalar(out=craw[:, 1, :, :], in0=craw[:, 1, :, :], scalar1=-1.0, scalar2=None,
                                               op0=ALU.mult), [Bcraw], [Bcraw])
            tt(Cm[:, 1, :, :].rearrange("p m (j c) -> p m j c", c=16), mask4,
               craw[:, 1, :, :].rearrange("p m (o c) -> p m o c", o=1).to_broadcast([128, 8, 8, 16]), ALU.mult,
               [Bmask, Bcraw], [BCm])
            tpb = Rot(nc, es2, "s5_tpb", [128, 1024], BF16, 2, psum=True)
            for ri in range(2):
                for g2 in range(2):
                    p_, Bp = tpb.next()
                    for j in range(4):
                        m = g2 * 4 + j
                        c.op("pe", lambda: nc.tensor.transpose(out=p_[:, j * 128:(j + 1) * 128], in_=Bd[:, ri, m, :],
                                                               identity=ident[:]), [BBd, Bid], [Bp])
                    c.copy(BT[:, ri, g2 * 4:(g2 + 1) * 4, :], p_[:, 0:512].rearrange("p (j t) -> p j t", j=4), [Bp], [BBT])
        c.barrier()
        T.dbg(c, "dbg_s5P", P[:], (128, 40, 8), F32, BP)
        if S5_STAGE <= 1:
            return
        w, Bw = sb(nc, es, "s5_w", [128, 2, 8, L], F32)
        Rb, BRb = sb(nc, es, "s5_Rb", [128, 8, L], F32)
        tA, BtA = sb(nc, es, "s5_tA", [128, 8, L], F32)
        tB, BtB = sb(nc, es, "s5_tB", [128, 8, L], F32)
        tC, BtC = sb(nc, es, "s5_tC", [128, 8, L], F32)
        tD, BtD = sb(nc, es, "s5_tD", [128, 8, L], F32)
        dv(lambda: nc.vector.tensor_copy(out=Rb[:], in_=P[:, MAG, :].rearrange("p (m o) -> p m o", o=1)
                                         .to_broadcast([128, 8, L])), [BP], [BRb])
        dv(lambda: nc.vector.memset(w[:, 0, :, 0:1], 1.0), [], [Bw])
        dv(lambda: nc.vector.memset(w[:, 1, :, 0:1], 0.0), [], [Bw])
        dv(lambda: nc.vector.tensor_copy(out=col(PR), in_=col(CS)), [BP], [BP])
        dv(lambda: nc.vector.tensor_copy(out=col(PI_), in_=col(SN)), [BP], [BP])
        k = 1
        cur = (PR, PI_)
        oth = (PR2, PI2)
        while k < L:
            prb = P[:, cur[0], :].rearrange("p (m o) -> p m o", o=1).to_broadcast([128, 8, k])
            pib = P[:, cur[1], :].rearrange("p (m o) -> p m o", o=1).to_broadcast([128, 8, k])
            wr0 = w[:, 0, :, 0:k]
            wi0 = w[:, 1, :, 0:k]
            tt(tA[:, :, 0:k], wr0, prb, ALU.mult, [Bw, BP], [BtA])
            tt(tB[:, :, 0:k], wi0, pib, ALU.mult, [Bw, BP], [BtB])
            tt(w[:, 0, :, k:2 * k], tA[:, :, 0:k], tB[:, :, 0:k], ALU.subtract, [BtA, BtB], [Bw])
            tt(tA[:, :, 0:k], wr0, pib, ALU.mult, [Bw, BP], [BtA])
            tt(tB[:, :, 0:k], wi0, prb, ALU.mult, [Bw, BP], [BtB])
            tt(w[:, 1, :, k:2 * k], tA[:, :, 0:k], tB[:, :, 0:k], ALU.add, [BtA, BtB], [Bw])
            csq(cur[0], cur[1], oth[0], oth[1])
            cur, oth = oth, cur
            k *= 2
        dv(lambda: nc.vector.tensor_copy(out=col(WLR), in_=col(cur[0])), [BP], [BP])
        dv(lambda: nc.vector.tensor_copy(out=col(WLI), in_=col(cur[1])), [BP], [BP])
        T.dbg(c, "dbg_s5w", w[:], (128, 2, 8, L), F32, Bw)
        gw, Bgw = sb(nc, es, "s5_gwt", [128, 2, 256], BF16)
        c.dma(gw[:], T["s5_glu_w"][l].rearrange("(kt p) n -> p kt n", p=128), reads=[T.B["s5_glu_w"]], writes=[Bgw], q="pool")
        gbc, Bgbc = sb(nc, es, "s5_gbt", [128, 2], F32)
        c.dma(gbc[:], T["s5_glu_b"][l].rearrange("(h p) -> p h", p=128), reads=[T.B["s5_glu_b"]], writes=[Bgbc],
              allow_slow_non_contiguous=True)
        dcl, Bdcl = sb(nc, es, "s5_dcol", [128, 2], F32)
        c.dma(dcl[:], T["s5_d"][l].rearrange("(h p) -> p h", p=128), reads=[T.B["s5_d"]], writes=[Bdcl],
              allow_slow_non_contiguous=True)
        if S5_STAGE <= 2:
            return
        uTr = Rot(nc, es, "s5_uT", [128, 2, L], BF16, 2)
        BUps = Rot(nc, es, "s5_BU", [128, 512], F32, 2, psum=True)
        Yps = Rot(nc, es, "s5_Y", [128, 512], F32, 2, psum=True)
        Tpf = Rot(nc, es, "s5_Tp", [128, 512], F32, 2, psum=True)
        d1 = Rot(nc, es, "s5_d1", [128, L], F32, 3)
        d2 = Rot(nc, es, "s5_d2", [128, L], F32, 3)
        zb = Rot(nc, es, "s5_zb", [128, 2, L], F32, 3)
        Z, BZ = sb(nc, es, "s5_Z", [128, 2, 8, L], F32)
        X, BX = sb(nc, es, "s5_X", [128, 2, 8, L], BF16)
        init = Rot(nc, es, "s5_init", [128, 2, 8], F32, 2)
        zt, Bzt = sb(nc, es, "s5_zt", [128, 2, L], F32)
        u2, Bu2 = sb(nc, es, "s5_u2", [128, 2, L], F32)
        hg, Bhg = sb(nc, es, "s5_hg", [128, 2, L], F32)
        hgb, Bhgb = sb(nc, es, "s5_hgb", [128, 2, L], BF16)
        sgt, Bsgt = sb(nc, es, "s5_sg", [128, 2, L], F32)
        ot, Bot = sb(nc, es, "s5_ot", [128, 2, L], F32)
        otm = Rot(nc, es, "s5_otm", [128, L // 128, 256], F32, 2)
        in_, Bin = init.next()
        dv(lambda: nc.vector.memset(in_[:], 0.0), [], [Bin])
        for ci in range(NCH):
            t0 = ci * L
            u_, Bu = uTr.next()
            c.dma(u_[:], T["projT"][uoff:uoff + 256, t0:t0 + L].rearrange("(h p) t -> p h t", p=128),
                  reads=[T.B["projT"]], writes=[Bu])
            for m in range(8):
                bk, Bbk = BUps.next()
                c.mm(bk[:, 0:L], BT[:, 0, m, :], u_[:, m // 4, :], True, True, [BBT, Bu], [Bbk])
                c.mm(bk[:, L:2 * L], BT[:, 1, m, :], u_[:, m // 4, :], True, True, [BBT, Bu], [Bbk])
                z_, Bz = zb.next()
                a1, Ba1 = d1.next()
                a2, Ba2 = d2.next()
                tt(a1[:], bk[:, 0:L], w[:, 0, m, :], ALU.mult, [Bbk, Bw], [Ba1])
                tt(a2[:], bk[:, L:2 * L], w[:, 1, m, :], ALU.mult, [Bbk, Bw], [Ba2])
                tt(z_[:, 0, :], a1[:], a2[:], ALU.add, [Ba1, Ba2], [Bz])
                a1, Ba1 = d1.next()
                a2, Ba2 = d2.next()
                tt(a1[:], bk[:, L:2 * L], w[:, 0, m, :], ALU.mult, [Bbk, Bw], [Ba1])
                tt(a2[:], bk[:, 0:L], w[:, 1, m, :], ALU.mult, [Bbk, Bw], [Ba2])
                tt(z_[:, 1, :], a1[:], a2[:], ALU.subtract, [Ba1, Ba2], [Bz])
                for ri in range(2):
                    dv(lambda: nc.vector.tensor_tensor_scan(out=Z[:, ri, m, :], data0=Rb[:, m, :], data1=z_[:, ri, :],
                                                            initial=in_[:, ri, m:m + 1], op0=ALU.mult, op1=ALU.add),
                       [BRb, Bz, Bin], [BZ])
            nx, Bnx = init.next()
            zr = Z[:, 0, :, L - 1]
            zi = Z[:, 1, :, L - 1]
            tt(col(X1), zr, col(WLR), ALU.mult, [BZ, BP], [BP])
            tt(col(X2), zi, col(WLI), ALU.mult, [BZ, BP], [BP])
            tt(nx[:, 0, :], col(X1), col(X2), ALU.subtract, [BP], [Bnx])
            tt(col(X1), zr, col(WLI), ALU.mult, [BZ, BP], [BP])
            tt(col(X2), zi, col(WLR), ALU.mult, [BZ, BP], [BP])
            tt(nx[:, 1, :], col(X1), col(X2), ALU.add, [BP], [Bnx])
            in_, Bin = nx, Bnx
            def pt(out, a, b, op, r, w_):
                return c.op("dve", lambda: nc.vector.tensor_tensor(out=out, in0=a, in1=b, op=op), r, w_)
            tt(tA[:], Z[:, 0, :, :], w[:, 0, :, :], ALU.mult, [BZ, Bw], [BtA])
            tt(tB[:], Z[:, 1, :, :], w[:, 1, :, :], ALU.mult, [BZ, Bw], [BtB])
            pt(X[:, 0, :, :], tA[:], tB[:], ALU.subtract, [BtA, BtB], [BX])
            tt(tC[:], Z[:, 0, :, :], w[:, 1, :, :], ALU.mult, [BZ, Bw], [BtC])
            tt(tD[:], Z[:, 1, :, :], w[:, 0, :, :], ALU.mult, [BZ, Bw], [BtD])
            pt(X[:, 1, :, :], tC[:], tD[:], ALU.add, [BtC, BtD], [BX])
            if ci == 0:
                T.dbg(c, "dbg_s5X", X[:], (128, 2, 8, L), BF16, BX)
            for half in range(2):
                yp, Byp = Yps.next()
                for mm_ in range(4):
                    m = half * 4 + mm_
                    c.mm(yp[:, 0:L], Cm[:, 0, m, :], X[:, 0, m, :], mm_ == 0, False, [BCm, BX], [Byp])
                    c.mm(yp[:, 0:L], Cm[:, 1, m, :], X[:, 1, m, :], False, mm_ == 3, [BCm, BX], [Byp])
                dv(lambda: nc.vector.scalar_tensor_tensor(out=zt[:, half, :], in0=u_[:, half, :], scalar=dcl[:, half:half + 1],
                                                          in1=yp[:, 0:L], op0=ALU.mult, op1=ALU.add),
                   [Bu, Bdcl, Byp], [Bzt])
            tt(u2[:], zt[:], zt[:], ALU.mult, [Bzt], [Bu2])
            dv(lambda: nc.vector.tensor_scalar(out=u2[:], in0=u2[:], scalar1=0.044715, scalar2=1.0, op0=ALU.mult,
                                               op1=ALU.add), [Bu2], [Bu2])
            tt(u2[:], u2[:], zt[:], ALU.mult, [Bu2, Bzt], [Bu2])
            c.op("act", lambda: nc.scalar.activation(out=u2[:], in_=u2[:], func=AF.Sigmoid, scale=1.5957691216057308),
                 [Bu2], [Bu2])
            tt(hg[:], u2[:], zt[:], ALU.mult, [Bu2, Bzt], [Bhg])
            c.op("act", lambda: nc.scalar.copy(out=hgb[:], in_=hg[:]), [Bhg], [Bhgb])
            for h2 in range(2):
                gp, Bgp = Yps.next()
                for kt_ in range(2):
                    c.mm(gp[:, 0:L], gw[:, kt_, h2 * 128:(h2 + 1) * 128], hgb[:, kt_, :], kt_ == 0, kt_ == 1, [Bgw, Bhgb], [Bgp])
                c.op("act", lambda: nc.scalar.activation(out=sgt[:, h2, :], in_=gp[:, 0:L], func=AF.Sigmoid,
                                                         bias=gbc[:, h2:h2 + 1]), [Bgp, Bgbc], [Bsgt])
            tt(ot[:], hg[:], sgt[:], ALU.mult, [Bhg, Bsgt], [Bot])
            tp_, Btp = Tpf.next()
            for ts_ in range(L // 128):
                for h2 in range(2):
                    c.op("pe", lambda: nc.tensor.transpose(out=tp_[:, ts_ * 256 + h2 * 128:ts_ * 256 + (h2 + 1) * 128],
                                                           in_=ot[:, h2, ts_ * 128:(ts_ + 1) * 128], identity=identf[:]),
                         [Bot, Bidf], [Btp])
            om, Bom = otm.next()
            c.copy(om[:], tp_[:, 0:(L // 128) * 256].rearrange("p (a e) -> p a e", e=256), [Btp], [Bom])
            c.dma(T["mix"][t0:t0 + L, 768:1024].rearrange("(a p) e -> p a e", p=128), om[:], reads=[Bom],
                  writes=[T.B["mix"]])


def norm_tile(c, x_t, Bx, gB, BgB, h_, Bh, s_, Bs, jk, Bjk, ngroups=1):
    nc = c.nc
    Wd = D // ngroups
    for g in range(ngroups):
        gs = slice(g * Wd, (g + 1) * Wd)
        c.op("act", lambda: nc.scalar.activation(out=jk[:, gs], in_=x_t[:, gs], func=AF.Square,
                                                 accum_out=s_[:, g:g + 1]), [Bx], [Bjk, Bs])
    c.op("act", lambda: nc.scalar.activation(out=s_[:, 4:4 + ngroups], in_=s_[:, 0:ngroups], func=AF.Sqrt,
                                             bias=gB[:, D:D + 1], scale=1.0 / Wd), [Bs, BgB], [Bs])
    c.op("dve", lambda: nc.vector.reciprocal(out=s_[:, 4:4 + ngroups], in_=s_[:, 4:4 + ngroups]), [Bs], [Bs])
    for g in range(ngroups):
        gs = slice(g * Wd, (g + 1) * Wd)
        c.op("dve", lambda: nc.vector.scalar_tensor_tensor(out=h_[:, gs], in0=x_t[:, gs], scalar=s_[:, 4 + g:5 + g],
                                                           in1=gB[:, gs], op0=ALU.mult, op1=ALU.mult),
             [Bx, Bs, BgB], [Bh])


def phase_out(c, T, l):
    nc = c.nc
    x_d = T["xin%d" % l]
    Bx_d = T.B["xin%d" % l]
    with scope(c) as es:
        W, BW = sb(nc, es, "po_W", [128, 8, D], BF16)
        ident, Bid = sb(nc, es, "po_id", [128, 128], BF16)
        c.dma(ident[:], T["ident"][:, :], reads=[T.B["ident"]], writes=[Bid])
        gB, BgB = load_gB(c, es, T["out_norm_g"][l], T.B["out_norm_g"], "po_g")
        wv = T["w_out"][l].rearrange("(kc p) n -> p kc n", p=128)
        for kc in range(8):
            c.dma(W[:, kc, :], wv[:, kc, :], reads=[T.B["w_out"]], writes=[BW], q="pool")
        mt = Rot(nc, es, "po_mt", [128, D], F32, 4)
        xt = Rot(nc, es, "po_xt", [128, D], F32, 5)
        jkr = Rot(nc, es, "po_jk", [128, D], BF16, 2)
        hb = Rot(nc, es, "po_hb", [128, D], BF16, 2)
        ss = Rot(nc, es, "po_ss", [128, 8], F32, 4)
        tp = Rot(nc, es, "po_tp", [128, 1024], BF16, 2, psum=True)
        hTr = Rot(nc, es, "po_hT", [128, 8, 128], BF16, 3)
        acc = Rot(nc, es, "po_acc", [128, 512], F32, 2, psum=True)
        xo = Rot(nc, es, "po_xo", [128, D], F32, 2)
        PF = 2
        loaded = {}
        staged = {}

        def load(t):
            rows = slice(t * 128, (t + 1) * 128)
            m_, Bm = mt.next()
            c.dma(m_[:], T["mix"][rows, :], reads=[T.B["mix"]], writes=[Bm])
            x_, Bx = xt.next()
            c.dma(x_[:], x_d[rows, :], reads=[Bx_d], writes=[Bx])
            loaded[t] = (m_, Bm, x_, Bx)

        def stage_a(t):
            m_, Bm, x_, Bx = loaded.pop(t)
            jk, Bjk = jkr.next()
            s_, Bs = ss.next()
            h_, Bh = hb.next()
            norm_tile(c, m_, Bm, gB, BgB, h_, Bh, s_, Bs, jk, Bjk, ngroups=4)
            hT, BhT = hTr.next()
            for g in range(2):
                p_, Bp = tp.next()
                for j in range(4):
                    kc = g * 4 + j
                    c.op("pe", lambda: nc.tensor.transpose(out=p_[:, j * 128:(j + 1) * 128],
                                                           in_=h_[:, kc * 128:(kc + 1) * 128],
                                                           identity=ident[:]), [Bh, Bid], [Bp])
                c.copy(hT[:, g * 4:(g + 1) * 4, :], p_[:, 0:512].rearrange("p (j t) -> p j t", j=4), [Bp], [BhT])
            staged[t] = (hT, BhT, x_, Bx)

        def stage_c(t):
            rows = slice(t * 128, (t + 1) * 128)
            hT, BhT, x_, Bx = staged.pop(t)
            o_, Bo = xo.next()
            for dc in range(2):
                ds_ = slice(dc * 512, (dc + 1) * 512)
                a_, Ba = acc.next()
                for k in range(8):
                    c.mm(a_[:, :], hT[:, k, :], W[:, k, ds_], k == 0, k == 7, [BW, BhT], [Ba])
                c.op("dve", lambda: nc.vector.tensor_tensor(out=o_[:, ds_], in0=a_[:, :], in1=x_[:, ds_], op=ALU.add),
                     [Ba, Bx], [Bo])
            c.dma(T["xmid"][rows, :], o_[:], reads=[Bo], writes=[T.B["xmid"]])

        for t in range(min(PF, NT)):
            load(t)
        for t in range(NT + 1):
            if t + PF < NT:
                load(t + PF)
            if t < NT:
                stage_a(t)
            if t >= 1:
                stage_c(t - 1)
    c.barrier()


FFN_TB = 1024


def phase_ffn(c, T, l, out_name):
    nc = c.nc
    moe = (l % 2 == 1)
    i = l // 2
    if moe:
        F = D_FFE
        experts = [(T["moe_w_gate"][i, e], T["moe_w_up"][i, e], T["moe_w_down"][i, e]) for e in range(N_EXP)]
        BWs = (T.B["moe_w_gate"], T.B["moe_w_up"], T.B["moe_w_down"])
    else:
        F = D_FF
        experts = [(T["ffn_w_gate"][i], T["ffn_w_up"][i], T["ffn_w_down"][i])]
        BWs = (T.B["ffn_w_gate"], T.B["ffn_w_up"], T.B["ffn_w_down"])
    TB = FFN_TB
    NTB = TB // 128
    FC = 512
    chunks = [(f0, min(FC, F - f0)) for f0 in range(0, F, FC)]
    with scope(c) as es:
        ident, Bid = sb(nc, es, "ff_id", [128, 128], BF16)
        c.dma(ident[:], T["ident"][:, :], reads=[T.B["ident"]], writes=[Bid])
        gB, BgB = load_gB(c, es, T["norm2_g"][l], T.B["norm2_g"], "ff_g")
        if moe:
            Wr, BWr = sb(nc, es, "ff_Wr", [128, 8, N_EXP], BF16)
            c.dma(Wr[:], T["moe_router_w"][i].rearrange("(kc p) e -> p kc e", p=128), reads=[T.B["moe_router_w"]],
                  writes=[BWr], q="pool")
            rb, Brb = sb(nc, es, "ff_rb", [128, N_EXP], F32)
            c.dma(rb[:], T["moe_router_b"][i].partition_broadcast(128), reads=[T.B["moe_router_b"]], writes=[Brb])
            combs = [sb(nc, es, "ff_comb%d" % i_, [128, NTB, N_EXP], F32) for i_ in range(2)]
            lg = Rot(nc, es, "ff_lg", [128, N_EXP], F32, 2)
            mx = Rot(nc, es, "ff_mx", [128, 8], F32, 2)
            ex = Rot(nc, es, "ff_ex", [128, 2 * N_EXP], F32, 2)
        wg = Rot(nc, es, "ff_wg", [128, 8, FC], BF16, 2)
        wu = Rot(nc, es, "ff_wu", [128, 8, FC], BF16, 2)
        wd = Rot(nc, es, "ff_wd", [128, 4, D], BF16, 2)
        Gp = Rot(nc, es, "ff_G", [128, 512], F32, 2, psum=True)
        Up = Rot(nc, es, "ff_U", [128, 512], F32, 2, psum=True)
        Dp = Rot(nc, es, "ff_D", [128, 512], F32, 2, psum=True)
        sg = Rot(nc, es, "ff_sg", [128, 512], F32, 2)
        act = Rot(nc, es, "ff_act", [128, 4, TB], BF16, 2)
        hTs = [sb(nc, es, "ff_hT%d" % i_, [128, 8, TB], BF16) for i_ in range(2)]
        accs = [sb(nc, es, "ff_acc%d" % i_, [128, NTB, D], F32) for i_ in range(2)]
        npools = norm_pools(nc, es, "ffn_", need_xt=False)
        NB = S // TB

        def prep_steps(tb):
            hT, BhT = hTs[tb % 2]
            acc, Bacc = accs[tb % 2]
            steps = norm_transpose_steps(c, npools, T["xmid"], T.B["xmid"], gB, BgB, ident, Bid, hT, BhT, tb * NTB, NTB,
                                         keep_x=(acc, Bacc))
            if moe:
                comb, Bcomb = combs[tb % 2]
                held = {}

                def make_rm(it):
                    def rm():
                        p_, Bp = Dp.next()
                        for k in range(8):
                            c.mm(p_[:, 0:N_EXP], hT[:, k, it * 128:(it + 1) * 128], Wr[:, k, :], k == 0, k == 7,
                                 [BhT, BWr], [Bp])
                        held[it] = (p_, Bp)
                    return rm

                def make_rp(it):
                    def rp():
                        p_, Bp = held.pop(it)
                        l_, Bl = lg.next()
                        c.op("dve", lambda: nc.vector.tensor_tensor(out=l_[:], in0=p_[:, 0:N_EXP], in1=rb[:], op=ALU.add),
                             [Bp, Brb], [Bl])
                        m_, Bm = mx.next()
                        c.op("dve", lambda: nc.vector.max(out=m_[:, 0:8], in_=l_[:]), [Bl], [Bm])
                        e_, Be = ex.next()
                        c.op("dve", lambda: nc.vector.tensor_scalar(out=e_[:, 0:8], in0=l_[:], scalar1=m_[:, 0:1], scalar2=None,
                                                                    op0=ALU.subtract), [Bl, Bm], [Be])
                        c.op("act", lambda: nc.scalar.activation(out=e_[:, 0:8], in_=e_[:, 0:8], func=AF.Exp), [Be], [Be])
                        c.op("dve", lambda: nc.vector.scalar_tensor_tensor(out=e_[:, 8:16], in0=l_[:], scalar=m_[:, 1:2],
                                                                           in1=e_[:, 0:8], op0=ALU.is_ge, op1=ALU.mult),
                             [Bl, Bm, Be], [Be])
                        c.op("dve", lambda: nc.vector.tensor_reduce(out=m_[:, 2:3], in_=e_[:, 8:16], axis=AX.X, op=ALU.add),
                             [Be], [Bm])
                        c.op("dve", lambda: nc.vector.reciprocal(out=m_[:, 3:4], in_=m_[:, 2:3]), [Bm], [Bm])
                        c.op("dve", lambda: nc.vector.tensor_scalar(out=comb[:, it, :], in0=e_[:, 8:16], scalar1=m_[:, 3:4],
                                                                    scalar2=None, op0=ALU.mult), [Be, Bm], [Bcomb])
                    return rp

                steps.append(lambda: None)
                def make_r(it):
                    rm_, rp_ = make_rm(it), make_rp(it)

                    def r():
                        rm_()
                        rp_()
                    return r

                for it in range(NTB):
                    steps.append(make_r(it))
            return steps

        for st_ in prep_steps(0):
            st_()
        pending = []
        for tb in range(NB):
            hT, BhT = hTs[tb % 2]
            acc, Bacc = accs[tb % 2]
            if moe:
                comb, Bcomb = combs[tb % 2]
            n_chunk = 0
            for e, (wg_d, wu_d, wd_d) in enumerate(experts):
                wgv = wg_d.rearrange("(kc p) f -> p kc f", p=128)
                wuv = wu_d.rearrange("(kc p) f -> p kc f", p=128)
                for (f0, fw) in chunks:
                    n_chunk += 1
                    if n_chunk == 2 and tb + 1 < NB:
                        pending = prep_steps(tb + 1)
                    nfull = fw // 128
                    rem = fw - nfull * 128
                    nft = nfull + (1 if rem else 0)
                    g_, Bg = wg.next()
                    c.dma(g_[:, :, 0:fw], wgv[:, :, f0:f0 + fw], reads=[BWs[0]], writes=[Bg], q="pool")
                    u_, Bu = wu.next()
                    c.dma(u_[:, :, 0:fw], wuv[:, :, f0:f0 + fw], reads=[BWs[1]], writes=[Bu], q="pool")
                    d_, Bd = wd.next()
                    if nfull:
                        c.dma(d_[:, 0:nfull, :], wd_d[f0:f0 + nfull * 128, :].rearrange("(ft p) d -> p ft d", p=128),
                              reads=[BWs[2]], writes=[Bd], q="pool")
                    if rem:
                        c.dma(d_[0:rem, nfull, :], wd_d[f0 + nfull * 128:f0 + fw, :], reads=[BWs[2]], writes=[Bd], q="pool")
                    a_, Ba = act.next()
                    for ft in range(nft):
                        M = min(128, fw - ft * 128)
                        fs = slice(ft * 128, ft * 128 + M)
                        for tt in range(TB // 512):
                            ts_ = slice(tt * 512, (tt + 1) * 512)
                            if pending:
                                pending.pop(0)()
                            G, BG = Gp.next()
                            for k in range(8):
                                c.mm(G[0:M, :], g_[:, k, fs], hT[:, k, ts_], k == 0, k == 7, [Bg, BhT], [BG])
                            U, BU = Up.next()
                            for k in range(8):
                                c.mm(U[0:M, :], u_[:, k, fs], hT[:, k, ts_], k == 0, k == 7, [Bu, BhT], [BU])
                            s_, Bs = sg.next()
                            c.op("act", lambda: nc.scalar.activation(out=s_[0:M, :], in_=G[0:M, :], func=AF.Silu), [BG], [Bs])
                            c.op("dve", lambda: nc.vector.tensor_tensor(out=a_[0:M, ft, ts_], in0=s_[0:M, :], in1=U[0:M, :],
                                                                        op=ALU.mult), [Bs, BU], [Ba])
                    for it in range(NTB):
                        for dc in range(2):
                            ds_ = slice(dc * 512, (dc + 1) * 512)
                            Dd, BD = Dp.next()
                            for ft in range(nft):
                                M = min(128, fw - ft * 128)
                                c.mm(Dd[:, :], a_[0:M, ft, it * 128:(it + 1) * 128], d_[0:M, ft, ds_], ft == 0, ft == nft - 1,
                                     [Ba, Bd], [BD])
                            if moe:
                                c.op("dve", lambda: nc.vector.scalar_tensor_tensor(
                                    out=acc[:, it, ds_], in0=Dd[:, :], scalar=comb[:, it, e:e + 1], in1=acc[:, it, ds_],
                                    op0=ALU.mult, op1=ALU.add), [BD, Bcomb, Bacc], [Bacc])
                            else:
                                c.op("dve", lambda: nc.vector.tensor_tensor(out=acc[:, it, ds_], in0=Dd[:, :],
                                                                            in1=acc[:, it, ds_], op=ALU.add),
                                     [BD, Bacc], [Bacc])
            while pending:
                pending.pop(0)()
            c.dma(T[out_name][tb * TB:(tb + 1) * TB, :].rearrange("(i p) d -> p i d", p=128), acc[:], reads=[Bacc],
                  writes=[T.B[out_name]])
            c.barrier()
    c.barrier()


class Tensors:
    def __init__(self, nc, ext_in=(), ext_out=()):
        self.nc = nc
        self.t = {}
        self.B = {}
        self.ext_in = set(ext_in)
        self.ext_out = set(ext_out)
        self.in_names = []
        self.out_names = []

    def add(self, name, shape, dtype, kind=None):
        if kind is None:
            if name in self.ext_in:
                kind = "ExternalInput"
            elif name in self.ext_out:
                kind = "ExternalOutput"
            else:
                kind = "Internal"
        if kind == "ExternalInput":
            self.in_names.append(name)
        if kind == "ExternalOutput":
            self.out_names.append(name)
        self.t[name] = self.nc.dram_tensor(name, list(shape), dtype, kind=kind).ap()
        self.B[name] = Buf(name)
        return self.t[name]

    def __getitem__(self, k):
        return self.t[k]

    def dbg(self, c, name, ap, shape, dtype, Bsrc):
        if name not in self.ext_out:
            return
        d = self.add(name, shape, dtype, kind="ExternalOutput")
        c.dma(d, ap, reads=[Bsrc], writes=[self.B[name]])


PARAM_SHAPES = {
    "norm1_g": (2, 1024), "w_in": (2, 1024, 2460), "dil_qk_g": (2, 2, 64), "nsa_qk_g": (2, 4, 64),
    "nsa_cmp_pos": (2, 2, 32, 64), "nsa_cmp_w1": (2, 2, 2048, 256), "nsa_cmp_w2": (2, 2, 256, 64),
    "gla_wa2": (2, 16, 128), "gla_ba": (2, 128), "gla_norm_g": (2, 64),
    "s5_a_re": (2, 16, 64), "s5_a_im": (2, 16, 64), "s5_b_re": (2, 16, 64, 16), "s5_b_im": (2, 16, 64, 16),
    "s5_c_re": (2, 16, 16, 64), "s5_c_im": (2, 16, 16, 64), "s5_d": (2, 256), "s5_log_dt": (2, 16),
    "s5_glu_w": (2, 256, 256), "s5_glu_b": (2, 256), "out_norm_g": (2, 1024), "w_out": (2, 1024, 1024),
    "norm2_g": (2, 1024), "ffn_w_gate": (1, 1024, 2752), "ffn_w_up": (1, 1024, 2752),
    "ffn_w_down": (1, 2752, 1024), "moe_router_w": (1, 1024, 8), "moe_router_b": (1, 8),
    "moe_w_gate": (1, 8, 1024, 3584), "moe_w_up": (1, 8, 1024, 3584), "moe_w_down": (1, 8, 3584, 1024),
}


def host_consts():
    cst = {}
    bf = ml_dtypes.bfloat16
    cst["ident"] = np.eye(128, dtype=np.float32).astype(bf)
    p = np.arange(S)
    loc = np.arange(128).astype(np.float32)
    KBd = np.zeros((4, 36, 128), np.float32)
    for di in range(36):
        KBd[0, di] = 128.0 * (di - 32)
        KBd[1, di] = loc
        KBd[2, di] = 1.0
        KBd[3, di] = 1.0
    cst["c_KBd"] = KBd.astype(bf)
    QB = np.zeros((4, 3, 4, 128), np.float32)
    for pi, (win, d) in enumerate(DIL_PATTERNS):
        for h in range(4):
            a = DIL_SLOPES[h] * d
            QB[0, pi, h] = a
            QB[1, pi, h] = a
            QB[2, pi, h] = 0.0
            QB[3, pi, h] = -a * loc
    cst["c_QBdil"] = QB.astype(bf)
    kl = np.arange(128)[:, None]
    ql = np.arange(128)[None, :]
    masks = np.stack([(kl <= ql), (kl >= ql), (kl > ql)], axis=1).astype(np.float32)
    cst["c_masks"] = ((masks - 1.0) * 30000.0).astype(bf)
    cst["c_mask01"] = (kl <= ql).astype(np.float32).astype(bf)
    KBS = np.zeros((68, S), np.float32)
    KBS[p // 64, p] = 32768.0
    KBS[64] = 128.0 * (p // 128)
    KBS[65] = p % 128
    KBS[66] = 1.0
    KBS[67] = 1.0
    cst["c_KBS"] = KBS.astype(bf)
    QBa = np.zeros((4, 4, S), np.float32)
    for h in range(4):
        a = NSA_SLOPES[h]
        QBa[0, h] = a
        QBa[1, h] = a
        QBa[2, h] = -a * 128.0 * (p // 128)
        QBa[3, h] = -a * (p % 128)
    cst["c_QBabs"] = QBa.astype(bf)
    KBc = np.zeros((4, 32, 128), np.float32)
    for m in range(32):
        KBc[0, m] = -128.0 * m
        KBc[1, m] = 16.0 * loc
        KBc[2, m] = 31.0
        KBc[3, m] = 1.0
    cst["c_KBc"] = KBc.astype(bf)
    QBc = np.zeros((4, 4, 128), np.float32)
    for h in range(4):
        a = NSA_SLOPES[h]
        QBc[0, h] = a
        QBc[1, h] = a
        QBc[2, h] = a
        QBc[3, h] = -a * loc
    cst["c_QBc"] = QBc.astype(bf)
    cm = np.zeros((128, 17, 128), np.float32)
    for m in range(17):
        valid = (ql - 16 * kl) >= (31 - 128 * m)
        cm[:, m, :] = np.where(valid, 0.0, -30000.0)
    cst["c_cmask"] = cm.astype(bf)
    cc = np.arange(256)
    jj = np.arange(64)
    cover = ((16 * cc[:, None] < 64 * jj[None, :] + 64) & (16 * cc[:, None] + 32 > 64 * jj[None, :])).astype(np.float32)
    cover[255] = 0.0
    cst["c_cover"] = np.ascontiguousarray(cover.reshape(2, 128, 64).transpose(1, 0, 2)).astype(bf)
    tt = (128 * np.arange(32)[None, :, None] + np.arange(128)[:, None, None])
    cur = tt // 64
    j3 = jj[None, None, :]
    forced = (j3 == 0) | (j3 == cur) | (j3 == cur - 1)
    adjv = np.where(j3 <= cur, np.where(forced, 1.0e4, 0.0), -1.0e30).astype(np.float32)
    cst["c_adj"] = np.ascontiguousarray(adjv)
    cst["identf"] = np.eye(128, dtype=np.float32)
    K2s = np.zeros((64, S), np.float32)
    K2s[p // 64, p] = 1.0
    cst["c_K2sel"] = K2s.astype(bf)
    K2w = np.zeros((64, S), np.float32)
    K2w[p // 128, p] = 1.0
    cst["c_K2win"] = K2w.astype(bf)
    QW = np.zeros((64, 4, S), np.float32)
    for h in range(4):
        QW[0:32, h, :] = NSA_SLOPES[h] * 128.0 * (np.arange(32)[:, None] - (p // 128)[None, :])
    cst["c_QW"] = QW.astype(bf)
    As = np.zeros((128, 32, 4), np.float32)
    for h in range(4):
        As[64:128, :, h] = NSA_SLOPES[h] * 64.0 * (np.arange(64)[:, None] - 2 * np.arange(32)[None, :] - 1) + 30000.0
    cst["c_Asel"] = As
    Ev = np.zeros((128, 2, 4), np.float64)
    for h in range(4):
        Ev[:, 0, h] = np.exp(NSA_SLOPES[h] * (np.arange(128) % 64))
        Ev[:, 1, h] = np.exp(NSA_SLOPES[h] * np.arange(128))
    cst["c_Ev"] = Ev.astype(np.float32)
    pp_ = np.arange(128)[:, None, None]
    mm_ = np.arange(8)[None, :, None]
    ch_ = np.arange(128)[None, None, :]
    cst["c_s5mask"] = ((ch_ // 16) == (2 * (mm_ % 4) + pp_ // 64)).astype(np.float32)
    return cst


CONST_SPECS = {"ident": ((128, 128), BF16), "c_KBd": ((4, 36, 128), BF16), "c_QBdil": ((4, 3, 4, 128), BF16),
               "c_masks": ((128, 3, 128), BF16), "c_KBS": ((68, S), BF16), "c_QBabs": ((4, 4, S), BF16),
               "c_KBc": ((4, 32, 128), BF16), "c_QBc": ((4, 4, 128), BF16), "c_cmask": ((128, 17, 128), BF16),
               "c_cover": ((128, 2, 64), BF16), "c_mask01": ((128, 128), BF16), "c_adj": ((128, 32, 64), F32),
               "identf": ((128, 128), F32), "c_s5mask": ((128, 8, 128), F32),
               "c_K2sel": ((64, S), BF16), "c_K2win": ((64, S), BF16), "c_QW": ((64, 4, S), BF16),
               "c_Asel": ((128, 32, 4), F32), "c_Ev": ((128, 2, 4), F32)}


def build(phases=("proj",), layers=(0, 1), ext_in=(), ext_out=()):
    nc = bass.Bass("TRN2", target_bir_lowering=False)
    _UID[0] = 0
    T = Tensors(nc, ext_in, ext_out)
    T.add("xin0", (S, D), F32, kind="ExternalInput")
    for n, shp in PARAM_SHAPES.items():
        T.add(n, shp, F32, kind="ExternalInput")
    for n, (shp, dt_) in CONST_SPECS.items():
        T.add(n, shp, dt_, kind="ExternalInput")
    T.add("projT", (PW, S), BF16)
    T.add("proj", (S, PW), BF16)
    T.add("xin1", (S, D), F32)
    T.add("dacc", (3, S, 264), F32)
    T.add("mix", (S, D), F32)
    T.add("xmid", (S, D), F32)
    T.add("xin2", (S, D), F32, kind="ExternalOutput")
    c = Ctx(nc)
    for l in layers:
        if "proj" in phases:
            phase_proj(c, T, l)
        if "dil" in phases:
            phase_dil(c, T, l)
        if "nsa" in phases:
            phase_nsa(c, T, l)
        if "gla" in phases:
            phase_gla(c, T, l)
        if "s5" in phases:
            phase_s5(c, T, l)
        if "out" in phases:
            phase_out(c, T, l)
        if "ffn" in phases:
            phase_ffn(c, T, l, "xin%d" % (l + 1))
    c.barrier()
    return nc, T


ALL_PHASES = ("proj", "dil", "nsa", "gla", "s5", "out", "ffn")


def kernel(**inputs):
    nc, T = build(phases=ALL_PHASES, layers=(0, 1))
    cst = host_consts()
    x = np.asarray(inputs["x"], dtype=np.float32)
    shared = {}
    for n in T.in_names:
        if n == "xin0":
            continue
        if n in cst:
            shared[n] = cst[n]
        else:
            shared[n] = np.ascontiguousarray(np.asarray(inputs[n], dtype=np.float32))
    maps = []
    for b in range(8):
        m = dict(shared)
        m["xin0"] = np.ascontiguousarray(x[b])
        maps.append(m)
    res = run_bass_kernel_spmd(nc, maps, core_ids=list(range(8)))
    return np.stack([np.asarray(r["xin2"], dtype=np.float32) for r in res.results], axis=0)
```

```python
from contextlib import ExitStack, contextmanager
import math
import numpy as np
import ml_dtypes
import concourse.bass as bass
import concourse.mybir as mybir
from concourse.bass_utils import run_bass_kernel_spmd

F32 = mybir.dt.float32
BF16 = mybir.dt.bfloat16
ALU = mybir.AluOpType
AF = mybir.ActivationFunctionType
AX = mybir.AxisListType

S = 4096
D = 1024
NT = S // 128
PW = 2460
EPS = 1e-6
D_FF = 2752
N_EXP = 8
D_FFE = 3584
OFF = {}
_o = 0
for _n, _w in (("dil_q", 256), ("dil_k", 256), ("dil_v", 256), ("nsa_q", 256),
               ("nsa_k_cmp", 64), ("nsa_v_cmp", 64), ("nsa_k_slc", 64), ("nsa_v_slc", 64),
               ("nsa_k_win", 64), ("nsa_v_win", 64), ("nsa_gate", 12),
               ("gla_q", 128), ("gla_k", 128), ("gla_v", 256), ("gla_a", 16), ("gla_r", 256),
               ("s5_u", 256)):
    OFF[_n] = _o
    _o += _w
assert _o == PW

NDMA = 24
SEM_LIMIT = 24000

FM_RANGES = [(0, 512), (768, 1216), (1280, 1344), (1420, 1676), (1932, 1948), (2204, 2460)]
TM_RANGES = [(512, 256), (1216, 64), (1344, 76), (1676, 256), (1948, 256)]
TM_STORES = [(512, 256), (1216, 64), (1344, 76), (1676, 256), (1948, 256)]
FM_TILES = []
for _a, _b in FM_RANGES:
    _c = _a
    while _c < _b:
        FM_TILES.append((_c, min(128, _b - _c)))
        _c += 128


class Buf:
    __slots__ = ("name", "w", "r")

    def __init__(self, name=""):
        self.name = name
        self.w = {}
        self.r = {}


class Ctx:
    ENG = ("pe", "act", "dve", "pool", "sp")

    def __init__(self, nc):
        self.nc = nc
        self.eng = {"pe": nc.tensor, "act": nc.scalar, "dve": nc.vector,
                    "pool": nc.gpsimd, "sp": nc.sync}
        self.gen = {e: 0 for e in self.ENG}
        self.sem = {e: nc.alloc_semaphore("s_%s_0" % e) for e in self.ENG}
        self.tick = {e: 0 for e in self.ENG}
        self.seen = {e: {} for e in self.ENG}
        self.dsem = [nc.alloc_semaphore("s_dma_%d" % i) for i in range(NDMA)]
        self.dval = [0] * NDMA
        self.dn = 0
        self.dgen = 0
        self.strict = {"act", "dve", "pool"}
        self.n_ops = 0
        self.rr = 0
        self.last_pool = None

    def _need(self, e, waits, key, val, same_ok):
        if key[0] == "e" and key[1] == e and not same_ok:
            return
        if self.seen[e].get(key, 0) >= val:
            return
        if waits.get(key, 0) < val:
            waits[key] = val

    def _ekey(self, e):
        return ("e", e, self.gen[e])

    def _emit_waits(self, e, waits):
        eng = self.eng[e]
        for key, val in waits.items():
            if key[0] == "d":
                if key[2] != self.dgen:
                    continue
                s = self.dsem[key[1]]
            else:
                if key[2] != self.gen[key[1]]:
                    continue
                s = self.sem[key[1]]
            eng.wait_ge(s, val)
            self.seen[e][key] = val

    def op(self, e, fn, reads=(), writes=()):
        waits = {}
        st = e in self.strict
        for b in reads:
            for key, val in b.w.items():
                self._need(e, waits, key, val, st)
        for b in writes:
            for key, val in b.w.items():
                self._need(e, waits, key, val, st)
            for key, val in b.r.items():
                self._need(e, waits, key, val, False)
        self._emit_waits(e, waits)
        ins = fn()
        self.tick[e] += 1
        ins.then_inc(self.sem[e], 1)
        key = self._ekey(e)
        val = self.tick[e]
        for b in reads:
            b.r[key] = val
        for b in writes:
            b.w[key] = val
            b.r = {}
        self.n_ops += 1
        return ins

    def dma(self, out, in_, reads=(), writes=(), q="sp", **kw):
        e = q
        waits = {}
        for b in reads:
            for key, val in b.w.items():
                self._need(e, waits, key, val, True)
        for b in writes:
            for key, val in b.w.items():
                if key[0] == "d":
                    continue
                self._need(e, waits, key, val, True)
            for key, val in b.r.items():
                self._need(e, waits, key, val, True)
        i = self.dn % NDMA
        self.dn += 1
        dkey = ("d", i, self.dgen)
        if self.dval[i] > 0:
            self._need(e, waits, dkey, self.dval[i], True)
        if e == "pool" and self.last_pool is not None:
            self._need(e, waits, self.last_pool[0], self.last_pool[1], True)
        self._emit_waits(e, waits)
        ins = self.eng[e].dma_start(out=out, in_=in_, **kw)
        self.dval[i] += 16
        ins.then_inc(self.dsem[i], 16)
        if e == "pool":
            self.last_pool = (dkey, self.dval[i])
        for b in reads:
            b.r[dkey] = self.dval[i]
        for b in writes:
            b.w[dkey] = self.dval[i]
        return ins

    def barrier(self):
        for e in self.ENG:
            waits = {}
            for o in self.ENG:
                if o != e and self.tick[o] > 0:
                    self._need(e, waits, self._ekey(o), self.tick[o], True)
            for i in range(NDMA):
                if self.dval[i] > 0:
                    self._need(e, waits, ("d", i, self.dgen), self.dval[i], True)
            self._emit_waits(e, waits)
        for e in self.ENG:
            if self.tick[e] > SEM_LIMIT:
                self.gen[e] += 1
                self.sem[e] = self.nc.alloc_semaphore("s_%s_%d" % (e, self.gen[e]))
                self.tick[e] = 0
        if any(v > SEM_LIMIT for v in self.dval):
            self.dgen += 1
            self.dsem = [self.nc.alloc_semaphore("s_dma_%d_g%d" % (i, self.dgen)) for i in range(NDMA)]
            self.dval = [0] * NDMA

    def evac_engine(self):
        self.rr += 1
        return "act" if (self.rr & 1) else "dve"

    def copy(self, out, in_, reads, writes, e=None):
        nc = self.nc
        e = e or self.evac_engine()
        if e == "act":
            return self.op("act", lambda: nc.scalar.copy(out=out, in_=in_), reads, writes)
        if e == "pool":
            return self.op("pool", lambda: nc.gpsimd.tensor_copy(out=out, in_=in_), reads, writes)
        return self.op("dve", lambda: nc.vector.tensor_copy(out=out, in_=in_), reads, writes)

    def mm(self, out, lhsT, rhs, start, stop, reads, writes, **kw):
        nc = self.nc
        return self.op("pe", lambda: nc.tensor.matmul(out, lhsT=lhsT, rhs=rhs, start=start, stop=stop, **kw),
                       reads, writes)


@contextmanager
def scope(c):
    with ExitStack() as es:
        yield es
        c.barrier()


_UID = [0]


def uniq(name):
    _UID[0] += 1
    return "%s_u%d" % (name, _UID[0])


class Rot:
    def __init__(self, nc, es, name, shape, dtype, n, psum=False):
        self.t = []
        self.b = []
        name = uniq(name)
        for i in range(n):
            nm = "%s_%d" % (name, i)
            if psum:
                t = es.enter_context(nc.psum_tensor(nm, shape, dtype))
            else:
                t = es.enter_context(nc.sbuf_tensor(nm, shape, dtype))
            self.t.append(t)
            self.b.append(Buf(nm))
        self.i = 0

    def next(self):
        k = self.i % len(self.t)
        self.i += 1
        return self.t[k], self.b[k]


def sb(nc, es, name, shape, dtype):
    name = uniq(name)
    return es.enter_context(nc.sbuf_tensor(name, shape, dtype)), Buf(name)


def ps(nc, es, name, shape, dtype):
    name = uniq(name)
    return es.enter_context(nc.psum_tensor(name, shape, dtype)), Buf(name)


def norm_pools(nc, es, pfx, need_xt=True):
    xt = Rot(nc, es, pfx + "xt", [128, D], F32, 2) if need_xt else None
    junk = Rot(nc, es, pfx + "jk", [128, D], BF16, 2)
    hb = Rot(nc, es, pfx + "hb", [128, D], BF16, 2)
    ss = Rot(nc, es, pfx + "ss", [128, 2], F32, 4)
    tp = Rot(nc, es, pfx + "tp", [128, 1024], BF16, 2, psum=True)
    return xt, junk, hb, ss, tp


def norm_transpose_steps(c, pools, x_d, Bx_d, gB, BgB, ident, Bid, hT, BhT, tile0, ntiles, keep_x=None):
    nc = c.nc
    xt, junk, hb, ss, tp = pools
    held = {}

    def make_n(i):
        def n_step():
            t = tile0 + i
            if keep_x is not None:
                x_t = keep_x[0][:, i, :]
                Bx = keep_x[1]
            else:
                xx, Bx = xt.next()
                x_t = xx[:]
            c.dma(x_t, x_d[t * 128:(t + 1) * 128, :], reads=[Bx_d], writes=[Bx])
            jk, Bjk = junk.next()
            s_, Bs = ss.next()
            c.op("act", lambda: nc.scalar.activation(out=jk[:], in_=x_t, func=AF.Square,
                                                     accum_out=s_[:, 0:1]), [Bx], [Bjk, Bs])
            c.op("act", lambda: nc.scalar.activation(out=s_[:, 1:2], in_=s_[:, 0:1], func=AF.Sqrt,
                                                     bias=gB[:, D:D + 1], scale=1.0 / D), [Bs, BgB], [Bs])
            c.op("dve", lambda: nc.vector.reciprocal(out=s_[:, 1:2], in_=s_[:, 1:2]), [Bs], [Bs])
            h_, Bh = hb.next()
            c.op("dve", lambda: nc.vector.scalar_tensor_tensor(out=h_[:], in0=x_t, scalar=s_[:, 1:2],
                                                               in1=gB[:, 0:D], op0=ALU.mult, op1=ALU.mult),
                 [Bx, Bs, BgB], [Bh])
            held[i] = (h_, Bh)
        return n_step

    def make_t(i):
        def t_step():
            h_, Bh = held.pop(i)
            for g in range(2):
                p_, Bp = tp.next()
                for j in range(4):
                    kc = g * 4 + j
                    c.op("pe", lambda: nc.tensor.transpose(out=p_[:, j * 128:(j + 1) * 128],
                                                           in_=h_[:, kc * 128:(kc + 1) * 128],
                                                           identity=ident[:]), [Bh, Bid], [Bp])
                c.copy(hT[:, g * 4:(g + 1) * 4, i * 128:(i + 1) * 128],
                       p_[:, 0:512].rearrange("p (j t) -> p j t", j=4), [Bp], [BhT])
        return t_step

    steps = []
    for i in range(ntiles):
        steps.append(make_n(i))
        if i >= 1:
            steps.append(make_t(i - 1))
    steps.append(make_t(ntiles - 1))
    return steps


def norm_transpose(c, es, x_d, Bx_d, gB, BgB, ident, Bid, hT, BhT, tile0, ntiles, pfx,
                   keep_x=None, pools=None):
    nc = c.nc
    if pools is None:
        pools = norm_pools(nc, es, pfx, keep_x is None)
    for st_ in norm_transpose_steps(c, pools, x_d, Bx_d, gB, BgB, ident, Bid, hT, BhT, tile0, ntiles, keep_x):
        st_()


def load_gB(c, es, g_row, Bg_d, name):
    nc = c.nc
    gB, BgB = sb(nc, es, name, [128, D + 1], F32)
    c.dma(gB[:, 0:D], g_row.partition_broadcast(128), reads=[Bg_d], writes=[BgB])
    c.op("dve", lambda: nc.vector.memset(gB[:, D:D + 1], EPS), [], [BgB])
    return gB, BgB


def phase_proj(c, T, l):
    nc = c.nc
    with scope(c) as es:
        W, BW = sb(nc, es, "pj_W", [128, 8, PW], BF16)
        ident, Bid = sb(nc, es, "pj_id", [128, 128], BF16)
        c.dma(ident[:], T["ident"][:, :], reads=[T.B["ident"]], writes=[Bid])
        gB, BgB = load_gB(c, es, T["norm1_g"][l], T.B["norm1_g"], "pj_g")
        wv = T["w_in"][l].rearrange("(kc p) n -> p kc n", p=128)
        for kc in range(8):
            c.dma(W[:, kc, :], wv[:, kc, :], reads=[T.B["w_in"]], writes=[BW], q="pool")
        hTs = [sb(nc, es, "pj_hT%d" % i, [128, 8, 2048], BF16) for i in range(2)]
        stf = Rot(nc, es, "pj_stf", [128, 2048], BF16, 2)
        stt = Rot(nc, es, "pj_stt", [128, PW], BF16, 2)
        acc = Rot(nc, es, "pj_acc", [128, 512], F32, 4, psum=True)
        x_d = T["xin%d" % l]
        npools = norm_pools(nc, es, "pj_", need_xt=True)

        def half_steps(half):
            return norm_transpose_steps(c, npools, x_d, T.B["xin%d" % l], gB, BgB, ident, Bid, hTs[half][0], hTs[half][1],
                                        half * 16, 16)

        for st_ in half_steps(0):
            st_()
        pending = []
        for half in range(2):
            hT, BhT = hTs[half]
            while pending:
                pending.pop(0)()
            if half == 0:
                pending = half_steps(1)
            for (c0, M) in FM_TILES:
                st, Bst = stf.next()
                for tt in range(4):
                    if pending:
                        pending.pop(0)()
                    a_, Ba = acc.next()
                    for k in range(8):
                        c.mm(a_[0:M, :], W[:, k, c0:c0 + M], hT[:, k, tt * 512:(tt + 1) * 512],
                             k == 0, k == 7, [BW, BhT], [Ba])
                    c.copy(st[0:M, tt * 512:(tt + 1) * 512], a_[0:M, :], [Ba], [Bst])
                c.dma(T["projT"][c0:c0 + M, half * 2048:(half + 1) * 2048], st[0:M, :],
                      reads=[Bst], writes=[T.B["projT"]])
            for i in range(16):
                st, Bst = stt.next()
                t = half * 16 + i
                for (c0, N) in TM_RANGES:
                    a_, Ba = acc.next()
                    for k in range(8):
                        c.mm(a_[:, 0:N], hT[:, k, i * 128:(i + 1) * 128], W[:, k, c0:c0 + N],
                             k == 0, k == 7, [BW, BhT], [Ba])
                    c.copy(st[:, c0:c0 + N], a_[:, 0:N], [Ba], [Bst])
                for (c0, N) in TM_STORES:
                    c.dma(T["proj"][t * 128:(t + 1) * 128, c0:c0 + N], st[:, c0:c0 + N], reads=[Bst],
                          writes=[T.B["proj"]])
    c.barrier()


DIL_SLOPES = [2.0 ** -2, 2.0 ** -4, 2.0 ** -6, 2.0 ** -8]
NSA_SLOPES = [2.0 ** -1, 2.0 ** -3, 2.0 ** -5, 2.0 ** -7]
DIL_PATTERNS = ((128, 1), (512, 4), (2048, 16))


def ssl(start, count, step):
    return slice(start, start + step * (count - 1) + 1, step)


def load_col(c, es, name, vec_ap, Bsrc, n, mul=None):
    nc = c.nc
    t, B = sb(nc, es, name, [n, 1], F32)
    c.dma(t[:, 0:1], vec_ap.rearrange("(p o) -> p o", o=1), reads=[Bsrc], writes=[B])
    if mul is not None:
        c.op("act", lambda: nc.scalar.mul(out=t[:], in_=t[:], mul=mul), [B], [B])
    return t, B


def prep_qk(c, es, T, row0, H, gcol, Bg, ones64, Bones, epsc, Beps, dst, Bdst, pfx):
    nc = c.nc
    raw = Rot(nc, es, pfx + "raw", [64, S], BF16, 2)
    sq = Rot(nc, es, pfx + "sq", [64, 512], BF16, 2)
    rs = Rot(nc, es, pfx + "rs", [64, 512], F32, 2)
    pp = Rot(nc, es, pfx + "pp", [64, 512], F32, 2, psum=True)
    for h in range(H):
        r_, Br = raw.next()
        c.dma(r_[:], T["projT"][row0 + h * 64:row0 + (h + 1) * 64, :], reads=[T.B["projT"]], writes=[Br])
        for tt in range(8):
            sl = slice(tt * 512, (tt + 1) * 512)
            q_, Bq = sq.next()
            c.op("act", lambda: nc.scalar.activation(out=q_[:], in_=r_[:, sl], func=AF.Square), [Br], [Bq])
            p_, Bp = pp.next()
            c.mm(p_[:], ones64[:], q_[:], True, True, [Bq, Bones], [Bp])
            s_, Bs = rs.next()
            c.op("act", lambda: nc.scalar.activation(out=s_[:], in_=p_[:], func=AF.Sqrt, bias=epsc[0:64, 0:1],
                                                     scale=1.0 / 64), [Bp, Beps], [Bs])
            c.op("dve", lambda: nc.vector.reciprocal(out=s_[:], in_=s_[:]), [Bs], [Bs])
            c.op("dve", lambda: nc.vector.scalar_tensor_tensor(out=dst[0:64, h, sl], in0=r_[:, sl],
                                                               scalar=gcol[:, 0:1], in1=s_[:],
                                                               op0=ALU.mult, op1=ALU.mult),
                 [Br, Bs, Bg], [Bdst])


def load_consts_small(c, es, T, pfx):
    nc = c.nc
    ones64, Bones = sb(nc, es, pfx + "ones64", [64, 64], BF16)
    c.op("dve", lambda: nc.vector.memset(ones64[:], 1.0), [], [Bones])
    epsc, Beps = sb(nc, es, pfx + "epsc", [128, 1], F32)
    c.op("dve", lambda: nc.vector.memset(epsc[:], EPS), [], [Beps])
    return ones64, Bones, epsc, Beps


def phase_dil(c, T, l):
    nc = c.nc
    with scope(c) as es:
        ones64, Bones, epsc, Beps = load_consts_small(c, es, T, "dl_")
        qT, BqT = sb(nc, es, "dl_qT", [64, 4, S], BF16)
        kT, BkT = sb(nc, es, "dl_kT", [64, 4, S], BF16)
        KB, BKB = sb(nc, es, "dl_KB", [4, 36, 128], BF16)
        c.dma(KB[:], T["c_KBd"][:, :, :], reads=[T.B["c_KBd"]], writes=[BKB])
        QB, BQB = sb(nc, es, "dl_QB", [4, 3, 4, 128], BF16)
        c.dma(QB[:], T["c_QBdil"][:, :, :, :], reads=[T.B["c_QBdil"]], writes=[BQB])
        msk, Bmsk = sb(nc, es, "dl_msk", [128, 3, 128], BF16)
        c.dma(msk[:], T["c_masks"][:, :, :], reads=[T.B["c_masks"]], writes=[Bmsk])
        with scope(c) as es2:
            gq, Bgq = load_col(c, es2, "dl_gq", T["dil_qk_g"][l, 0], T.B["dil_qk_g"], 64, mul=0.125)
            gk, Bgk = load_col(c, es2, "dl_gk", T["dil_qk_g"][l, 1], T.B["dil_qk_g"], 64)
            with scope(c) as es3:
                prep_qk(c, es3, T, OFF["dil_q"], 4, gq, Bgq, ones64, Bones, epsc, Beps, qT, BqT, "dlq")
            c.barrier()
            with scope(c) as es3:
                prep_qk(c, es3, T, OFF["dil_k"], 4, gk, Bgk, ones64, Bones, epsc, Beps, kT, BkT, "dlk")
        T.dbg(c, "dbg_qT", qT[:], (64, 4, S), BF16, BqT)
        T.dbg(c, "dbg_kT", kT[:], (64, 4, S), BF16, BkT)
        Vp = []
        for pi in range(3):
            v_, Bv = sb(nc, es, "dl_V%d" % pi, [128, 32, 4, 66], BF16)
            c.op("dve", lambda: nc.vector.memset(v_[:], 1.0), [], [Bv])
            Vp.append((v_, Bv))
        with scope(c) as es2:
            stg = Rot(nc, es2, "dl_vst", [128, 8, 256], BF16, 2)
            for pi, (win, d) in enumerate(DIL_PATTERNS):
                v_, Bv = Vp[pi]
                bpc = 32 // d
                for r in range(d):
                    for b0 in range(0, bpc, 8):
                        nb = min(8, bpc - b0)
                        s_, Bs = stg.next()
                        src = T["proj"][ssl(r + d * 128 * b0, 128 * nb, d), OFF["dil_v"]:OFF["dil_v"] + 256]
                        c.dma(s_[:, 0:nb, :], src.rearrange("(b k) c -> k b c", k=128),
                              reads=[T.B["proj"]], writes=[Bs])
                        blk0 = r * bpc + b0
                        c.copy(v_[:, blk0:blk0 + nb, :, 0:64],
                               s_[:, 0:nb, :].rearrange("k b (h e) -> k b h e", h=4), [Bs], [Bv])
        c.barrier()
        for pi in range(3):
            T.dbg(c, "dbg_V%d" % pi, Vp[pi][0][:], (128, 32, 4, 66), BF16, Vp[pi][1])
        Sps = Rot(nc, es, "dl_S", [128, 512], F32, 3, psum=True)
        Ops = Rot(nc, es, "dl_O", [128, 512], F32, 2, psum=True)
        Pt = Rot(nc, es, "dl_P", [128, 512], BF16, 5)
        Mt = Rot(nc, es, "dl_M", [128, 512], F32, 3)
        Osb = Rot(nc, es, "dl_Osb", [128, 264], F32, 3)
        SKEW = 2
        tasks = []
        for pi, (win, d) in enumerate(DIL_PATTERNS):
            bpc = 32 // d
            for r in range(d):
                for bi in range(bpc):
                    kbs = [bi - 1, bi] if bi >= 1 else [bi]
                    for i_, kbi in enumerate(kbs):
                        tasks.append((pi, d, bpc, r, bi, kbi, i_ == 0, i_ == len(kbs) - 1))
        st = {}
        cur_o = [None]

        def stage_a(ti):
            pi, d, bpc, r, bi, kbi, is_first, is_last = tasks[ti]
            qsl = ssl(r + d * 128 * bi, 128, d)
            ksl = ssl(r + d * 128 * kbi, 128, d)
            s_, Bs = Sps.next()
            for h in range(4):
                c.mm(s_[:, h * 128:(h + 1) * 128], kT[:, h, ksl], qT[:, h, qsl], True, False,
                     [BkT, BqT], [Bs])
                c.mm(s_[:, h * 128:(h + 1) * 128], KB[:, 32 + kbi - bi, :],
                     QB[:, pi, h, :], False, True, [BKB, BQB], [Bs])
            p_, Bp = Pt.next()
            mi = 0 if kbi == bi else 1
            m_, Bm = Mt.next()
            c.op("dve", lambda: nc.vector.tensor_tensor(
                out=m_[:].rearrange("k (h q) -> k h q", h=4),
                in0=s_[:].rearrange("k (h q) -> k h q", h=4),
                in1=msk[:, mi:mi + 1, :].to_broadcast([128, 4, 128]), op=ALU.add), [Bs, Bmsk], [Bm])
            c.op("act", lambda: nc.scalar.activation(out=p_[:], in_=m_[:], func=AF.Exp), [Bm], [Bp])
            st[ti] = (p_, Bp)

        def stage_c(ti):
            pi, d, bpc, r, bi, kbi, is_first, is_last = tasks[ti]
            v_, Bv = Vp[pi]
            kblk = r * bpc + kbi
            p_, Bp = st.pop(ti)
            if is_first:
                cur_o[0] = Ops.next()
            o_, Bo = cur_o[0]
            for h in range(4):
                c.mm(o_[:, h * 66:(h + 1) * 66], p_[:, h * 128:(h + 1) * 128], v_[:, kblk, h, :],
                     is_first and h == 0, False, [Bp, Bv], [Bo], skip_group_check=True)
            if is_last:
                ob, Bob = Osb.next()
                c.copy(ob[:], o_[:, 0:264], [Bo], [Bob])
                dst = T["dacc"][pi, ssl(r + d * 128 * bi, 128, d), :]
                c.dma(dst, ob[:], reads=[Bob], writes=[T.B["dacc"]])

        for ti in range(len(tasks) + SKEW):
            if ti < len(tasks):
                stage_a(ti)
            if ti - SKEW >= 0:
                stage_c(ti - SKEW)
    c.barrier()
    with scope(c) as es:
        a3 = Rot(nc, es, "dc_a", [128, 3, 264], F32, 5)
        rc = Rot(nc, es, "dc_r", [128, 4], F32, 3)
        yo = Rot(nc, es, "dc_y", [128, 256], F32, 3)
        PF = 3
        loaded = {}

        def load(t):
            a_, Ba = a3.next()
            c.dma(a_[:], T["dacc"][:, t * 128:(t + 1) * 128, :].rearrange("p t c -> t p c"),
                  reads=[T.B["dacc"]], writes=[Ba])
            loaded[t] = (a_, Ba)

        for t in range(min(PF, NT)):
            load(t)
        for t in range(NT):
            if t + PF < NT:
                load(t + PF)
            a_, Ba = loaded.pop(t)
            c.op("dve", lambda: nc.vector.tensor_tensor(out=a_[:, 0, :], in0=a_[:, 0, :], in1=a_[:, 1, :], op=ALU.add),
                 [Ba], [Ba])
            c.op("dve", lambda: nc.vector.tensor_tensor(out=a_[:, 0, :], in0=a_[:, 0, :], in1=a_[:, 2, :], op=ALU.add),
                 [Ba], [Ba])
            av = a_[:, 0, :].rearrange("t (h e) -> t h e", h=4)
            r_, Br = rc.next()
            c.op("dve", lambda: nc.vector.reciprocal(out=r_[:].rearrange("t (h o) -> t h o", o=1), in_=av[:, :, 64:65]),
                 [Ba], [Br])
            y_, By = yo.next()
            c.op("dve", lambda: nc.vector.tensor_tensor(
                out=y_[:].rearrange("t (h e) -> t h e", h=4), in0=av[:, :, 0:64],
                in1=r_[:].rearrange("t (h o) -> t h o", o=1).to_broadcast([128, 4, 64]), op=ALU.mult),
                [Ba, Br], [By])
            c.dma(T["mix"][t * 128:(t + 1) * 128, 0:256], y_[:], reads=[By], writes=[T.B["mix"]])
    c.barrier()


NSA_STAGE = 99


def phase_nsa(c, T, l):
    _phase_nsa(c, T, l)
    c.barrier()


def _phase_nsa(c, T, l):
    nc = c.nc
    with scope(c) as es:
        ones64, Bones, epsc, Beps = load_consts_small(c, es, T, "ns_")
        ident, Bid = sb(nc, es, "ns_id", [128, 128], BF16)
        c.dma(ident[:], T["ident"][:, :], reads=[T.B["ident"]], writes=[Bid])
        qT, BqT = sb(nc, es, "ns_Q2s", [128, 4, S], BF16)
        Q2w, BQ2w = sb(nc, es, "ns_Q2w", [128, 4, S], BF16)
        c.dma(Q2w[64:128, :, :], T["c_QW"][:, :, :], reads=[T.B["c_QW"]], writes=[BQ2w])
        ksT, BksT = sb(nc, es, "ns_Ks2", [128, 1, S], BF16)
        c.dma(ksT[64:128, 0, :], T["c_K2sel"][:, :], reads=[T.B["c_K2sel"]], writes=[BksT])
        kwT, BkwT = sb(nc, es, "ns_Kw2", [128, 1, S], BF16)
        c.dma(kwT[64:128, 0, :], T["c_K2win"][:, :], reads=[T.B["c_K2win"]], writes=[BkwT])
        Asel, BAsel = sb(nc, es, "ns_Asel", [128, 32, 4], F32)
        c.dma(Asel[:], T["c_Asel"][:, :, :], reads=[T.B["c_Asel"]], writes=[BAsel])
        Vs, BVs = sb(nc, es, "ns_Vsh", [128, 32, 4, 66], BF16)
        Vw, BVw = sb(nc, es, "ns_Vwh", [128, 32, 4, 66], BF16)
        with scope(c) as es2:
            Ev, BEv = sb(nc, es2, "ns_Ev", [128, 2, 4], F32)
            c.dma(Ev[:], T["c_Ev"][:, :, :], reads=[T.B["c_Ev"]], writes=[BEv])
            for vi, (vh_, Bvh, nm) in enumerate(((Vs, BVs, "nsa_v_slc"), (Vw, BVw, "nsa_v_win"))):
                v_, Bv = sb(nc, es2, "ns_Vst%d" % vi, [128, 32, 66], BF16)
                c.op("dve", lambda: nc.vector.memset(v_[:], 1.0), [], [Bv])
                c.dma(v_[:, :, 0:64], T["proj"][:, OFF[nm]:OFF[nm] + 64].rearrange("(b k) e -> k b e", k=128),
                      reads=[T.B["proj"]], writes=[Bv])
                for h in range(4):
                    c.op("dve", lambda: nc.vector.tensor_scalar(out=vh_[:, :, h, :], in0=v_[:], scalar1=Ev[:, vi, h:h + 1],
                                                                scalar2=None, op0=ALU.mult), [Bv, BEv], [Bvh])
        kcT, BkcT = sb(nc, es, "ns_kcT", [64, 256], BF16)
        Vc, BVc = sb(nc, es, "ns_Vc", [128, 2, 130], BF16)
        c.op("dve", lambda: nc.vector.memset(Vc[:], 0.0), [], [BVc])
        c.op("dve", lambda: nc.vector.memset(Vc[:, :, 128:130], 1.0), [], [BVc])
        c.dma(Vc[:, :, 64:128], T["c_cover"][:, :, :], reads=[T.B["c_cover"]], writes=[BVc])
        with scope(c) as es2:
            g0, Bg0 = load_col(c, es2, "ns_g0", T["nsa_qk_g"][l, 0], T.B["nsa_qk_g"], 64, mul=0.125)
            g2, Bg2 = load_col(c, es2, "ns_g2", T["nsa_qk_g"][l, 2], T.B["nsa_qk_g"], 64)
            g3, Bg3 = load_col(c, es2, "ns_g3", T["nsa_qk_g"][l, 3], T.B["nsa_qk_g"], 64)
            with scope(c) as es3:
                prep_qk(c, es3, T, OFF["nsa_q"], 4, g0, Bg0, ones64, Bones, epsc, Beps, qT, BqT, "nsq")
            c.barrier()
            for h in range(4):
                c.op("act", lambda: nc.scalar.copy(out=Q2w[0:64, h, :], in_=qT[0:64, h, :]), [BqT], [BQ2w])
            with scope(c) as es3:
                prep_qk(c, es3, T, OFF["nsa_k_slc"], 1, g2, Bg2, ones64, Bones, epsc, Beps, ksT, BksT, "nss")
            with scope(c) as es3:
                prep_qk(c, es3, T, OFF["nsa_k_win"], 1, g3, Bg3, ones64, Bones, epsc, Beps, kwT, BkwT, "nsw")
            c.barrier()
        c.barrier()
        if NSA_STAGE <= 1:
            return
        with scope(c) as es2:
            g1, Bg1 = load_col(c, es2, "ns_g1", T["nsa_qk_g"][l, 1], T.B["nsa_qk_g"], 64)
            raw, Braw = sb(nc, es2, "ns_raw", [64, 2, S], BF16)
            c.dma(raw[:], T["projT"][OFF["nsa_k_cmp"]:OFF["nsa_k_cmp"] + 128, :].rearrange("(i d) t -> d i t", i=2),
                  reads=[T.B["projT"]], writes=[Braw])
            w1, Bw1 = sb(nc, es2, "ns_w1", [64, 2, 32, 256], BF16)
            w2, Bw2 = sb(nc, es2, "ns_w2", [128, 2, 2, 64], BF16)
            posT, BposT = sb(nc, es2, "ns_posT", [64, 2, 32], BF16)
            for i in range(2):
                c.dma(w1[:, i, :, :], T["nsa_cmp_w1"][l, i].rearrange("(l d) h -> d l h", d=64),
                      reads=[T.B["nsa_cmp_w1"]], writes=[Bw1], q="pool")
                c.dma(w2[:, i, :, :], T["nsa_cmp_w2"][l, i].rearrange("(hc p) e -> p hc e", p=128),
                      reads=[T.B["nsa_cmp_w2"]], writes=[Bw2], q="pool")
                c.dma(posT[:, i, :], T["nsa_cmp_pos"][l, i].rearrange("l d -> d l"),
                      reads=[T.B["nsa_cmp_pos"]], writes=[BposT], q="pool", allow_slow_non_contiguous=True)
            Hps = Rot(nc, es2, "ns_Hps", [128, 512], F32, 2, psum=True)
            bps = Rot(nc, es2, "ns_bps", [128, 512], F32, 2, psum=True)
            b1 = Rot(nc, es2, "ns_b1", [128, 2], F32, 2)
            zt = Rot(nc, es2, "ns_z", [128, 256], F32, 2)
            ut = Rot(nc, es2, "ns_u", [128, 256], F32, 2)
            G, BG = sb(nc, es2, "ns_G", [128, 2, 2, 256], BF16)
            c.op("dve", lambda: nc.vector.memset(G[:], 0.0), [], [BG])
            for i in range(2):
                for hc in range(2):
                    h_, Bh = Hps.next()
                    for ll in range(32):
                        c.mm(h_[:, 0:255], w1[:, i, ll, hc * 128:(hc + 1) * 128], raw[:, i, ssl(ll, 255, 16)],
                             ll == 0, ll == 31, [Bw1, Braw], [Bh])
                    bp, Bbp = bps.next()
                    for ll in range(32):
                        c.mm(bp[:, 0:2], w1[:, i, ll, hc * 128:(hc + 1) * 128], posT[:, i, ll:ll + 1].to_broadcast([64, 2]),
                             ll == 0, ll == 31, [Bw1, BposT], [Bbp])
                    b_, Bb = b1.next()
                    c.op("dve", lambda: nc.vector.tensor_copy(out=b_[:], in_=bp[:, 0:2]), [Bbp], [Bb])
                    z_, Bz = zt.next()
                    c.op("act", lambda: nc.scalar.activation(out=z_[:, 0:255], in_=h_[:, 0:255], func=AF.Identity,
                                                             bias=b_[:, 0:1]), [Bh, Bb], [Bz])
                    u_, Bu = ut.next()
                    c.op("dve", lambda: nc.vector.tensor_tensor(out=u_[:, 0:255], in0=z_[:, 0:255], in1=z_[:, 0:255],
                                                                op=ALU.mult), [Bz], [Bu])
                    c.op("dve", lambda: nc.vector.tensor_scalar(out=u_[:, 0:255], in0=u_[:, 0:255], scalar1=0.044715,
                                                                scalar2=1.0, op0=ALU.mult, op1=ALU.add), [Bu], [Bu])
                    c.op("dve", lambda: nc.vector.tensor_tensor(out=u_[:, 0:255], in0=u_[:, 0:255], in1=z_[:, 0:255],
                                                                op=ALU.mult), [Bu, Bz], [Bu])
                    c.op("act", lambda: nc.scalar.activation(out=u_[:, 0:255], in_=u_[:, 0:255], func=AF.Sigmoid,
                                                             scale=1.5957691216057308), [Bu], [Bu])
                    c.op("dve", lambda: nc.vector.tensor_tensor(out=G[:, i, hc, 0:255], in0=u_[:, 0:255],
                                                                in1=z_[:, 0:255], op=ALU.mult), [Bu, Bz], [BG])
            T.dbg(c, "dbg_w1", w1[:], (64, 2, 32, 256), BF16, Bw1)
            T.dbg(c, "dbg_posT", posT[:], (64, 2, 32), BF16, BposT)
            T.dbg(c, "dbg_G", G[:], (128, 2, 2, 256), BF16, BG)
            T.dbg(c, "dbg_w2", w2[:], (128, 2, 2, 64), BF16, Bw2)
            kp, Bkp = Hps.next()
            for hc in range(2):
                c.mm(kp[0:64, 0:256], w2[:, 0, hc, :], G[:, 0, hc, :], hc == 0, hc == 1, [Bw2, BG], [Bkp])
            kraw, Bkraw = sb(nc, es2, "ns_kraw", [64, 256], F32)
            c.op("dve", lambda: nc.vector.tensor_copy(out=kraw[:], in_=kp[0:64, 0:256]), [Bkp], [Bkraw])
            ksq, Bksq = sb(nc, es2, "ns_ksq", [64, 256], BF16)
            c.op("act", lambda: nc.scalar.activation(out=ksq[:], in_=kraw[:], func=AF.Square), [Bkraw], [Bksq])
            sp_, Bsp = bps.next()
            c.mm(sp_[0:64, 0:256], ones64[:], ksq[:], True, True, [Bksq, Bones], [Bsp])
            krs, Bkrs = sb(nc, es2, "ns_krs", [64, 256], F32)
            c.op("act", lambda: nc.scalar.activation(out=krs[:], in_=sp_[0:64, 0:256], func=AF.Sqrt,
                                                     bias=epsc[0:64, 0:1], scale=1.0 / 64), [Bsp, Beps], [Bkrs])
            c.op("dve", lambda: nc.vector.reciprocal(out=krs[:], in_=krs[:]), [Bkrs], [Bkrs])
            c.op("dve", lambda: nc.vector.scalar_tensor_tensor(out=kcT[:], in0=kraw[:], scalar=g1[:, 0:1], in1=krs[:],
                                                               op0=ALU.mult, op1=ALU.mult), [Bkraw, Bkrs, Bg1], [BkcT])
            for ct in range(2):
                rows = 128 if ct == 0 else 127
                vp_, Bvp = Hps.next()
                for hc in range(2):
                    c.mm(vp_[0:rows, 0:64], G[:, 1, hc, ct * 128:ct * 128 + rows], w2[:, 1, hc, :], hc == 0, hc == 1,
                         [BG, Bw2], [Bvp])
                c.op("dve", lambda: nc.vector.tensor_copy(out=Vc[0:rows, ct, 0:64], in_=vp_[0:rows, 0:64]), [Bvp], [BVc])
        c.barrier()
        KBc, BKBc = sb(nc, es, "ns_KBc", [4, 32, 128], BF16)
        c.dma(KBc[:], T["c_KBc"][:, :, :], reads=[T.B["c_KBc"]], writes=[BKBc])
        QBc, BQBc = sb(nc, es, "ns_QBc", [4, 4, 128], BF16)
        c.dma(QBc[:], T["c_QBc"][:, :, :], reads=[T.B["c_QBc"]], writes=[BQBc])
        cmask, Bcmask = sb(nc, es, "ns_cmask", [128, 17, 128], BF16)
        c.dma(cmask[:], T["c_cmask"][:, :, :], reads=[T.B["c_cmask"]], writes=[Bcmask])
        msk, Bmsk = sb(nc, es, "ns_msk", [128, 3, 128], BF16)
        c.dma(msk[:], T["c_masks"][:, :, :], reads=[T.B["c_masks"]], writes=[Bmsk])
        adj, Badj = sb(nc, es, "ns_adj", [128, 32, 64], F32)
        c.dma(adj[:], T["c_adj"][:, :, :], reads=[T.B["c_adj"]], writes=[Badj])
        gates, Bgates = sb(nc, es, "ns_gates", [128, 32, 12], F32)
        yacc, Byacc = sb(nc, es, "ns_yacc", [128, 32, 256], F32)
        with scope(c) as es2:
            gst, Bgst = sb(nc, es2, "ns_gst", [128, 32, 12], BF16)
            c.dma(gst[:], T["proj"][:, OFF["nsa_gate"]:OFF["nsa_gate"] + 12].rearrange("(b k) e -> k b e", k=128),
                  reads=[T.B["proj"]], writes=[Bgst])
            c.op("act", lambda: nc.scalar.activation(out=gates[:], in_=gst[:], func=AF.Sigmoid), [Bgst], [Bgates])
        c.barrier()
        T.dbg(c, "dbg_kcT", kcT[:], (64, 256), BF16, BkcT)
        T.dbg(c, "dbg_Vc", Vc[:], (128, 2, 130), BF16, BVc)
        if NSA_STAGE <= 2:
            return
        Sps = Rot(nc, es, "ns_S", [128, 512], F32, 3, psum=True)
        Ops = Rot(nc, es, "ns_O", [128, 512], F32, 2, psum=True)
        Tps = Rot(nc, es, "ns_T", [128, 256], BF16, 1, psum=True)
        Pt = Rot(nc, es, "ns_P", [128, 512], BF16, 5)
        Mt = Rot(nc, es, "ns_M", [128, 512], F32, 2)
        Ocs = Rot(nc, es, "ns_Oc", [128, 4, 130], F32, 3)
        Osb = Rot(nc, es, "ns_Osb", [128, 264], F32, 2)
        sm = Rot(nc, es, "ns_sm", [128, 16], F32, 3)
        impt = Rot(nc, es, "ns_imp", [128, 64], F32, 2)
        imp2 = Rot(nc, es, "ns_imp2", [128, 64], F32, 2)
        mx = Rot(nc, es, "ns_mx", [128, 16], F32, 2)
        selb = Rot(nc, es, "ns_selb", [128, 128], BF16, 2)
        for _i in range(2):
            c.op("dve", lambda: nc.vector.memset(selb.t[_i][:], 0.0), [], [selb.b[_i]])
        tmpy = Rot(nc, es, "ns_tmpy", [128, 256], F32, 2)

        def softmax_tile(s_, Bs, mask_ap, Bmask):
            p_, Bp = Pt.next()
            if mask_ap is not None:
                m_, Bm = Mt.next()
                c.op("dve", lambda: nc.vector.tensor_tensor(
                    out=m_[:].rearrange("k (h q) -> k h q", h=4), in0=s_[:].rearrange("k (h q) -> k h q", h=4),
                    in1=mask_ap.to_broadcast([128, 4, 128]), op=ALU.add), [Bs, Bmask], [Bm])
                c.op("act", lambda: nc.scalar.activation(out=p_[:], in_=m_[:], func=AF.Exp), [Bm], [Bp])
            else:
                c.op("act", lambda: nc.scalar.activation(out=p_[:], in_=s_[:], func=AF.Exp), [Bs], [Bp])
            return p_, Bp

        def finish_branch(o_ap, Bo, bq, br, first):
            num, den = o_ap
            s_, Bs_ = sm.next()
            c.op("dve", lambda: nc.vector.tensor_scalar(out=s_[:, 0:4].rearrange("t (h o) -> t h o", o=1), in0=den,
                                                        scalar1=1e-30, scalar2=None, op0=ALU.max), [Bo], [Bs_])
            c.op("dve", lambda: nc.vector.reciprocal(out=s_[:, 4:8], in_=s_[:, 0:4]), [Bs_], [Bs_])
            c.op("dve", lambda: nc.vector.tensor_tensor(out=s_[:, 8:12], in0=s_[:, 4:8], in1=gates[:, bq, ssl(br, 4, 3)],
                                                        op=ALU.mult), [Bs_, Bgates], [Bs_])
            wb = s_[:, 8:12].rearrange("t (h o) -> t h o", o=1).to_broadcast([128, 4, 64])
            yv = yacc[:, bq, :].rearrange("t (h e) -> t h e", h=4)
            if first:
                c.op("dve", lambda: nc.vector.tensor_tensor(out=yv, in0=num, in1=wb, op=ALU.mult), [Bo, Bs_], [Byacc])
            else:
                t_, Bt = tmpy.next()
                tv = t_[:].rearrange("t (h e) -> t h e", h=4)
                c.op("dve", lambda: nc.vector.tensor_tensor(out=tv, in0=num, in1=wb, op=ALU.mult), [Bo, Bs_], [Bt])
                c.op("dve", lambda: nc.vector.tensor_tensor(out=yacc[:, bq, :], in0=yacc[:, bq, :], in1=t_[:],
                                                            op=ALU.add), [Bt, Byacc], [Byacc])
            return s_, Bs_

        BqS = Buf("ns_qsel_rows")
        cmp_held = {}

        def cmp_a(bq):
            cts = [0] if bq < 16 else [0, 1]
            oA, BoA = Ops.next()
            oB, BoB = Ops.next()
            first = True
            for ct in cts:
                m = bq - 16 * ct
                s_, Bs = Sps.next()
                s4 = s_[:].rearrange("k (h q) -> k h q", h=4)
                c.mm(s4, kcT[:, ct * 128:(ct + 1) * 128], qT[0:64, :, bq * 128:(bq + 1) * 128],
                     True, False, [BkcT, BqT], [Bs])
                c.mm(s4, KBc[:, m, :], QBc[:, :, :], False, True, [BKBc, BQBc], [Bs])
                p_, Bp = softmax_tile(s_, Bs, cmask[:, m:m + 1, :] if m <= 16 else None, Bcmask)
                for h in range(4):
                    o_, Bo = (oA, BoA) if h < 2 else (oB, BoB)
                    hh = h % 2
                    c.mm(o_[:, hh * 130:(hh + 1) * 130], p_[:, h * 128:(h + 1) * 128], Vc[:, ct, :],
                         first and hh == 0, False, [Bp, BVc], [Bo], skip_group_check=True)
                first = False
            oc, Boc = Ocs.next()
            c.copy(oc[:, 0:2, :], oA[:, 0:260].rearrange("t (h e) -> t h e", h=2), [BoA], [Boc])
            c.copy(oc[:, 2:4, :], oB[:, 0:260].rearrange("t (h e) -> t h e", h=2), [BoB], [Boc])
            cmp_held[bq] = (oc, Boc)

        def cmp_b(bq):
            oc, Boc = cmp_held.pop(bq)
            s_, Bs_ = finish_branch((oc[:, :, 0:64], oc[:, :, 128:129]), Boc, bq, 0, True)
            im, Bim = impt.next()
            c.op("dve", lambda: nc.vector.scalar_tensor_tensor(out=im[:], in0=oc[:, 0, 64:128], scalar=s_[:, 4:5],
                                                               in1=adj[:, bq, :], op0=ALU.mult, op1=ALU.add),
                 [Boc, Bs_, Badj], [Bim])
            for h in range(1, 4):
                c.op("dve", lambda: nc.vector.scalar_tensor_tensor(out=im[:], in0=oc[:, h, 64:128], scalar=s_[:, 4 + h:5 + h],
                                                                   in1=im[:], op0=ALU.mult, op1=ALU.add),
                     [Boc, Bs_, Bim], [Bim])
            mx_, Bmx = mx.next()
            c.op("dve", lambda: nc.vector.max(out=mx_[:, 0:8], in_=im[:]), [Bim], [Bmx])
            i2, Bi2 = imp2.next()
            c.op("dve", lambda: nc.vector.match_replace(out=i2[:], in_to_replace=mx_[:, 0:8], in_values=im[:],
                                                        imm_value=-3.0e38), [Bim, Bmx], [Bi2])
            c.op("dve", lambda: nc.vector.max(out=mx_[:, 8:16], in_=i2[:]), [Bi2], [Bmx])
            sb_, Bsb = selb.next()
            c.op("dve", lambda: nc.vector.tensor_scalar(out=sb_[:, 64:128], in0=im[:], scalar1=mx_[:, 15:16], scalar2=None,
                                                        op0=ALU.is_ge), [Bim, Bmx], [Bsb])
            tp_, Btp = Tps.next()
            c.op("pe", lambda: nc.tensor.transpose(out=tp_[:, 0:128], in_=sb_[:], identity=ident[:]), [Bsb, Bid], [Btp])
            for h in range(4):
                c.op("dve", lambda: nc.vector.tensor_scalar(
                    out=qT[64:128, h, bq * 128:(bq + 1) * 128], in0=tp_[64:128, 0:128], scalar1=Asel[64:128, bq, h:h + 1],
                    scalar2=-30000.0, op0=ALU.mult, op1=ALU.add), [Btp, BAsel], [BqS])

        for bq in range(NT + 1):
            if bq < NT:
                cmp_a(bq)
            if bq >= 1:
                cmp_b(bq - 1)
        T.dbg(c, "dbg_ycmp", yacc[:], (128, 32, 256), F32, Byacc)
        if NSA_STAGE <= 3:
            return
        SKEW = 2
        tasks = []
        for bq in range(NT):
            for br in (1, 2):
                kbs = list(range(0, bq + 1)) if br == 1 else list(range(max(0, bq - 4), bq + 1))
                for i_, kb in enumerate(kbs):
                    tasks.append((bq, br, kb, i_ == 0, i_ == len(kbs) - 1))
        opnd = {1: (ksT, BksT, Vs, BVs, qT, BqS), 2: (kwT, BkwT, Vw, BVw, Q2w, BQ2w)}
        st = {}
        cur_o = [None]

        def stage_a(ti):
            bq, br, kb, is_first, is_last = tasks[ti]
            kT_, BkT_, V_, BV_, Q_, BQ_ = opnd[br]
            s_, Bs = Sps.next()
            s4 = s_[:].rearrange("k (h q) -> k h q", h=4)
            c.mm(s4, kT_[:, 0, kb * 128:(kb + 1) * 128], Q_[:, :, bq * 128:(bq + 1) * 128], True, True,
                 [BkT_, BQ_, BqT], [Bs])
            mask_ap = None
            if kb == bq:
                mask_ap = msk[:, 0:1, :]
            elif br == 2 and kb == bq - 4:
                mask_ap = msk[:, 2:3, :]
            st[ti] = softmax_tile(s_, Bs, mask_ap, Bmsk)

        def stage_c(ti):
            bq, br, kb, is_first, is_last = tasks[ti]
            kT_, BkT_, V_, BV_, Q_, BQ_ = opnd[br]
            p_, Bp = st.pop(ti)
            if is_first:
                cur_o[0] = Ops.next()
            o_, Bo = cur_o[0]
            for h in range(4):
                c.mm(o_[:, h * 66:(h + 1) * 66], p_[:, h * 128:(h + 1) * 128], V_[:, kb, h, :],
                     is_first and h == 0, False, [Bp, BV_], [Bo], skip_group_check=True)
            if is_last:
                ob, Bob = Osb.next()
                c.copy(ob[:], o_[:, 0:264], [Bo], [Bob])
                ov = ob[:].rearrange("t (h e) -> t h e", h=4)
                finish_branch((ov[:, :, 0:64], ov[:, :, 64:65]), Bob, bq, br, False)

        for ti in range(len(tasks) + SKEW):
            if ti < len(tasks):
                stage_a(ti)
            if ti - SKEW >= 0:
                stage_c(ti - SKEW)
        c.dma(T["mix"][:, 256:512].rearrange("(b q) e -> q b e", q=128), yacc[:], reads=[Byacc], writes=[T.B["mix"]])
    c.barrier()


GLA_STAGE = 99


def phase_gla(c, T, l):
    _phase_gla(c, T, l)
    c.barrier()


def _phase_gla(c, T, l):
    nc = c.nc
    with scope(c) as es:
        ident, Bid = sb(nc, es, "gl_id", [128, 128], BF16)
        c.dma(ident[:], T["ident"][:, :], reads=[T.B["ident"]], writes=[Bid])
        m01, Bm01 = sb(nc, es, "gl_m01", [128, 128], BF16)
        c.dma(m01[:], T["c_mask01"][:, :], reads=[T.B["c_mask01"]], writes=[Bm01])
        epsc, Beps = sb(nc, es, "gl_eps", [128, 1], F32)
        c.op("dve", lambda: nc.vector.memset(epsc[:], EPS), [], [Beps])
        gnB, BgnB = sb(nc, es, "gl_gnB", [128, 64], F32)
        c.dma(gnB[:], T["gla_norm_g"][l].partition_broadcast(128), reads=[T.B["gla_norm_g"]], writes=[BgnB])
        qt, Bqt = sb(nc, es, "gl_qt", [32, 4, S], BF16)
        kt, Bkt = sb(nc, es, "gl_kt", [32, 4, S], BF16)
        ktm, Bktm = sb(nc, es, "gl_ktm", [128, NT, 128], BF16)
        ebl, Bebl = sb(nc, es, "gl_ebl", [32, 4, NT], F32)
        with scope(c) as es2:
            wa, Bwa = sb(nc, es2, "gl_wa", [16, 128], BF16)
            c.dma(wa[:], T["gla_wa2"][l], reads=[T.B["gla_wa2"]], writes=[Bwa], q="pool")
            bac, Bbac = sb(nc, es2, "gl_ba", [32, 4], F32)
            c.dma(bac[:], T["gla_ba"][l].rearrange("(g p) -> p g", g=4), reads=[T.B["gla_ba"]], writes=[Bbac],
                  allow_slow_non_contiguous=True)
            aT, BaT = sb(nc, es2, "gl_aT", [16, S], BF16)
            c.dma(aT[:], T["projT"][OFF["gla_a"]:OFF["gla_a"] + 16, :], reads=[T.B["projT"]], writes=[BaT])
            seg, Bseg = sb(nc, es2, "gl_seg", [32, S], BF16)
            c.op("dve", lambda: nc.vector.memset(seg[:], 1.0), [], [Bseg])
            c.op("dve", lambda: nc.vector.memset(seg[:, ssl(0, NT, 128)], 0.0), [], [Bseg])
            zp = Rot(nc, es2, "gl_zp", [128, 512], F32, 2, psum=True)
            qkr = Rot(nc, es2, "gl_qk", [32, 2, S], BF16, 2)
            lar = Rot(nc, es2, "gl_la", [32, S], F32, 2)
            bbr = Rot(nc, es2, "gl_bb", [32, S], F32, 2)
            qkv = T["projT"][OFF["gla_q"]:OFF["gla_q"] + 256, :].rearrange("(i p) t -> p i t", i=8)
            for g in range(4):
                qk, Bqk = qkr.next()
                c.dma(qk[:, 0, :], qkv[:, g, :], reads=[T.B["projT"]], writes=[Bqk])
                c.dma(qk[:, 1, :], qkv[:, 4 + g, :], reads=[T.B["projT"]], writes=[Bqk])
                la, Bla = lar.next()
                bb, Bbb = bbr.next()
                for tt in range(8):
                    sl = slice(tt * 512, (tt + 1) * 512)
                    z_, Bz = zp.next()
                    c.mm(z_[0:32, :], wa[:, g * 32:(g + 1) * 32], aT[:, sl], True, True, [Bwa, BaT], [Bz])
                    c.op("act", lambda: nc.scalar.activation(out=la[:, sl], in_=z_[0:32, :], func=AF.Sigmoid,
                                                             bias=bac[:, g:g + 1]), [Bz, Bbac], [Bla])
                c.op("act", lambda: nc.scalar.activation(out=la[:], in_=la[:], func=AF.Ln), [Bla], [Bla])
                c.op("act", lambda: nc.scalar.mul(out=la[:], in_=la[:], mul=1.0 / 16.0), [Bla], [Bla])
                c.op("dve", lambda: nc.vector.tensor_tensor_scan(out=bb[:], data0=seg[:], data1=la[:],
                                                                 initial=0.0, op0=ALU.mult, op1=ALU.add),
                     [Bseg, Bla], [Bbb])
                c.op("act", lambda: nc.scalar.activation(out=la[:], in_=bb[:], func=AF.Exp), [Bbb], [Bla])
                c.op("dve", lambda: nc.vector.tensor_copy(out=ebl[:, g, :], in_=la[:, ssl(127, NT, 128)]), [Bla], [Bebl])
                c.op("dve", lambda: nc.vector.scalar_tensor_tensor(out=qt[:, g, :], in0=qk[:, 0, :], scalar=32.0 ** -0.5,
                                                                   in1=la[:], op0=ALU.mult, op1=ALU.mult),
                     [Bqk, Bla], [Bqt])
                c.op("act", lambda: nc.scalar.activation(out=bb[:], in_=bb[:], func=AF.Exp, scale=-1.0),
                     [Bbb], [Bbb])
                c.op("dve", lambda: nc.vector.tensor_tensor(out=kt[:, g, :], in0=qk[:, 1, :], in1=bb[:],
                                                            op=ALU.mult), [Bqk, Bbb], [Bkt])
            tp = Rot(nc, es2, "gl_tp", [128, 1024], BF16, 2, psum=True)
            for n in range(NT):
                p_, Bp = tp.next()
                for g in range(4):
                    c.op("pe", lambda: nc.tensor.transpose(out=p_[:, g * 32:(g + 1) * 32],
                                                           in_=kt[:, g, n * 128:(n + 1) * 128],
                                                           identity=ident[0:32, 0:32]), [Bkt, Bid], [Bp])
                c.copy(ktm[:, n, :], p_[:, 0:128], [Bp], [Bktm])
        v, Bv = sb(nc, es, "gl_v", [128, NT, 256], BF16)
        c.dma(v[:], T["proj"][:, OFF["gla_v"]:OFF["gla_v"] + 256].rearrange("(n j) e -> j n e", j=128),
              reads=[T.B["proj"]], writes=[Bv])
        oall, Boall = sb(nc, es, "gl_oall", [128, NT, 256], F32)
        Sps = Rot(nc, es, "gl_S", [128, 512], F32, 2, psum=True)
        Ops = Rot(nc, es, "gl_O", [128, 512], F32, 2, psum=True)
        Pps = Rot(nc, es, "gl_P", [128, 512], F32, 2, psum=True)
        At = Rot(nc, es, "gl_A", [128, 512], BF16, 3)
        Sf, BSf = sb(nc, es, "gl_Sf", [32, 4, 64], F32)
        Sf2, BSf2 = sb(nc, es, "gl_Sf2", [32, 4, 64], F32)
        Sf3, BSf3 = sb(nc, es, "gl_Sf3", [32, 4, 64], F32)
        Sst, BSst = sb(nc, es, "gl_Sst", [32, 4, 64], BF16)
        c.op("dve", lambda: nc.vector.memset(Sf[:], 0.0), [], [BSf])
        c.op("dve", lambda: nc.vector.memset(Sst[:], 0.0), [], [BSst])
        for n in range(NT):
            sl = slice(n * 128, (n + 1) * 128)
            s_, Bs = Sps.next()
            for h in range(4):
                c.mm(s_[:, h * 128:(h + 1) * 128], kt[:, h, sl], qt[:, h, sl], True, True, [Bkt, Bqt], [Bs])
            pp, Bpp = Pps.next()
            for h in range(4):
                c.mm(pp[0:32, h * 64:(h + 1) * 64], ktm[:, n, 32 * h:32 * h + 32], v[:, n, 64 * h:64 * h + 64],
                     True, True, [Bktm, Bv], [Bpp])
            a_, Ba = At.next()
            c.op("dve", lambda: nc.vector.tensor_tensor(
                out=a_[:].rearrange("k (h q) -> k h q", h=4), in0=s_[:].rearrange("k (h q) -> k h q", h=4),
                in1=m01[:].rearrange("k (o q) -> k o q", o=1).to_broadcast([128, 4, 128]), op=ALU.mult), [Bs, Bm01], [Ba])
            o_, Bo = Ops.next()
            for h in range(4):
                c.mm(o_[:, 64 * h:64 * h + 64], a_[:, h * 128:(h + 1) * 128], v[:, n, 64 * h:64 * h + 64],
                     True, False, [Ba, Bv], [Bo])
                c.mm(o_[:, 64 * h:64 * h + 64], qt[:, h, sl], Sst[:, h, :], False, True, [Bqt, BSst], [Bo])
            c.copy(oall[:, n, :], o_[:, 0:256], [Bo], [Boall])
            eb = ebl[:, :, n:n + 1].to_broadcast([32, 4, 64])
            c.op("dve", lambda: nc.vector.tensor_tensor(out=Sf2[:], in0=Sf[:], in1=eb, op=ALU.mult), [BSf, Bebl], [BSf2])
            c.op("dve", lambda: nc.vector.tensor_tensor(out=Sf3[:], in0=pp[0:32, 0:256].rearrange("p (h e) -> p h e", h=4),
                                                        in1=eb, op=ALU.mult), [Bpp, Bebl], [BSf3])
            c.op("dve", lambda: nc.vector.tensor_tensor(out=Sf[:], in0=Sf2[:], in1=Sf3[:], op=ALU.add), [BSf2, BSf3], [BSf])
            c.op("dve", lambda: nc.vector.tensor_copy(out=Sst[:], in_=Sf[:]), [BSf], [BSst])
        if GLA_STAGE <= 5:
            return
        with scope(c) as es2:
            r_, Br = sb(nc, es2, "gl_r", [128, NT, 256], BF16)
            c.dma(r_[:], T["proj"][:, OFF["gla_r"]:OFF["gla_r"] + 256].rearrange("(n j) e -> j n e", j=128),
                  reads=[T.B["proj"]], writes=[Br])
            sq, Bsq = sb(nc, es2, "gl_sq", [128, NT, 256], F32)
            ss, Bss = sb(nc, es2, "gl_ss", [128, NT * 4], F32)
            c.op("act", lambda: nc.scalar.activation(out=sq[:], in_=oall[:], func=AF.Square), [Boall], [Bsq])
            c.op("dve", lambda: nc.vector.tensor_reduce(out=ss[:], in_=sq[:].rearrange("t n (h e) -> t (n h) e", h=4),
                                                        axis=AX.X, op=ALU.add), [Bsq], [Bss])
            c.op("act", lambda: nc.scalar.activation(out=ss[:], in_=ss[:], func=AF.Sqrt, bias=epsc[:, 0:1], scale=1.0 / 64),
                 [Bss, Beps], [Bss])
            c.op("dve", lambda: nc.vector.reciprocal(out=ss[:], in_=ss[:]), [Bss], [Bss])
            ov = oall[:].rearrange("t n (h e) -> t (n h) e", h=4)
            c.op("dve", lambda: nc.vector.tensor_tensor(
                out=ov, in0=ov, in1=ss[:].rearrange("t (m o) -> t m o", o=1).to_broadcast([128, NT * 4, 64]), op=ALU.mult),
                [Boall, Bss], [Boall])
            c.op("dve", lambda: nc.vector.tensor_tensor(
                out=ov, in0=ov, in1=gnB[:].rearrange("t (o e) -> t o e", o=1).to_broadcast([128, NT * 4, 64]), op=ALU.mult),
                [Boall, BgnB], [Boall])
            c.op("act", lambda: nc.scalar.activation(out=sq[:], in_=r_[:], func=AF.Silu), [Br], [Bsq])
            c.op("dve", lambda: nc.vector.tensor_tensor(out=oall[:], in0=oall[:], in1=sq[:], op=ALU.mult),
                 [Boall, Bsq], [Boall])
            c.dma(T["mix"][:, 512:768].rearrange("(n j) e -> j n e", j=128), oall[:], reads=[Boall], writes=[T.B["mix"]])


S5_L = 256
S5_STAGE = 99


def phase_s5(c, T, l):
    _phase_s5(c, T, l)
    c.barrier()


def _phase_s5(c, T, l):
    nc = c.nc
    L = S5_L
    NCH = S // L
    uoff = OFF["s5_u"]

    def dv(fn, r, w):
        return c.op("dve", fn, r, w)

    def tt(out, a, b, op, r, w):
        return c.op("dve", lambda: nc.vector.tensor_tensor(out=out, in0=a, in1=b, op=op), r, w)

    with scope(c) as es:
        ident, Bid = sb(nc, es, "s5_id", [128, 128], BF16)
        c.dma(ident[:], T["ident"][:, :], reads=[T.B["ident"]], writes=[Bid])
        identf, Bidf = sb(nc, es, "s5_idf", [128, 128], F32)
        c.dma(identf[:], T["identf"][:, :], reads=[T.B["identf"]], writes=[Bidf])
        mask, Bmask = sb(nc, es, "s5_mask", [128, 8, 128], F32)
        c.dma(mask[:], T["c_s5mask"][:, :, :], reads=[T.B["c_s5mask"]], writes=[Bmask])
        P, BP = sb(nc, es, "s5_P", [128, 40, 8], F32)
        hp, Bhp = sb(nc, es, "s5_hp", [128, 1], F32)
        dv(lambda: nc.vector.memset(hp[:], math.pi / 2.0), [], [Bhp])
        (ARE, AIM, LDT, DT, MAG, TH, CS, SN, C2, S2, TA, TB_, ABR, ABI, DEN, AM1, FRE, FIM, PR, PI_, PR2, PI2,
         WLR, WLI, X1, X2) = range(26)

        def col(i):
            return P[:, i, :]

        for gg in range(2):
            ps_ = slice(gg * 64, (gg + 1) * 64)
            c.dma(P[ps_, ARE, :], T["s5_a_re"][l].rearrange("(m gg) n -> gg n m", gg=2)[gg], reads=[T.B["s5_a_re"]],
                  writes=[BP], allow_slow_non_contiguous=True)
            c.dma(P[ps_, AIM, :], T["s5_a_im"][l].rearrange("(m gg) n -> gg n m", gg=2)[gg], reads=[T.B["s5_a_im"]],
                  writes=[BP], allow_slow_non_contiguous=True)
            c.dma(P[ps_, LDT, :], T["s5_log_dt"][l].rearrange("(m gg) -> gg m", gg=2)[gg].partition_broadcast(64),
                  reads=[T.B["s5_log_dt"]], writes=[BP], allow_slow_non_contiguous=True)
        c.op("act", lambda: nc.scalar.activation(out=col(DT), in_=col(LDT), func=AF.Exp), [BP], [BP])
        tt(col(TA), col(DT), col(ARE), ALU.mult, [BP], [BP])
        c.op("act", lambda: nc.scalar.activation(out=col(MAG), in_=col(TA), func=AF.Exp), [BP], [BP])
        tt(col(TH), col(DT), col(AIM), ALU.mult, [BP], [BP])
        c.op("act", lambda: nc.scalar.activation(out=col(SN), in_=col(TH), func=AF.Sin, scale=1.0 / 16.0), [BP], [BP])
        c.op("act", lambda: nc.scalar.activation(out=col(CS), in_=col(TH), func=AF.Sin, scale=1.0 / 16.0,
                                                 bias=hp[:, 0:1]), [BP, Bhp], [BP])

        def csq(cr, ci, orr, oi):
            tt(col(TA), col(cr), col(cr), ALU.mult, [BP], [BP])
            tt(col(TB_), col(ci), col(ci), ALU.mult, [BP], [BP])
            dv(lambda: nc.vector.scalar_tensor_tensor(out=col(oi), in0=col(cr), scalar=2.0, in1=col(ci),
                                                      op0=ALU.mult, op1=ALU.mult), [BP], [BP])
            tt(col(orr), col(TA), col(TB_), ALU.subtract, [BP], [BP])

        csq(CS, SN, C2, S2)
        csq(C2, S2, CS, SN)
        csq(CS, SN, C2, S2)
        csq(C2, S2, CS, SN)
        tt(col(ABR), col(MAG), col(CS), ALU.mult, [BP], [BP])
        tt(col(ABI), col(MAG), col(SN), ALU.mult, [BP], [BP])
        tt(col(TA), col(ARE), col(ARE), ALU.mult, [BP], [BP])
        tt(col(TB_), col(AIM), col(AIM), ALU.mult, [BP], [BP])
        tt(col(DEN), col(TA), col(TB_), ALU.add, [BP], [BP])
        dv(lambda: nc.vector.reciprocal(out=col(DEN), in_=col(DEN)), [BP], [BP])
        dv(lambda: nc.vector.tensor_scalar(out=col(AM1), in0=col(ABR), scalar1=-1.0, scalar2=None, op0=ALU.add), [BP], [BP])
        tt(col(TA), col(AM1), col(ARE), ALU.mult, [BP], [BP])
        tt(col(TB_), col(ABI), col(AIM), ALU.mult, [BP], [BP])
        tt(col(FRE), col(TA), col(TB_), ALU.add, [BP], [BP])
        tt(col(FRE), col(FRE), col(DEN), ALU.mult, [BP], [BP])
        tt(col(TA), col(ABI), col(ARE), ALU.mult, [BP], [BP])
        tt(col(TB_), col(AM1), col(AIM), ALU.mult, [BP], [BP])
        tt(col(FIM), col(TA), col(TB_), ALU.subtract, [BP], [BP])
        tt(col(FIM), col(FIM), col(DEN), ALU.mult, [BP], [BP])
        BT, BBT = sb(nc, es, "s5_BT", [128, 2, 8, 128], BF16)
        Cm, BCm = sb(nc, es, "s5_Cm", [128, 2, 8, 128], BF16)
        with scope(c) as es2:
            braw, Bbraw = sb(nc, es2, "s5_braw", [128, 2, 8, 16], F32)
            craw, Bcraw = sb(nc, es2, "s5_craw", [128, 2, 8, 16], F32)
            for gg in range(2):
                ps_ = slice(gg * 64, (gg + 1) * 64)
                for ri, nm in enumerate(("s5_b_re", "s5_b_im")):
                    c.dma(braw[ps_, ri, :, :], T[nm][l].rearrange("(m gg) n c -> gg n m c", gg=2)[gg], reads=[T.B[nm]],
                          writes=[Bbraw], allow_slow_non_contiguous=True)
                for ri, nm in enumerate(("s5_c_re", "s5_c_im")):
                    for m in range(8):
                        c.dma(craw[ps_, ri, m, :], T[nm][l, 2 * m + gg].rearrange("c n -> n c"), reads=[T.B[nm]],
                              writes=[Bcraw], allow_slow_non_contiguous=True)
            bbc, Bbbc = sb(nc, es2, "s5_bbc", [128, 2, 8, 16], F32)
            t1, Bt1 = sb(nc, es2, "s5_t1", [128, 8, 16], F32)
            t2, Bt2 = sb(nc, es2, "s5_t2", [128, 8, 16], F32)
            fre_b = P[:, FRE, :].rearrange("p (m o) -> p m o", o=1).to_broadcast([128, 8, 16])
            fim_b = P[:, FIM, :].rearrange("p (m o) -> p m o", o=1).to_broadcast([128, 8, 16])
            tt(t1[:], braw[:, 0, :, :], fre_b, ALU.mult, [Bbraw, BP], [Bt1])
            tt(t2[:], braw[:, 1, :, :], fim_b, ALU.mult, [Bbraw, BP], [Bt2])
            tt(bbc[:, 0, :, :], t1[:], t2[:], ALU.subtract, [Bt1, Bt2], [Bbbc])
            tt(t1[:], braw[:, 1, :, :], fre_b, ALU.mult, [Bbraw, BP], [Bt1])
            tt(t2[:], braw[:, 0, :, :], fim_b, ALU.mult, [Bbraw, BP], [Bt2])
            tt(bbc[:, 1, :, :], t1[:], t2[:], ALU.add, [Bt1, Bt2], [Bbbc])
            Bd, BBd = sb(nc, es2, "s5_Bd", [128, 2, 8, 128], BF16)
            mask4 = mask[:].rearrange("p m (j c) -> p m j c", c=16)
            for ri in range(2):
                tt(Bd[:, ri, :, :].rearrange("p m (j c) -> p m j c", c=16), mask4,
                   bbc[:, ri, :, :].rearrange("p m (o c) -> p m o c", o=1).to_broadcast([128, 8, 8, 16]), ALU.mult,
                   [Bmask, Bbbc], [BBd])
            tt(Cm[:, 0, :, :].rearrange("p m (j c) -> p m j c", c=16), mask4,
               craw[:, 0, :, :].rearrange("p m (o c) -> p m o c", o=1).to_broadcast([128, 8, 8, 16]), ALU.mult,
               [Bmask, Bcraw], [BCm])
            dv(lambda: nc.vector.tensor_scalar(out=craw[:, 1, :, :], in0=craw[:, 1, :, :], scalar1=-1.0, scalar2=None,
                                               op0=ALU.mult), [Bcraw], [Bcraw])
            tt(Cm[:, 1, :, :].rearrange("p m (j c) -> p m j c", c=16), mask4,
               craw[:, 1, :, :].rearrange("p m (o c) -> p m o c", o=1).to_broadcast([128, 8, 8, 16]), ALU.mult,
               [Bmask, Bcraw], [BCm])
            tpb = Rot(nc, es2, "s5_tpb", [128, 1024], BF16, 2, psum=True)
            for ri in range(2):
                for g2 in range(2):
                    p_, Bp = tpb.next()
                    for j in range(4):
                        m = g2 * 4 + j
                        c.op("pe", lambda: nc.tensor.transpose(out=p_[:, j * 128:(j + 1) * 128], in_=Bd[:, ri, m, :],
                                                               identity=ident[:]), [BBd, Bid], [Bp])
                    c.copy(BT[:, ri, g2 * 4:(g2 + 1) * 4, :], p_[:, 0:512].rearrange("p (j t) -> p j t", j=4), [Bp], [BBT])
        c.barrier()
        T.dbg(c, "dbg_s5P", P[:], (128, 40, 8), F32, BP)
        if S5_STAGE <= 1:
            return
        w, Bw = sb(nc, es, "s5_w", [128, 2, 8, L], F32)
        Rb, BRb = sb(nc, es, "s5_Rb", [128, 8, L], F32)
        tA, BtA = sb(nc, es, "s5_tA", [128, 8, L], F32)
        tB, BtB = sb(nc, es, "s5_tB", [128, 8, L], F32)
        tC, BtC = sb(nc, es, "s5_tC", [128, 8, L], F32)
        tD, BtD = sb(nc, es, "s5_tD", [128, 8, L], F32)
        dv(lambda: nc.vector.tensor_copy(out=Rb[:], in_=P[:, MAG, :].rearrange("p (m o) -> p m o", o=1)
                                         .to_broadcast([128, 8, L])), [BP], [BRb])
        dv(lambda: nc.vector.memset(w[:, 0, :, 0:1], 1.0), [], [Bw])
        dv(lambda: nc.vector.memset(w[:, 1, :, 0:1], 0.0), [], [Bw])
        dv(lambda: nc.vector.tensor_copy(out=col(PR), in_=col(CS)), [BP], [BP])
        dv(lambda: nc.vector.tensor_copy(out=col(PI_), in_=col(SN)), [BP], [BP])
        k = 1
        cur = (PR, PI_)
        oth = (PR2, PI2)
        while k < L:
            prb = P[:, cur[0], :].rearrange("p (m o) -> p m o", o=1).to_broadcast([128, 8, k])
            pib = P[:, cur[1], :].rearrange("p (m o) -> p m o", o=1).to_broadcast([128, 8, k])
            wr0 = w[:, 0, :, 0:k]
            wi0 = w[:, 1, :, 0:k]
            tt(tA[:, :, 0:k], wr0, prb, ALU.mult, [Bw, BP], [BtA])
            tt(tB[:, :, 0:k], wi0, pib, ALU.mult, [Bw, BP], [BtB])
            tt(w[:, 0, :, k:2 * k], tA[:, :, 0:k], tB[:, :, 0:k], ALU.subtract, [BtA, BtB], [Bw])
            tt(tA[:, :, 0:k], wr0, pib, ALU.mult, [Bw, BP], [BtA])
            tt(tB[:, :, 0:k], wi0, prb, ALU.mult, [Bw, BP], [BtB])
            tt(w[:, 1, :, k:2 * k], tA[:, :, 0:k], tB[:, :, 0:k], ALU.add, [BtA, BtB], [Bw])
            csq(cur[0], cur[1], oth[0], oth[1])
            cur, oth = oth, cur
            k *= 2
        dv(lambda: nc.vector.tensor_copy(out=col(WLR), in_=col(cur[0])), [BP], [BP])
        dv(lambda: nc.vector.tensor_copy(out=col(WLI), in_=col(cur[1])), [BP], [BP])
        T.dbg(c, "dbg_s5w", w[:], (128, 2, 8, L), F32, Bw)
        gw, Bgw = sb(nc, es, "s5_gwt", [128, 2, 256], BF16)
        c.dma(gw[:], T["s5_glu_w"][l].rearrange("(kt p) n -> p kt n", p=128), reads=[T.B["s5_glu_w"]], writes=[Bgw], q="pool")
        gbc, Bgbc = sb(nc, es, "s5_gbt", [128, 2], F32)
        c.dma(gbc[:], T["s5_glu_b"][l].rearrange("(h p) -> p h", p=128), reads=[T.B["s5_glu_b"]], writes=[Bgbc],
              allow_slow_non_contiguous=True)
        dcl, Bdcl = sb(nc, es, "s5_dcol", [128, 2], F32)
        c.dma(dcl[:], T["s5_d"][l].rearrange("(h p) -> p h", p=128), reads=[T.B["s5_d"]], writes=[Bdcl],
              allow_slow_non_contiguous=True)
        if S5_STAGE <= 2:
            return
        uTr = Rot(nc, es, "s5_uT", [128, 2, L], BF16, 2)
        BUps = Rot(nc, es, "s5_BU", [128, 512], F32, 2, psum=True)
        Yps = Rot(nc, es, "s5_Y", [128, 512], F32, 2, psum=True)
        Tpf = Rot(nc, es, "s5_Tp", [128, 512], F32, 2, psum=True)
        d1 = Rot(nc, es, "s5_d1", [128, L], F32, 3)
        d2 = Rot(nc, es, "s5_d2", [128, L], F32, 3)
        zb = Rot(nc, es, "s5_zb", [128, 2, L], F32, 3)
        Z, BZ = sb(nc, es, "s5_Z", [128, 2, 8, L], F32)
        X, BX = sb(nc, es, "s5_X", [128, 2, 8, L], BF16)
        init = Rot(nc, es, "s5_init", [128, 2, 8], F32, 2)
        zt, Bzt = sb(nc, es, "s5_zt", [128, 2, L], F32)
        u2, Bu2 = sb(nc, es, "s5_u2", [128, 2, L], F32)
        hg, Bhg = sb(nc, es, "s5_hg", [128, 2, L], F32)
        hgb, Bhgb = sb(nc, es, "s5_hgb", [128, 2, L], BF16)
        sgt, Bsgt = sb(nc, es, "s5_sg", [128, 2, L], F32)
        ot, Bot = sb(nc, es, "s5_ot", [128, 2, L], F32)
        otm = Rot(nc, es, "s5_otm", [128, L // 128, 256], F32, 2)
        in_, Bin = init.next()
        dv(lambda: nc.vector.memset(in_[:], 0.0), [], [Bin])
        for ci in range(NCH):
            t0 = ci * L
            u_, Bu = uTr.next()
            c.dma(u_[:], T["projT"][uoff:uoff + 256, t0:t0 + L].rearrange("(h p) t -> p h t", p=128),
                  reads=[T.B["projT"]], writes=[Bu])
            for m in range(8):
                bk, Bbk = BUps.next()
                c.mm(bk[:, 0:L], BT[:, 0, m, :], u_[:, m // 4, :], True, True, [BBT, Bu], [Bbk])
                c.mm(bk[:, L:2 * L], BT[:, 1, m, :], u_[:, m // 4, :], True, True, [BBT, Bu], [Bbk])
                z_, Bz = zb.next()
                a1, Ba1 = d1.next()
                a2, Ba2 = d2.next()
                tt(a1[:], bk[:, 0:L], w[:, 0, m, :], ALU.mult, [Bbk, Bw], [Ba1])
                tt(a2[:], bk[:, L:2 * L], w[:, 1, m, :], ALU.mult, [Bbk, Bw], [Ba2])
                tt(z_[:, 0, :], a1[:], a2[:], ALU.add, [Ba1, Ba2], [Bz])
                a1, Ba1 = d1.next()
                a2, Ba2 = d2.next()
                tt(a1[:], bk[:, L:2 * L], w[:, 0, m, :], ALU.mult, [Bbk, Bw], [Ba1])
                tt(a2[:], bk[:, 0:L], w[:, 1, m, :], ALU.mult, [Bbk, Bw], [Ba2])
                tt(z_[:, 1, :], a1[:], a2[:], ALU.subtract, [Ba1, Ba2], [Bz])
                for ri in range(2):
                    dv(lambda: nc.vector.tensor_tensor_scan(out=Z[:, ri, m, :], data0=Rb[:, m, :], data1=z_[:, ri, :],
                                                            initial=in_[:, ri, m:m + 1], op0=ALU.mult, op1=ALU.add),
                       [BRb, Bz, Bin], [BZ])
            nx, Bnx = init.next()
            zr = Z[:, 0, :, L - 1]
            zi = Z[:, 1, :, L - 1]
            tt(col(X1), zr, col(WLR), ALU.mult, [BZ, BP], [BP])
            tt(col(X2), zi, col(WLI), ALU.mult, [BZ, BP], [BP])
            tt(nx[:, 0, :], col(X1), col(X2), ALU.subtract, [BP], [Bnx])
            tt(col(X1), zr, col(WLI), ALU.mult, [BZ, BP], [BP])
            tt(col(X2), zi, col(WLR), ALU.mult, [BZ, BP], [BP])
            tt(nx[:, 1, :], col(X1), col(X2), ALU.add, [BP], [Bnx])
            in_, Bin = nx, Bnx
            def pt(out, a, b, op, r, w_):
                return c.op("dve", lambda: nc.vector.tensor_tensor(out=out, in0=a, in1=b, op=op), r, w_)
            tt(tA[:], Z[:, 0, :, :], w[:, 0, :, :], ALU.mult, [BZ, Bw], [BtA])
            tt(tB[:], Z[:, 1, :, :], w[:, 1, :, :], ALU.mult, [BZ, Bw], [BtB])
            pt(X[:, 0, :, :], tA[:], tB[:], ALU.subtract, [BtA, BtB], [BX])
            tt(tC[:], Z[:, 0, :, :], w[:, 1, :, :], ALU.mult, [BZ, Bw], [BtC])
            tt(tD[:], Z[:, 1, :, :], w[:, 0, :, :], ALU.mult, [BZ, Bw], [BtD])
            pt(X[:, 1, :, :], tC[:], tD[:], ALU.add, [BtC, BtD], [BX])
            if ci == 0:
                T.dbg(c, "dbg_s5X", X[:], (128, 2, 8, L), BF16, BX)
            for half in range(2):
                yp, Byp = Yps.next()
                for mm_ in range(4):
                    m = half * 4 + mm_
                    c.mm(yp[:, 0:L], Cm[:, 0, m, :], X[:, 0, m, :], mm_ == 0, False, [BCm, BX], [Byp])
                    c.mm(yp[:, 0:L], Cm[:, 1, m, :], X[:, 1, m, :], False, mm_ == 3, [BCm, BX], [Byp])
                dv(lambda: nc.vector.scalar_tensor_tensor(out=zt[:, half, :], in0=u_[:, half, :], scalar=dcl[:, half:half + 1],
                                                          in1=yp[:, 0:L], op0=ALU.mult, op1=ALU.add),
                   [Bu, Bdcl, Byp], [Bzt])
            tt(u2[:], zt[:], zt[:], ALU.mult, [Bzt], [Bu2])
            dv(lambda: nc.vector.tensor_scalar(out=u2[:], in0=u2[:], scalar1=0.044715, scalar2=1.0, op0=ALU.mult,
                                               op1=ALU.add), [Bu2], [Bu2])
            tt(u2[:], u2[:], zt[:], ALU.mult, [Bu2, Bzt], [Bu2])
            c.op("act", lambda: nc.scalar.activation(out=u2[:], in_=u2[:], func=AF.Sigmoid, scale=1.5957691216057308),
                 [Bu2], [Bu2])
            tt(hg[:], u2[:], zt[:], ALU.mult, [Bu2, Bzt], [Bhg])
            c.op("act", lambda: nc.scalar.copy(out=hgb[:], in_=hg[:]), [Bhg], [Bhgb])
            for h2 in range(2):
                gp, Bgp = Yps.next()
                for kt_ in range(2):
                    c.mm(gp[:, 0:L], gw[:, kt_, h2 * 128:(h2 + 1) * 128], hgb[:, kt_, :], kt_ == 0, kt_ == 1, [Bgw, Bhgb], [Bgp])
                c.op("act", lambda: nc.scalar.activation(out=sgt[:, h2, :], in_=gp[:, 0:L], func=AF.Sigmoid,
                                                         bias=gbc[:, h2:h2 + 1]), [Bgp, Bgbc], [Bsgt])
            tt(ot[:], hg[:], sgt[:], ALU.mult, [Bhg, Bsgt], [Bot])
            tp_, Btp = Tpf.next()
            for ts_ in range(L // 128):
                for h2 in range(2):
                    c.op("pe", lambda: nc.tensor.transpose(out=tp_[:, ts_ * 256 + h2 * 128:ts_ * 256 + (h2 + 1) * 128],
                                                           in_=ot[:, h2, ts_ * 128:(ts_ + 1) * 128], identity=identf[:]),
                         [Bot, Bidf], [Btp])
            om, Bom = otm.next()
            c.copy(om[:], tp_[:, 0:(L // 128) * 256].rearrange("p (a e) -> p a e", e=256), [Btp], [Bom])
            c.dma(T["mix"][t0:t0 + L, 768:1024].rearrange("(a p) e -> p a e", p=128), om[:], reads=[Bom],
                  writes=[T.B["mix"]])


def norm_tile(c, x_t, Bx, gB, BgB, h_, Bh, s_, Bs, jk, Bjk, ngroups=1):
    nc = c.nc
    Wd = D // ngroups
    for g in range(ngroups):
        gs = slice(g * Wd, (g + 1) * Wd)
        c.op("act", lambda: nc.scalar.activation(out=jk[:, gs], in_=x_t[:, gs], func=AF.Square,
                                                 accum_out=s_[:, g:g + 1]), [Bx], [Bjk, Bs])
    c.op("act", lambda: nc.scalar.activation(out=s_[:, 4:4 + ngroups], in_=s_[:, 0:ngroups], func=AF.Sqrt,
                                             bias=gB[:, D:D + 1], scale=1.0 / Wd), [Bs, BgB], [Bs])
    c.op("dve", lambda: nc.vector.reciprocal(out=s_[:, 4:4 + ngroups], in_=s_[:, 4:4 + ngroups]), [Bs], [Bs])
    for g in range(ngroups):
        gs = slice(g * Wd, (g + 1) * Wd)
        c.op("dve", lambda: nc.vector.scalar_tensor_tensor(out=h_[:, gs], in0=x_t[:, gs], scalar=s_[:, 4 + g:5 + g],
                                                           in1=gB[:, gs], op0=ALU.mult, op1=ALU.mult),
             [Bx, Bs, BgB], [Bh])


def phase_out(c, T, l):
    nc = c.nc
    x_d = T["xin%d" % l]
    Bx_d = T.B["xin%d" % l]
    with scope(c) as es:
        W, BW = sb(nc, es, "po_W", [128, 8, D], BF16)
        ident, Bid = sb(nc, es, "po_id", [128, 128], BF16)
        c.dma(ident[:], T["ident"][:, :], reads=[T.B["ident"]], writes=[Bid])
        gB, BgB = load_gB(c, es, T["out_norm_g"][l], T.B["out_norm_g"], "po_g")
        wv = T["w_out"][l].rearrange("(kc p) n -> p kc n", p=128)
        for kc in range(8):
            c.dma(W[:, kc, :], wv[:, kc, :], reads=[T.B["w_out"]], writes=[BW], q="pool")
        mt = Rot(nc, es, "po_mt", [128, D], F32, 4)
        xt = Rot(nc, es, "po_xt", [128, D], F32, 5)
        jkr = Rot(nc, es, "po_jk", [128, D], BF16, 2)
        hb = Rot(nc, es, "po_hb", [128, D], BF16, 2)
        ss = Rot(nc, es, "po_ss", [128, 8], F32, 4)
        tp = Rot(nc, es, "po_tp", [128, 1024], BF16, 2, psum=True)
        hTr = Rot(nc, es, "po_hT", [128, 8, 128], BF16, 3)
        acc = Rot(nc, es, "po_acc", [128, 512], F32, 2, psum=True)
        xo = Rot(nc, es, "po_xo", [128, D], F32, 2)
        PF = 2
        loaded = {}
        staged = {}

        def load(t):
            rows = slice(t * 128, (t + 1) * 128)
            m_, Bm = mt.next()
            c.dma(m_[:], T["mix"][rows, :], reads=[T.B["mix"]], writes=[Bm])
            x_, Bx = xt.next()
            c.dma(x_[:], x_d[rows, :], reads=[Bx_d], writes=[Bx])
            loaded[t] = (m_, Bm, x_, Bx)

        def stage_a(t):
            m_, Bm, x_, Bx = loaded.pop(t)
            jk, Bjk = jkr.next()
            s_, Bs = ss.next()
            h_, Bh = hb.next()
            norm_tile(c, m_, Bm, gB, BgB, h_, Bh, s_, Bs, jk, Bjk, ngroups=4)
            hT, BhT = hTr.next()
            for g in range(2):
                p_, Bp = tp.next()
                for j in range(4):
                    kc = g * 4 + j
                    c.op("pe", lambda: nc.tensor.transpose(out=p_[:, j * 128:(j + 1) * 128],
                                                           in_=h_[:, kc * 128:(kc + 1) * 128],
                                                           identity=ident[:]), [Bh, Bid], [Bp])
                c.copy(hT[:, g * 4:(g + 1) * 4, :], p_[:, 0:512].rearrange("p (j t) -> p j t", j=4), [Bp], [BhT])
            staged[t] = (hT, BhT, x_, Bx)

        def stage_c(t):
            rows = slice(t * 128, (t + 1) * 128)
            hT, BhT, x_, Bx = staged.pop(t)
            o_, Bo = xo.next()
            for dc in range(2):
                ds_ = slice(dc * 512, (dc + 1) * 512)
                a_, Ba = acc.next()
                for k in range(8):
                    c.mm(a_[:, :], hT[:, k, :], W[:, k, ds_], k == 0, k == 7, [BW, BhT], [Ba])
                c.op("dve", lambda: nc.vector.tensor_tensor(out=o_[:, ds_], in0=a_[:, :], in1=x_[:, ds_], op=ALU.add),
                     [Ba, Bx], [Bo])
            c.dma(T["xmid"][rows, :], o_[:], reads=[Bo], writes=[T.B["xmid"]])

        for t in range(min(PF, NT)):
            load(t)
        for t in range(NT + 1):
            if t + PF < NT:
                load(t + PF)
            if t < NT:
                stage_a(t)
            if t >= 1:
                stage_c(t - 1)
    c.barrier()


FFN_TB = 1024


def phase_ffn(c, T, l, out_name):
    nc = c.nc
    moe = (l % 2 == 1)
    i = l // 2
    if moe:
        F = D_FFE
        experts = [(T["moe_w_gate"][i, e], T["moe_w_up"][i, e], T["moe_w_down"][i, e]) for e in range(N_EXP)]
        BWs = (T.B["moe_w_gate"], T.B["moe_w_up"], T.B["moe_w_down"])
    else:
        F = D_FF
        experts = [(T["ffn_w_gate"][i], T["ffn_w_up"][i], T["ffn_w_down"][i])]
        BWs = (T.B["ffn_w_gate"], T.B["ffn_w_up"], T.B["ffn_w_down"])
    TB = FFN_TB
    NTB = TB // 128
    FC = 512
    chunks = [(f0, min(FC, F - f0)) for f0 in range(0, F, FC)]
    with scope(c) as es:
        ident, Bid = sb(nc, es, "ff_id", [128, 128], BF16)
        c.dma(ident[:], T["ident"][:, :], reads=[T.B["ident"]], writes=[Bid])
        gB, BgB = load_gB(c, es, T["norm2_g"][l], T.B["norm2_g"], "ff_g")
        if moe:
            Wr, BWr = sb(nc, es, "ff_Wr", [128, 8, N_EXP], BF16)
            c.dma(Wr[:], T["moe_router_w"][i].rearrange("(kc p) e -> p kc e", p=128), reads=[T.B["moe_router_w"]],
                  writes=[BWr], q="pool")
            rb, Brb = sb(nc, es, "ff_rb", [128, N_EXP], F32)
            c.dma(rb[:], T["moe_router_b"][i].partition_broadcast(128), reads=[T.B["moe_router_b"]], writes=[Brb])
            combs = [sb(nc, es, "ff_comb%d" % i_, [128, NTB, N_EXP], F32) for i_ in range(2)]
            lg = Rot(nc, es, "ff_lg", [128, N_EXP], F32, 2)
            mx = Rot(nc, es, "ff_mx", [128, 8], F32, 2)
            ex = Rot(nc, es, "ff_ex", [128, 2 * N_EXP], F32, 2)
        wg = Rot(nc, es, "ff_wg", [128, 8, FC], BF16, 2)
        wu = Rot(nc, es, "ff_wu", [128, 8, FC], BF16, 2)
        wd = Rot(nc, es, "ff_wd", [128, 4, D], BF16, 3)
        Gp = Rot(nc, es, "ff_G", [128, 512], F32, 2, psum=True)
        Up = Rot(nc, es, "ff_U", [128, 512], F32, 2, psum=True)
        Dp = Rot(nc, es, "ff_D", [128, 512], F32, 2, psum=True)
        sg = Rot(nc, es, "ff_sg", [128, 512], F32, 2)
        act = Rot(nc, es, "ff_act", [128, 4, TB], BF16, 3)
        hTs = [sb(nc, es, "ff_hT%d" % i_, [128, 8, TB], BF16) for i_ in range(2)]
        accs = [sb(nc, es, "ff_acc%d" % i_, [128, NTB, D], F32) for i_ in range(2)]
        npools = norm_pools(nc, es, "ffn_", need_xt=False)
        NB = S // TB

        def prep_steps(tb):
            hT, BhT = hTs[tb % 2]
            acc, Bacc = accs[tb % 2]
            steps = norm_transpose_steps(c, npools, T["xmid"], T.B["xmid"], gB, BgB, ident, Bid, hT, BhT, tb * NTB, NTB,
                                         keep_x=(acc, Bacc))
            if moe:
                comb, Bcomb = combs[tb % 2]
                held = {}

                def make_rm(it):
                    def rm():
                        p_, Bp = Dp.next()
                        for k in range(8):
                            c.mm(p_[:, 0:N_EXP], hT[:, k, it * 128:(it + 1) * 128], Wr[:, k, :], k == 0, k == 7,
                                 [BhT, BWr], [Bp])
                        held[it] = (p_, Bp)
                    return rm

                def make_rp(it):
                    def rp():
                        p_, Bp = held.pop(it)
                        l_, Bl = lg.next()
                        c.op("dve", lambda: nc.vector.tensor_tensor(out=l_[:], in0=p_[:, 0:N_EXP], in1=rb[:], op=ALU.add),
                             [Bp, Brb], [Bl])
                        m_, Bm = mx.next()
                        c.op("dve", lambda: nc.vector.max(out=m_[:, 0:8], in_=l_[:]), [Bl], [Bm])
                        e_, Be = ex.next()
                        c.op("dve", lambda: nc.vector.tensor_scalar(out=e_[:, 0:8], in0=l_[:], scalar1=m_[:, 0:1], scalar2=None,
                                                                    op0=ALU.subtract), [Bl, Bm], [Be])
                        c.op("act", lambda: nc.scalar.activation(out=e_[:, 0:8], in_=e_[:, 0:8], func=AF.Exp), [Be], [Be])
                        c.op("dve", lambda: nc.vector.scalar_tensor_tensor(out=e_[:, 8:16], in0=l_[:], scalar=m_[:, 1:2],
                                                                           in1=e_[:, 0:8], op0=ALU.is_ge, op1=ALU.mult),
                             [Bl, Bm, Be], [Be])
                        c.op("dve", lambda: nc.vector.tensor_reduce(out=m_[:, 2:3], in_=e_[:, 8:16], axis=AX.X, op=ALU.add),
                             [Be], [Bm])
                        c.op("dve", lambda: nc.vector.reciprocal(out=m_[:, 3:4], in_=m_[:, 2:3]), [Bm], [Bm])
                        c.op("dve", lambda: nc.vector.tensor_scalar(out=comb[:, it, :], in0=e_[:, 8:16], scalar1=m_[:, 3:4],
                                                                    scalar2=None, op0=ALU.mult), [Be, Bm], [Bcomb])
                    return rp

                steps.append(lambda: None)
                def make_r(it):
                    rm_, rp_ = make_rm(it), make_rp(it)

                    def r():
                        rm_()
                        rp_()
                    return r

                for it in range(NTB):
                    steps.append(make_r(it))
            return steps

        for st_ in prep_steps(0):
            st_()
        pending = []
        for tb in range(NB):
            hT, BhT = hTs[tb % 2]
            acc, Bacc = accs[tb % 2]
            if moe:
                comb, Bcomb = combs[tb % 2]
            n_chunk = 0

            def stage_down(ctx):
                e, a_, Ba, d_, Bd, nft, fw = ctx
                for it in range(NTB):
                    for dc in range(2):
                        ds_ = slice(dc * 512, (dc + 1) * 512)
                        Dd, BD = Dp.next()
                        for ft in range(nft):
                            M = min(128, fw - ft * 128)
                            c.mm(Dd[:, :], a_[0:M, ft, it * 128:(it + 1) * 128], d_[0:M, ft, ds_], ft == 0, ft == nft - 1,
                                 [Ba, Bd], [BD])
                        if moe:
                            c.op("dve", lambda: nc.vector.scalar_tensor_tensor(
                                out=acc[:, it, ds_], in0=Dd[:, :], scalar=comb[:, it, e:e + 1], in1=acc[:, it, ds_],
                                op0=ALU.mult, op1=ALU.add), [BD, Bcomb, Bacc], [Bacc])
                        else:
                            c.op("dve", lambda: nc.vector.tensor_tensor(out=acc[:, it, ds_], in0=Dd[:, :],
                                                                        in1=acc[:, it, ds_], op=ALU.add),
                                 [BD, Bacc], [Bacc])

            prev_ctx = None
            for e, (wg_d, wu_d, wd_d) in enumerate(experts):
                wgv = wg_d.rearrange("(kc p) f -> p kc f", p=128)
                wuv = wu_d.rearrange("(kc p) f -> p kc f", p=128)
                for (f0, fw) in chunks:
                    n_chunk += 1
                    if n_chunk == 2 and tb + 1 < NB:
                        pending = prep_steps(tb + 1)
                    nfull = fw // 128
                    rem = fw - nfull * 128
                    nft = nfull + (1 if rem else 0)
                    g_, Bg = wg.next()
                    c.dma(g_[:, :, 0:fw], wgv[:, :, f0:f0 + fw], reads=[BWs[0]], writes=[Bg], q="pool")
                    u_, Bu = wu.next()
                    c.dma(u_[:, :, 0:fw], wuv[:, :, f0:f0 + fw], reads=[BWs[1]], writes=[Bu], q="pool")
                    d_, Bd = wd.next()
                    if nfull:
                        c.dma(d_[:, 0:nfull, :], wd_d[f0:f0 + nfull * 128, :].rearrange("(ft p) d -> p ft d", p=128),
                              reads=[BWs[2]], writes=[Bd], q="pool")
                    if rem:
                        c.dma(d_[0:rem, nfull, :], wd_d[f0 + nfull * 128:f0 + fw, :], reads=[BWs[2]], writes=[Bd], q="pool")
                    a_, Ba = act.next()
                    n_inner = 0
                    for ft in range(nft):
                        M = min(128, fw - ft * 128)
                        fs = slice(ft * 128, ft * 128 + M)
                        for tt in range(TB // 512):
                            ts_ = slice(tt * 512, (tt + 1) * 512)
                            if pending:
                                pending.pop(0)()
                            G, BG = Gp.next()
                            for k in range(8):
                                c.mm(G[0:M, :], g_[:, k, fs], hT[:, k, ts_], k == 0, k == 7, [Bg, BhT], [BG])
                            U, BU = Up.next()
                            for k in range(8):
                                c.mm(U[0:M, :], u_[:, k, fs], hT[:, k, ts_], k == 0, k == 7, [Bu, BhT], [BU])
                            s_, Bs = sg.next()
                            c.op("act", lambda: nc.scalar.activation(out=s_[0:M, :], in_=G[0:M, :], func=AF.Silu), [BG], [Bs])
                            c.op("dve", lambda: nc.vector.tensor_tensor(out=a_[0:M, ft, ts_], in0=s_[0:M, :], in1=U[0:M, :],
                                                                        op=ALU.mult), [Bs, BU], [Ba])
                            n_inner += 1
                            if n_inner == 1 and prev_ctx is not None:
                                stage_down(prev_ctx)
                                prev_ctx = None
                    prev_ctx = (e, a_, Ba, d_, Bd, nft, fw)
            stage_down(prev_ctx)
            while pending:
                pending.pop(0)()
            c.dma(T[out_name][tb * TB:(tb + 1) * TB, :].rearrange("(i p) d -> p i d", p=128), acc[:], reads=[Bacc],
                  writes=[T.B[out_name]])
            c.barrier()
    c.barrier()


class Tensors:
    def __init__(self, nc, ext_in=(), ext_out=()):
        self.nc = nc
        self.t = {}
        self.B = {}
        self.ext_in = set(ext_in)
        self.ext_out = set(ext_out)
        self.in_names = []
        self.out_names = []

    def add(self, name, shape, dtype, kind=None):
        if kind is None:
            if name in self.ext_in:
                kind = "ExternalInput"
            elif name in self.ext_out:
                kind = "ExternalOutput"
            else:
                kind = "Internal"
        if kind == "ExternalInput":
            self.in_names.append(name)
        if kind == "ExternalOutput":
            self.out_names.append(name)
        self.t[name] = self.nc.dram_tensor(name, list(shape), dtype, kind=kind).ap()
        self.B[name] = Buf(name)
        return self.t[name]

    def __getitem__(self, k):
        return self.t[k]

    def dbg(self, c, name, ap, shape, dtype, Bsrc):
        if name not in self.ext_out:
            return
        d = self.add(name, shape, dtype, kind="ExternalOutput")
        c.dma(d, ap, reads=[Bsrc], writes=[self.B[name]])


PARAM_SHAPES = {
    "norm1_g": (2, 1024), "w_in": (2, 1024, 2460), "dil_qk_g": (2, 2, 64), "nsa_qk_g": (2, 4, 64),
    "nsa_cmp_pos": (2, 2, 32, 64), "nsa_cmp_w1": (2, 2, 2048, 256), "nsa_cmp_w2": (2, 2, 256, 64),
    "gla_wa2": (2, 16, 128), "gla_ba": (2, 128), "gla_norm_g": (2, 64),
    "s5_a_re": (2, 16, 64), "s5_a_im": (2, 16, 64), "s5_b_re": (2, 16, 64, 16), "s5_b_im": (2, 16, 64, 16),
    "s5_c_re": (2, 16, 16, 64), "s5_c_im": (2, 16, 16, 64), "s5_d": (2, 256), "s5_log_dt": (2, 16),
    "s5_glu_w": (2, 256, 256), "s5_glu_b": (2, 256), "out_norm_g": (2, 1024), "w_out": (2, 1024, 1024),
    "norm2_g": (2, 1024), "ffn_w_gate": (1, 1024, 2752), "ffn_w_up": (1, 1024, 2752),
    "ffn_w_down": (1, 2752, 1024), "moe_router_w": (1, 1024, 8), "moe_router_b": (1, 8),
    "moe_w_gate": (1, 8, 1024, 3584), "moe_w_up": (1, 8, 1024, 3584), "moe_w_down": (1, 8, 3584, 1024),
}


def host_consts():
    cst = {}
    bf = ml_dtypes.bfloat16
    cst["ident"] = np.eye(128, dtype=np.float32).astype(bf)
    p = np.arange(S)
    loc = np.arange(128).astype(np.float32)
    KBd = np.zeros((4, 36, 128), np.float32)
    for di in range(36):
        KBd[0, di] = 128.0 * (di - 32)
        KBd[1, di] = loc
        KBd[2, di] = 1.0
        KBd[3, di] = 1.0
    cst["c_KBd"] = KBd.astype(bf)
    QB = np.zeros((4, 3, 4, 128), np.float32)
    for pi, (win, d) in enumerate(DIL_PATTERNS):
        for h in range(4):
            a = DIL_SLOPES[h] * d
            QB[0, pi, h] = a
            QB[1, pi, h] = a
            QB[2, pi, h] = 0.0
            QB[3, pi, h] = -a * loc
    cst["c_QBdil"] = QB.astype(bf)
    kl = np.arange(128)[:, None]
    ql = np.arange(128)[None, :]
    masks = np.stack([(kl <= ql), (kl >= ql), (kl > ql)], axis=1).astype(np.float32)
    cst["c_masks"] = ((masks - 1.0) * 30000.0).astype(bf)
    cst["c_mask01"] = (kl <= ql).astype(np.float32).astype(bf)
    KBS = np.zeros((68, S), np.float32)
    KBS[p // 64, p] = 32768.0
    KBS[64] = 128.0 * (p // 128)
    KBS[65] = p % 128
    KBS[66] = 1.0
    KBS[67] = 1.0
    cst["c_KBS"] = KBS.astype(bf)
    QBa = np.zeros((4, 4, S), np.float32)
    for h in range(4):
        a = NSA_SLOPES[h]
        QBa[0, h] = a
        QBa[1, h] = a
        QBa[2, h] = -a * 128.0 * (p // 128)
        QBa[3, h] = -a * (p % 128)
    cst["c_QBabs"] = QBa.astype(bf)
    KBc = np.zeros((4, 32, 128), np.float32)
    for m in range(32):
        KBc[0, m] = -128.0 * m
        KBc[1, m] = 16.0 * loc
        KBc[2, m] = 31.0
        KBc[3, m] = 1.0
    cst["c_KBc"] = KBc.astype(bf)
    QBc = np.zeros((4, 4, 128), np.float32)
    for h in range(4):
        a = NSA_SLOPES[h]
        QBc[0, h] = a
        QBc[1, h] = a
        QBc[2, h] = a
        QBc[3, h] = -a * loc
    cst["c_QBc"] = QBc.astype(bf)
    cm = np.zeros((128, 17, 128), np.float32)
    for m in range(17):
        valid = (ql - 16 * kl) >= (31 - 128 * m)
        cm[:, m, :] = np.where(valid, 0.0, -30000.0)
    cst["c_cmask"] = cm.astype(bf)
    cc = np.arange(256)
    jj = np.arange(64)
    cover = ((16 * cc[:, None] < 64 * jj[None, :] + 64) & (16 * cc[:, None] + 32 > 64 * jj[None, :])).astype(np.float32)
    cover[255] = 0.0
    cst["c_cover"] = np.ascontiguousarray(cover.reshape(2, 128, 64).transpose(1, 0, 2)).astype(bf)
    tt = (128 * np.arange(32)[None, :, None] + np.arange(128)[:, None, None])
    cur = tt // 64
    j3 = jj[None, None, :]
    forced = (j3 == 0) | (j3 == cur) | (j3 == cur - 1)
    adjv = np.where(j3 <= cur, np.where(forced, 1.0e4, 0.0), -1.0e30).astype(np.float32)
    cst["c_adj"] = np.ascontiguousarray(adjv)
    cst["identf"] = np.eye(128, dtype=np.float32)
    K2s = np.zeros((64, S), np.float32)
    K2s[p // 64, p] = 1.0
    cst["c_K2sel"] = K2s.astype(bf)
    K2w = np.zeros((64, S), np.float32)
    K2w[p // 128, p] = 1.0
    cst["c_K2win"] = K2w.astype(bf)
    QW = np.zeros((64, 4, S), np.float32)
    for h in range(4):
        QW[0:32, h, :] = NSA_SLOPES[h] * 128.0 * (np.arange(32)[:, None] - (p // 128)[None, :])
    cst["c_QW"] = QW.astype(bf)
    As = np.zeros((128, 32, 4), np.float32)
    for h in range(4):
        As[64:128, :, h] = NSA_SLOPES[h] * 64.0 * (np.arange(64)[:, None] - 2 * np.arange(32)[None, :] - 1) + 30000.0
    cst["c_Asel"] = As
    Ev = np.zeros((128, 2, 4), np.float64)
    for h in range(4):
        Ev[:, 0, h] = np.exp(NSA_SLOPES[h] * (np.arange(128) % 64))
        Ev[:, 1, h] = np.exp(NSA_SLOPES[h] * np.arange(128))
    cst["c_Ev"] = Ev.astype(np.float32)
    pp_ = np.arange(128)[:, None, None]
    mm_ = np.arange(8)[None, :, None]
    ch_ = np.arange(128)[None, None, :]
    cst["c_s5mask"] = ((ch_ // 16) == (2 * (mm_ % 4) + pp_ // 64)).astype(np.float32)
    return cst


CONST_SPECS = {"ident": ((128, 128), BF16), "c_KBd": ((4, 36, 128), BF16), "c_QBdil": ((4, 3, 4, 128), BF16),
               "c_masks": ((128, 3, 128), BF16), "c_KBS": ((68, S), BF16), "c_QBabs": ((4, 4, S), BF16),
               "c_KBc": ((4, 32, 128), BF16), "c_QBc": ((4, 4, 128), BF16), "c_cmask": ((128, 17, 128), BF16),
               "c_cover": ((128, 2, 64), BF16), "c_mask01": ((128, 128), BF16), "c_adj": ((128, 32, 64), F32),
               "identf": ((128, 128), F32), "c_s5mask": ((128, 8, 128), F32),
               "c_K2sel": ((64, S), BF16), "c_K2win": ((64, S), BF16), "c_QW": ((64, 4, S), BF16),
               "c_Asel": ((128, 32, 4), F32), "c_Ev": ((128, 2, 4), F32)}


def build(phases=("proj",), layers=(0, 1), ext_in=(), ext_out=()):
    nc = bass.Bass("TRN2", target_bir_lowering=False)
    _UID[0] = 0
    T = Tensors(nc, ext_in, ext_out)
    T.add("xin0", (S, D), F32, kind="ExternalInput")
    for n, shp in PARAM_SHAPES.items():
        T.add(n, shp, F32, kind="ExternalInput")
    for n, (shp, dt_) in CONST_SPECS.items():
        T.add(n, shp, dt_, kind="ExternalInput")
    T.add("projT", (PW, S), BF16)
    T.add("proj", (S, PW), BF16)
    T.add("xin1", (S, D), F32)
    T.add("dacc", (3, S, 264), F32)
    T.add("mix", (S, D), F32)
    T.add("xmid", (S, D), F32)
    T.add("xin2", (S, D), F32, kind="ExternalOutput")
    c = Ctx(nc)
    for l in layers:
        if "proj" in phases:
            phase_proj(c, T, l)
        if "dil" in phases:
            phase_dil(c, T, l)
        if "nsa" in phases:
            phase_nsa(c, T, l)
        if "gla" in phases:
            phase_gla(c, T, l)
        if "s5" in phases:
            phase_s5(c, T, l)
        if "out" in phases:
            phase_out(c, T, l)
        if "ffn" in phases:
            phase_ffn(c, T, l, "xin%d" % (l + 1))
    c.barrier()
    return nc, T


ALL_PHASES = ("proj", "dil", "nsa", "gla", "s5", "out", "ffn")


def kernel(**inputs):
    nc, T = build(phases=ALL_PHASES, layers=(0, 1))
    cst = host_consts()
    x = np.asarray(inputs["x"], dtype=np.float32)
    shared = {}
    for n in T.in_names:
        if n == "xin0":
            continue
        if n in cst:
            shared[n] = cst[n]
        else:
            shared[n] = np.ascontiguousarray(np.asarray(inputs[n], dtype=np.float32))
    maps = []
    for b in range(8):
        m = dict(shared)
        m["xin0"] = np.ascontiguousarray(x[b])
        maps.append(m)
    res = run_bass_kernel_spmd(nc, maps, core_ids=list(range(8)))
    return np.stack([np.asarray(r["xin2"], dtype=np.float32) for r in res.results], axis=0)
```

```python
from contextlib import ExitStack, contextmanager
import math
import numpy as np
import ml_dtypes
import concourse.bass as bass
import concourse.mybir as mybir
from concourse.bass_utils import run_bass_kernel_spmd

F32 = mybir.dt.float32
BF16 = mybir.dt.bfloat16
ALU = mybir.AluOpType
AF = mybir.ActivationFunctionType
AX = mybir.AxisListType

S = 4096
D = 1024
NT = S // 128
PW = 2460
EPS = 1e-6
D_FF = 2752
N_EXP = 8
D_FFE = 3584
OFF = {}
_o = 0
for _n, _w in (("dil_q", 256), ("dil_k", 256), ("dil_v", 256), ("nsa_q", 256),
               ("nsa_k_cmp", 64), ("nsa_v_cmp", 64), ("nsa_k_slc", 64), ("nsa_v_slc", 64),
               ("nsa_k_win", 64), ("nsa_v_win", 64), ("nsa_gate", 12),
               ("gla_q", 128), ("gla_k", 128), ("gla_v", 256), ("gla_a", 16), ("gla_r", 256),
               ("s5_u", 256)):
    OFF[_n] = _o
    _o += _w
assert _o == PW

NDMA = 24
SEM_LIMIT = 24000

FM_RANGES = [(0, 512), (768, 1216), (1280, 1344), (1420, 1676), (1932, 1948), (2204, 2460)]
TM_RANGES = [(512, 256), (1216, 64), (1344, 76), (1676, 256), (1948, 256)]
TM_STORES = [(512, 256), (1216, 64), (1344, 76), (1676, 256), (1948, 256)]
FM_TILES = []
for _a, _b in FM_RANGES:
    _c = _a
    while _c < _b:
        FM_TILES.append((_c, min(128, _b - _c)))
        _c += 128


class Buf:
    __slots__ = ("name", "w", "r")

    def __init__(self, name=""):
        self.name = name
        self.w = {}
        self.r = {}


class Ctx:
    ENG = ("pe", "act", "dve", "pool", "sp")

    def __init__(self, nc):
        self.nc = nc
        self.eng = {"pe": nc.tensor, "act": nc.scalar, "dve": nc.vector,
                    "pool": nc.gpsimd, "sp": nc.sync}
        self.gen = {e: 0 for e in self.ENG}
        self.sem = {e: nc.alloc_semaphore("s_%s_0" % e) for e in self.ENG}
        self.tick = {e: 0 for e in self.ENG}
        self.seen = {e: {} for e in self.ENG}
        self.dsem = [nc.alloc_semaphore("s_dma_%d" % i) for i in range(NDMA)]
        self.dval = [0] * NDMA
        self.dn = 0
        self.dgen = 0
        self.strict = {"act", "dve", "pool"}
        self.n_ops = 0
        self.rr = 0
        self.last_pool = None

    def _need(self, e, waits, key, val, same_ok):
        if key[0] == "e" and key[1] == e and not same_ok:
            return
        if self.seen[e].get(key, 0) >= val:
            return
        if waits.get(key, 0) < val:
            waits[key] = val

    def _ekey(self, e):
        return ("e", e, self.gen[e])

    def _emit_waits(self, e, waits):
        eng = self.eng[e]
        for key, val in waits.items():
            if key[0] == "d":
                if key[2] != self.dgen:
                    continue
                s = self.dsem[key[1]]
            else:
                if key[2] != self.gen[key[1]]:
                    continue
                s = self.sem[key[1]]
            eng.wait_ge(s, val)
            self.seen[e][key] = val

    def op(self, e, fn, reads=(), writes=()):
        waits = {}
        st = e in self.strict
        for b in reads:
            for key, val in b.w.items():
                self._need(e, waits, key, val, st)
        for b in writes:
            for key, val in b.w.items():
                self._need(e, waits, key, val, st)
            for key, val in b.r.items():
                self._need(e, waits, key, val, False)
        self._emit_waits(e, waits)
        ins = fn()
        self.tick[e] += 1
        ins.then_inc(self.sem[e], 1)
        key = self._ekey(e)
        val = self.tick[e]
        for b in reads:
            b.r[key] = val
        for b in writes:
            b.w[key] = val
            b.r = {}
        self.n_ops += 1
        return ins

    def dma(self, out, in_, reads=(), writes=(), q="sp", **kw):
        e = q
        waits = {}
        for b in reads:
            for key, val in b.w.items():
                self._need(e, waits, key, val, True)
        for b in writes:
            for key, val in b.w.items():
                if key[0] == "d":
                    continue
                self._need(e, waits, key, val, True)
            for key, val in b.r.items():
                self._need(e, waits, key, val, True)
        i = self.dn % NDMA
        self.dn += 1
        dkey = ("d", i, self.dgen)
        if self.dval[i] > 0:
            self._need(e, waits, dkey, self.dval[i], True)
        if e == "pool" and self.last_pool is not None:
            self._need(e, waits, self.last_pool[0], self.last_pool[1], True)
        self._emit_waits(e, waits)
        ins = self.eng[e].dma_start(out=out, in_=in_, **kw)
        self.dval[i] += 16
        ins.then_inc(self.dsem[i], 16)
        if e == "pool":
            self.last_pool = (dkey, self.dval[i])
        for b in reads:
            b.r[dkey] = self.dval[i]
        for b in writes:
            b.w[dkey] = self.dval[i]
        return ins

    def barrier(self):
        for e in self.ENG:
            waits = {}
            for o in self.ENG:
                if o != e and self.tick[o] > 0:
                    self._need(e, waits, self._ekey(o), self.tick[o], True)
            for i in range(NDMA):
                if self.dval[i] > 0:
                    self._need(e, waits, ("d", i, self.dgen), self.dval[i], True)
            self._emit_waits(e, waits)
        for e in self.ENG:
            if self.tick[e] > SEM_LIMIT:
                self.gen[e] += 1
                self.sem[e] = self.nc.alloc_semaphore("s_%s_%d" % (e, self.gen[e]))
                self.tick[e] = 0
        if any(v > SEM_LIMIT for v in self.dval):
            self.dgen += 1
            self.dsem = [self.nc.alloc_semaphore("s_dma_%d_g%d" % (i, self.dgen)) for i in range(NDMA)]
            self.dval = [0] * NDMA

    def evac_engine(self):
        self.rr += 1
        return "act" if (self.rr & 1) else "dve"

    def copy(self, out, in_, reads, writes, e=None):
        nc = self.nc
        e = e or self.evac_engine()
        if e == "act":
            return self.op("act", lambda: nc.scalar.copy(out=out, in_=in_), reads, writes)
        if e == "pool":
            return self.op("pool", lambda: nc.gpsimd.tensor_copy(out=out, in_=in_), reads, writes)
        return self.op("dve", lambda: nc.vector.tensor_copy(out=out, in_=in_), reads, writes)

    def mm(self, out, lhsT, rhs, start, stop, reads, writes, **kw):
        nc = self.nc
        return self.op("pe", lambda: nc.tensor.matmul(out, lhsT=lhsT, rhs=rhs, start=start, stop=stop, **kw),
                       reads, writes)


@contextmanager
def scope(c):
    with ExitStack() as es:
        yield es
        c.barrier()


_UID = [0]


def uniq(name):
    _UID[0] += 1
    return "%s_u%d" % (name, _UID[0])


class Rot:
    def __init__(self, nc, es, name, shape, dtype, n, psum=False):
        self.t = []
        self.b = []
        name = uniq(name)
        for i in range(n):
            nm = "%s_%d" % (name, i)
            if psum:
                t = es.enter_context(nc.psum_tensor(nm, shape, dtype))
            else:
                t = es.enter_context(nc.sbuf_tensor(nm, shape, dtype))
            self.t.append(t)
            self.b.append(Buf(nm))
        self.i = 0

    def next(self):
        k = self.i % len(self.t)
        self.i += 1
        return self.t[k], self.b[k]


def sb(nc, es, name, shape, dtype):
    name = uniq(name)
    return es.enter_context(nc.sbuf_tensor(name, shape, dtype)), Buf(name)


def ps(nc, es, name, shape, dtype):
    name = uniq(name)
    return es.enter_context(nc.psum_tensor(name, shape, dtype)), Buf(name)


def norm_pools(nc, es, pfx, need_xt=True):
    xt = Rot(nc, es, pfx + "xt", [128, D], F32, 2) if need_xt else None
    junk = Rot(nc, es, pfx + "jk", [128, D], BF16, 2)
    hb = Rot(nc, es, pfx + "hb", [128, D], BF16, 2)
    ss = Rot(nc, es, pfx + "ss", [128, 2], F32, 4)
    tp = Rot(nc, es, pfx + "tp", [128, 1024], BF16, 2, psum=True)
    return xt, junk, hb, ss, tp


def norm_transpose_steps(c, pools, x_d, Bx_d, gB, BgB, ident, Bid, hT, BhT, tile0, ntiles, keep_x=None):
    nc = c.nc
    xt, junk, hb, ss, tp = pools
    held = {}

    def make_n(i):
        def n_step():
            t = tile0 + i
            if keep_x is not None:
                x_t = keep_x[0][:, i, :]
                Bx = keep_x[1]
            else:
                xx, Bx = xt.next()
                x_t = xx[:]
            c.dma(x_t, x_d[t * 128:(t + 1) * 128, :], reads=[Bx_d], writes=[Bx])
            jk, Bjk = junk.next()
            s_, Bs = ss.next()
            c.op("act", lambda: nc.scalar.activation(out=jk[:], in_=x_t, func=AF.Square,
                                                     accum_out=s_[:, 0:1]), [Bx], [Bjk, Bs])
            c.op("act", lambda: nc.scalar.activation(out=s_[:, 1:2], in_=s_[:, 0:1], func=AF.Sqrt,
                                                     bias=gB[:, D:D + 1], scale=1.0 / D), [Bs, BgB], [Bs])
            c.op("dve", lambda: nc.vector.reciprocal(out=s_[:, 1:2], in_=s_[:, 1:2]), [Bs], [Bs])
            h_, Bh = hb.next()
            c.op("dve", lambda: nc.vector.scalar_tensor_tensor(out=h_[:], in0=x_t, scalar=s_[:, 1:2],
                                                               in1=gB[:, 0:D], op0=ALU.mult, op1=ALU.mult),
                 [Bx, Bs, BgB], [Bh])
            held[i] = (h_, Bh)
        return n_step

    def make_t(i):
        def t_step():
            h_, Bh = held.pop(i)
            for g in range(2):
                p_, Bp = tp.next()
                for j in range(4):
                    kc = g * 4 + j
                    c.op("pe", lambda: nc.tensor.transpose(out=p_[:, j * 128:(j + 1) * 128],
                                                           in_=h_[:, kc * 128:(kc + 1) * 128],
                                                           identity=ident[:]), [Bh, Bid], [Bp])
                c.copy(hT[:, g * 4:(g + 1) * 4, i * 128:(i + 1) * 128],
                       p_[:, 0:512].rearrange("p (j t) -> p j t", j=4), [Bp], [BhT])
        return t_step

    steps = []
    for i in range(ntiles):
        steps.append(make_n(i))
        if i >= 1:
            steps.append(make_t(i - 1))
    steps.append(make_t(ntiles - 1))
    return steps


def norm_transpose(c, es, x_d, Bx_d, gB, BgB, ident, Bid, hT, BhT, tile0, ntiles, pfx,
                   keep_x=None, pools=None):
    nc = c.nc
    if pools is None:
        pools = norm_pools(nc, es, pfx, keep_x is None)
    for st_ in norm_transpose_steps(c, pools, x_d, Bx_d, gB, BgB, ident, Bid, hT, BhT, tile0, ntiles, keep_x):
        st_()


def load_gB(c, es, g_row, Bg_d, name):
    nc = c.nc
    gB, BgB = sb(nc, es, name, [128, D + 1], F32)
    c.dma(gB[:, 0:D], g_row.partition_broadcast(128), reads=[Bg_d], writes=[BgB])
    c.op("dve", lambda: nc.vector.memset(gB[:, D:D + 1], EPS), [], [BgB])
    return gB, BgB


def phase_proj(c, T, l):
    nc = c.nc
    with scope(c) as es:
        W, BW = sb(nc, es, "pj_W", [128, 8, PW], BF16)
        ident, Bid = sb(nc, es, "pj_id", [128, 128], BF16)
        c.dma(ident[:], T["ident"][:, :], reads=[T.B["ident"]], writes=[Bid])
        gB, BgB = load_gB(c, es, T["norm1_g"][l], T.B["norm1_g"], "pj_g")
        wv = T["w_in"][l].rearrange("(kc p) n -> p kc n", p=128)
        for kc in range(8):
            c.dma(W[:, kc, :], wv[:, kc, :], reads=[T.B["w_in"]], writes=[BW], q="pool")
        hTs = [sb(nc, es, "pj_hT%d" % i, [128, 8, 2048], BF16) for i in range(2)]
        stf = Rot(nc, es, "pj_stf", [128, 2048], BF16, 2)
        stt = Rot(nc, es, "pj_stt", [128, PW], BF16, 2)
        acc = Rot(nc, es, "pj_acc", [128, 512], F32, 4, psum=True)
        x_d = T["xin%d" % l]
        npools = norm_pools(nc, es, "pj_", need_xt=True)

        def half_steps(half):
            return norm_transpose_steps(c, npools, x_d, T.B["xin%d" % l], gB, BgB, ident, Bid, hTs[half][0], hTs[half][1],
                                        half * 16, 16)

        for st_ in half_steps(0):
            st_()
        pending = []
        for half in range(2):
            hT, BhT = hTs[half]
            while pending:
                pending.pop(0)()
            if half == 0:
                pending = half_steps(1)
            for (c0, M) in FM_TILES:
                st, Bst = stf.next()
                for tt in range(4):
                    if pending:
                        pending.pop(0)()
                    a_, Ba = acc.next()
                    for k in range(8):
                        c.mm(a_[0:M, :], W[:, k, c0:c0 + M], hT[:, k, tt * 512:(tt + 1) * 512],
                             k == 0, k == 7, [BW, BhT], [Ba])
                    c.copy(st[0:M, tt * 512:(tt + 1) * 512], a_[0:M, :], [Ba], [Bst])
                c.dma(T["projT"][c0:c0 + M, half * 2048:(half + 1) * 2048], st[0:M, :],
                      reads=[Bst], writes=[T.B["projT"]])
            for i in range(16):
                st, Bst = stt.next()
                t = half * 16 + i
                for (c0, N) in TM_RANGES:
                    a_, Ba = acc.next()
                    for k in range(8):
                        c.mm(a_[:, 0:N], hT[:, k, i * 128:(i + 1) * 128], W[:, k, c0:c0 + N],
                             k == 0, k == 7, [BW, BhT], [Ba])
                    c.copy(st[:, c0:c0 + N], a_[:, 0:N], [Ba], [Bst])
                for (c0, N) in TM_STORES:
                    c.dma(T["proj"][t * 128:(t + 1) * 128, c0:c0 + N], st[:, c0:c0 + N], reads=[Bst],
                          writes=[T.B["proj"]])
    c.barrier()


DIL_SLOPES = [2.0 ** -2, 2.0 ** -4, 2.0 ** -6, 2.0 ** -8]
NSA_SLOPES = [2.0 ** -1, 2.0 ** -3, 2.0 ** -5, 2.0 ** -7]
DIL_PATTERNS = ((128, 1), (512, 4), (2048, 16))


def ssl(start, count, step):
    return slice(start, start + step * (count - 1) + 1, step)


def load_col(c, es, name, vec_ap, Bsrc, n, mul=None):
    nc = c.nc
    t, B = sb(nc, es, name, [n, 1], F32)
    c.dma(t[:, 0:1], vec_ap.rearrange("(p o) -> p o", o=1), reads=[Bsrc], writes=[B])
    if mul is not None:
        c.op("act", lambda: nc.scalar.mul(out=t[:], in_=t[:], mul=mul), [B], [B])
    return t, B


def prep_qk(c, es, T, row0, H, gcol, Bg, ones64, Bones, epsc, Beps, dst, Bdst, pfx):
    nc = c.nc
    raw = Rot(nc, es, pfx + "raw", [64, S], BF16, 2)
    sq = Rot(nc, es, pfx + "sq", [64, 512], BF16, 2)
    rs = Rot(nc, es, pfx + "rs", [64, 512], F32, 2)
    pp = Rot(nc, es, pfx + "pp", [64, 512], F32, 2, psum=True)
    for h in range(H):
        r_, Br = raw.next()
        c.dma(r_[:], T["projT"][row0 + h * 64:row0 + (h + 1) * 64, :], reads=[T.B["projT"]], writes=[Br])
        for tt in range(8):
            sl = slice(tt * 512, (tt + 1) * 512)
            q_, Bq = sq.next()
            c.op("act", lambda: nc.scalar.activation(out=q_[:], in_=r_[:, sl], func=AF.Square), [Br], [Bq])
            p_, Bp = pp.next()
            c.mm(p_[:], ones64[:], q_[:], True, True, [Bq, Bones], [Bp])
            s_, Bs = rs.next()
            c.op("act", lambda: nc.scalar.activation(out=s_[:], in_=p_[:], func=AF.Ln, bias=epsc[0:64, 0:1],
                                                     scale=1.0 / 64), [Bp, Beps], [Bs])
            c.op("act", lambda: nc.scalar.activation(out=s_[:], in_=s_[:], func=AF.Exp, scale=-0.5), [Bs], [Bs])
            c.op("dve", lambda: nc.vector.scalar_tensor_tensor(out=dst[0:64, h, sl], in0=r_[:, sl],
                                                               scalar=gcol[:, 0:1], in1=s_[:],
                                                               op0=ALU.mult, op1=ALU.mult),
                 [Br, Bs, Bg], [Bdst])


def load_consts_small(c, es, T, pfx):
    nc = c.nc
    ones64, Bones = sb(nc, es, pfx + "ones64", [64, 64], BF16)
    c.op("dve", lambda: nc.vector.memset(ones64[:], 1.0), [], [Bones])
    epsc, Beps = sb(nc, es, pfx + "epsc", [128, 1], F32)
    c.op("dve", lambda: nc.vector.memset(epsc[:], EPS), [], [Beps])
    return ones64, Bones, epsc, Beps


def phase_dil(c, T, l):
    nc = c.nc
    with scope(c) as es:
        ones64, Bones, epsc, Beps = load_consts_small(c, es, T, "dl_")
        qT, BqT = sb(nc, es, "dl_qT", [64, 4, S], BF16)
        kT, BkT = sb(nc, es, "dl_kT", [64, 4, S], BF16)
        KB, BKB = sb(nc, es, "dl_KB", [4, 36, 128], BF16)
        c.dma(KB[:], T["c_KBd"][:, :, :], reads=[T.B["c_KBd"]], writes=[BKB])
        QB, BQB = sb(nc, es, "dl_QB", [4, 3, 4, 128], BF16)
        c.dma(QB[:], T["c_QBdil"][:, :, :, :], reads=[T.B["c_QBdil"]], writes=[BQB])
        msk, Bmsk = sb(nc, es, "dl_msk", [128, 3, 128], BF16)
        c.dma(msk[:], T["c_masks"][:, :, :], reads=[T.B["c_masks"]], writes=[Bmsk])
        with scope(c) as es2:
            gq, Bgq = load_col(c, es2, "dl_gq", T["dil_qk_g"][l, 0], T.B["dil_qk_g"], 64, mul=0.125)
            gk, Bgk = load_col(c, es2, "dl_gk", T["dil_qk_g"][l, 1], T.B["dil_qk_g"], 64)
            with scope(c) as es3:
                prep_qk(c, es3, T, OFF["dil_q"], 4, gq, Bgq, ones64, Bones, epsc, Beps, qT, BqT, "dlq")
            c.barrier()
            with scope(c) as es3:
                prep_qk(c, es3, T, OFF["dil_k"], 4, gk, Bgk, ones64, Bones, epsc, Beps, kT, BkT, "dlk")
        T.dbg(c, "dbg_qT", qT[:], (64, 4, S), BF16, BqT)
        T.dbg(c, "dbg_kT", kT[:], (64, 4, S), BF16, BkT)
        Vp = []
        for pi in range(3):
            v_, Bv = sb(nc, es, "dl_V%d" % pi, [128, 32, 4, 66], BF16)
            c.op("dve", lambda: nc.vector.memset(v_[:], 1.0), [], [Bv])
            Vp.append((v_, Bv))
        with scope(c) as es2:
            stg = Rot(nc, es2, "dl_vst", [128, 8, 256], BF16, 2)
            for pi, (win, d) in enumerate(DIL_PATTERNS):
                v_, Bv = Vp[pi]
                bpc = 32 // d
                for r in range(d):
                    for b0 in range(0, bpc, 8):
                        nb = min(8, bpc - b0)
                        s_, Bs = stg.next()
                        src = T["proj"][ssl(r + d * 128 * b0, 128 * nb, d), OFF["dil_v"]:OFF["dil_v"] + 256]
                        c.dma(s_[:, 0:nb, :], src.rearrange("(b k) c -> k b c", k=128),
                              reads=[T.B["proj"]], writes=[Bs])
                        blk0 = r * bpc + b0
                        c.copy(v_[:, blk0:blk0 + nb, :, 0:64],
                               s_[:, 0:nb, :].rearrange("k b (h e) -> k b h e", h=4), [Bs], [Bv])
        c.barrier()
        for pi in range(3):
            T.dbg(c, "dbg_V%d" % pi, Vp[pi][0][:], (128, 32, 4, 66), BF16, Vp[pi][1])
        Sps = Rot(nc, es, "dl_S", [128, 512], F32, 3, psum=True)
        Ops = Rot(nc, es, "dl_O", [128, 512], F32, 2, psum=True)
        Pt = Rot(nc, es, "dl_P", [128, 512], BF16, 5)
        Mt = Rot(nc, es, "dl_M", [128, 512], F32, 3)
        Osb = Rot(nc, es, "dl_Osb", [128, 264], F32, 3)
        SKEW = 2
        tasks = []
        for pi, (win, d) in enumerate(DIL_PATTERNS):
            bpc = 32 // d
            for r in range(d):
                for bi in range(bpc):
                    kbs = [bi - 1, bi] if bi >= 1 else [bi]
                    for i_, kbi in enumerate(kbs):
                        tasks.append((pi, d, bpc, r, bi, kbi, i_ == 0, i_ == len(kbs) - 1))
        st = {}
        cur_o = [None]

        def stage_a(ti):
            pi, d, bpc, r, bi, kbi, is_first, is_last = tasks[ti]
            qsl = ssl(r + d * 128 * bi, 128, d)
            ksl = ssl(r + d * 128 * kbi, 128, d)
            s_, Bs = Sps.next()
            for h in range(4):
                c.mm(s_[:, h * 128:(h + 1) * 128], kT[:, h, ksl], qT[:, h, qsl], True, False,
                     [BkT, BqT], [Bs])
                c.mm(s_[:, h * 128:(h + 1) * 128], KB[:, 32 + kbi - bi, :],
                     QB[:, pi, h, :], False, True, [BKB, BQB], [Bs])
            p_, Bp = Pt.next()
            mi = 0 if kbi == bi else 1
            m_, Bm = Mt.next()
            c.op("dve", lambda: nc.vector.tensor_tensor(
                out=m_[:].rearrange("k (h q) -> k h q", h=4),
                in0=s_[:].rearrange("k (h q) -> k h q", h=4),
                in1=msk[:, mi:mi + 1, :].to_broadcast([128, 4, 128]), op=ALU.add), [Bs, Bmsk], [Bm])
            c.op("act", lambda: nc.scalar.activation(out=p_[:], in_=m_[:], func=AF.Exp), [Bm], [Bp])
            st[ti] = (p_, Bp)

        def stage_c(ti):
            pi, d, bpc, r, bi, kbi, is_first, is_last = tasks[ti]
            v_, Bv = Vp[pi]
            kblk = r * bpc + kbi
            p_, Bp = st.pop(ti)
            if is_first:
                cur_o[0] = Ops.next()
            o_, Bo = cur_o[0]
            for h in range(4):
                c.mm(o_[:, h * 66:(h + 1) * 66], p_[:, h * 128:(h + 1) * 128], v_[:, kblk, h, :],
                     is_first and h == 0, False, [Bp, Bv], [Bo], skip_group_check=True)
            if is_last:
                ob, Bob = Osb.next()
                c.copy(ob[:], o_[:, 0:264], [Bo], [Bob])
                dst = T["dacc"][pi, ssl(r + d * 128 * bi, 128, d), :]
                c.dma(dst, ob[:], reads=[Bob], writes=[T.B["dacc"]])

        for ti in range(len(tasks) + SKEW):
            if ti < len(tasks):
                stage_a(ti)
            if ti - SKEW >= 0:
                stage_c(ti - SKEW)
    c.barrier()
    with scope(c) as es:
        a3 = Rot(nc, es, "dc_a", [128, 3, 264], F32, 5)
        rc = Rot(nc, es, "dc_r", [128, 4], F32, 3)
        yo = Rot(nc, es, "dc_y", [128, 256], F32, 3)
        PF = 3
        loaded = {}

        def load(t):
            a_, Ba = a3.next()
            c.dma(a_[:], T["dacc"][:, t * 128:(t + 1) * 128, :].rearrange("p t c -> t p c"),
                  reads=[T.B["dacc"]], writes=[Ba])
            loaded[t] = (a_, Ba)

        for t in range(min(PF, NT)):
            load(t)
        for t in range(NT):
            if t + PF < NT:
                load(t + PF)
            a_, Ba = loaded.pop(t)
            c.op("dve", lambda: nc.vector.tensor_tensor(out=a_[:, 0, :], in0=a_[:, 0, :], in1=a_[:, 1, :], op=ALU.add),
                 [Ba], [Ba])
            c.op("dve", lambda: nc.vector.tensor_tensor(out=a_[:, 0, :], in0=a_[:, 0, :], in1=a_[:, 2, :], op=ALU.add),
                 [Ba], [Ba])
            av = a_[:, 0, :].rearrange("t (h e) -> t h e", h=4)
            r_, Br = rc.next()
            c.op("dve", lambda: nc.vector.reciprocal(out=r_[:].rearrange("t (h o) -> t h o", o=1), in_=av[:, :, 64:65]),
                 [Ba], [Br])
            y_, By = yo.next()
            c.op("dve", lambda: nc.vector.tensor_tensor(
                out=y_[:].rearrange("t (h e) -> t h e", h=4), in0=av[:, :, 0:64],
                in1=r_[:].rearrange("t (h o) -> t h o", o=1).to_broadcast([128, 4, 64]), op=ALU.mult),
                [Ba, Br], [By])
            c.dma(T["mix"][t * 128:(t + 1) * 128, 0:256], y_[:], reads=[By], writes=[T.B["mix"]])
    c.barrier()


NSA_STAGE = 99


def phase_nsa(c, T, l):
    _phase_nsa(c, T, l)
    c.barrier()


def _phase_nsa(c, T, l):
    nc = c.nc
    with scope(c) as es:
        ones64, Bones, epsc, Beps = load_consts_small(c, es, T, "ns_")
        ident, Bid = sb(nc, es, "ns_id", [128, 128], BF16)
        c.dma(ident[:], T["ident"][:, :], reads=[T.B["ident"]], writes=[Bid])
        qT, BqT = sb(nc, es, "ns_Q2s", [128, 4, S], BF16)
        Q2w, BQ2w = sb(nc, es, "ns_Q2w", [128, 4, S], BF16)
        c.dma(Q2w[64:128, :, :], T["c_QW"][:, :, :], reads=[T.B["c_QW"]], writes=[BQ2w])
        ksT, BksT = sb(nc, es, "ns_Ks2", [128, 1, S], BF16)
        c.dma(ksT[64:128, 0, :], T["c_K2sel"][:, :], reads=[T.B["c_K2sel"]], writes=[BksT])
        kwT, BkwT = sb(nc, es, "ns_Kw2", [128, 1, S], BF16)
        c.dma(kwT[64:128, 0, :], T["c_K2win"][:, :], reads=[T.B["c_K2win"]], writes=[BkwT])
        Asel, BAsel = sb(nc, es, "ns_Asel", [128, 32, 4], F32)
        c.dma(Asel[:], T["c_Asel"][:, :, :], reads=[T.B["c_Asel"]], writes=[BAsel])
        Vs, BVs = sb(nc, es, "ns_Vsh", [128, 32, 4, 66], BF16)
        Vw, BVw = sb(nc, es, "ns_Vwh", [128, 32, 4, 66], BF16)
        with scope(c) as es2:
            Ev, BEv = sb(nc, es2, "ns_Ev", [128, 2, 4], F32)
            c.dma(Ev[:], T["c_Ev"][:, :, :], reads=[T.B["c_Ev"]], writes=[BEv])
            for vi, (vh_, Bvh, nm) in enumerate(((Vs, BVs, "nsa_v_slc"), (Vw, BVw, "nsa_v_win"))):
                v_, Bv = sb(nc, es2, "ns_Vst%d" % vi, [128, 32, 66], BF16)
                c.op("dve", lambda: nc.vector.memset(v_[:], 1.0), [], [Bv])
                c.dma(v_[:, :, 0:64], T["proj"][:, OFF[nm]:OFF[nm] + 64].rearrange("(b k) e -> k b e", k=128),
                      reads=[T.B["proj"]], writes=[Bv])
                for h in range(4):
                    c.op("dve", lambda: nc.vector.tensor_scalar(out=vh_[:, :, h, :], in0=v_[:], scalar1=Ev[:, vi, h:h + 1],
                                                                scalar2=None, op0=ALU.mult), [Bv, BEv], [Bvh])
        kcT, BkcT = sb(nc, es, "ns_kcT", [64, 256], BF16)
        Vc, BVc = sb(nc, es, "ns_Vc", [128, 2, 130], BF16)
        c.op("dve", lambda: nc.vector.memset(Vc[:], 0.0), [], [BVc])
        c.op("dve", lambda: nc.vector.memset(Vc[:, :, 128:130], 1.0), [], [BVc])
        c.dma(Vc[:, :, 64:128], T["c_cover"][:, :, :], reads=[T.B["c_cover"]], writes=[BVc])
        with scope(c) as es2:
            g0, Bg0 = load_col(c, es2, "ns_g0", T["nsa_qk_g"][l, 0], T.B["nsa_qk_g"], 64, mul=0.125)
            g2, Bg2 = load_col(c, es2, "ns_g2", T["nsa_qk_g"][l, 2], T.B["nsa_qk_g"], 64)
            g3, Bg3 = load_col(c, es2, "ns_g3", T["nsa_qk_g"][l, 3], T.B["nsa_qk_g"], 64)
            with scope(c) as es3:
                prep_qk(c, es3, T, OFF["nsa_q"], 4, g0, Bg0, ones64, Bones, epsc, Beps, qT, BqT, "nsq")
            c.barrier()
            for h in range(4):
                c.op("act", lambda: nc.scalar.copy(out=Q2w[0:64, h, :], in_=qT[0:64, h, :]), [BqT], [BQ2w])
            with scope(c) as es3:
                prep_qk(c, es3, T, OFF["nsa_k_slc"], 1, g2, Bg2, ones64, Bones, epsc, Beps, ksT, BksT, "nss")
            with scope(c) as es3:
                prep_qk(c, es3, T, OFF["nsa_k_win"], 1, g3, Bg3, ones64, Bones, epsc, Beps, kwT, BkwT, "nsw")
            c.barrier()
        c.barrier()
        if NSA_STAGE <= 1:
            return
        with scope(c) as es2:
            g1, Bg1 = load_col(c, es2, "ns_g1", T["nsa_qk_g"][l, 1], T.B["nsa_qk_g"], 64)
            raw, Braw = sb(nc, es2, "ns_raw", [64, 2, S], BF16)
            c.dma(raw[:], T["projT"][OFF["nsa_k_cmp"]:OFF["nsa_k_cmp"] + 128, :].rearrange("(i d) t -> d i t", i=2),
                  reads=[T.B["projT"]], writes=[Braw])
            w1, Bw1 = sb(nc, es2, "ns_w1", [64, 2, 32, 256], BF16)
            w2, Bw2 = sb(nc, es2, "ns_w2", [128, 2, 2, 64], BF16)
            posT, BposT = sb(nc, es2, "ns_posT", [64, 2, 32], BF16)
            for i in range(2):
                c.dma(w1[:, i, :, :], T["nsa_cmp_w1"][l, i].rearrange("(l d) h -> d l h", d=64),
                      reads=[T.B["nsa_cmp_w1"]], writes=[Bw1], q="pool")
                c.dma(w2[:, i, :, :], T["nsa_cmp_w2"][l, i].rearrange("(hc p) e -> p hc e", p=128),
                      reads=[T.B["nsa_cmp_w2"]], writes=[Bw2], q="pool")
                c.dma(posT[:, i, :], T["nsa_cmp_pos"][l, i].rearrange("l d -> d l"),
                      reads=[T.B["nsa_cmp_pos"]], writes=[BposT], q="pool", allow_slow_non_contiguous=True)
            Hps = Rot(nc, es2, "ns_Hps", [128, 512], F32, 2, psum=True)
            bps = Rot(nc, es2, "ns_bps", [128, 512], F32, 2, psum=True)
            b1 = Rot(nc, es2, "ns_b1", [128, 2], F32, 2)
            zt = Rot(nc, es2, "ns_z", [128, 256], F32, 2)
            ut = Rot(nc, es2, "ns_u", [128, 256], F32, 2)
            G, BG = sb(nc, es2, "ns_G", [128, 2, 2, 256], BF16)
            c.op("dve", lambda: nc.vector.memset(G[:], 0.0), [], [BG])
            for i in range(2):
                for hc in range(2):
                    h_, Bh = Hps.next()
                    for ll in range(32):
                        c.mm(h_[:, 0:255], w1[:, i, ll, hc * 128:(hc + 1) * 128], raw[:, i, ssl(ll, 255, 16)],
                             ll == 0, ll == 31, [Bw1, Braw], [Bh])
                    bp, Bbp = bps.next()
                    for ll in range(32):
                        c.mm(bp[:, 0:2], w1[:, i, ll, hc * 128:(hc + 1) * 128], posT[:, i, ll:ll + 1].to_broadcast([64, 2]),
                             ll == 0, ll == 31, [Bw1, BposT], [Bbp])
                    b_, Bb = b1.next()
                    c.op("dve", lambda: nc.vector.tensor_copy(out=b_[:], in_=bp[:, 0:2]), [Bbp], [Bb])
                    z_, Bz = zt.next()
                    c.op("act", lambda: nc.scalar.activation(out=z_[:, 0:255], in_=h_[:, 0:255], func=AF.Identity,
                                                             bias=b_[:, 0:1]), [Bh, Bb], [Bz])
                    u_, Bu = ut.next()
                    c.op("dve", lambda: nc.vector.tensor_tensor(out=u_[:, 0:255], in0=z_[:, 0:255], in1=z_[:, 0:255],
                                                                op=ALU.mult), [Bz], [Bu])
                    c.op("dve", lambda: nc.vector.tensor_scalar(out=u_[:, 0:255], in0=u_[:, 0:255], scalar1=0.044715,
                                                                scalar2=1.0, op0=ALU.mult, op1=ALU.add), [Bu], [Bu])
                    c.op("dve", lambda: nc.vector.tensor_tensor(out=u_[:, 0:255], in0=u_[:, 0:255], in1=z_[:, 0:255],
                                                                op=ALU.mult), [Bu, Bz], [Bu])
                    c.op("act", lambda: nc.scalar.activation(out=u_[:, 0:255], in_=u_[:, 0:255], func=AF.Sigmoid,
                                                             scale=1.5957691216057308), [Bu], [Bu])
                    c.op("dve", lambda: nc.vector.tensor_tensor(out=G[:, i, hc, 0:255], in0=u_[:, 0:255],
                                                                in1=z_[:, 0:255], op=ALU.mult), [Bu, Bz], [BG])
            T.dbg(c, "dbg_w1", w1[:], (64, 2, 32, 256), BF16, Bw1)
            T.dbg(c, "dbg_posT", posT[:], (64, 2, 32), BF16, BposT)
            T.dbg(c, "dbg_G", G[:], (128, 2, 2, 256), BF16, BG)
            T.dbg(c, "dbg_w2", w2[:], (128, 2, 2, 64), BF16, Bw2)
            kp, Bkp = Hps.next()
            for hc in range(2):
                c.mm(kp[0:64, 0:256], w2[:, 0, hc, :], G[:, 0, hc, :], hc == 0, hc == 1, [Bw2, BG], [Bkp])
            kraw, Bkraw = sb(nc, es2, "ns_kraw", [64, 256], F32)
            c.op("dve", lambda: nc.vector.tensor_copy(out=kraw[:], in_=kp[0:64, 0:256]), [Bkp], [Bkraw])
            ksq, Bksq = sb(nc, es2, "ns_ksq", [64, 256], BF16)
            c.op("act", lambda: nc.scalar.activation(out=ksq[:], in_=kraw[:], func=AF.Square), [Bkraw], [Bksq])
            sp_, Bsp = bps.next()
            c.mm(sp_[0:64, 0:256], ones64[:], ksq[:], True, True, [Bksq, Bones], [Bsp])
            krs, Bkrs = sb(nc, es2, "ns_krs", [64, 256], F32)
            c.op("act", lambda: nc.scalar.activation(out=krs[:], in_=sp_[0:64, 0:256], func=AF.Sqrt,
                                                     bias=epsc[0:64, 0:1], scale=1.0 / 64), [Bsp, Beps], [Bkrs])
            c.op("dve", lambda: nc.vector.reciprocal(out=krs[:], in_=krs[:]), [Bkrs], [Bkrs])
            c.op("dve", lambda: nc.vector.scalar_tensor_tensor(out=kcT[:], in0=kraw[:], scalar=g1[:, 0:1], in1=krs[:],
                                                               op0=ALU.mult, op1=ALU.mult), [Bkraw, Bkrs, Bg1], [BkcT])
            for ct in range(2):
                rows = 128 if ct == 0 else 127
                vp_, Bvp = Hps.next()
                for hc in range(2):
                    c.mm(vp_[0:rows, 0:64], G[:, 1, hc, ct * 128:ct * 128 + rows], w2[:, 1, hc, :], hc == 0, hc == 1,
                         [BG, Bw2], [Bvp])
                c.op("dve", lambda: nc.vector.tensor_copy(out=Vc[0:rows, ct, 0:64], in_=vp_[0:rows, 0:64]), [Bvp], [BVc])
        c.barrier()
        KBc, BKBc = sb(nc, es, "ns_KBc", [4, 32, 128], BF16)
        c.dma(KBc[:], T["c_KBc"][:, :, :], reads=[T.B["c_KBc"]], writes=[BKBc])
        QBc, BQBc = sb(nc, es, "ns_QBc", [4, 4, 128], BF16)
        c.dma(QBc[:], T["c_QBc"][:, :, :], reads=[T.B["c_QBc"]], writes=[BQBc])
        cmask, Bcmask = sb(nc, es, "ns_cmask", [128, 17, 128], BF16)
        c.dma(cmask[:], T["c_cmask"][:, :, :], reads=[T.B["c_cmask"]], writes=[Bcmask])
        msk, Bmsk = sb(nc, es, "ns_msk", [128, 3, 128], BF16)
        c.dma(msk[:], T["c_masks"][:, :, :], reads=[T.B["c_masks"]], writes=[Bmsk])
        adj, Badj = sb(nc, es, "ns_adj", [128, 32, 64], F32)
        c.dma(adj[:], T["c_adj"][:, :, :], reads=[T.B["c_adj"]], writes=[Badj])
        gates, Bgates = sb(nc, es, "ns_gates", [128, 32, 12], F32)
        yacc, Byacc = sb(nc, es, "ns_yacc", [128, 32, 256], F32)
        with scope(c) as es2:
            gst, Bgst = sb(nc, es2, "ns_gst", [128, 32, 12], BF16)
            c.dma(gst[:], T["proj"][:, OFF["nsa_gate"]:OFF["nsa_gate"] + 12].rearrange("(b k) e -> k b e", k=128),
                  reads=[T.B["proj"]], writes=[Bgst])
            c.op("act", lambda: nc.scalar.activation(out=gates[:], in_=gst[:], func=AF.Sigmoid), [Bgst], [Bgates])
        c.barrier()
        T.dbg(c, "dbg_kcT", kcT[:], (64, 256), BF16, BkcT)
        T.dbg(c, "dbg_Vc", Vc[:], (128, 2, 130), BF16, BVc)
        if NSA_STAGE <= 2:
            return
        Sps = Rot(nc, es, "ns_S", [128, 512], F32, 3, psum=True)
        Ops = Rot(nc, es, "ns_O", [128, 512], F32, 2, psum=True)
        Tps = Rot(nc, es, "ns_T", [128, 256], BF16, 1, psum=True)
        Pt = Rot(nc, es, "ns_P", [128, 512], BF16, 5)
        Mt = Rot(nc, es, "ns_M", [128, 512], F32, 2)
        Ocs = Rot(nc, es, "ns_Oc", [128, 4, 130], F32, 3)
        Osb = Rot(nc, es, "ns_Osb", [128, 264], F32, 2)
        sm = Rot(nc, es, "ns_sm", [128, 16], F32, 3)
        impt = Rot(nc, es, "ns_imp", [128, 64], F32, 2)
        imp2 = Rot(nc, es, "ns_imp2", [128, 64], F32, 2)
        mx = Rot(nc, es, "ns_mx", [128, 16], F32, 2)
        selb = Rot(nc, es, "ns_selb", [128, 128], BF16, 2)
        for _i in range(2):
            c.op("dve", lambda: nc.vector.memset(selb.t[_i][:], 0.0), [], [selb.b[_i]])
        tmpy = Rot(nc, es, "ns_tmpy", [128, 256], F32, 2)

        def softmax_tile(s_, Bs, mask_ap, Bmask):
            p_, Bp = Pt.next()
            if mask_ap is not None:
                m_, Bm = Mt.next()
                c.op("dve", lambda: nc.vector.tensor_tensor(
                    out=m_[:].rearrange("k (h q) -> k h q", h=4), in0=s_[:].rearrange("k (h q) -> k h q", h=4),
                    in1=mask_ap.to_broadcast([128, 4, 128]), op=ALU.add), [Bs, Bmask], [Bm])
                c.op("act", lambda: nc.scalar.activation(out=p_[:], in_=m_[:], func=AF.Exp), [Bm], [Bp])
            else:
                c.op("act", lambda: nc.scalar.activation(out=p_[:], in_=s_[:], func=AF.Exp), [Bs], [Bp])
            return p_, Bp

        def finish_branch(o_ap, Bo, bq, br, first):
            num, den = o_ap
            s_, Bs_ = sm.next()
            c.op("dve", lambda: nc.vector.tensor_scalar(out=s_[:, 0:4].rearrange("t (h o) -> t h o", o=1), in0=den,
                                                        scalar1=1e-30, scalar2=None, op0=ALU.max), [Bo], [Bs_])
            c.op("dve", lambda: nc.vector.reciprocal(out=s_[:, 4:8], in_=s_[:, 0:4]), [Bs_], [Bs_])
            c.op("dve", lambda: nc.vector.tensor_tensor(out=s_[:, 8:12], in0=s_[:, 4:8], in1=gates[:, bq, ssl(br, 4, 3)],
                                                        op=ALU.mult), [Bs_, Bgates], [Bs_])
            wb = s_[:, 8:12].rearrange("t (h o) -> t h o", o=1).to_broadcast([128, 4, 64])
            yv = yacc[:, bq, :].rearrange("t (h e) -> t h e", h=4)
            if first:
                c.op("dve", lambda: nc.vector.tensor_tensor(out=yv, in0=num, in1=wb, op=ALU.mult), [Bo, Bs_], [Byacc])
            else:
                t_, Bt = tmpy.next()
                tv = t_[:].rearrange("t (h e) -> t h e", h=4)
                c.op("dve", lambda: nc.vector.tensor_tensor(out=tv, in0=num, in1=wb, op=ALU.mult), [Bo, Bs_], [Bt])
                c.op("dve", lambda: nc.vector.tensor_tensor(out=yacc[:, bq, :], in0=yacc[:, bq, :], in1=t_[:],
                                                            op=ALU.add), [Bt, Byacc], [Byacc])
            return s_, Bs_

        BqS = Buf("ns_qsel_rows")
        cmp_held = {}

        def cmp_a(bq):
            cts = [0] if bq < 16 else [0, 1]
            oA, BoA = Ops.next()
            oB, BoB = Ops.next()
            first = True
            for ct in cts:
                m = bq - 16 * ct
                s_, Bs = Sps.next()
                s4 = s_[:].rearrange("k (h q) -> k h q", h=4)
                c.mm(s4, kcT[:, ct * 128:(ct + 1) * 128], qT[0:64, :, bq * 128:(bq + 1) * 128],
                     True, False, [BkcT, BqT], [Bs])
                c.mm(s4, KBc[:, m, :], QBc[:, :, :], False, True, [BKBc, BQBc], [Bs])
                p_, Bp = softmax_tile(s_, Bs, cmask[:, m:m + 1, :] if m <= 16 else None, Bcmask)
                for h in range(4):
                    o_, Bo = (oA, BoA) if h < 2 else (oB, BoB)
                    hh = h % 2
                    c.mm(o_[:, hh * 130:(hh + 1) * 130], p_[:, h * 128:(h + 1) * 128], Vc[:, ct, :],
                         first and hh == 0, False, [Bp, BVc], [Bo], skip_group_check=True)
                first = False
            oc, Boc = Ocs.next()
            c.copy(oc[:, 0:2, :], oA[:, 0:260].rearrange("t (h e) -> t h e", h=2), [BoA], [Boc])
            c.copy(oc[:, 2:4, :], oB[:, 0:260].rearrange("t (h e) -> t h e", h=2), [BoB], [Boc])
            cmp_held[bq] = (oc, Boc)

        def cmp_b(bq):
            oc, Boc = cmp_held.pop(bq)
            s_, Bs_ = finish_branch((oc[:, :, 0:64], oc[:, :, 128:129]), Boc, bq, 0, True)
            im, Bim = impt.next()
            c.op("dve", lambda: nc.vector.scalar_tensor_tensor(out=im[:], in0=oc[:, 0, 64:128], scalar=s_[:, 4:5],
                                                               in1=adj[:, bq, :], op0=ALU.mult, op1=ALU.add),
                 [Boc, Bs_, Badj], [Bim])
            for h in range(1, 4):
                c.op("dve", lambda: nc.vector.scalar_tensor_tensor(out=im[:], in0=oc[:, h, 64:128], scalar=s_[:, 4 + h:5 + h],
                                                                   in1=im[:], op0=ALU.mult, op1=ALU.add),
                     [Boc, Bs_, Bim], [Bim])
            mx_, Bmx = mx.next()
            c.op("dve", lambda: nc.vector.max(out=mx_[:, 0:8], in_=im[:]), [Bim], [Bmx])
            i2, Bi2 = imp2.next()
            c.op("dve", lambda: nc.vector.match_replace(out=i2[:], in_to_replace=mx_[:, 0:8], in_values=im[:],
                                                        imm_value=-3.0e38), [Bim, Bmx], [Bi2])
            c.op("dve", lambda: nc.vector.max(out=mx_[:, 8:16], in_=i2[:]), [Bi2], [Bmx])
            sb_, Bsb = selb.next()
            c.op("dve", lambda: nc.vector.tensor_scalar(out=sb_[:, 64:128], in0=im[:], scalar1=mx_[:, 15:16], scalar2=None,
                                                        op0=ALU.is_ge), [Bim, Bmx], [Bsb])
            tp_, Btp = Tps.next()
            c.op("pe", lambda: nc.tensor.transpose(out=tp_[:, 0:128], in_=sb_[:], identity=ident[:]), [Bsb, Bid], [Btp])
            for h in range(4):
                c.op("dve", lambda: nc.vector.tensor_scalar(
                    out=qT[64:128, h, bq * 128:(bq + 1) * 128], in0=tp_[64:128, 0:128], scalar1=Asel[64:128, bq, h:h + 1],
                    scalar2=-30000.0, op0=ALU.mult, op1=ALU.add), [Btp, BAsel], [BqS])

        for bq in range(NT + 1):
            if bq < NT:
                cmp_a(bq)
            if bq >= 1:
                cmp_b(bq - 1)
        T.dbg(c, "dbg_ycmp", yacc[:], (128, 32, 256), F32, Byacc)
        if NSA_STAGE <= 3:
            return
        SKEW = 2
        tasks = []
        for bq in range(NT):
            for br in (1, 2):
                kbs = list(range(0, bq + 1)) if br == 1 else list(range(max(0, bq - 4), bq + 1))
                for i_, kb in enumerate(kbs):
                    tasks.append((bq, br, kb, i_ == 0, i_ == len(kbs) - 1))
        opnd = {1: (ksT, BksT, Vs, BVs, qT, BqS), 2: (kwT, BkwT, Vw, BVw, Q2w, BQ2w)}
        st = {}
        cur_o = [None]

        def stage_a(ti):
            bq, br, kb, is_first, is_last = tasks[ti]
            kT_, BkT_, V_, BV_, Q_, BQ_ = opnd[br]
            s_, Bs = Sps.next()
            s4 = s_[:].rearrange("k (h q) -> k h q", h=4)
            c.mm(s4, kT_[:, 0, kb * 128:(kb + 1) * 128], Q_[:, :, bq * 128:(bq + 1) * 128], True, True,
                 [BkT_, BQ_, BqT], [Bs])
            mask_ap = None
            if kb == bq:
                mask_ap = msk[:, 0:1, :]
            elif br == 2 and kb == bq - 4:
                mask_ap = msk[:, 2:3, :]
            st[ti] = softmax_tile(s_, Bs, mask_ap, Bmsk)

        def stage_c(ti):
            bq, br, kb, is_first, is_last = tasks[ti]
            kT_, BkT_, V_, BV_, Q_, BQ_ = opnd[br]
            p_, Bp = st.pop(ti)
            if is_first:
                cur_o[0] = Ops.next()
            o_, Bo = cur_o[0]
            for h in range(4):
                c.mm(o_[:, h * 66:(h + 1) * 66], p_[:, h * 128:(h + 1) * 128], V_[:, kb, h, :],
                     is_first and h == 0, False, [Bp, BV_], [Bo], skip_group_check=True)
            if is_last:
                ob, Bob = Osb.next()
                c.copy(ob[:], o_[:, 0:264], [Bo], [Bob])
                ov = ob[:].rearrange("t (h e) -> t h e", h=4)
                finish_branch((ov[:, :, 0:64], ov[:, :, 64:65]), Bob, bq, br, False)

        for ti in range(len(tasks) + SKEW):
            if ti < len(tasks):
                stage_a(ti)
            if ti - SKEW >= 0:
                stage_c(ti - SKEW)
        c.dma(T["mix"][:, 256:512].rearrange("(b q) e -> q b e", q=128), yacc[:], reads=[Byacc], writes=[T.B["mix"]])
    c.barrier()


GLA_STAGE = 99


def phase_gla(c, T, l):
    _phase_gla(c, T, l)
    c.barrier()


def _phase_gla(c, T, l):
    nc = c.nc
    with scope(c) as es:
        ident, Bid = sb(nc, es, "gl_id", [128, 128], BF16)
        c.dma(ident[:], T["ident"][:, :], reads=[T.B["ident"]], writes=[Bid])
        m01, Bm01 = sb(nc, es, "gl_m01", [128, 128], BF16)
        c.dma(m01[:], T["c_mask01"][:, :], reads=[T.B["c_mask01"]], writes=[Bm01])
        epsc, Beps = sb(nc, es, "gl_eps", [128, 1], F32)
        c.op("dve", lambda: nc.vector.memset(epsc[:], EPS), [], [Beps])
        gnB, BgnB = sb(nc, es, "gl_gnB", [128, 64], F32)
        c.dma(gnB[:], T["gla_norm_g"][l].partition_broadcast(128), reads=[T.B["gla_norm_g"]], writes=[BgnB])
        qt, Bqt = sb(nc, es, "gl_qt", [32, 4, S], BF16)
        kt, Bkt = sb(nc, es, "gl_kt", [32, 4, S], BF16)
        ktm, Bktm = sb(nc, es, "gl_ktm", [128, NT, 128], BF16)
        ebl, Bebl = sb(nc, es, "gl_ebl", [32, 4, NT], F32)
        with scope(c) as es2:
            wa, Bwa = sb(nc, es2, "gl_wa", [16, 128], BF16)
            c.dma(wa[:], T["gla_wa2"][l], reads=[T.B["gla_wa2"]], writes=[Bwa], q="pool")
            bac, Bbac = sb(nc, es2, "gl_ba", [32, 4], F32)
            c.dma(bac[:], T["gla_ba"][l].rearrange("(g p) -> p g", g=4), reads=[T.B["gla_ba"]], writes=[Bbac],
                  allow_slow_non_contiguous=True)
            aT, BaT = sb(nc, es2, "gl_aT", [16, S], BF16)
            c.dma(aT[:], T["projT"][OFF["gla_a"]:OFF["gla_a"] + 16, :], reads=[T.B["projT"]], writes=[BaT])
            seg, Bseg = sb(nc, es2, "gl_seg", [32, S], BF16)
            c.op("dve", lambda: nc.vector.memset(seg[:], 1.0), [], [Bseg])
            c.op("dve", lambda: nc.vector.memset(seg[:, ssl(0, NT, 128)], 0.0), [], [Bseg])
            zp = Rot(nc, es2, "gl_zp", [128, 512], F32, 2, psum=True)
            qkr = Rot(nc, es2, "gl_qk", [32, 2, S], BF16, 2)
            lar = Rot(nc, es2, "gl_la", [32, S], F32, 2)
            bbr = Rot(nc, es2, "gl_bb", [32, S], F32, 2)
            qkv = T["projT"][OFF["gla_q"]:OFF["gla_q"] + 256, :].rearrange("(i p) t -> p i t", i=8)
            for g in range(4):
                qk, Bqk = qkr.next()
                c.dma(qk[:, 0, :], qkv[:, g, :], reads=[T.B["projT"]], writes=[Bqk])
                c.dma(qk[:, 1, :], qkv[:, 4 + g, :], reads=[T.B["projT"]], writes=[Bqk])
                la, Bla = lar.next()
                bb, Bbb = bbr.next()
                for tt in range(8):
                    sl = slice(tt * 512, (tt + 1) * 512)
                    z_, Bz = zp.next()
                    c.mm(z_[0:32, :], wa[:, g * 32:(g + 1) * 32], aT[:, sl], True, True, [Bwa, BaT], [Bz])
                    c.op("act", lambda: nc.scalar.activation(out=la[:, sl], in_=z_[0:32, :], func=AF.Sigmoid,
                                                             bias=bac[:, g:g + 1]), [Bz, Bbac], [Bla])
                c.op("act", lambda: nc.scalar.activation(out=la[:], in_=la[:], func=AF.Ln), [Bla], [Bla])
                c.op("act", lambda: nc.scalar.mul(out=la[:], in_=la[:], mul=1.0 / 16.0), [Bla], [Bla])
                c.op("dve", lambda: nc.vector.tensor_tensor_scan(out=bb[:], data0=seg[:], data1=la[:],
                                                                 initial=0.0, op0=ALU.mult, op1=ALU.add),
                     [Bseg, Bla], [Bbb])
                c.op("act", lambda: nc.scalar.activation(out=la[:], in_=bb[:], func=AF.Exp), [Bbb], [Bla])
                c.op("dve", lambda: nc.vector.tensor_copy(out=ebl[:, g, :], in_=la[:, ssl(127, NT, 128)]), [Bla], [Bebl])
                c.op("dve", lambda: nc.vector.scalar_tensor_tensor(out=qt[:, g, :], in0=qk[:, 0, :], scalar=32.0 ** -0.5,
                                                                   in1=la[:], op0=ALU.mult, op1=ALU.mult),
                     [Bqk, Bla], [Bqt])
                c.op("act", lambda: nc.scalar.activation(out=bb[:], in_=bb[:], func=AF.Exp, scale=-1.0),
                     [Bbb], [Bbb])
                c.op("dve", lambda: nc.vector.tensor_tensor(out=kt[:, g, :], in0=qk[:, 1, :], in1=bb[:],
                                                            op=ALU.mult), [Bqk, Bbb], [Bkt])
            tp = Rot(nc, es2, "gl_tp", [128, 1024], BF16, 2, psum=True)
            for n in range(NT):
                p_, Bp = tp.next()
                for g in range(4):
                    c.op("pe", lambda: nc.tensor.transpose(out=p_[:, g * 32:(g + 1) * 32],
                                                           in_=kt[:, g, n * 128:(n + 1) * 128],
                                                           identity=ident[0:32, 0:32]), [Bkt, Bid], [Bp])
                c.copy(ktm[:, n, :], p_[:, 0:128], [Bp], [Bktm])
        v, Bv = sb(nc, es, "gl_v", [128, NT, 256], BF16)
        c.dma(v[:], T["proj"][:, OFF["gla_v"]:OFF["gla_v"] + 256].rearrange("(n j) e -> j n e", j=128),
              reads=[T.B["proj"]], writes=[Bv])
        oall, Boall = sb(nc, es, "gl_oall", [128, NT, 256], F32)
        Sps = Rot(nc, es, "gl_S", [128, 512], F32, 2, psum=True)
        Ops = Rot(nc, es, "gl_O", [128, 512], F32, 2, psum=True)
        Pps = Rot(nc, es, "gl_P", [128, 512], F32, 2, psum=True)
        At = Rot(nc, es, "gl_A", [128, 512], BF16, 3)
        Sf, BSf = sb(nc, es, "gl_Sf", [32, 4, 64], F32)
        Sf2, BSf2 = sb(nc, es, "gl_Sf2", [32, 4, 64], F32)
        Sf3, BSf3 = sb(nc, es, "gl_Sf3", [32, 4, 64], F32)
        Sst, BSst = sb(nc, es, "gl_Sst", [32, 4, 64], BF16)
        c.op("dve", lambda: nc.vector.memset(Sf[:], 0.0), [], [BSf])
        c.op("dve", lambda: nc.vector.memset(Sst[:], 0.0), [], [BSst])
        for n in range(NT):
            sl = slice(n * 128, (n + 1) * 128)
            s_, Bs = Sps.next()
            for h in range(4):
                c.mm(s_[:, h * 128:(h + 1) * 128], kt[:, h, sl], qt[:, h, sl], True, True, [Bkt, Bqt], [Bs])
            pp, Bpp = Pps.next()
            for h in range(4):
                c.mm(pp[0:32, h * 64:(h + 1) * 64], ktm[:, n, 32 * h:32 * h + 32], v[:, n, 64 * h:64 * h + 64],
                     True, True, [Bktm, Bv], [Bpp])
            a_, Ba = At.next()
            c.op("dve", lambda: nc.vector.tensor_tensor(
                out=a_[:].rearrange("k (h q) -> k h q", h=4), in0=s_[:].rearrange("k (h q) -> k h q", h=4),
                in1=m01[:].rearrange("k (o q) -> k o q", o=1).to_broadcast([128, 4, 128]), op=ALU.mult), [Bs, Bm01], [Ba])
            o_, Bo = Ops.next()
            for h in range(4):
                c.mm(o_[:, 64 * h:64 * h + 64], a_[:, h * 128:(h + 1) * 128], v[:, n, 64 * h:64 * h + 64],
                     True, False, [Ba, Bv], [Bo])
                c.mm(o_[:, 64 * h:64 * h + 64], qt[:, h, sl], Sst[:, h, :], False, True, [Bqt, BSst], [Bo])
            c.copy(oall[:, n, :], o_[:, 0:256], [Bo], [Boall])
            eb = ebl[:, :, n:n + 1].to_broadcast([32, 4, 64])
            c.op("dve", lambda: nc.vector.tensor_tensor(out=Sf2[:], in0=Sf[:], in1=eb, op=ALU.mult), [BSf, Bebl], [BSf2])
            c.op("dve", lambda: nc.vector.tensor_tensor(out=Sf3[:], in0=pp[0:32, 0:256].rearrange("p (h e) -> p h e", h=4),
                                                        in1=eb, op=ALU.mult), [Bpp, Bebl], [BSf3])
            c.op("dve", lambda: nc.vector.tensor_tensor(out=Sf[:], in0=Sf2[:], in1=Sf3[:], op=ALU.add), [BSf2, BSf3], [BSf])
            c.op("dve", lambda: nc.vector.tensor_copy(out=Sst[:], in_=Sf[:]), [BSf], [BSst])
        if GLA_STAGE <= 5:
            return
        with scope(c) as es2:
            r_, Br = sb(nc, es2, "gl_r", [128, NT, 256], BF16)
            c.dma(r_[:], T["proj"][:, OFF["gla_r"]:OFF["gla_r"] + 256].rearrange("(n j) e -> j n e", j=128),
                  reads=[T.B["proj"]], writes=[Br])
            sq, Bsq = sb(nc, es2, "gl_sq", [128, NT, 256], F32)
            ss, Bss = sb(nc, es2, "gl_ss", [128, NT * 4], F32)
            c.op("act", lambda: nc.scalar.activation(out=sq[:], in_=oall[:], func=AF.Square), [Boall], [Bsq])
            c.op("dve", lambda: nc.vector.tensor_reduce(out=ss[:], in_=sq[:].rearrange("t n (h e) -> t (n h) e", h=4),
                                                        axis=AX.X, op=ALU.add), [Bsq], [Bss])
            c.op("act", lambda: nc.scalar.activation(out=ss[:], in_=ss[:], func=AF.Sqrt, bias=epsc[:, 0:1], scale=1.0 / 64),
                 [Bss, Beps], [Bss])
            c.op("dve", lambda: nc.vector.reciprocal(out=ss[:], in_=ss[:]), [Bss], [Bss])
            ov = oall[:].rearrange("t n (h e) -> t (n h) e", h=4)
            c.op("dve", lambda: nc.vector.tensor_tensor(
                out=ov, in0=ov, in1=ss[:].rearrange("t (m o) -> t m o", o=1).to_broadcast([128, NT * 4, 64]), op=ALU.mult),
                [Boall, Bss], [Boall])
            c.op("dve", lambda: nc.vector.tensor_tensor(
                out=ov, in0=ov, in1=gnB[:].rearrange("t (o e) -> t o e", o=1).to_broadcast([128, NT * 4, 64]), op=ALU.mult),
                [Boall, BgnB], [Boall])
            c.op("act", lambda: nc.scalar.activation(out=sq[:], in_=r_[:], func=AF.Silu), [Br], [Bsq])
            c.op("dve", lambda: nc.vector.tensor_tensor(out=oall[:], in0=oall[:], in1=sq[:], op=ALU.mult),
                 [Boall, Bsq], [Boall])
            c.dma(T["mix"][:, 512:768].rearrange("(n j) e -> j n e", j=128), oall[:], reads=[Boall], writes=[T.B["mix"]])


S5_L = 256
S5_STAGE = 99


def phase_s5(c, T, l):
    _phase_s5(c, T, l)
    c.barrier()


def _phase_s5(c, T, l):
    nc = c.nc
    L = S5_L
    NCH = S // L
    uoff = OFF["s5_u"]

    def dv(fn, r, w):
        return c.op("dve", fn, r, w)

    def tt(out, a, b, op, r, w):
        return c.op("dve", lambda: nc.vector.tensor_tensor(out=out, in0=a, in1=b, op=op), r, w)

    with scope(c) as es:
        ident, Bid = sb(nc, es, "s5_id", [128, 128], BF16)
        c.dma(ident[:], T["ident"][:, :], reads=[T.B["ident"]], writes=[Bid])
        identf, Bidf = sb(nc, es, "s5_idf", [128, 128], F32)
        c.dma(identf[:], T["identf"][:, :], reads=[T.B["identf"]], writes=[Bidf])
        mask, Bmask = sb(nc, es, "s5_mask", [128, 8, 128], F32)
        c.dma(mask[:], T["c_s5mask"][:, :, :], reads=[T.B["c_s5mask"]], writes=[Bmask])
        P, BP = sb(nc, es, "s5_P", [128, 40, 8], F32)
        hp, Bhp = sb(nc, es, "s5_hp", [128, 1], F32)
        dv(lambda: nc.vector.memset(hp[:], math.pi / 2.0), [], [Bhp])
        (ARE, AIM, LDT, DT, MAG, TH, CS, SN, C2, S2, TA, TB_, ABR, ABI, DEN, AM1, FRE, FIM, PR, PI_, PR2, PI2,
         WLR, WLI, X1, X2) = range(26)

        def col(i):
            return P[:, i, :]

        for gg in range(2):
            ps_ = slice(gg * 64, (gg + 1) * 64)
            c.dma(P[ps_, ARE, :], T["s5_a_re"][l].rearrange("(m gg) n -> gg n m", gg=2)[gg], reads=[T.B["s5_a_re"]],
                  writes=[BP], allow_slow_non_contiguous=True)
            c.dma(P[ps_, AIM, :], T["s5_a_im"][l].rearrange("(m gg) n -> gg n m", gg=2)[gg], reads=[T.B["s5_a_im"]],
                  writes=[BP], allow_slow_non_contiguous=True)
            c.dma(P[ps_, LDT, :], T["s5_log_dt"][l].rearrange("(m gg) -> gg m", gg=2)[gg].partition_broadcast(64),
                  reads=[T.B["s5_log_dt"]], writes=[BP], allow_slow_non_contiguous=True)
        c.op("act", lambda: nc.scalar.activation(out=col(DT), in_=col(LDT), func=AF.Exp), [BP], [BP])
        tt(col(TA), col(DT), col(ARE), ALU.mult, [BP], [BP])
        c.op("act", lambda: nc.scalar.activation(out=col(MAG), in_=col(TA), func=AF.Exp), [BP], [BP])
        tt(col(TH), col(DT), col(AIM), ALU.mult, [BP], [BP])
        c.op("act", lambda: nc.scalar.activation(out=col(SN), in_=col(TH), func=AF.Sin, scale=1.0 / 16.0), [BP], [BP])
        c.op("act", lambda: nc.scalar.activation(out=col(CS), in_=col(TH), func=AF.Sin, scale=1.0 / 16.0,
                                                 bias=hp[:, 0:1]), [BP, Bhp], [BP])

        def csq(cr, ci, orr, oi):
            tt(col(TA), col(cr), col(cr), ALU.mult, [BP], [BP])
            tt(col(TB_), col(ci), col(ci), ALU.mult, [BP], [BP])
            dv(lambda: nc.vector.scalar_tensor_tensor(out=col(oi), in0=col(cr), scalar=2.0, in1=col(ci),
                                                      op0=ALU.mult, op1=ALU.mult), [BP], [BP])
            tt(col(orr), col(TA), col(TB_), ALU.subtract, [BP], [BP])

        csq(CS, SN, C2, S2)
        csq(C2, S2, CS, SN)
        csq(CS, SN, C2, S2)
        csq(C2, S2, CS, SN)
        tt(col(ABR), col(MAG), col(CS), ALU.mult, [BP], [BP])
        tt(col(ABI), col(MAG), col(SN), ALU.mult, [BP], [BP])
        tt(col(TA), col(ARE), col(ARE), ALU.mult, [BP], [BP])
        tt(col(TB_), col(AIM), col(AIM), ALU.mult, [BP], [BP])
        tt(col(DEN), col(TA), col(TB_), ALU.add, [BP], [BP])
        dv(lambda: nc.vector.reciprocal(out=col(DEN), in_=col(DEN)), [BP], [BP])
        dv(lambda: nc.vector.tensor_scalar(out=col(AM1), in0=col(ABR), scalar1=-1.0, scalar2=None, op0=ALU.add), [BP], [BP])
        tt(col(TA), col(AM1), col(ARE), ALU.mult, [BP], [BP])
        tt(col(TB_), col(ABI), col(AIM), ALU.mult, [BP], [BP])
        tt(col(FRE), col(TA), col(TB_), ALU.add, [BP], [BP])
        tt(col(FRE), col(FRE), col(DEN), ALU.mult, [BP], [BP])
        tt(col(TA), col(ABI), col(ARE), ALU.mult, [BP], [BP])
        tt(col(TB_), col(AM1), col(AIM), ALU.mult, [BP], [BP])
        tt(col(FIM), col(TA), col(TB_), ALU.subtract, [BP], [BP])
        tt(col(FIM), col(FIM), col(DEN), ALU.mult, [BP], [BP])
        BT, BBT = sb(nc, es, "s5_BT", [128, 2, 8, 128], BF16)
        Cm, BCm = sb(nc, es, "s5_Cm", [128, 2, 8, 128], BF16)
        with scope(c) as es2:
            braw, Bbraw = sb(nc, es2, "s5_braw", [128, 2, 8, 16], F32)
            craw, Bcraw = sb(nc, es2, "s5_craw", [128, 2, 8, 16], F32)
            for gg in range(2):
                ps_ = slice(gg * 64, (gg + 1) * 64)
                for ri, nm in enumerate(("s5_b_re", "s5_b_im")):
                    c.dma(braw[ps_, ri, :, :], T[nm][l].rearrange("(m gg) n c -> gg n m c", gg=2)[gg], reads=[T.B[nm]],
                          writes=[Bbraw], allow_slow_non_contiguous=True)
                for ri, nm in enumerate(("s5_c_re", "s5_c_im")):
                    for m in range(8):
                        c.dma(craw[ps_, ri, m, :], T[nm][l, 2 * m + gg].rearrange("c n -> n c"), reads=[T.B[nm]],
                              writes=[Bcraw], allow_slow_non_contiguous=True)
            bbc, Bbbc = sb(nc, es2, "s5_bbc", [128, 2, 8, 16], F32)
            t1, Bt1 = sb(nc, es2, "s5_t1", [128, 8, 16], F32)
            t2, Bt2 = sb(nc, es2, "s5_t2", [128, 8, 16], F32)
            fre_b = P[:, FRE, :].rearrange("p (m o) -> p m o", o=1).to_broadcast([128, 8, 16])
            fim_b = P[:, FIM, :].rearrange("p (m o) -> p m o", o=1).to_broadcast([128, 8, 16])
            tt(t1[:], braw[:, 0, :, :], fre_b, ALU.mult, [Bbraw, BP], [Bt1])
            tt(t2[:], braw[:, 1, :, :], fim_b, ALU.mult, [Bbraw, BP], [Bt2])
            tt(bbc[:, 0, :, :], t1[:], t2[:], ALU.subtract, [Bt1, Bt2], [Bbbc])
            tt(t1[:], braw[:, 1, :, :], fre_b, ALU.mult, [Bbraw, BP], [Bt1])
            tt(t2[:], braw[:, 0, :, :], fim_b, ALU.mult, [Bbraw, BP], [Bt2])
            tt(bbc[:, 1, :, :], t1[:], t2[:], ALU.add, [Bt1, Bt2], [Bbbc])
            Bd, BBd = sb(nc, es2, "s5_Bd", [128, 2, 8, 128], BF16)
            mask4 = mask[:].rearrange("p m (j c) -> p m j c", c=16)
            for ri in range(2):
                tt(Bd[:, ri, :, :].rearrange("p m (j c) -> p m j c", c=16), mask4,
                   bbc[:, ri, :, :].rearrange("p m (o c) -> p m o c", o=1).to_broadcast([128, 8, 8, 16]), ALU.mult,
                   [Bmask, Bbbc], [BBd])
            tt(Cm[:, 0, :, :].rearrange("p m (j c) -> p m j c", c=16), mask4,
               craw[:, 0, :, :].rearrange("p m (o c) -> p m o c", o=1).to_broadcast([128, 8, 8, 16]), ALU.mult,
               [Bmask, Bcraw], [BCm])
            dv(lambda: nc.vector.tensor_scalar(out=craw[:, 1, :, :], in0=craw[:, 1, :, :], scalar1=-1.0, scalar2=None,
                                               op0=ALU.mult), [Bcraw], [Bcraw])
            tt(Cm[:, 1, :, :].rearrange("p m (j c) -> p m j c", c=16), mask4,
               craw[:, 1, :, :].rearrange("p m (o c) -> p m o c", o=1).to_broadcast([128, 8, 8, 16]), ALU.mult,
               [Bmask, Bcraw], [BCm])
            tpb = Rot(nc, es2, "s5_tpb", [128, 1024], BF16, 2, psum=True)
            for ri in range(2):
                for g2 in range(2):
                    p_, Bp = tpb.next()
                    for j in range(4):
                        m = g2 * 4 + j
                        c.op("pe", lambda: nc.tensor.transpose(out=p_[:, j * 128:(j + 1) * 128], in_=Bd[:, ri, m, :],
                                                               identity=ident[:]), [BBd, Bid], [Bp])
                    c.copy(BT[:, ri, g2 * 4:(g2 + 1) * 4, :], p_[:, 0:512].rearrange("p (j t) -> p j t", j=4), [Bp], [BBT])
        c.barrier()
        T.dbg(c, "dbg_s5P", P[:], (128, 40, 8), F32, BP)
        if S5_STAGE <= 1:
            return
        w, Bw = sb(nc, es, "s5_w", [128, 2, 8, L], F32)
        Rb, BRb = sb(nc, es, "s5_Rb", [128, 8, L], F32)
        tA, BtA = sb(nc, es, "s5_tA", [128, 8, L], F32)
        tB, BtB = sb(nc, es, "s5_tB", [128, 8, L], F32)
        tC, BtC = sb(nc, es, "s5_tC", [128, 8, L], F32)
        tD, BtD = sb(nc, es, "s5_tD", [128, 8, L], F32)
        dv(lambda: nc.vector.tensor_copy(out=Rb[:], in_=P[:, MAG, :].rearrange("p (m o) -> p m o", o=1)
                                         .to_broadcast([128, 8, L])), [BP], [BRb])
        dv(lambda: nc.vector.memset(w[:, 0, :, 0:1], 1.0), [], [Bw])
        dv(lambda: nc.vector.memset(w[:, 1, :, 0:1], 0.0), [], [Bw])
        dv(lambda: nc.vector.tensor_copy(out=col(PR), in_=col(CS)), [BP], [BP])
        dv(lambda: nc.vector.tensor_copy(out=col(PI_), in_=col(SN)), [BP], [BP])
        k = 1
        cur = (PR, PI_)
        oth = (PR2, PI2)
        while k < L:
            prb = P[:, cur[0], :].rearrange("p (m o) -> p m o", o=1).to_broadcast([128, 8, k])
            pib = P[:, cur[1], :].rearrange("p (m o) -> p m o", o=1).to_broadcast([128, 8, k])
            wr0 = w[:, 0, :, 0:k]
            wi0 = w[:, 1, :, 0:k]
            tt(tA[:, :, 0:k], wr0, prb, ALU.mult, [Bw, BP], [BtA])
            tt(tB[:, :, 0:k], wi0, pib, ALU.mult, [Bw, BP], [BtB])
            tt(w[:, 0, :, k:2 * k], tA[:, :, 0:k], tB[:, :, 0:k], ALU.subtract, [BtA, BtB], [Bw])
            tt(tA[:, :, 0:k], wr0, pib, ALU.mult, [Bw, BP], [BtA])
            tt(tB[:, :, 0:k], wi0, prb, ALU.mult, [Bw, BP], [BtB])
            tt(w[:, 1, :, k:2 * k], tA[:, :, 0:k], tB[:, :, 0:k], ALU.add, [BtA, BtB], [Bw])
            csq(cur[0], cur[1], oth[0], oth[1])
            cur, oth = oth, cur
            k *= 2
        dv(lambda: nc.vector.tensor_copy(out=col(WLR), in_=col(cur[0])), [BP], [BP])
        dv(lambda: nc.vector.tensor_copy(out=col(WLI), in_=col(cur[1])), [BP], [BP])
        T.dbg(c, "dbg_s5w", w[:], (128, 2, 8, L), F32, Bw)
        gw, Bgw = sb(nc, es, "s5_gwt", [128, 2, 256], BF16)
        c.dma(gw[:], T["s5_glu_w"][l].rearrange("(kt p) n -> p kt n", p=128), reads=[T.B["s5_glu_w"]], writes=[Bgw], q="pool")
        gbc, Bgbc = sb(nc, es, "s5_gbt", [128, 2], F32)
        c.dma(gbc[:], T["s5_glu_b"][l].rearrange("(h p) -> p h", p=128), reads=[T.B["s5_glu_b"]], writes=[Bgbc],
              allow_slow_non_contiguous=True)
        dcl, Bdcl = sb(nc, es, "s5_dcol", [128, 2], F32)
        c.dma(dcl[:], T["s5_d"][l].rearrange("(h p) -> p h", p=128), reads=[T.B["s5_d"]], writes=[Bdcl],
              allow_slow_non_contiguous=True)
        if S5_STAGE <= 2:
            return
        uTr = Rot(nc, es, "s5_uT", [128, 2, L], BF16, 3)
        BUps = Rot(nc, es, "s5_BU", [128, 512], F32, 2, psum=True)
        Yps = Rot(nc, es, "s5_Y", [128, 512], F32, 2, psum=True)
        Tpf = Rot(nc, es, "s5_Tp", [128, 512], F32, 2, psum=True)
        d1 = Rot(nc, es, "s5_d1", [128, L], F32, 3)
        d2 = Rot(nc, es, "s5_d2", [128, L], F32, 3)
        zb = Rot(nc, es, "s5_zb", [128, 2, L], F32, 3)
        Z, BZ = sb(nc, es, "s5_Z", [128, 2, 8, L], F32)
        bus = Rot(nc, es, "s5_bus", [128, 2, 8, L], F32, 2)
        zbs, Bzbs = sb(nc, es, "s5_zbs", [128, 2, 8, L], F32)
        Xr = Rot(nc, es, "s5_X", [128, 2, 8, L], BF16, 2)
        stash = {}
        init = Rot(nc, es, "s5_init", [128, 2, 8], F32, 2)
        zt, Bzt = sb(nc, es, "s5_zt", [128, 2, L], F32)
        u2, Bu2 = sb(nc, es, "s5_u2", [128, 2, L], F32)
        hg, Bhg = sb(nc, es, "s5_hg", [128, 2, L], F32)
        hgb, Bhgb = sb(nc, es, "s5_hgb", [128, 2, L], BF16)
        sgt, Bsgt = sb(nc, es, "s5_sg", [128, 2, L], F32)
        ot, Bot = sb(nc, es, "s5_ot", [128, 2, L], F32)
        otm = Rot(nc, es, "s5_otm", [128, L // 128, 256], F32, 2)
        in_, Bin = init.next()
        dv(lambda: nc.vector.memset(in_[:], 0.0), [], [Bin])
        def s5_a(ci):
            nonlocal in_, Bin
            t0 = ci * L
            X, BX = Xr.next()
            u_, Bu = uTr.next()
            c.dma(u_[:], T["projT"][uoff:uoff + 256, t0:t0 + L].rearrange("(h p) t -> p h t", p=128),
                  reads=[T.B["projT"]], writes=[Bu])
            bu_, Bbu = bus.next()
            for m in range(8):
                bk, Bbk = BUps.next()
                c.mm(bk[:, 0:L], BT[:, 0, m, :], u_[:, m // 4, :], True, True, [BBT, Bu], [Bbk])
                c.mm(bk[:, L:2 * L], BT[:, 1, m, :], u_[:, m // 4, :], True, True, [BBT, Bu], [Bbk])
                c.op("act", lambda: nc.scalar.copy(out=bu_[:, :, m, :], in_=bk[:, 0:2 * L].rearrange("p (r l) -> p r l", r=2)),
                     [Bbk], [Bbu])
            tt(tA[:], bu_[:, 0, :, :], w[:, 0, :, :], ALU.mult, [Bbu, Bw], [BtA])
            tt(tB[:], bu_[:, 1, :, :], w[:, 1, :, :], ALU.mult, [Bbu, Bw], [BtB])
            tt(zbs[:, 0, :, :], tA[:], tB[:], ALU.add, [BtA, BtB], [Bzbs])
            tt(tC[:], bu_[:, 1, :, :], w[:, 0, :, :], ALU.mult, [Bbu, Bw], [BtC])
            tt(tD[:], bu_[:, 0, :, :], w[:, 1, :, :], ALU.mult, [Bbu, Bw], [BtD])
            tt(zbs[:, 1, :, :], tC[:], tD[:], ALU.subtract, [BtC, BtD], [Bzbs])
            for m in range(8):
                for ri in range(2):
                    dv(lambda: nc.vector.tensor_tensor_scan(out=Z[:, ri, m, :], data0=Rb[:, m, :], data1=zbs[:, ri, m, :],
                                                            initial=in_[:, ri, m:m + 1], op0=ALU.mult, op1=ALU.add),
                       [BRb, Bzbs, Bin], [BZ])
            nx, Bnx = init.next()
            zr = Z[:, 0, :, L - 1]
            zi = Z[:, 1, :, L - 1]
            tt(col(X1), zr, col(WLR), ALU.mult, [BZ, BP], [BP])
            tt(col(X2), zi, col(WLI), ALU.mult, [BZ, BP], [BP])
            tt(nx[:, 0, :], col(X1), col(X2), ALU.subtract, [BP], [Bnx])
            tt(col(X1), zr, col(WLI), ALU.mult, [BZ, BP], [BP])
            tt(col(X2), zi, col(WLR), ALU.mult, [BZ, BP], [BP])
            tt(nx[:, 1, :], col(X1), col(X2), ALU.add, [BP], [Bnx])
            in_, Bin = nx, Bnx
            def pt(out, a, b, op, r, w_):
                return c.op("dve", lambda: nc.vector.tensor_tensor(out=out, in0=a, in1=b, op=op), r, w_)
            tt(tA[:], Z[:, 0, :, :], w[:, 0, :, :], ALU.mult, [BZ, Bw], [BtA])
            tt(tB[:], Z[:, 1, :, :], w[:, 1, :, :], ALU.mult, [BZ, Bw], [BtB])
            pt(X[:, 0, :, :], tA[:], tB[:], ALU.subtract, [BtA, BtB], [BX])
            tt(tC[:], Z[:, 0, :, :], w[:, 1, :, :], ALU.mult, [BZ, Bw], [BtC])
            tt(tD[:], Z[:, 1, :, :], w[:, 0, :, :], ALU.mult, [BZ, Bw], [BtD])
            pt(X[:, 1, :, :], tC[:], tD[:], ALU.add, [BtC, BtD], [BX])
            if ci == 0:
                T.dbg(c, "dbg_s5X", X[:], (128, 2, 8, L), BF16, BX)
            stash[ci] = (u_, Bu, X, BX, t0)

        def s5_b(ci):
            u_, Bu, X, BX, t0 = stash.pop(ci)
            for half in range(2):
                yp, Byp = Yps.next()
                for mm_ in range(4):
                    m = half * 4 + mm_
                    c.mm(yp[:, 0:L], Cm[:, 0, m, :], X[:, 0, m, :], mm_ == 0, False, [BCm, BX], [Byp])
                    c.mm(yp[:, 0:L], Cm[:, 1, m, :], X[:, 1, m, :], False, mm_ == 3, [BCm, BX], [Byp])
                dv(lambda: nc.vector.scalar_tensor_tensor(out=zt[:, half, :], in0=u_[:, half, :], scalar=dcl[:, half:half + 1],
                                                          in1=yp[:, 0:L], op0=ALU.mult, op1=ALU.add),
                   [Bu, Bdcl, Byp], [Bzt])
            tt(u2[:], zt[:], zt[:], ALU.mult, [Bzt], [Bu2])
            dv(lambda: nc.vector.tensor_scalar(out=u2[:], in0=u2[:], scalar1=0.044715, scalar2=1.0, op0=ALU.mult,
                                               op1=ALU.add), [Bu2], [Bu2])
            tt(u2[:], u2[:], zt[:], ALU.mult, [Bu2, Bzt], [Bu2])
            c.op("act", lambda: nc.scalar.activation(out=u2[:], in_=u2[:], func=AF.Sigmoid, scale=1.5957691216057308),
                 [Bu2], [Bu2])
            tt(hg[:], u2[:], zt[:], ALU.mult, [Bu2, Bzt], [Bhg])
            c.op("act", lambda: nc.scalar.copy(out=hgb[:], in_=hg[:]), [Bhg], [Bhgb])
            for h2 in range(2):
                gp, Bgp = Yps.next()
                for kt_ in range(2):
                    c.mm(gp[:, 0:L], gw[:, kt_, h2 * 128:(h2 + 1) * 128], hgb[:, kt_, :], kt_ == 0, kt_ == 1, [Bgw, Bhgb], [Bgp])
                c.op("act", lambda: nc.scalar.activation(out=sgt[:, h2, :], in_=gp[:, 0:L], func=AF.Sigmoid,
                                                         bias=gbc[:, h2:h2 + 1]), [Bgp, Bgbc], [Bsgt])
            tt(ot[:], hg[:], sgt[:], ALU.mult, [Bhg, Bsgt], [Bot])
            tp_, Btp = Tpf.next()
            for ts_ in range(L // 128):
                for h2 in range(2):
                    c.op("pe", lambda: nc.tensor.transpose(out=tp_[:, ts_ * 256 + h2 * 128:ts_ * 256 + (h2 + 1) * 128],
                                                           in_=ot[:, h2, ts_ * 128:(ts_ + 1) * 128], identity=identf[:]),
                         [Bot, Bidf], [Btp])
            om, Bom = otm.next()
            c.copy(om[:], tp_[:, 0:(L // 128) * 256].rearrange("p (a e) -> p a e", e=256), [Btp], [Bom])
            c.dma(T["mix"][t0:t0 + L, 768:1024].rearrange("(a p) e -> p a e", p=128), om[:], reads=[Bom],
                  writes=[T.B["mix"]])

        for ci in range(NCH + 1):
            if ci < NCH:
                s5_a(ci)
            if ci >= 1:
                s5_b(ci - 1)


def norm_tile(c, x_t, Bx, gB, BgB, h_, Bh, s_, Bs, jk, Bjk, ngroups=1):
    nc = c.nc
    Wd = D // ngroups
    for g in range(ngroups):
        gs = slice(g * Wd, (g + 1) * Wd)
        c.op("act", lambda: nc.scalar.activation(out=jk[:, gs], in_=x_t[:, gs], func=AF.Square,
                                                 accum_out=s_[:, g:g + 1]), [Bx], [Bjk, Bs])
    c.op("act", lambda: nc.scalar.activation(out=s_[:, 4:4 + ngroups], in_=s_[:, 0:ngroups], func=AF.Sqrt,
                                             bias=gB[:, D:D + 1], scale=1.0 / Wd), [Bs, BgB], [Bs])
    c.op("dve", lambda: nc.vector.reciprocal(out=s_[:, 4:4 + ngroups], in_=s_[:, 4:4 + ngroups]), [Bs], [Bs])
    for g in range(ngroups):
        gs = slice(g * Wd, (g + 1) * Wd)
        c.op("dve", lambda: nc.vector.scalar_tensor_tensor(out=h_[:, gs], in0=x_t[:, gs], scalar=s_[:, 4 + g:5 + g],
                                                           in1=gB[:, gs], op0=ALU.mult, op1=ALU.mult),
             [Bx, Bs, BgB], [Bh])


def phase_out(c, T, l):
    nc = c.nc
    x_d = T["xin%d" % l]
    Bx_d = T.B["xin%d" % l]
    with scope(c) as es:
        W, BW = sb(nc, es, "po_W", [128, 8, D], BF16)
        ident, Bid = sb(nc, es, "po_id", [128, 128], BF16)
        c.dma(ident[:], T["ident"][:, :], reads=[T.B["ident"]], writes=[Bid])
        gB, BgB = load_gB(c, es, T["out_norm_g"][l], T.B["out_norm_g"], "po_g")
        wv = T["w_out"][l].rearrange("(kc p) n -> p kc n", p=128)
        for kc in range(8):
            c.dma(W[:, kc, :], wv[:, kc, :], reads=[T.B["w_out"]], writes=[BW], q="pool")
        mt = Rot(nc, es, "po_mt", [128, D], F32, 4)
        xt = Rot(nc, es, "po_xt", [128, D], F32, 5)
        jkr = Rot(nc, es, "po_jk", [128, D], BF16, 2)
        hb = Rot(nc, es, "po_hb", [128, D], BF16, 2)
        ss = Rot(nc, es, "po_ss", [128, 8], F32, 4)
        tp = Rot(nc, es, "po_tp", [128, 1024], BF16, 2, psum=True)
        hTr = Rot(nc, es, "po_hT", [128, 8, 128], BF16, 3)
        acc = Rot(nc, es, "po_acc", [128, 512], F32, 2, psum=True)
        xo = Rot(nc, es, "po_xo", [128, D], F32, 2)
        PF = 2
        loaded = {}
        staged = {}

        def load(t):
            rows = slice(t * 128, (t + 1) * 128)
            m_, Bm = mt.next()
            c.dma(m_[:], T["mix"][rows, :], reads=[T.B["mix"]], writes=[Bm])
            x_, Bx = xt.next()
            c.dma(x_[:], x_d[rows, :], reads=[Bx_d], writes=[Bx])
            loaded[t] = (m_, Bm, x_, Bx)

        def stage_a(t):
            m_, Bm, x_, Bx = loaded.pop(t)
            jk, Bjk = jkr.next()
            s_, Bs = ss.next()
            h_, Bh = hb.next()
            norm_tile(c, m_, Bm, gB, BgB, h_, Bh, s_, Bs, jk, Bjk, ngroups=4)
            hT, BhT = hTr.next()
            for g in range(2):
                p_, Bp = tp.next()
                for j in range(4):
                    kc = g * 4 + j
                    c.op("pe", lambda: nc.tensor.transpose(out=p_[:, j * 128:(j + 1) * 128],
                                                           in_=h_[:, kc * 128:(kc + 1) * 128],
                                                           identity=ident[:]), [Bh, Bid], [Bp])
                c.copy(hT[:, g * 4:(g + 1) * 4, :], p_[:, 0:512].rearrange("p (j t) -> p j t", j=4), [Bp], [BhT])
            staged[t] = (hT, BhT, x_, Bx)

        def stage_c(t):
            rows = slice(t * 128, (t + 1) * 128)
            hT, BhT, x_, Bx = staged.pop(t)
            o_, Bo = xo.next()
            for dc in range(2):
                ds_ = slice(dc * 512, (dc + 1) * 512)
                a_, Ba = acc.next()
                for k in range(8):
                    c.mm(a_[:, :], hT[:, k, :], W[:, k, ds_], k == 0, k == 7, [BW, BhT], [Ba])
                c.op("dve", lambda: nc.vector.tensor_tensor(out=o_[:, ds_], in0=a_[:, :], in1=x_[:, ds_], op=ALU.add),
                     [Ba, Bx], [Bo])
            c.dma(T["xmid"][rows, :], o_[:], reads=[Bo], writes=[T.B["xmid"]])

        for t in range(min(PF, NT)):
            load(t)
        for t in range(NT + 1):
            if t + PF < NT:
                load(t + PF)
            if t < NT:
                stage_a(t)
            if t >= 1:
                stage_c(t - 1)
    c.barrier()


FFN_TB = 1024


def phase_ffn(c, T, l, out_name):
    nc = c.nc
    moe = (l % 2 == 1)
    i = l // 2
    if moe:
        F = D_FFE
        experts = [(T["moe_w_gate"][i, e], T["moe_w_up"][i, e], T["moe_w_down"][i, e]) for e in range(N_EXP)]
        BWs = (T.B["moe_w_gate"], T.B["moe_w_up"], T.B["moe_w_down"])
    else:
        F = D_FF
        experts = [(T["ffn_w_gate"][i], T["ffn_w_up"][i], T["ffn_w_down"][i])]
        BWs = (T.B["ffn_w_gate"], T.B["ffn_w_up"], T.B["ffn_w_down"])
    TB = FFN_TB
    NTB = TB // 128
    FC = 512
    chunks = [(f0, min(FC, F - f0)) for f0 in range(0, F, FC)]
    with scope(c) as es:
        ident, Bid = sb(nc, es, "ff_id", [128, 128], BF16)
        c.dma(ident[:], T["ident"][:, :], reads=[T.B["ident"]], writes=[Bid])
        gB, BgB = load_gB(c, es, T["norm2_g"][l], T.B["norm2_g"], "ff_g")
        if moe:
            Wr, BWr = sb(nc, es, "ff_Wr", [128, 8, N_EXP], BF16)
            c.dma(Wr[:], T["moe_router_w"][i].rearrange("(kc p) e -> p kc e", p=128), reads=[T.B["moe_router_w"]],
                  writes=[BWr], q="pool")
            rb, Brb = sb(nc, es, "ff_rb", [128, N_EXP], F32)
            c.dma(rb[:], T["moe_router_b"][i].partition_broadcast(128), reads=[T.B["moe_router_b"]], writes=[Brb])
            combs = [sb(nc, es, "ff_comb%d" % i_, [128, NTB, N_EXP], F32) for i_ in range(2)]
            lg = Rot(nc, es, "ff_lg", [128, N_EXP], F32, 2)
            mx = Rot(nc, es, "ff_mx", [128, 8], F32, 2)
            ex = Rot(nc, es, "ff_ex", [128, 2 * N_EXP], F32, 2)
        wg = Rot(nc, es, "ff_wg", [128, 8, FC], BF16, 2)
        wu = Rot(nc, es, "ff_wu", [128, 8, FC], BF16, 2)
        wd = Rot(nc, es, "ff_wd", [128, 4, D], BF16, 3)
        Gp = Rot(nc, es, "ff_G", [128, 512], F32, 2, psum=True)
        Up = Rot(nc, es, "ff_U", [128, 512], F32, 2, psum=True)
        Dp = Rot(nc, es, "ff_D", [128, 512], F32, 2, psum=True)
        sg = Rot(nc, es, "ff_sg", [128, 512], F32, 2)
        act = Rot(nc, es, "ff_act", [128, 4, TB], BF16, 3)
        hTs = [sb(nc, es, "ff_hT%d" % i_, [128, 8, TB], BF16) for i_ in range(2)]
        accs = [sb(nc, es, "ff_acc%d" % i_, [128, NTB, D], F32) for i_ in range(2)]
        npools = norm_pools(nc, es, "ffn_", need_xt=False)
        NB = S // TB

        def prep_steps(tb):
            hT, BhT = hTs[tb % 2]
            acc, Bacc = accs[tb % 2]
            steps = norm_transpose_steps(c, npools, T["xmid"], T.B["xmid"], gB, BgB, ident, Bid, hT, BhT, tb * NTB, NTB,
                                         keep_x=(acc, Bacc))
            if moe:
                comb, Bcomb = combs[tb % 2]
                held = {}

                def make_rm(it):
                    def rm():
                        p_, Bp = Dp.next()
                        for k in range(8):
                            c.mm(p_[:, 0:N_EXP], hT[:, k, it * 128:(it + 1) * 128], Wr[:, k, :], k == 0, k == 7,
                                 [BhT, BWr], [Bp])
                        held[it] = (p_, Bp)
                    return rm

                def make_rp(it):
                    def rp():
                        p_, Bp = held.pop(it)
                        l_, Bl = lg.next()
                        c.op("dve", lambda: nc.vector.tensor_tensor(out=l_[:], in0=p_[:, 0:N_EXP], in1=rb[:], op=ALU.add),
                             [Bp, Brb], [Bl])
                        m_, Bm = mx.next()
                        c.op("dve", lambda: nc.vector.max(out=m_[:, 0:8], in_=l_[:]), [Bl], [Bm])
                        e_, Be = ex.next()
                        c.op("dve", lambda: nc.vector.tensor_scalar(out=e_[:, 0:8], in0=l_[:], scalar1=m_[:, 0:1], scalar2=None,
                                                                    op0=ALU.subtract), [Bl, Bm], [Be])
                        c.op("act", lambda: nc.scalar.activation(out=e_[:, 0:8], in_=e_[:, 0:8], func=AF.Exp), [Be], [Be])
                        c.op("dve", lambda: nc.vector.scalar_tensor_tensor(out=e_[:, 8:16], in0=l_[:], scalar=m_[:, 1:2],
                                                                           in1=e_[:, 0:8], op0=ALU.is_ge, op1=ALU.mult),
                             [Bl, Bm, Be], [Be])
                        c.op("dve", lambda: nc.vector.tensor_reduce(out=m_[:, 2:3], in_=e_[:, 8:16], axis=AX.X, op=ALU.add),
                             [Be], [Bm])
                        c.op("dve", lambda: nc.vector.reciprocal(out=m_[:, 3:4], in_=m_[:, 2:3]), [Bm], [Bm])
                        c.op("dve", lambda: nc.vector.tensor_scalar(out=comb[:, it, :], in0=e_[:, 8:16], scalar1=m_[:, 3:4],
                                                                    scalar2=None, op0=ALU.mult), [Be, Bm], [Bcomb])
                    return rp

                steps.append(lambda: None)
                def make_r(it):
                    rm_, rp_ = make_rm(it), make_rp(it)

                    def r():
                        rm_()
                        rp_()
                    return r

                for it in range(NTB):
                    steps.append(make_r(it))
            return steps

        for st_ in prep_steps(0):
            st_()
        pending = []
        for tb in range(NB):
            hT, BhT = hTs[tb % 2]
            acc, Bacc = accs[tb % 2]
            if moe:
                comb, Bcomb = combs[tb % 2]
            n_chunk = 0

            def stage_down(ctx):
                e, a_, Ba, d_, Bd, nft, fw = ctx
                for it in range(NTB):
                    for dc in range(2):
                        ds_ = slice(dc * 512, (dc + 1) * 512)
                        Dd, BD = Dp.next()
                        for ft in range(nft):
                            M = min(128, fw - ft * 128)
                            c.mm(Dd[:, :], a_[0:M, ft, it * 128:(it + 1) * 128], d_[0:M, ft, ds_], ft == 0, ft == nft - 1,
                                 [Ba, Bd], [BD])
                        if moe:
                            c.op("dve", lambda: nc.vector.scalar_tensor_tensor(
                                out=acc[:, it, ds_], in0=Dd[:, :], scalar=comb[:, it, e:e + 1], in1=acc[:, it, ds_],
                                op0=ALU.mult, op1=ALU.add), [BD, Bcomb, Bacc], [Bacc])
                        else:
                            c.op("dve", lambda: nc.vector.tensor_tensor(out=acc[:, it, ds_], in0=Dd[:, :],
                                                                        in1=acc[:, it, ds_], op=ALU.add),
                                 [BD, Bacc], [Bacc])

            prev_ctx = None
            for e, (wg_d, wu_d, wd_d) in enumerate(experts):
                wgv = wg_d.rearrange("(kc p) f -> p kc f", p=128)
                wuv = wu_d.rearrange("(kc p) f -> p kc f", p=128)
                for (f0, fw) in chunks:
                    n_chunk += 1
                    if n_chunk == 2 and tb + 1 < NB:
                        pending = prep_steps(tb + 1)
                    nfull = fw // 128
                    rem = fw - nfull * 128
                    nft = nfull + (1 if rem else 0)
                    g_, Bg = wg.next()
                    c.dma(g_[:, :, 0:fw], wgv[:, :, f0:f0 + fw], reads=[BWs[0]], writes=[Bg], q="pool")
                    u_, Bu = wu.next()
                    c.dma(u_[:, :, 0:fw], wuv[:, :, f0:f0 + fw], reads=[BWs[1]], writes=[Bu], q="pool")
                    d_, Bd = wd.next()
                    if nfull:
                        c.dma(d_[:, 0:nfull, :], wd_d[f0:f0 + nfull * 128, :].rearrange("(ft p) d -> p ft d", p=128),
                              reads=[BWs[2]], writes=[Bd], q="pool")
                    if rem:
                        c.dma(d_[0:rem, nfull, :], wd_d[f0 + nfull * 128:f0 + fw, :], reads=[BWs[2]], writes=[Bd], q="pool")
                    a_, Ba = act.next()
                    n_inner = 0
                    for ft in range(nft):
                        M = min(128, fw - ft * 128)
                        fs = slice(ft * 128, ft * 128 + M)
                        for tt in range(TB // 512):
                            ts_ = slice(tt * 512, (tt + 1) * 512)
                            if pending:
                                pending.pop(0)()
                            G, BG = Gp.next()
                            for k in range(8):
                                c.mm(G[0:M, :], g_[:, k, fs], hT[:, k, ts_], k == 0, k == 7, [Bg, BhT], [BG])
                            U, BU = Up.next()
                            for k in range(8):
                                c.mm(U[0:M, :], u_[:, k, fs], hT[:, k, ts_], k == 0, k == 7, [Bu, BhT], [BU])
                            s_, Bs = sg.next()
                            c.op("act", lambda: nc.scalar.activation(out=s_[0:M, :], in_=G[0:M, :], func=AF.Silu), [BG], [Bs])
                            c.op("dve", lambda: nc.vector.tensor_tensor(out=a_[0:M, ft, ts_], in0=s_[0:M, :], in1=U[0:M, :],
                                                                        op=ALU.mult), [Bs, BU], [Ba])
                            n_inner += 1
                            if n_inner == 1 and prev_ctx is not None:
                                stage_down(prev_ctx)
                                prev_ctx = None
                    prev_ctx = (e, a_, Ba, d_, Bd, nft, fw)
            stage_down(prev_ctx)
            while pending:
                pending.pop(0)()
            c.dma(T[out_name][tb * TB:(tb + 1) * TB, :].rearrange("(i p) d -> p i d", p=128), acc[:], reads=[Bacc],
                  writes=[T.B[out_name]])
            c.barrier()
    c.barrier()


class Tensors:
    def __init__(self, nc, ext_in=(), ext_out=()):
        self.nc = nc
        self.t = {}
        self.B = {}
        self.ext_in = set(ext_in)
        self.ext_out = set(ext_out)
        self.in_names = []
        self.out_names = []

    def add(self, name, shape, dtype, kind=None):
        if kind is None:
            if name in self.ext_in:
                kind = "ExternalInput"
            elif name in self.ext_out:
                kind = "ExternalOutput"
            else:
                kind = "Internal"
        if kind == "ExternalInput":
            self.in_names.append(name)
        if kind == "ExternalOutput":
            self.out_names.append(name)
        self.t[name] = self.nc.dram_tensor(name, list(shape), dtype, kind=kind).ap()
        self.B[name] = Buf(name)
        return self.t[name]

    def __getitem__(self, k):
        return self.t[k]

    def dbg(self, c, name, ap, shape, dtype, Bsrc):
        if name not in self.ext_out:
            return
        d = self.add(name, shape, dtype, kind="ExternalOutput")
        c.dma(d, ap, reads=[Bsrc], writes=[self.B[name]])


PARAM_SHAPES = {
    "norm1_g": (2, 1024), "w_in": (2, 1024, 2460), "dil_qk_g": (2, 2, 64), "nsa_qk_g": (2, 4, 64),
    "nsa_cmp_pos": (2, 2, 32, 64), "nsa_cmp_w1": (2, 2, 2048, 256), "nsa_cmp_w2": (2, 2, 256, 64),
    "gla_wa2": (2, 16, 128), "gla_ba": (2, 128), "gla_norm_g": (2, 64),
    "s5_a_re": (2, 16, 64), "s5_a_im": (2, 16, 64), "s5_b_re": (2, 16, 64, 16), "s5_b_im": (2, 16, 64, 16),
    "s5_c_re": (2, 16, 16, 64), "s5_c_im": (2, 16, 16, 64), "s5_d": (2, 256), "s5_log_dt": (2, 16),
    "s5_glu_w": (2, 256, 256), "s5_glu_b": (2, 256), "out_norm_g": (2, 1024), "w_out": (2, 1024, 1024),
    "norm2_g": (2, 1024), "ffn_w_gate": (1, 1024, 2752), "ffn_w_up": (1, 1024, 2752),
    "ffn_w_down": (1, 2752, 1024), "moe_router_w": (1, 1024, 8), "moe_router_b": (1, 8),
    "moe_w_gate": (1, 8, 1024, 3584), "moe_w_up": (1, 8, 1024, 3584), "moe_w_down": (1, 8, 3584, 1024),
}


def host_consts():
    cst = {}
    bf = ml_dtypes.bfloat16
    cst["ident"] = np.eye(128, dtype=np.float32).astype(bf)
    p = np.arange(S)
    loc = np.arange(128).astype(np.float32)
    KBd = np.zeros((4, 36, 128), np.float32)
    for di in range(36):
        KBd[0, di] = 128.0 * (di - 32)
        KBd[1, di] = loc
        KBd[2, di] = 1.0
        KBd[3, di] = 1.0
    cst["c_KBd"] = KBd.astype(bf)
    QB = np.zeros((4, 3, 4, 128), np.float32)
    for pi, (win, d) in enumerate(DIL_PATTERNS):
        for h in range(4):
            a = DIL_SLOPES[h] * d
            QB[0, pi, h] = a
            QB[1, pi, h] = a
            QB[2, pi, h] = 0.0
            QB[3, pi, h] = -a * loc
    cst["c_QBdil"] = QB.astype(bf)
    kl = np.arange(128)[:, None]
    ql = np.arange(128)[None, :]
    masks = np.stack([(kl <= ql), (kl >= ql), (kl > ql)], axis=1).astype(np.float32)
    cst["c_masks"] = ((masks - 1.0) * 30000.0).astype(bf)
    cst["c_mask01"] = (kl <= ql).astype(np.float32).astype(bf)
    KBS = np.zeros((68, S), np.float32)
    KBS[p // 64, p] = 32768.0
    KBS[64] = 128.0 * (p // 128)
    KBS[65] = p % 128
    KBS[66] = 1.0
    KBS[67] = 1.0
    cst["c_KBS"] = KBS.astype(bf)
    QBa = np.zeros((4, 4, S), np.float32)
    for h in range(4):
        a = NSA_SLOPES[h]
        QBa[0, h] = a
        QBa[1, h] = a
        QBa[2, h] = -a * 128.0 * (p // 128)
        QBa[3, h] = -a * (p % 128)
    cst["c_QBabs"] = QBa.astype(bf)
    KBc = np.zeros((4, 32, 128), np.float32)
    for m in range(32):
        KBc[0, m] = -128.0 * m
        KBc[1, m] = 16.0 * loc
        KBc[2, m] = 31.0
        KBc[3, m] = 1.0
    cst["c_KBc"] = KBc.astype(bf)
    QBc = np.zeros((4, 4, 128), np.float32)
    for h in range(4):
        a = NSA_SLOPES[h]
        QBc[0, h] = a
        QBc[1, h] = a
        QBc[2, h] = a
        QBc[3, h] = -a * loc
    cst["c_QBc"] = QBc.astype(bf)
    cm = np.zeros((128, 17, 128), np.float32)
    for m in range(17):
        valid = (ql - 16 * kl) >= (31 - 128 * m)
        cm[:, m, :] = np.where(valid, 0.0, -30000.0)
    cst["c_cmask"] = cm.astype(bf)
    cc = np.arange(256)
    jj = np.arange(64)
    cover = ((16 * cc[:, None] < 64 * jj[None, :] + 64) & (16 * cc[:, None] + 32 > 64 * jj[None, :])).astype(np.float32)
    cover[255] = 0.0
    cst["c_cover"] = np.ascontiguousarray(cover.reshape(2, 128, 64).transpose(1, 0, 2)).astype(bf)
    tt = (128 * np.arange(32)[None, :, None] + np.arange(128)[:, None, None])
    cur = tt // 64
    j3 = jj[None, None, :]
    forced = (j3 == 0) | (j3 == cur) | (j3 == cur - 1)
    adjv = np.where(j3 <= cur, np.where(forced, 1.0e4, 0.0), -1.0e30).astype(np.float32)
    cst["c_adj"] = np.ascontiguousarray(adjv)
    cst["identf"] = np.eye(128, dtype=np.float32)
    K2s = np.zeros((64, S), np.float32)
    K2s[p // 64, p] = 1.0
    cst["c_K2sel"] = K2s.astype(bf)
    K2w = np.zeros((64, S), np.float32)
    K2w[p // 128, p] = 1.0
    cst["c_K2win"] = K2w.astype(bf)
    QW = np.zeros((64, 4, S), np.float32)
    for h in range(4):
        QW[0:32, h, :] = NSA_SLOPES[h] * 128.0 * (np.arange(32)[:, None] - (p // 128)[None, :])
    cst["c_QW"] = QW.astype(bf)
    As = np.zeros((128, 32, 4), np.float32)
    for h in range(4):
        As[64:128, :, h] = NSA_SLOPES[h] * 64.0 * (np.arange(64)[:, None] - 2 * np.arange(32)[None, :] - 1) + 30000.0
    cst["c_Asel"] = As
    Ev = np.zeros((128, 2, 4), np.float64)
    for h in range(4):
        Ev[:, 0, h] = np.exp(NSA_SLOPES[h] * (np.arange(128) % 64))
        Ev[:, 1, h] = np.exp(NSA_SLOPES[h] * np.arange(128))
    cst["c_Ev"] = Ev.astype(np.float32)
    pp_ = np.arange(128)[:, None, None]
    mm_ = np.arange(8)[None, :, None]
    ch_ = np.arange(128)[None, None, :]
    cst["c_s5mask"] = ((ch_ // 16) == (2 * (mm_ % 4) + pp_ // 64)).astype(np.float32)
    return cst


CONST_SPECS = {"ident": ((128, 128), BF16), "c_KBd": ((4, 36, 128), BF16), "c_QBdil": ((4, 3, 4, 128), BF16),
               "c_masks": ((128, 3, 128), BF16), "c_KBS": ((68, S), BF16), "c_QBabs": ((4, 4, S), BF16),
               "c_KBc": ((4, 32, 128), BF16), "c_QBc": ((4, 4, 128), BF16), "c_cmask": ((128, 17, 128), BF16),
               "c_cover": ((128, 2, 64), BF16), "c_mask01": ((128, 128), BF16), "c_adj": ((128, 32, 64), F32),
               "identf": ((128, 128), F32), "c_s5mask": ((128, 8, 128), F32),
               "c_K2sel": ((64, S), BF16), "c_K2win": ((64, S), BF16), "c_QW": ((64, 4, S), BF16),
               "c_Asel": ((128, 32, 4), F32), "c_Ev": ((128, 2, 4), F32)}


def build(phases=("proj",), layers=(0, 1), ext_in=(), ext_out=()):
    nc = bass.Bass("TRN2", target_bir_lowering=False)
    _UID[0] = 0
    T = Tensors(nc, ext_in, ext_out)
    T.add("xin0", (S, D), F32, kind="ExternalInput")
    for n, shp in PARAM_SHAPES.items():
        T.add(n, shp, F32, kind="ExternalInput")
    for n, (shp, dt_) in CONST_SPECS.items():
        T.add(n, shp, dt_, kind="ExternalInput")
    T.add("projT", (PW, S), BF16)
    T.add("proj", (S, PW), BF16)
    T.add("xin1", (S, D), F32)
    T.add("dacc", (3, S, 264), F32)
    T.add("mix", (S, D), F32)
    T.add("xmid", (S, D), F32)
    T.add("xin2", (S, D), F32, kind="ExternalOutput")
    c = Ctx(nc)
    for l in layers:
        if "proj" in phases:
            phase_proj(c, T, l)
        if "dil" in phases:
            phase_dil(c, T, l)
        if "nsa" in phases:
            phase_nsa(c, T, l)
        if "gla" in phases:
            phase_gla(c, T, l)
        if "s5" in phases:
            phase_s5(c, T, l)
        if "out" in phases:
            phase_out(c, T, l)
        if "ffn" in phases:
            phase_ffn(c, T, l, "xin%d" % (l + 1))
    c.barrier()
    return nc, T


ALL_PHASES = ("proj", "dil", "nsa", "gla", "s5", "out", "ffn")


def kernel(**inputs):
    nc, T = build(phases=ALL_PHASES, layers=(0, 1))
    cst = host_consts()
    x = np.asarray(inputs["x"], dtype=np.float32)
    shared = {}
    for n in T.in_names:
        if n == "xin0":
            continue
        if n in cst:
            shared[n] = cst[n]
        else:
            shared[n] = np.ascontiguousarray(np.asarray(inputs[n], dtype=np.float32))
    maps = []
    for b in range(8):
        m = dict(shared)
        m["xin0"] = np.ascontiguousarray(x[b])
        maps.append(m)
    res = run_bass_kernel_spmd(nc, maps, core_ids=list(range(8)))
    return np.stack([np.asarray(r["xin2"], dtype=np.float32) for r in res.results], axis=0)
```

```python
from contextlib import ExitStack, contextmanager
import math
import numpy as np
import ml_dtypes
import concourse.bass as bass
import concourse.mybir as mybir
from concourse.bass_utils import run_bass_kernel_spmd

F32 = mybir.dt.float32
BF16 = mybir.dt.bfloat16
ALU = mybir.AluOpType
AF = mybir.ActivationFunctionType
AX = mybir.AxisListType

S = 4096
D = 1024
NT = S // 128
PW = 2460
EPS = 1e-6
D_FF = 2752
N_EXP = 8
D_FFE = 3584
OFF = {}
_o = 0
for _n, _w in (("dil_q", 256), ("dil_k", 256), ("dil_v", 256), ("nsa_q", 256),
               ("nsa_k_cmp", 64), ("nsa_v_cmp", 64), ("nsa_k_slc", 64), ("nsa_v_slc", 64),
               ("nsa_k_win", 64), ("nsa_v_win", 64), ("nsa_gate", 12),
               ("gla_q", 128), ("gla_k", 128), ("gla_v", 256), ("gla_a", 16), ("gla_r", 256),
               ("s5_u", 256)):
    OFF[_n] = _o
    _o += _w
assert _o == PW

NDMA = 24
SEM_LIMIT = 24000

FM_RANGES = [(0, 512), (768, 1216), (1280, 1344), (1420, 1676), (1932, 1948), (2204, 2460)]
TM_RANGES = [(512, 256), (1216, 64), (1344, 76), (1676, 256), (1948, 256)]
TM_STORES = [(512, 256), (1216, 64), (1344, 76), (1676, 256), (1948, 256)]
FM_TILES = []
for _a, _b in FM_RANGES:
    _c = _a
    while _c < _b:
        FM_TILES.append((_c, min(128, _b - _c)))
        _c += 128


class Buf:
    __slots__ = ("name", "w", "r")

    def __init__(self, name=""):
        self.name = name
        self.w = {}
        self.r = {}


class Ctx:
    ENG = ("pe", "act", "dve", "pool", "sp")

    def __init__(self, nc):
        self.nc = nc
        self.eng = {"pe": nc.tensor, "act": nc.scalar, "dve": nc.vector,
                    "pool": nc.gpsimd, "sp": nc.sync}
        self.gen = {e: 0 for e in self.ENG}
        self.sem = {e: nc.alloc_semaphore("s_%s_0" % e) for e in self.ENG}
        self.tick = {e: 0 for e in self.ENG}
        self.seen = {e: {} for e in self.ENG}
        self.dsem = [nc.alloc_semaphore("s_dma_%d" % i) for i in range(NDMA)]
        self.dval = [0] * NDMA
        self.dn = 0
        self.dgen = 0
        self.strict = {"act", "dve", "pool"}
        self.n_ops = 0
        self.rr = 0
        self.last_pool = None

    def _need(self, e, waits, key, val, same_ok):
        if key[0] == "e" and key[1] == e and not same_ok:
            return
        if self.seen[e].get(key, 0) >= val:
            return
        if waits.get(key, 0) < val:
            waits[key] = val

    def _ekey(self, e):
        return ("e", e, self.gen[e])

    def _emit_waits(self, e, waits):
        eng = self.eng[e]
        for key, val in waits.items():
            if key[0] == "d":
                if key[2] != self.dgen:
                    continue
                s = self.dsem[key[1]]
            else:
                if key[2] != self.gen[key[1]]:
                    continue
                s = self.sem[key[1]]
            eng.wait_ge(s, val)
            self.seen[e][key] = val

    def op(self, e, fn, reads=(), writes=()):
        waits = {}
        st = e in self.strict
        for b in reads:
            for key, val in b.w.items():
                self._need(e, waits, key, val, st)
        for b in writes:
            for key, val in b.w.items():
                self._need(e, waits, key, val, st)
            for key, val in b.r.items():
                self._need(e, waits, key, val, False)
        self._emit_waits(e, waits)
        ins = fn()
        self.tick[e] += 1
        ins.then_inc(self.sem[e], 1)
        key = self._ekey(e)
        val = self.tick[e]
        for b in reads:
            b.r[key] = val
        for b in writes:
            b.w[key] = val
            b.r = {}
        self.n_ops += 1
        return ins

    def dma(self, out, in_, reads=(), writes=(), q="sp", **kw):
        e = q
        waits = {}
        for b in reads:
            for key, val in b.w.items():
                self._need(e, waits, key, val, True)
        for b in writes:
            for key, val in b.w.items():
                if key[0] == "d":
                    continue
                self._need(e, waits, key, val, True)
            for key, val in b.r.items():
                self._need(e, waits, key, val, True)
        i = self.dn % NDMA
        self.dn += 1
        dkey = ("d", i, self.dgen)
        if self.dval[i] > 0:
            self._need(e, waits, dkey, self.dval[i], True)
        if e == "pool" and self.last_pool is not None:
            self._need(e, waits, self.last_pool[0], self.last_pool[1], True)
        self._emit_waits(e, waits)
        ins = self.eng[e].dma_start(out=out, in_=in_, **kw)
        self.dval[i] += 16
        ins.then_inc(self.dsem[i], 16)
        if e == "pool":
            self.last_pool = (dkey, self.dval[i])
        for b in reads:
            b.r[dkey] = self.dval[i]
        for b in writes:
            b.w[dkey] = self.dval[i]
        return ins

    def barrier(self):
        for e in self.ENG:
            waits = {}
            for o in self.ENG:
                if o != e and self.tick[o] > 0:
                    self._need(e, waits, self._ekey(o), self.tick[o], True)
            for i in range(NDMA):
                if self.dval[i] > 0:
                    self._need(e, waits, ("d", i, self.dgen), self.dval[i], True)
            self._emit_waits(e, waits)
        for e in self.ENG:
            if self.tick[e] > SEM_LIMIT:
                self.gen[e] += 1
                self.sem[e] = self.nc.alloc_semaphore("s_%s_%d" % (e, self.gen[e]))
                self.tick[e] = 0
        if any(v > SEM_LIMIT for v in self.dval):
            self.dgen += 1
            self.dsem = [self.nc.alloc_semaphore("s_dma_%d_g%d" % (i, self.dgen)) for i in range(NDMA)]
            self.dval = [0] * NDMA

    def evac_engine(self):
        self.rr += 1
        return "act" if (self.rr & 1) else "dve"

    def copy(self, out, in_, reads, writes, e=None):
        nc = self.nc
        e = e or self.evac_engine()
        if e == "act":
            return self.op("act", lambda: nc.scalar.copy(out=out, in_=in_), reads, writes)
        if e == "pool":
            return self.op("pool", lambda: nc.gpsimd.tensor_copy(out=out, in_=in_), reads, writes)
        return self.op("dve", lambda: nc.vector.tensor_copy(out=out, in_=in_), reads, writes)

    def mm(self, out, lhsT, rhs, start, stop, reads, writes, **kw):
        nc = self.nc
        return self.op("pe", lambda: nc.tensor.matmul(out, lhsT=lhsT, rhs=rhs, start=start, stop=stop, **kw),
                       reads, writes)


@contextmanager
def scope(c):
    with ExitStack() as es:
        yield es
        c.barrier()


_UID = [0]


def uniq(name):
    _UID[0] += 1
    return "%s_u%d" % (name, _UID[0])


class Rot:
    def __init__(self, nc, es, name, shape, dtype, n, psum=False):
        self.t = []
        self.b = []
        name = uniq(name)
        for i in range(n):
            nm = "%s_%d" % (name, i)
            if psum:
                t = es.enter_context(nc.psum_tensor(nm, shape, dtype))
            else:
                t = es.enter_context(nc.sbuf_tensor(nm, shape, dtype))
            self.t.append(t)
            self.b.append(Buf(nm))
        self.i = 0

    def next(self):
        k = self.i % len(self.t)
        self.i += 1
        return self.t[k], self.b[k]


def sb(nc, es, name, shape, dtype):
    name = uniq(name)
    return es.enter_context(nc.sbuf_tensor(name, shape, dtype)), Buf(name)


def ps(nc, es, name, shape, dtype):
    name = uniq(name)
    return es.enter_context(nc.psum_tensor(name, shape, dtype)), Buf(name)


def norm_pools(nc, es, pfx, need_xt=True):
    xt = Rot(nc, es, pfx + "xt", [128, D], F32, 2) if need_xt else None
    junk = Rot(nc, es, pfx + "jk", [128, D], BF16, 2)
    hb = Rot(nc, es, pfx + "hb", [128, D], BF16, 2)
    ss = Rot(nc, es, pfx + "ss", [128, 2], F32, 4)
    tp = Rot(nc, es, pfx + "tp", [128, 1024], BF16, 2, psum=True)
    return xt, junk, hb, ss, tp


def norm_transpose_steps(c, pools, x_d, Bx_d, gB, BgB, ident, Bid, hT, BhT, tile0, ntiles, keep_x=None):
    nc = c.nc
    xt, junk, hb, ss, tp = pools
    held = {}

    def make_n(i):
        def n_step():
            t = tile0 + i
            if keep_x is not None:
                x_t = keep_x[0][:, i, :]
                Bx = keep_x[1]
            else:
                xx, Bx = xt.next()
                x_t = xx[:]
            c.dma(x_t, x_d[t * 128:(t + 1) * 128, :], reads=[Bx_d], writes=[Bx])
            jk, Bjk = junk.next()
            s_, Bs = ss.next()
            c.op("act", lambda: nc.scalar.activation(out=jk[:], in_=x_t, func=AF.Square,
                                                     accum_out=s_[:, 0:1]), [Bx], [Bjk, Bs])
            c.op("act", lambda: nc.scalar.activation(out=s_[:, 1:2], in_=s_[:, 0:1], func=AF.Sqrt,
                                                     bias=gB[:, D:D + 1], scale=1.0 / D), [Bs, BgB], [Bs])
            c.op("dve", lambda: nc.vector.reciprocal(out=s_[:, 1:2], in_=s_[:, 1:2]), [Bs], [Bs])
            h_, Bh = hb.next()
            c.op("dve", lambda: nc.vector.scalar_tensor_tensor(out=h_[:], in0=x_t, scalar=s_[:, 1:2],
                                                               in1=gB[:, 0:D], op0=ALU.mult, op1=ALU.mult),
                 [Bx, Bs, BgB], [Bh])
            held[i] = (h_, Bh)
        return n_step

    def make_t(i):
        def t_step():
            h_, Bh = held.pop(i)
            for g in range(2):
                p_, Bp = tp.next()
                for j in range(4):
                    kc = g * 4 + j
                    c.op("pe", lambda: nc.tensor.transpose(out=p_[:, j * 128:(j + 1) * 128],
                                                           in_=h_[:, kc * 128:(kc + 1) * 128],
                                                           identity=ident[:]), [Bh, Bid], [Bp])
                c.copy(hT[:, g * 4:(g + 1) * 4, i * 128:(i + 1) * 128],
                       p_[:, 0:512].rearrange("p (j t) -> p j t", j=4), [Bp], [BhT])
        return t_step

    steps = []
    for i in range(ntiles):
        steps.append(make_n(i))
        if i >= 1:
            steps.append(make_t(i - 1))
    steps.append(make_t(ntiles - 1))
    return steps


def norm_transpose(c, es, x_d, Bx_d, gB, BgB, ident, Bid, hT, BhT, tile0, ntiles, pfx,
                   keep_x=None, pools=None):
    nc = c.nc
    if pools is None:
        pools = norm_pools(nc, es, pfx, keep_x is None)
    for st_ in norm_transpose_steps(c, pools, x_d, Bx_d, gB, BgB, ident, Bid, hT, BhT, tile0, ntiles, keep_x):
        st_()


def load_gB(c, es, g_row, Bg_d, name):
    nc = c.nc
    gB, BgB = sb(nc, es, name, [128, D + 1], F32)
    c.dma(gB[:, 0:D], g_row.partition_broadcast(128), reads=[Bg_d], writes=[BgB])
    c.op("dve", lambda: nc.vector.memset(gB[:, D:D + 1], EPS), [], [BgB])
    return gB, BgB


def phase_proj(c, T, l):
    nc = c.nc
    with scope(c) as es:
        W, BW = sb(nc, es, "pj_W", [128, 8, PW], BF16)
        ident, Bid = sb(nc, es, "pj_id", [128, 128], BF16)
        c.dma(ident[:], T["ident"][:, :], reads=[T.B["ident"]], writes=[Bid])
        gB, BgB = load_gB(c, es, T["norm1_g"][l], T.B["norm1_g"], "pj_g")
        wv = T["w_in"][l].rearrange("(kc p) n -> p kc n", p=128)
        for kc in range(8):
            c.dma(W[:, kc, :], wv[:, kc, :], reads=[T.B["w_in"]], writes=[BW], q="pool")
        hTs = [sb(nc, es, "pj_hT%d" % i, [128, 8, 2048], BF16) for i in range(2)]
        stf = Rot(nc, es, "pj_stf", [128, 2048], BF16, 2)
        stt = Rot(nc, es, "pj_stt", [128, PW], BF16, 2)
        acc = Rot(nc, es, "pj_acc", [128, 512], F32, 4, psum=True)
        x_d = T["xin%d" % l]
        npools = norm_pools(nc, es, "pj_", need_xt=True)

        def half_steps(half):
            return norm_transpose_steps(c, npools, x_d, T.B["xin%d" % l], gB, BgB, ident, Bid, hTs[half][0], hTs[half][1],
                                        half * 16, 16)

        for st_ in half_steps(0):
            st_()
        pending = []
        for half in range(2):
            hT, BhT = hTs[half]
            while pending:
                pending.pop(0)()
            if half == 0:
                pending = half_steps(1)
            for (c0, M) in FM_TILES:
                st, Bst = stf.next()
                for tt in range(4):
                    if pending:
                        pending.pop(0)()
                    a_, Ba = acc.next()
                    for k in range(8):
                        c.mm(a_[0:M, :], W[:, k, c0:c0 + M], hT[:, k, tt * 512:(tt + 1) * 512],
                             k == 0, k == 7, [BW, BhT], [Ba])
                    c.copy(st[0:M, tt * 512:(tt + 1) * 512], a_[0:M, :], [Ba], [Bst])
                c.dma(T["projT"][c0:c0 + M, half * 2048:(half + 1) * 2048], st[0:M, :],
                      reads=[Bst], writes=[T.B["projT"]])
            for i in range(16):
                st, Bst = stt.next()
                t = half * 16 + i
                for (c0, N) in TM_RANGES:
                    a_, Ba = acc.next()
                    for k in range(8):
                        c.mm(a_[:, 0:N], hT[:, k, i * 128:(i + 1) * 128], W[:, k, c0:c0 + N],
                             k == 0, k == 7, [BW, BhT], [Ba])
                    c.copy(st[:, c0:c0 + N], a_[:, 0:N], [Ba], [Bst])
                for (c0, N) in TM_STORES:
                    c.dma(T["proj"][t * 128:(t + 1) * 128, c0:c0 + N], st[:, c0:c0 + N], reads=[Bst],
                          writes=[T.B["proj"]])
    c.barrier()


DIL_SLOPES = [2.0 ** -2, 2.0 ** -4, 2.0 ** -6, 2.0 ** -8]
NSA_SLOPES = [2.0 ** -1, 2.0 ** -3, 2.0 ** -5, 2.0 ** -7]
DIL_PATTERNS = ((128, 1), (512, 4), (2048, 16))


def ssl(start, count, step):
    return slice(start, start + step * (count - 1) + 1, step)


def load_col(c, es, name, vec_ap, Bsrc, n, mul=None):
    nc = c.nc
    t, B = sb(nc, es, name, [n, 1], F32)
    c.dma(t[:, 0:1], vec_ap.rearrange("(p o) -> p o", o=1), reads=[Bsrc], writes=[B])
    if mul is not None:
        c.op("act", lambda: nc.scalar.mul(out=t[:], in_=t[:], mul=mul), [B], [B])
    return t, B


def prep_qk(c, es, T, row0, H, gcol, Bg, ones64, Bones, epsc, Beps, dst, Bdst, pfx):
    nc = c.nc
    raw = Rot(nc, es, pfx + "raw", [64, S], BF16, 2)
    sq = Rot(nc, es, pfx + "sq", [64, 512], BF16, 3)
    rs = Rot(nc, es, pfx + "rs", [64, 512], F32, 2)
    pp = Rot(nc, es, pfx + "pp", [64, 512], F32, 2, psum=True)
    tasks = []
    for h in range(H):
        for tt in range(8):
            tasks.append((h, tt))
    held = {}
    raws = {}

    def st_a(i):
        h, tt = tasks[i]
        if tt == 0:
            r_, Br = raw.next()
            c.dma(r_[:], T["projT"][row0 + h * 64:row0 + (h + 1) * 64, :], reads=[T.B["projT"]], writes=[Br])
            raws[h] = (r_, Br)
        r_, Br = raws[h]
        sl = slice(tt * 512, (tt + 1) * 512)
        q_, Bq = sq.next()
        c.op("act", lambda: nc.scalar.activation(out=q_[:], in_=r_[:, sl], func=AF.Square), [Br], [Bq])
        p_, Bp = pp.next()
        c.mm(p_[:], ones64[:], q_[:], True, True, [Bq, Bones], [Bp])
        held[i] = (p_, Bp)

    def st_b(i):
        h, tt = tasks[i]
        r_, Br = raws[h]
        sl = slice(tt * 512, (tt + 1) * 512)
        p_, Bp = held.pop(i)
        s_, Bs = rs.next()
        c.op("act", lambda: nc.scalar.activation(out=s_[:], in_=p_[:], func=AF.Ln, bias=epsc[0:64, 0:1],
                                                 scale=1.0 / 64), [Bp, Beps], [Bs])
        c.op("act", lambda: nc.scalar.activation(out=s_[:], in_=s_[:], func=AF.Exp, scale=-0.5), [Bs], [Bs])
        c.op("dve", lambda: nc.vector.scalar_tensor_tensor(out=dst[0:64, h, sl], in0=r_[:, sl],
                                                           scalar=gcol[:, 0:1], in1=s_[:],
                                                           op0=ALU.mult, op1=ALU.mult),
             [Br, Bs, Bg], [Bdst])

    for i in range(len(tasks) + 1):
        if i < len(tasks):
            st_a(i)
        if i >= 1:
            st_b(i - 1)


def load_consts_small(c, es, T, pfx):
    nc = c.nc
    ones64, Bones = sb(nc, es, pfx + "ones64", [64, 64], BF16)
    c.op("dve", lambda: nc.vector.memset(ones64[:], 1.0), [], [Bones])
    epsc, Beps = sb(nc, es, pfx + "epsc", [128, 1], F32)
    c.op("dve", lambda: nc.vector.memset(epsc[:], EPS), [], [Beps])
    return ones64, Bones, epsc, Beps


def phase_dil(c, T, l):
    nc = c.nc
    with scope(c) as es:
        ones64, Bones, epsc, Beps = load_consts_small(c, es, T, "dl_")
        qT, BqT = sb(nc, es, "dl_qT", [64, 4, S], BF16)
        kT, BkT = sb(nc, es, "dl_kT", [64, 4, S], BF16)
        KB, BKB = sb(nc, es, "dl_KB", [4, 36, 128], BF16)
        c.dma(KB[:], T["c_KBd"][:, :, :], reads=[T.B["c_KBd"]], writes=[BKB])
        QB, BQB = sb(nc, es, "dl_QB", [4, 3, 4, 128], BF16)
        c.dma(QB[:], T["c_QBdil"][:, :, :, :], reads=[T.B["c_QBdil"]], writes=[BQB])
        msk, Bmsk = sb(nc, es, "dl_msk", [128, 3, 128], BF16)
        c.dma(msk[:], T["c_masks"][:, :, :], reads=[T.B["c_masks"]], writes=[Bmsk])
        Vp = []
        for pi in range(3):
            v_, Bv = sb(nc, es, "dl_V%d" % pi, [128, 32, 4, 66], BF16)
            c.op("dve", lambda: nc.vector.memset(v_[:], 1.0), [], [Bv])
            Vp.append((v_, Bv))
        with scope(c) as es2:
            stg = Rot(nc, es2, "dl_vst", [128, 8, 256], BF16, 2)
            for pi, (win, d) in enumerate(DIL_PATTERNS):
                v_, Bv = Vp[pi]
                bpc = 32 // d
                for r in range(d):
                    for b0 in range(0, bpc, 8):
                        nb = min(8, bpc - b0)
                        s_, Bs = stg.next()
                        src = T["proj"][ssl(r + d * 128 * b0, 128 * nb, d), OFF["dil_v"]:OFF["dil_v"] + 256]
                        c.dma(s_[:, 0:nb, :], src.rearrange("(b k) c -> k b c", k=128),
                              reads=[T.B["proj"]], writes=[Bs])
                        blk0 = r * bpc + b0
                        c.copy(v_[:, blk0:blk0 + nb, :, 0:64],
                               s_[:, 0:nb, :].rearrange("k b (h e) -> k b h e", h=4), [Bs], [Bv])
        with scope(c) as es2:
            gq, Bgq = load_col(c, es2, "dl_gq", T["dil_qk_g"][l, 0], T.B["dil_qk_g"], 64, mul=0.125)
            gk, Bgk = load_col(c, es2, "dl_gk", T["dil_qk_g"][l, 1], T.B["dil_qk_g"], 64)
            with scope(c) as es3:
                prep_qk(c, es3, T, OFF["dil_q"], 4, gq, Bgq, ones64, Bones, epsc, Beps, qT, BqT, "dlq")
            c.barrier()
            with scope(c) as es3:
                prep_qk(c, es3, T, OFF["dil_k"], 4, gk, Bgk, ones64, Bones, epsc, Beps, kT, BkT, "dlk")
        T.dbg(c, "dbg_qT", qT[:], (64, 4, S), BF16, BqT)
        T.dbg(c, "dbg_kT", kT[:], (64, 4, S), BF16, BkT)
        c.barrier()
        for pi in range(3):
            T.dbg(c, "dbg_V%d" % pi, Vp[pi][0][:], (128, 32, 4, 66), BF16, Vp[pi][1])
        Sps = Rot(nc, es, "dl_S", [128, 512], F32, 3, psum=True)
        Ops = Rot(nc, es, "dl_O", [128, 512], F32, 2, psum=True)
        Pt = Rot(nc, es, "dl_P", [128, 512], BF16, 5)
        Mt = Rot(nc, es, "dl_M", [128, 512], F32, 3)
        Osb = Rot(nc, es, "dl_Osb", [128, 264], F32, 3)
        SKEW = 2
        tasks = []
        for pi, (win, d) in enumerate(DIL_PATTERNS):
            bpc = 32 // d
            for r in range(d):
                for bi in range(bpc):
                    kbs = [bi - 1, bi] if bi >= 1 else [bi]
                    for i_, kbi in enumerate(kbs):
                        tasks.append((pi, d, bpc, r, bi, kbi, i_ == 0, i_ == len(kbs) - 1))
        st = {}
        cur_o = [None]

        def stage_a(ti):
            pi, d, bpc, r, bi, kbi, is_first, is_last = tasks[ti]
            qsl = ssl(r + d * 128 * bi, 128, d)
            ksl = ssl(r + d * 128 * kbi, 128, d)
            s_, Bs = Sps.next()
            for h in range(4):
                c.mm(s_[:, h * 128:(h + 1) * 128], kT[:, h, ksl], qT[:, h, qsl], True, False,
                     [BkT, BqT], [Bs])
                c.mm(s_[:, h * 128:(h + 1) * 128], KB[:, 32 + kbi - bi, :],
                     QB[:, pi, h, :], False, True, [BKB, BQB], [Bs])
            p_, Bp = Pt.next()
            mi = 0 if kbi == bi else 1
            m_, Bm = Mt.next()
            c.op("dve", lambda: nc.vector.tensor_tensor(
                out=m_[:].rearrange("k (h q) -> k h q", h=4),
                in0=s_[:].rearrange("k (h q) -> k h q", h=4),
                in1=msk[:, mi:mi + 1, :].to_broadcast([128, 4, 128]), op=ALU.add), [Bs, Bmsk], [Bm])
            c.op("act", lambda: nc.scalar.activation(out=p_[:], in_=m_[:], func=AF.Exp), [Bm], [Bp])
            st[ti] = (p_, Bp)

        def stage_c(ti):
            pi, d, bpc, r, bi, kbi, is_first, is_last = tasks[ti]
            v_, Bv = Vp[pi]
            kblk = r * bpc + kbi
            p_, Bp = st.pop(ti)
            if is_first:
                cur_o[0] = Ops.next()
            o_, Bo = cur_o[0]
            for h in range(4):
                c.mm(o_[:, h * 66:(h + 1) * 66], p_[:, h * 128:(h + 1) * 128], v_[:, kblk, h, :],
                     is_first and h == 0, False, [Bp, Bv], [Bo], skip_group_check=True)
            if is_last:
                ob, Bob = Osb.next()
                c.copy(ob[:], o_[:, 0:264], [Bo], [Bob])
                dst = T["dacc"][pi, ssl(r + d * 128 * bi, 128, d), :]
                c.dma(dst, ob[:], reads=[Bob], writes=[T.B["dacc"]])

        for ti in range(len(tasks) + SKEW):
            if ti < len(tasks):
                stage_a(ti)
            if ti - SKEW >= 0:
                stage_c(ti - SKEW)
    c.barrier()
    with scope(c) as es:
        a3 = Rot(nc, es, "dc_a", [128, 3, 264], F32, 5)
        rc = Rot(nc, es, "dc_r", [128, 4], F32, 3)
        yo = Rot(nc, es, "dc_y", [128, 256], F32, 3)
        PF = 3
        loaded = {}

        def load(t):
            a_, Ba = a3.next()
            c.dma(a_[:], T["dacc"][:, t * 128:(t + 1) * 128, :].rearrange("p t c -> t p c"),
                  reads=[T.B["dacc"]], writes=[Ba])
            loaded[t] = (a_, Ba)

        for t in range(min(PF, NT)):
            load(t)
        for t in range(NT):
            if t + PF < NT:
                load(t + PF)
            a_, Ba = loaded.pop(t)
            c.op("dve", lambda: nc.vector.tensor_tensor(out=a_[:, 0, :], in0=a_[:, 0, :], in1=a_[:, 1, :], op=ALU.add),
                 [Ba], [Ba])
            c.op("dve", lambda: nc.vector.tensor_tensor(out=a_[:, 0, :], in0=a_[:, 0, :], in1=a_[:, 2, :], op=ALU.add),
                 [Ba], [Ba])
            av = a_[:, 0, :].rearrange("t (h e) -> t h e", h=4)
            r_, Br = rc.next()
            c.op("dve", lambda: nc.vector.reciprocal(out=r_[:].rearrange("t (h o) -> t h o", o=1), in_=av[:, :, 64:65]),
                 [Ba], [Br])
            y_, By = yo.next()
            c.op("dve", lambda: nc.vector.tensor_tensor(
                out=y_[:].rearrange("t (h e) -> t h e", h=4), in0=av[:, :, 0:64],
                in1=r_[:].rearrange("t (h o) -> t h o", o=1).to_broadcast([128, 4, 64]), op=ALU.mult),
                [Ba, Br], [By])
            c.dma(T["mix"][t * 128:(t + 1) * 128, 0:256], y_[:], reads=[By], writes=[T.B["mix"]])
    c.barrier()


NSA_STAGE = 99


def phase_nsa(c, T, l):
    _phase_nsa(c, T, l)
    c.barrier()


def _phase_nsa(c, T, l):
    nc = c.nc
    with scope(c) as es:
        ones64, Bones, epsc, Beps = load_consts_small(c, es, T, "ns_")
        ident, Bid = sb(nc, es, "ns_id", [128, 128], BF16)
        c.dma(ident[:], T["ident"][:, :], reads=[T.B["ident"]], writes=[Bid])
        qT, BqT = sb(nc, es, "ns_Q2s", [128, 4, S], BF16)
        Q2w, BQ2w = sb(nc, es, "ns_Q2w", [128, 4, S], BF16)
        c.dma(Q2w[64:128, :, :], T["c_QW"][:, :, :], reads=[T.B["c_QW"]], writes=[BQ2w])
        ksT, BksT = sb(nc, es, "ns_Ks2", [128, 1, S], BF16)
        c.dma(ksT[64:128, 0, :], T["c_K2sel"][:, :], reads=[T.B["c_K2sel"]], writes=[BksT])
        kwT, BkwT = sb(nc, es, "ns_Kw2", [128, 1, S], BF16)
        c.dma(kwT[64:128, 0, :], T["c_K2win"][:, :], reads=[T.B["c_K2win"]], writes=[BkwT])
        Asel, BAsel = sb(nc, es, "ns_Asel", [128, 32, 4], F32)
        c.dma(Asel[:], T["c_Asel"][:, :, :], reads=[T.B["c_Asel"]], writes=[BAsel])
        Vs, BVs = sb(nc, es, "ns_Vsh", [128, 32, 4, 66], BF16)
        Vw, BVw = sb(nc, es, "ns_Vwh", [128, 32, 4, 66], BF16)
        with scope(c) as es2:
            Ev, BEv = sb(nc, es2, "ns_Ev", [128, 2, 4], F32)
            c.dma(Ev[:], T["c_Ev"][:, :, :], reads=[T.B["c_Ev"]], writes=[BEv])
            for vi, (vh_, Bvh, nm) in enumerate(((Vs, BVs, "nsa_v_slc"), (Vw, BVw, "nsa_v_win"))):
                v_, Bv = sb(nc, es2, "ns_Vst%d" % vi, [128, 32, 66], BF16)
                c.op("dve", lambda: nc.vector.memset(v_[:], 1.0), [], [Bv])
                c.dma(v_[:, :, 0:64], T["proj"][:, OFF[nm]:OFF[nm] + 64].rearrange("(b k) e -> k b e", k=128),
                      reads=[T.B["proj"]], writes=[Bv])
                for h in range(4):
                    c.op("dve", lambda: nc.vector.tensor_scalar(out=vh_[:, :, h, :], in0=v_[:], scalar1=Ev[:, vi, h:h + 1],
                                                                scalar2=None, op0=ALU.mult), [Bv, BEv], [Bvh])
        kcT, BkcT = sb(nc, es, "ns_kcT", [64, 256], BF16)
        Vc, BVc = sb(nc, es, "ns_Vc", [128, 2, 130], BF16)
        c.op("dve", lambda: nc.vector.memset(Vc[:], 0.0), [], [BVc])
        c.op("dve", lambda: nc.vector.memset(Vc[:, :, 128:130], 1.0), [], [BVc])
        c.dma(Vc[:, :, 64:128], T["c_cover"][:, :, :], reads=[T.B["c_cover"]], writes=[BVc])
        with scope(c) as es2:
            g0, Bg0 = load_col(c, es2, "ns_g0", T["nsa_qk_g"][l, 0], T.B["nsa_qk_g"], 64, mul=0.125)
            g2, Bg2 = load_col(c, es2, "ns_g2", T["nsa_qk_g"][l, 2], T.B["nsa_qk_g"], 64)
            g3, Bg3 = load_col(c, es2, "ns_g3", T["nsa_qk_g"][l, 3], T.B["nsa_qk_g"], 64)
            with scope(c) as es3:
                prep_qk(c, es3, T, OFF["nsa_q"], 4, g0, Bg0, ones64, Bones, epsc, Beps, qT, BqT, "nsq")
            c.barrier()
            for h in range(4):
                c.op("act", lambda: nc.scalar.copy(out=Q2w[0:64, h, :], in_=qT[0:64, h, :]), [BqT], [BQ2w])
            with scope(c) as es3:
                prep_qk(c, es3, T, OFF["nsa_k_slc"], 1, g2, Bg2, ones64, Bones, epsc, Beps, ksT, BksT, "nss")
            with scope(c) as es3:
                prep_qk(c, es3, T, OFF["nsa_k_win"], 1, g3, Bg3, ones64, Bones, epsc, Beps, kwT, BkwT, "nsw")
            c.barrier()
        c.barrier()
        if NSA_STAGE <= 1:
            return
        with scope(c) as es2:
            g1, Bg1 = load_col(c, es2, "ns_g1", T["nsa_qk_g"][l, 1], T.B["nsa_qk_g"], 64)
            raw, Braw = sb(nc, es2, "ns_raw", [64, 2, S], BF16)
            c.dma(raw[:], T["projT"][OFF["nsa_k_cmp"]:OFF["nsa_k_cmp"] + 128, :].rearrange("(i d) t -> d i t", i=2),
                  reads=[T.B["projT"]], writes=[Braw])
            w1, Bw1 = sb(nc, es2, "ns_w1", [64, 2, 32, 256], BF16)
            w2, Bw2 = sb(nc, es2, "ns_w2", [128, 2, 2, 64], BF16)
            posT, BposT = sb(nc, es2, "ns_posT", [64, 2, 32], BF16)
            for i in range(2):
                c.dma(w1[:, i, :, :], T["nsa_cmp_w1"][l, i].rearrange("(l d) h -> d l h", d=64),
                      reads=[T.B["nsa_cmp_w1"]], writes=[Bw1], q="pool")
                c.dma(w2[:, i, :, :], T["nsa_cmp_w2"][l, i].rearrange("(hc p) e -> p hc e", p=128),
                      reads=[T.B["nsa_cmp_w2"]], writes=[Bw2], q="pool")
                c.dma(posT[:, i, :], T["nsa_cmp_pos"][l, i].rearrange("l d -> d l"),
                      reads=[T.B["nsa_cmp_pos"]], writes=[BposT], q="pool", allow_slow_non_contiguous=True)
            Hps = Rot(nc, es2, "ns_Hps", [128, 512], F32, 2, psum=True)
            bps = Rot(nc, es2, "ns_bps", [128, 512], F32, 2, psum=True)
            b1 = Rot(nc, es2, "ns_b1", [128, 2], F32, 2)
            zt = Rot(nc, es2, "ns_z", [128, 256], F32, 2)
            ut = Rot(nc, es2, "ns_u", [128, 256], F32, 2)
            G, BG = sb(nc, es2, "ns_G", [128, 2, 2, 256], BF16)
            c.op("dve", lambda: nc.vector.memset(G[:], 0.0), [], [BG])
            for i in range(2):
                for hc in range(2):
                    h_, Bh = Hps.next()
                    for ll in range(32):
                        c.mm(h_[:, 0:255], w1[:, i, ll, hc * 128:(hc + 1) * 128], raw[:, i, ssl(ll, 255, 16)],
                             ll == 0, ll == 31, [Bw1, Braw], [Bh])
                    bp, Bbp = bps.next()
                    for ll in range(32):
                        c.mm(bp[:, 0:2], w1[:, i, ll, hc * 128:(hc + 1) * 128], posT[:, i, ll:ll + 1].to_broadcast([64, 2]),
                             ll == 0, ll == 31, [Bw1, BposT], [Bbp])
                    b_, Bb = b1.next()
                    c.op("dve", lambda: nc.vector.tensor_copy(out=b_[:], in_=bp[:, 0:2]), [Bbp], [Bb])
                    z_, Bz = zt.next()
                    c.op("act", lambda: nc.scalar.activation(out=z_[:, 0:255], in_=h_[:, 0:255], func=AF.Identity,
                                                             bias=b_[:, 0:1]), [Bh, Bb], [Bz])
                    u_, Bu = ut.next()
                    c.op("dve", lambda: nc.vector.tensor_tensor(out=u_[:, 0:255], in0=z_[:, 0:255], in1=z_[:, 0:255],
                                                                op=ALU.mult), [Bz], [Bu])
                    c.op("dve", lambda: nc.vector.tensor_scalar(out=u_[:, 0:255], in0=u_[:, 0:255], scalar1=0.044715,
                                                                scalar2=1.0, op0=ALU.mult, op1=ALU.add), [Bu], [Bu])
                    c.op("dve", lambda: nc.vector.tensor_tensor(out=u_[:, 0:255], in0=u_[:, 0:255], in1=z_[:, 0:255],
                                                                op=ALU.mult), [Bu, Bz], [Bu])
                    c.op("act", lambda: nc.scalar.activation(out=u_[:, 0:255], in_=u_[:, 0:255], func=AF.Sigmoid,
                                                             scale=1.5957691216057308), [Bu], [Bu])
                    c.op("dve", lambda: nc.vector.tensor_tensor(out=G[:, i, hc, 0:255], in0=u_[:, 0:255],
                                                                in1=z_[:, 0:255], op=ALU.mult), [Bu, Bz], [BG])
            T.dbg(c, "dbg_w1", w1[:], (64, 2, 32, 256), BF16, Bw1)
            T.dbg(c, "dbg_posT", posT[:], (64, 2, 32), BF16, BposT)
            T.dbg(c, "dbg_G", G[:], (128, 2, 2, 256), BF16, BG)
            T.dbg(c, "dbg_w2", w2[:], (128, 2, 2, 64), BF16, Bw2)
            kp, Bkp = Hps.next()
            for hc in range(2):
                c.mm(kp[0:64, 0:256], w2[:, 0, hc, :], G[:, 0, hc, :], hc == 0, hc == 1, [Bw2, BG], [Bkp])
            kraw, Bkraw = sb(nc, es2, "ns_kraw", [64, 256], F32)
            c.op("dve", lambda: nc.vector.tensor_copy(out=kraw[:], in_=kp[0:64, 0:256]), [Bkp], [Bkraw])
            ksq, Bksq = sb(nc, es2, "ns_ksq", [64, 256], BF16)
            c.op("act", lambda: nc.scalar.activation(out=ksq[:], in_=kraw[:], func=AF.Square), [Bkraw], [Bksq])
            sp_, Bsp = bps.next()
            c.mm(sp_[0:64, 0:256], ones64[:], ksq[:], True, True, [Bksq, Bones], [Bsp])
            krs, Bkrs = sb(nc, es2, "ns_krs", [64, 256], F32)
            c.op("act", lambda: nc.scalar.activation(out=krs[:], in_=sp_[0:64, 0:256], func=AF.Sqrt,
                                                     bias=epsc[0:64, 0:1], scale=1.0 / 64), [Bsp, Beps], [Bkrs])
            c.op("dve", lambda: nc.vector.reciprocal(out=krs[:], in_=krs[:]), [Bkrs], [Bkrs])
            c.op("dve", lambda: nc.vector.scalar_tensor_tensor(out=kcT[:], in0=kraw[:], scalar=g1[:, 0:1], in1=krs[:],
                                                               op0=ALU.mult, op1=ALU.mult), [Bkraw, Bkrs, Bg1], [BkcT])
            for ct in range(2):
                rows = 128 if ct == 0 else 127
                vp_, Bvp = Hps.next()
                for hc in range(2):
                    c.mm(vp_[0:rows, 0:64], G[:, 1, hc, ct * 128:ct * 128 + rows], w2[:, 1, hc, :], hc == 0, hc == 1,
                         [BG, Bw2], [Bvp])
                c.op("dve", lambda: nc.vector.tensor_copy(out=Vc[0:rows, ct, 0:64], in_=vp_[0:rows, 0:64]), [Bvp], [BVc])
        c.barrier()
        KBc, BKBc = sb(nc, es, "ns_KBc", [4, 32, 128], BF16)
        c.dma(KBc[:], T["c_KBc"][:, :, :], reads=[T.B["c_KBc"]], writes=[BKBc])
        QBc, BQBc = sb(nc, es, "ns_QBc", [4, 4, 128], BF16)
        c.dma(QBc[:], T["c_QBc"][:, :, :], reads=[T.B["c_QBc"]], writes=[BQBc])
        cmask, Bcmask = sb(nc, es, "ns_cmask", [128, 17, 128], BF16)
        c.dma(cmask[:], T["c_cmask"][:, :, :], reads=[T.B["c_cmask"]], writes=[Bcmask])
        msk, Bmsk = sb(nc, es, "ns_msk", [128, 3, 128], BF16)
        c.dma(msk[:], T["c_masks"][:, :, :], reads=[T.B["c_masks"]], writes=[Bmsk])
        adj, Badj = sb(nc, es, "ns_adj", [128, 32, 64], F32)
        c.dma(adj[:], T["c_adj"][:, :, :], reads=[T.B["c_adj"]], writes=[Badj])
        gates, Bgates = sb(nc, es, "ns_gates", [128, 32, 12], F32)
        yacc, Byacc = sb(nc, es, "ns_yacc", [128, 32, 256], F32)
        with scope(c) as es2:
            gst, Bgst = sb(nc, es2, "ns_gst", [128, 32, 12], BF16)
            c.dma(gst[:], T["proj"][:, OFF["nsa_gate"]:OFF["nsa_gate"] + 12].rearrange("(b k) e -> k b e", k=128),
                  reads=[T.B["proj"]], writes=[Bgst])
            c.op("act", lambda: nc.scalar.activation(out=gates[:], in_=gst[:], func=AF.Sigmoid), [Bgst], [Bgates])
        c.barrier()
        T.dbg(c, "dbg_kcT", kcT[:], (64, 256), BF16, BkcT)
        T.dbg(c, "dbg_Vc", Vc[:], (128, 2, 130), BF16, BVc)
        if NSA_STAGE <= 2:
            return
        Sps = Rot(nc, es, "ns_S", [128, 512], F32, 3, psum=True)
        Ops = Rot(nc, es, "ns_O", [128, 512], F32, 2, psum=True)
        Tps = Rot(nc, es, "ns_T", [128, 256], BF16, 1, psum=True)
        Pt = Rot(nc, es, "ns_P", [128, 512], BF16, 5)
        Mt = Rot(nc, es, "ns_M", [128, 512], F32, 2)
        Ocs = Rot(nc, es, "ns_Oc", [128, 4, 130], F32, 3)
        Osb = Rot(nc, es, "ns_Osb", [128, 264], F32, 2)
        sm = Rot(nc, es, "ns_sm", [128, 16], F32, 3)
        impt = Rot(nc, es, "ns_imp", [128, 64], F32, 2)
        imp2 = Rot(nc, es, "ns_imp2", [128, 64], F32, 2)
        mx = Rot(nc, es, "ns_mx", [128, 16], F32, 2)
        selb = Rot(nc, es, "ns_selb", [128, 128], BF16, 2)
        for _i in range(2):
            c.op("dve", lambda: nc.vector.memset(selb.t[_i][:], 0.0), [], [selb.b[_i]])
        tmpy = Rot(nc, es, "ns_tmpy", [128, 256], F32, 2)

        def softmax_tile(s_, Bs, mask_ap, Bmask):
            p_, Bp = Pt.next()
            if mask_ap is not None:
                m_, Bm = Mt.next()
                c.op("dve", lambda: nc.vector.tensor_tensor(
                    out=m_[:].rearrange("k (h q) -> k h q", h=4), in0=s_[:].rearrange("k (h q) -> k h q", h=4),
                    in1=mask_ap.to_broadcast([128, 4, 128]), op=ALU.add), [Bs, Bmask], [Bm])
                c.op("act", lambda: nc.scalar.activation(out=p_[:], in_=m_[:], func=AF.Exp), [Bm], [Bp])
            else:
                c.op("act", lambda: nc.scalar.activation(out=p_[:], in_=s_[:], func=AF.Exp), [Bs], [Bp])
            return p_, Bp

        def finish_branch(o_ap, Bo, bq, br, first):
            num, den = o_ap
            s_, Bs_ = sm.next()
            c.op("dve", lambda: nc.vector.tensor_scalar(out=s_[:, 0:4].rearrange("t (h o) -> t h o", o=1), in0=den,
                                                        scalar1=1e-30, scalar2=None, op0=ALU.max), [Bo], [Bs_])
            c.op("dve", lambda: nc.vector.reciprocal(out=s_[:, 4:8], in_=s_[:, 0:4]), [Bs_], [Bs_])
            c.op("dve", lambda: nc.vector.tensor_tensor(out=s_[:, 8:12], in0=s_[:, 4:8], in1=gates[:, bq, ssl(br, 4, 3)],
                                                        op=ALU.mult), [Bs_, Bgates], [Bs_])
            wb = s_[:, 8:12].rearrange("t (h o) -> t h o", o=1).to_broadcast([128, 4, 64])
            yv = yacc[:, bq, :].rearrange("t (h e) -> t h e", h=4)
            if first:
                c.op("dve", lambda: nc.vector.tensor_tensor(out=yv, in0=num, in1=wb, op=ALU.mult), [Bo, Bs_], [Byacc])
            else:
                t_, Bt = tmpy.next()
                tv = t_[:].rearrange("t (h e) -> t h e", h=4)
                c.op("dve", lambda: nc.vector.tensor_tensor(out=tv, in0=num, in1=wb, op=ALU.mult), [Bo, Bs_], [Bt])
                c.op("dve", lambda: nc.vector.tensor_tensor(out=yacc[:, bq, :], in0=yacc[:, bq, :], in1=t_[:],
                                                            op=ALU.add), [Bt, Byacc], [Byacc])
            return s_, Bs_

        BqS = Buf("ns_qsel_rows")
        cmp_held = {}

        def cmp_a(bq):
            cts = [0] if bq < 16 else [0, 1]
            oA, BoA = Ops.next()
            oB, BoB = Ops.next()
            first = True
            for ct in cts:
                m = bq - 16 * ct
                s_, Bs = Sps.next()
                s4 = s_[:].rearrange("k (h q) -> k h q", h=4)
                c.mm(s4, kcT[:, ct * 128:(ct + 1) * 128], qT[0:64, :, bq * 128:(bq + 1) * 128],
                     True, False, [BkcT, BqT], [Bs])
                c.mm(s4, KBc[:, m, :], QBc[:, :, :], False, True, [BKBc, BQBc], [Bs])
                p_, Bp = softmax_tile(s_, Bs, cmask[:, m:m + 1, :] if m <= 16 else None, Bcmask)
                for h in range(4):
                    o_, Bo = (oA, BoA) if h < 2 else (oB, BoB)
                    hh = h % 2
                    c.mm(o_[:, hh * 130:(hh + 1) * 130], p_[:, h * 128:(h + 1) * 128], Vc[:, ct, :],
                         first and hh == 0, False, [Bp, BVc], [Bo], skip_group_check=True)
                first = False
            oc, Boc = Ocs.next()
            c.copy(oc[:, 0:2, :], oA[:, 0:260].rearrange("t (h e) -> t h e", h=2), [BoA], [Boc])
            c.copy(oc[:, 2:4, :], oB[:, 0:260].rearrange("t (h e) -> t h e", h=2), [BoB], [Boc])
            cmp_held[bq] = (oc, Boc)

        def cmp_b(bq):
            oc, Boc = cmp_held.pop(bq)
            s_, Bs_ = finish_branch((oc[:, :, 0:64], oc[:, :, 128:129]), Boc, bq, 0, True)
            im, Bim = impt.next()
            c.op("dve", lambda: nc.vector.scalar_tensor_tensor(out=im[:], in0=oc[:, 0, 64:128], scalar=s_[:, 4:5],
                                                               in1=adj[:, bq, :], op0=ALU.mult, op1=ALU.add),
                 [Boc, Bs_, Badj], [Bim])
            for h in range(1, 4):
                c.op("dve", lambda: nc.vector.scalar_tensor_tensor(out=im[:], in0=oc[:, h, 64:128], scalar=s_[:, 4 + h:5 + h],
                                                                   in1=im[:], op0=ALU.mult, op1=ALU.add),
                     [Boc, Bs_, Bim], [Bim])
            mx_, Bmx = mx.next()
            c.op("dve", lambda: nc.vector.max(out=mx_[:, 0:8], in_=im[:]), [Bim], [Bmx])
            i2, Bi2 = imp2.next()
            c.op("dve", lambda: nc.vector.match_replace(out=i2[:], in_to_replace=mx_[:, 0:8], in_values=im[:],
                                                        imm_value=-3.0e38), [Bim, Bmx], [Bi2])
            c.op("dve", lambda: nc.vector.max(out=mx_[:, 8:16], in_=i2[:]), [Bi2], [Bmx])
            sb_, Bsb = selb.next()
            c.op("dve", lambda: nc.vector.tensor_scalar(out=sb_[:, 64:128], in0=im[:], scalar1=mx_[:, 15:16], scalar2=None,
                                                        op0=ALU.is_ge), [Bim, Bmx], [Bsb])
            tp_, Btp = Tps.next()
            c.op("pe", lambda: nc.tensor.transpose(out=tp_[:, 0:128], in_=sb_[:], identity=ident[:]), [Bsb, Bid], [Btp])
            for h in range(4):
                c.op("dve", lambda: nc.vector.tensor_scalar(
                    out=qT[64:128, h, bq * 128:(bq + 1) * 128], in0=tp_[64:128, 0:128], scalar1=Asel[64:128, bq, h:h + 1],
                    scalar2=-30000.0, op0=ALU.mult, op1=ALU.add), [Btp, BAsel], [BqS])

        for bq in range(NT + 1):
            if bq < NT:
                cmp_a(bq)
            if bq >= 1:
                cmp_b(bq - 1)
        T.dbg(c, "dbg_ycmp", yacc[:], (128, 32, 256), F32, Byacc)
        if NSA_STAGE <= 3:
            return
        SKEW = 2
        tasks = []
        for bq in range(NT):
            for br in (1, 2):
                kbs = list(range(0, bq + 1)) if br == 1 else list(range(max(0, bq - 4), bq + 1))
                for i_, kb in enumerate(kbs):
                    tasks.append((bq, br, kb, i_ == 0, i_ == len(kbs) - 1))
        opnd = {1: (ksT, BksT, Vs, BVs, qT, BqS), 2: (kwT, BkwT, Vw, BVw, Q2w, BQ2w)}
        st = {}
        cur_o = [None]

        def stage_a(ti):
            bq, br, kb, is_first, is_last = tasks[ti]
            kT_, BkT_, V_, BV_, Q_, BQ_ = opnd[br]
            s_, Bs = Sps.next()
            s4 = s_[:].rearrange("k (h q) -> k h q", h=4)
            c.mm(s4, kT_[:, 0, kb * 128:(kb + 1) * 128], Q_[:, :, bq * 128:(bq + 1) * 128], True, True,
                 [BkT_, BQ_, BqT], [Bs])
            mask_ap = None
            if kb == bq:
                mask_ap = msk[:, 0:1, :]
            elif br == 2 and kb == bq - 4:
                mask_ap = msk[:, 2:3, :]
            st[ti] = softmax_tile(s_, Bs, mask_ap, Bmsk)

        def stage_c(ti):
            bq, br, kb, is_first, is_last = tasks[ti]
            kT_, BkT_, V_, BV_, Q_, BQ_ = opnd[br]
            p_, Bp = st.pop(ti)
            if is_first:
                cur_o[0] = Ops.next()
            o_, Bo = cur_o[0]
            for h in range(4):
                c.mm(o_[:, h * 66:(h + 1) * 66], p_[:, h * 128:(h + 1) * 128], V_[:, kb, h, :],
                     is_first and h == 0, False, [Bp, BV_], [Bo], skip_group_check=True)
            if is_last:
                ob, Bob = Osb.next()
                c.copy(ob[:], o_[:, 0:264], [Bo], [Bob])
                ov = ob[:].rearrange("t (h e) -> t h e", h=4)
                finish_branch((ov[:, :, 0:64], ov[:, :, 64:65]), Bob, bq, br, False)

        for ti in range(len(tasks) + SKEW):
            if ti < len(tasks):
                stage_a(ti)
            if ti - SKEW >= 0:
                stage_c(ti - SKEW)
        c.dma(T["mix"][:, 256:512].rearrange("(b q) e -> q b e", q=128), yacc[:], reads=[Byacc], writes=[T.B["mix"]])
    c.barrier()


GLA_STAGE = 99


def phase_gla(c, T, l):
    _phase_gla(c, T, l)
    c.barrier()


def _phase_gla(c, T, l):
    nc = c.nc
    with scope(c) as es:
        ident, Bid = sb(nc, es, "gl_id", [128, 128], BF16)
        c.dma(ident[:], T["ident"][:, :], reads=[T.B["ident"]], writes=[Bid])
        m01, Bm01 = sb(nc, es, "gl_m01", [128, 128], BF16)
        c.dma(m01[:], T["c_mask01"][:, :], reads=[T.B["c_mask01"]], writes=[Bm01])
        epsc, Beps = sb(nc, es, "gl_eps", [128, 1], F32)
        c.op("dve", lambda: nc.vector.memset(epsc[:], EPS), [], [Beps])
        gnB, BgnB = sb(nc, es, "gl_gnB", [128, 64], F32)
        c.dma(gnB[:], T["gla_norm_g"][l].partition_broadcast(128), reads=[T.B["gla_norm_g"]], writes=[BgnB])
        qt, Bqt = sb(nc, es, "gl_qt", [32, 4, S], BF16)
        kt, Bkt = sb(nc, es, "gl_kt", [32, 4, S], BF16)
        ktm, Bktm = sb(nc, es, "gl_ktm", [128, NT, 128], BF16)
        ebl, Bebl = sb(nc, es, "gl_ebl", [32, 4, NT], F32)
        with scope(c) as es2:
            wa, Bwa = sb(nc, es2, "gl_wa", [16, 128], BF16)
            c.dma(wa[:], T["gla_wa2"][l], reads=[T.B["gla_wa2"]], writes=[Bwa], q="pool")
            bac, Bbac = sb(nc, es2, "gl_ba", [32, 4], F32)
            c.dma(bac[:], T["gla_ba"][l].rearrange("(g p) -> p g", g=4), reads=[T.B["gla_ba"]], writes=[Bbac],
                  allow_slow_non_contiguous=True)
            aT, BaT = sb(nc, es2, "gl_aT", [16, S], BF16)
            c.dma(aT[:], T["projT"][OFF["gla_a"]:OFF["gla_a"] + 16, :], reads=[T.B["projT"]], writes=[BaT])
            seg, Bseg = sb(nc, es2, "gl_seg", [32, S], BF16)
            c.op("dve", lambda: nc.vector.memset(seg[:], 1.0), [], [Bseg])
            c.op("dve", lambda: nc.vector.memset(seg[:, ssl(0, NT, 128)], 0.0), [], [Bseg])
            zp = Rot(nc, es2, "gl_zp", [128, 512], F32, 2, psum=True)
            qkr = Rot(nc, es2, "gl_qk", [32, 2, S], BF16, 2)
            lar = Rot(nc, es2, "gl_la", [32, S], F32, 2)
            bbr = Rot(nc, es2, "gl_bb", [32, S], F32, 2)
            qkv = T["projT"][OFF["gla_q"]:OFF["gla_q"] + 256, :].rearrange("(i p) t -> p i t", i=8)
            for g in range(4):
                qk, Bqk = qkr.next()
                c.dma(qk[:, 0, :], qkv[:, g, :], reads=[T.B["projT"]], writes=[Bqk])
                c.dma(qk[:, 1, :], qkv[:, 4 + g, :], reads=[T.B["projT"]], writes=[Bqk])
                la, Bla = lar.next()
                bb, Bbb = bbr.next()
                for tt in range(8):
                    sl = slice(tt * 512, (tt + 1) * 512)
                    z_, Bz = zp.next()
                    c.mm(z_[0:32, :], wa[:, g * 32:(g + 1) * 32], aT[:, sl], True, True, [Bwa, BaT], [Bz])
                    c.op("act", lambda: nc.scalar.activation(out=la[:, sl], in_=z_[0:32, :], func=AF.Sigmoid,
                                                             bias=bac[:, g:g + 1]), [Bz, Bbac], [Bla])
                c.op("act", lambda: nc.scalar.activation(out=la[:], in_=la[:], func=AF.Ln), [Bla], [Bla])
                c.op("act", lambda: nc.scalar.mul(out=la[:], in_=la[:], mul=1.0 / 16.0), [Bla], [Bla])
                c.op("dve", lambda: nc.vector.tensor_tensor_scan(out=bb[:], data0=seg[:], data1=la[:],
                                                                 initial=0.0, op0=ALU.mult, op1=ALU.add),
                     [Bseg, Bla], [Bbb])
                c.op("act", lambda: nc.scalar.activation(out=la[:], in_=bb[:], func=AF.Exp), [Bbb], [Bla])
                c.op("dve", lambda: nc.vector.tensor_copy(out=ebl[:, g, :], in_=la[:, ssl(127, NT, 128)]), [Bla], [Bebl])
                c.op("dve", lambda: nc.vector.scalar_tensor_tensor(out=qt[:, g, :], in0=qk[:, 0, :], scalar=32.0 ** -0.5,
                                                                   in1=la[:], op0=ALU.mult, op1=ALU.mult),
                     [Bqk, Bla], [Bqt])
                c.op("act", lambda: nc.scalar.activation(out=bb[:], in_=bb[:], func=AF.Exp, scale=-1.0),
                     [Bbb], [Bbb])
                c.op("dve", lambda: nc.vector.tensor_tensor(out=kt[:, g, :], in0=qk[:, 1, :], in1=bb[:],
                                                            op=ALU.mult), [Bqk, Bbb], [Bkt])
            tp = Rot(nc, es2, "gl_tp", [128, 1024], BF16, 2, psum=True)
            for n in range(NT):
                p_, Bp = tp.next()
                for g in range(4):
                    c.op("pe", lambda: nc.tensor.transpose(out=p_[:, g * 32:(g + 1) * 32],
                                                           in_=kt[:, g, n * 128:(n + 1) * 128],
                                                           identity=ident[0:32, 0:32]), [Bkt, Bid], [Bp])
                c.copy(ktm[:, n, :], p_[:, 0:128], [Bp], [Bktm])
        v, Bv = sb(nc, es, "gl_v", [128, NT, 256], BF16)
        c.dma(v[:], T["proj"][:, OFF["gla_v"]:OFF["gla_v"] + 256].rearrange("(n j) e -> j n e", j=128),
              reads=[T.B["proj"]], writes=[Bv])
        oall, Boall = sb(nc, es, "gl_oall", [128, NT, 256], F32)
        Sps = Rot(nc, es, "gl_S", [128, 512], F32, 2, psum=True)
        Ops = Rot(nc, es, "gl_O", [128, 512], F32, 2, psum=True)
        Pps = Rot(nc, es, "gl_P", [128, 512], F32, 2, psum=True)
        At = Rot(nc, es, "gl_A", [128, 512], BF16, 3)
        Sf, BSf = sb(nc, es, "gl_Sf", [32, 4, 64], F32)
        Sf2, BSf2 = sb(nc, es, "gl_Sf2", [32, 4, 64], F32)
        Sf3, BSf3 = sb(nc, es, "gl_Sf3", [32, 4, 64], F32)
        Sst, BSst = sb(nc, es, "gl_Sst", [32, 4, 64], BF16)
        c.op("dve", lambda: nc.vector.memset(Sf[:], 0.0), [], [BSf])
        c.op("dve", lambda: nc.vector.memset(Sst[:], 0.0), [], [BSst])
        for n in range(NT):
            sl = slice(n * 128, (n + 1) * 128)
            s_, Bs = Sps.next()
            for h in range(4):
                c.mm(s_[:, h * 128:(h + 1) * 128], kt[:, h, sl], qt[:, h, sl], True, True, [Bkt, Bqt], [Bs])
            pp, Bpp = Pps.next()
            for h in range(4):
                c.mm(pp[0:32, h * 64:(h + 1) * 64], ktm[:, n, 32 * h:32 * h + 32], v[:, n, 64 * h:64 * h + 64],
                     True, True, [Bktm, Bv], [Bpp])
            a_, Ba = At.next()
            c.op("dve", lambda: nc.vector.tensor_tensor(
                out=a_[:].rearrange("k (h q) -> k h q", h=4), in0=s_[:].rearrange("k (h q) -> k h q", h=4),
                in1=m01[:].rearrange("k (o q) -> k o q", o=1).to_broadcast([128, 4, 128]), op=ALU.mult), [Bs, Bm01], [Ba])
            o_, Bo = Ops.next()
            for h in range(4):
                c.mm(o_[:, 64 * h:64 * h + 64], a_[:, h * 128:(h + 1) * 128], v[:, n, 64 * h:64 * h + 64],
                     True, False, [Ba, Bv], [Bo])
                c.mm(o_[:, 64 * h:64 * h + 64], qt[:, h, sl], Sst[:, h, :], False, True, [Bqt, BSst], [Bo])
            c.copy(oall[:, n, :], o_[:, 0:256], [Bo], [Boall])
            eb = ebl[:, :, n:n + 1].to_broadcast([32, 4, 64])
            c.op("dve", lambda: nc.vector.tensor_tensor(out=Sf2[:], in0=Sf[:], in1=eb, op=ALU.mult), [BSf, Bebl], [BSf2])
            c.op("dve", lambda: nc.vector.tensor_tensor(out=Sf3[:], in0=pp[0:32, 0:256].rearrange("p (h e) -> p h e", h=4),
                                                        in1=eb, op=ALU.mult), [Bpp, Bebl], [BSf3])
            c.op("dve", lambda: nc.vector.tensor_tensor(out=Sf[:], in0=Sf2[:], in1=Sf3[:], op=ALU.add), [BSf2, BSf3], [BSf])
            c.op("dve", lambda: nc.vector.tensor_copy(out=Sst[:], in_=Sf[:]), [BSf], [BSst])
        if GLA_STAGE <= 5:
            return
        with scope(c) as es2:
            r_, Br = sb(nc, es2, "gl_r", [128, NT, 256], BF16)
            c.dma(r_[:], T["proj"][:, OFF["gla_r"]:OFF["gla_r"] + 256].rearrange("(n j) e -> j n e", j=128),
                  reads=[T.B["proj"]], writes=[Br])
            sq, Bsq = sb(nc, es2, "gl_sq", [128, NT, 256], F32)
            ss, Bss = sb(nc, es2, "gl_ss", [128, NT * 4], F32)
            c.op("act", lambda: nc.scalar.activation(out=sq[:], in_=oall[:], func=AF.Square), [Boall], [Bsq])
            c.op("dve", lambda: nc.vector.tensor_reduce(out=ss[:], in_=sq[:].rearrange("t n (h e) -> t (n h) e", h=4),
                                                        axis=AX.X, op=ALU.add), [Bsq], [Bss])
            c.op("act", lambda: nc.scalar.activation(out=ss[:], in_=ss[:], func=AF.Sqrt, bias=epsc[:, 0:1], scale=1.0 / 64),
                 [Bss, Beps], [Bss])
            c.op("dve", lambda: nc.vector.reciprocal(out=ss[:], in_=ss[:]), [Bss], [Bss])
            ov = oall[:].rearrange("t n (h e) -> t (n h) e", h=4)
            c.op("dve", lambda: nc.vector.tensor_tensor(
                out=ov, in0=ov, in1=ss[:].rearrange("t (m o) -> t m o", o=1).to_broadcast([128, NT * 4, 64]), op=ALU.mult),
                [Boall, Bss], [Boall])
            c.op("dve", lambda: nc.vector.tensor_tensor(
                out=ov, in0=ov, in1=gnB[:].rearrange("t (o e) -> t o e", o=1).to_broadcast([128, NT * 4, 64]), op=ALU.mult),
                [Boall, BgnB], [Boall])
            c.op("act", lambda: nc.scalar.activation(out=sq[:], in_=r_[:], func=AF.Silu), [Br], [Bsq])
            c.op("dve", lambda: nc.vector.tensor_tensor(out=oall[:], in0=oall[:], in1=sq[:], op=ALU.mult),
                 [Boall, Bsq], [Boall])
            c.dma(T["mix"][:, 512:768].rearrange("(n j) e -> j n e", j=128), oall[:], reads=[Boall], writes=[T.B["mix"]])


S5_L = 256
S5_STAGE = 99


def phase_s5(c, T, l):
    _phase_s5(c, T, l)
    c.barrier()


def _phase_s5(c, T, l):
    nc = c.nc
    L = S5_L
    NCH = S // L
    uoff = OFF["s5_u"]

    def dv(fn, r, w):
        return c.op("dve", fn, r, w)

    def tt(out, a, b, op, r, w):
        return c.op("dve", lambda: nc.vector.tensor_tensor(out=out, in0=a, in1=b, op=op), r, w)

    with scope(c) as es:
        ident, Bid = sb(nc, es, "s5_id", [128, 128], BF16)
        c.dma(ident[:], T["ident"][:, :], reads=[T.B["ident"]], writes=[Bid])
        identf, Bidf = sb(nc, es, "s5_idf", [128, 128], F32)
        c.dma(identf[:], T["identf"][:, :], reads=[T.B["identf"]], writes=[Bidf])
        mask, Bmask = sb(nc, es, "s5_mask", [128, 8, 128], F32)
        c.dma(mask[:], T["c_s5mask"][:, :, :], reads=[T.B["c_s5mask"]], writes=[Bmask])
        P, BP = sb(nc, es, "s5_P", [128, 40, 8], F32)
        hp, Bhp = sb(nc, es, "s5_hp", [128, 1], F32)
        dv(lambda: nc.vector.memset(hp[:], math.pi / 2.0), [], [Bhp])
        (ARE, AIM, LDT, DT, MAG, TH, CS, SN, C2, S2, TA, TB_, ABR, ABI, DEN, AM1, FRE, FIM, PR, PI_, PR2, PI2,
         WLR, WLI, X1, X2) = range(26)

        def col(i):
            return P[:, i, :]

        for gg in range(2):
            ps_ = slice(gg * 64, (gg + 1) * 64)
            c.dma(P[ps_, ARE, :], T["s5_a_re"][l].rearrange("(m gg) n -> gg n m", gg=2)[gg], reads=[T.B["s5_a_re"]],
                  writes=[BP], allow_slow_non_contiguous=True)
            c.dma(P[ps_, AIM, :], T["s5_a_im"][l].rearrange("(m gg) n -> gg n m", gg=2)[gg], reads=[T.B["s5_a_im"]],
                  writes=[BP], allow_slow_non_contiguous=True)
            c.dma(P[ps_, LDT, :], T["s5_log_dt"][l].rearrange("(m gg) -> gg m", gg=2)[gg].partition_broadcast(64),
                  reads=[T.B["s5_log_dt"]], writes=[BP], allow_slow_non_contiguous=True)
        c.op("act", lambda: nc.scalar.activation(out=col(DT), in_=col(LDT), func=AF.Exp), [BP], [BP])
        tt(col(TA), col(DT), col(ARE), ALU.mult, [BP], [BP])
        c.op("act", lambda: nc.scalar.activation(out=col(MAG), in_=col(TA), func=AF.Exp), [BP], [BP])
        tt(col(TH), col(DT), col(AIM), ALU.mult, [BP], [BP])
        c.op("act", lambda: nc.scalar.activation(out=col(SN), in_=col(TH), func=AF.Sin, scale=1.0 / 16.0), [BP], [BP])
        c.op("act", lambda: nc.scalar.activation(out=col(CS), in_=col(TH), func=AF.Sin, scale=1.0 / 16.0,
                                                 bias=hp[:, 0:1]), [BP, Bhp], [BP])

        def csq(cr, ci, orr, oi):
            tt(col(TA), col(cr), col(cr), ALU.mult, [BP], [BP])
            tt(col(TB_), col(ci), col(ci), ALU.mult, [BP], [BP])
            dv(lambda: nc.vector.scalar_tensor_tensor(out=col(oi), in0=col(cr), scalar=2.0, in1=col(ci),
                                                      op0=ALU.mult, op1=ALU.mult), [BP], [BP])
            tt(col(orr), col(TA), col(TB_), ALU.subtract, [BP], [BP])

        csq(CS, SN, C2, S2)
        csq(C2, S2, CS, SN)
        csq(CS, SN, C2, S2)
        csq(C2, S2, CS, SN)
        tt(col(ABR), col(MAG), col(CS), ALU.mult, [BP], [BP])
        tt(col(ABI), col(MAG), col(SN), ALU.mult, [BP], [BP])
        tt(col(TA), col(ARE), col(ARE), ALU.mult, [BP], [BP])
        tt(col(TB_), col(AIM), col(AIM), ALU.mult, [BP], [BP])
        tt(col(DEN), col(TA), col(TB_), ALU.add, [BP], [BP])
        dv(lambda: nc.vector.reciprocal(out=col(DEN), in_=col(DEN)), [BP], [BP])
        dv(lambda: nc.vector.tensor_scalar(out=col(AM1), in0=col(ABR), scalar1=-1.0, scalar2=None, op0=ALU.add), [BP], [BP])
        tt(col(TA), col(AM1), col(ARE), ALU.mult, [BP], [BP])
        tt(col(TB_), col(ABI), col(AIM), ALU.mult, [BP], [BP])
        tt(col(FRE), col(TA), col(TB_), ALU.add, [BP], [BP])
        tt(col(FRE), col(FRE), col(DEN), ALU.mult, [BP], [BP])
        tt(col(TA), col(ABI), col(ARE), ALU.mult, [BP], [BP])
        tt(col(TB_), col(AM1), col(AIM), ALU.mult, [BP], [BP])
        tt(col(FIM), col(TA), col(TB_), ALU.subtract, [BP], [BP])
        tt(col(FIM), col(FIM), col(DEN), ALU.mult, [BP], [BP])
        BT, BBT = sb(nc, es, "s5_BT", [128, 2, 8, 128], BF16)
        Cm, BCm = sb(nc, es, "s5_Cm", [128, 2, 8, 128], BF16)
        with scope(c) as es2:
            braw, Bbraw = sb(nc, es2, "s5_braw", [128, 2, 8, 16], F32)
            craw, Bcraw = sb(nc, es2, "s5_craw", [128, 2, 8, 16], F32)
            for gg in range(2):
                ps_ = slice(gg * 64, (gg + 1) * 64)
                for ri, nm in enumerate(("s5_b_re", "s5_b_im")):
                    c.dma(braw[ps_, ri, :, :], T[nm][l].rearrange("(m gg) n c -> gg n m c", gg=2)[gg], reads=[T.B[nm]],
                          writes=[Bbraw], allow_slow_non_contiguous=True)
                for ri, nm in enumerate(("s5_c_re", "s5_c_im")):
                    for m in range(8):
                        c.dma(craw[ps_, ri, m, :], T[nm][l, 2 * m + gg].rearrange("c n -> n c"), reads=[T.B[nm]],
                              writes=[Bcraw], allow_slow_non_contiguous=True)
            bbc, Bbbc = sb(nc, es2, "s5_bbc", [128, 2, 8, 16], F32)
            t1, Bt1 = sb(nc, es2, "s5_t1", [128, 8, 16], F32)
            t2, Bt2 = sb(nc, es2, "s5_t2", [128, 8, 16], F32)
            fre_b = P[:, FRE, :].rearrange("p (m o) -> p m o", o=1).to_broadcast([128, 8, 16])
            fim_b = P[:, FIM, :].rearrange("p (m o) -> p m o", o=1).to_broadcast([128, 8, 16])
            tt(t1[:], braw[:, 0, :, :], fre_b, ALU.mult, [Bbraw, BP], [Bt1])
            tt(t2[:], braw[:, 1, :, :], fim_b, ALU.mult, [Bbraw, BP], [Bt2])
            tt(bbc[:, 0, :, :], t1[:], t2[:], ALU.subtract, [Bt1, Bt2], [Bbbc])
            tt(t1[:], braw[:, 1, :, :], fre_b, ALU.mult, [Bbraw, BP], [Bt1])
            tt(t2[:], braw[:, 0, :, :], fim_b, ALU.mult, [Bbraw, BP], [Bt2])
            tt(bbc[:, 1, :, :], t1[:], t2[:], ALU.add, [Bt1, Bt2], [Bbbc])
            Bd, BBd = sb(nc, es2, "s5_Bd", [128, 2, 8, 128], BF16)
            mask4 = mask[:].rearrange("p m (j c) -> p m j c", c=16)
            for ri in range(2):
                tt(Bd[:, ri, :, :].rearrange("p m (j c) -> p m j c", c=16), mask4,
                   bbc[:, ri, :, :].rearrange("p m (o c) -> p m o c", o=1).to_broadcast([128, 8, 8, 16]), ALU.mult,
                   [Bmask, Bbbc], [BBd])
            tt(Cm[:, 0, :, :].rearrange("p m (j c) -> p m j c", c=16), mask4,
               craw[:, 0, :, :].rearrange("p m (o c) -> p m o c", o=1).to_broadcast([128, 8, 8, 16]), ALU.mult,
               [Bmask, Bcraw], [BCm])
            dv(lambda: nc.vector.tensor_scalar(out=craw[:, 1, :, :], in0=craw[:, 1, :, :], scalar1=-1.0, scalar2=None,
                                               op0=ALU.mult), [Bcraw], [Bcraw])
            tt(Cm[:, 1, :, :].rearrange("p m (j c) -> p m j c", c=16), mask4,
               craw[:, 1, :, :].rearrange("p m (o c) -> p m o c", o=1).to_broadcast([128, 8, 8, 16]), ALU.mult,
               [Bmask, Bcraw], [BCm])
            tpb = Rot(nc, es2, "s5_tpb", [128, 1024], BF16, 2, psum=True)
            for ri in range(2):
                for g2 in range(2):
                    p_, Bp = tpb.next()
                    for j in range(4):
                        m = g2 * 4 + j
                        c.op("pe", lambda: nc.tensor.transpose(out=p_[:, j * 128:(j + 1) * 128], in_=Bd[:, ri, m, :],
                                                               identity=ident[:]), [BBd, Bid], [Bp])
                    c.copy(BT[:, ri, g2 * 4:(g2 + 1) * 4, :], p_[:, 0:512].rearrange("p (j t) -> p j t", j=4), [Bp], [BBT])
        c.barrier()
        T.dbg(c, "dbg_s5P", P[:], (128, 40, 8), F32, BP)
        if S5_STAGE <= 1:
            return
        w, Bw = sb(nc, es, "s5_w", [128, 2, 8, L], F32)
        Rb, BRb = sb(nc, es, "s5_Rb", [128, 8, L], F32)
        tA, BtA = sb(nc, es, "s5_tA", [128, 8, L], F32)
        tB, BtB = sb(nc, es, "s5_tB", [128, 8, L], F32)
        tC, BtC = sb(nc, es, "s5_tC", [128, 8, L], F32)
        tD, BtD = sb(nc, es, "s5_tD", [128, 8, L], F32)
        dv(lambda: nc.vector.tensor_copy(out=Rb[:], in_=P[:, MAG, :].rearrange("p (m o) -> p m o", o=1)
                                         .to_broadcast([128, 8, L])), [BP], [BRb])
        dv(lambda: nc.vector.memset(w[:, 0, :, 0:1], 1.0), [], [Bw])
        dv(lambda: nc.vector.memset(w[:, 1, :, 0:1], 0.0), [], [Bw])
        dv(lambda: nc.vector.tensor_copy(out=col(PR), in_=col(CS)), [BP], [BP])
        dv(lambda: nc.vector.tensor_copy(out=col(PI_), in_=col(SN)), [BP], [BP])
        k = 1
        cur = (PR, PI_)
        oth = (PR2, PI2)
        while k < L:
            prb = P[:, cur[0], :].rearrange("p (m o) -> p m o", o=1).to_broadcast([128, 8, k])
            pib = P[:, cur[1], :].rearrange("p (m o) -> p m o", o=1).to_broadcast([128, 8, k])
            wr0 = w[:, 0, :, 0:k]
            wi0 = w[:, 1, :, 0:k]
            tt(tA[:, :, 0:k], wr0, prb, ALU.mult, [Bw, BP], [BtA])
            tt(tB[:, :, 0:k], wi0, pib, ALU.mult, [Bw, BP], [BtB])
            tt(w[:, 0, :, k:2 * k], tA[:, :, 0:k], tB[:, :, 0:k], ALU.subtract, [BtA, BtB], [Bw])
            tt(tA[:, :, 0:k], wr0, pib, ALU.mult, [Bw, BP], [BtA])
            tt(tB[:, :, 0:k], wi0, prb, ALU.mult, [Bw, BP], [BtB])
            tt(w[:, 1, :, k:2 * k], tA[:, :, 0:k], tB[:, :, 0:k], ALU.add, [BtA, BtB], [Bw])
            csq(cur[0], cur[1], oth[0], oth[1])
            cur, oth = oth, cur
            k *= 2
        dv(lambda: nc.vector.tensor_copy(out=col(WLR), in_=col(cur[0])), [BP], [BP])
        dv(lambda: nc.vector.tensor_copy(out=col(WLI), in_=col(cur[1])), [BP], [BP])
        T.dbg(c, "dbg_s5w", w[:], (128, 2, 8, L), F32, Bw)
        gw, Bgw = sb(nc, es, "s5_gwt", [128, 2, 256], BF16)
        c.dma(gw[:], T["s5_glu_w"][l].rearrange("(kt p) n -> p kt n", p=128), reads=[T.B["s5_glu_w"]], writes=[Bgw], q="pool")
        gbc, Bgbc = sb(nc, es, "s5_gbt", [128, 2], F32)
        c.dma(gbc[:], T["s5_glu_b"][l].rearrange("(h p) -> p h", p=128), reads=[T.B["s5_glu_b"]], writes=[Bgbc],
              allow_slow_non_contiguous=True)
        dcl, Bdcl = sb(nc, es, "s5_dcol", [128, 2], F32)
        c.dma(dcl[:], T["s5_d"][l].rearrange("(h p) -> p h", p=128), reads=[T.B["s5_d"]], writes=[Bdcl],
              allow_slow_non_contiguous=True)
        if S5_STAGE <= 2:
            return
        uTr = Rot(nc, es, "s5_uT", [128, 2, L], BF16, 3)
        BUps = Rot(nc, es, "s5_BU", [128, 512], F32, 2, psum=True)
        Yps = Rot(nc, es, "s5_Y", [128, 512], F32, 2, psum=True)
        Tpf = Rot(nc, es, "s5_Tp", [128, 512], F32, 2, psum=True)
        d1 = Rot(nc, es, "s5_d1", [128, L], F32, 3)
        d2 = Rot(nc, es, "s5_d2", [128, L], F32, 3)
        zb = Rot(nc, es, "s5_zb", [128, 2, L], F32, 3)
        Z, BZ = sb(nc, es, "s5_Z", [128, 2, 8, L], F32)
        bus = Rot(nc, es, "s5_bus", [128, 2, 8, L], F32, 2)
        zbs, Bzbs = sb(nc, es, "s5_zbs", [128, 2, 8, L], F32)
        Xr = Rot(nc, es, "s5_X", [128, 2, 8, L], BF16, 2)
        stash = {}
        init = Rot(nc, es, "s5_init", [128, 2, 8], F32, 2)
        zt, Bzt = sb(nc, es, "s5_zt", [128, 2, L], F32)
        u2, Bu2 = sb(nc, es, "s5_u2", [128, 2, L], F32)
        hg, Bhg = sb(nc, es, "s5_hg", [128, 2, L], F32)
        hgb, Bhgb = sb(nc, es, "s5_hgb", [128, 2, L], BF16)
        sgt, Bsgt = sb(nc, es, "s5_sg", [128, 2, L], F32)
        ot, Bot = sb(nc, es, "s5_ot", [128, 2, L], F32)
        otm = Rot(nc, es, "s5_otm", [128, L // 128, 256], F32, 2)
        in_, Bin = init.next()
        dv(lambda: nc.vector.memset(in_[:], 0.0), [], [Bin])
        def s5_a(ci):
            nonlocal in_, Bin
            t0 = ci * L
            X, BX = Xr.next()
            u_, Bu = uTr.next()
            c.dma(u_[:], T["projT"][uoff:uoff + 256, t0:t0 + L].rearrange("(h p) t -> p h t", p=128),
                  reads=[T.B["projT"]], writes=[Bu])
            bu_, Bbu = bus.next()
            for m in range(8):
                bk, Bbk = BUps.next()
                c.mm(bk[:, 0:L], BT[:, 0, m, :], u_[:, m // 4, :], True, True, [BBT, Bu], [Bbk])
                c.mm(bk[:, L:2 * L], BT[:, 1, m, :], u_[:, m // 4, :], True, True, [BBT, Bu], [Bbk])
                c.op("act", lambda: nc.scalar.copy(out=bu_[:, :, m, :], in_=bk[:, 0:2 * L].rearrange("p (r l) -> p r l", r=2)),
                     [Bbk], [Bbu])
            tt(tA[:], bu_[:, 0, :, :], w[:, 0, :, :], ALU.mult, [Bbu, Bw], [BtA])
            tt(tB[:], bu_[:, 1, :, :], w[:, 1, :, :], ALU.mult, [Bbu, Bw], [BtB])
            tt(zbs[:, 0, :, :], tA[:], tB[:], ALU.add, [BtA, BtB], [Bzbs])
            tt(tC[:], bu_[:, 1, :, :], w[:, 0, :, :], ALU.mult, [Bbu, Bw], [BtC])
            tt(tD[:], bu_[:, 0, :, :], w[:, 1, :, :], ALU.mult, [Bbu, Bw], [BtD])
            tt(zbs[:, 1, :, :], tC[:], tD[:], ALU.subtract, [BtC, BtD], [Bzbs])
            for m in range(8):
                for ri in range(2):
                    dv(lambda: nc.vector.tensor_tensor_scan(out=Z[:, ri, m, :], data0=Rb[:, m, :], data1=zbs[:, ri, m, :],
                                                            initial=in_[:, ri, m:m + 1], op0=ALU.mult, op1=ALU.add),
                       [BRb, Bzbs, Bin], [BZ])
            nx, Bnx = init.next()
            zr = Z[:, 0, :, L - 1]
            zi = Z[:, 1, :, L - 1]
            tt(col(X1), zr, col(WLR), ALU.mult, [BZ, BP], [BP])
            tt(col(X2), zi, col(WLI), ALU.mult, [BZ, BP], [BP])
            tt(nx[:, 0, :], col(X1), col(X2), ALU.subtract, [BP], [Bnx])
            tt(col(X1), zr, col(WLI), ALU.mult, [BZ, BP], [BP])
            tt(col(X2), zi, col(WLR), ALU.mult, [BZ, BP], [BP])
            tt(nx[:, 1, :], col(X1), col(X2), ALU.add, [BP], [Bnx])
            in_, Bin = nx, Bnx
            def pt(out, a, b, op, r, w_):
                return c.op("dve", lambda: nc.vector.tensor_tensor(out=out, in0=a, in1=b, op=op), r, w_)
            tt(tA[:], Z[:, 0, :, :], w[:, 0, :, :], ALU.mult, [BZ, Bw], [BtA])
            tt(tB[:], Z[:, 1, :, :], w[:, 1, :, :], ALU.mult, [BZ, Bw], [BtB])
            pt(X[:, 0, :, :], tA[:], tB[:], ALU.subtract, [BtA, BtB], [BX])
            tt(tC[:], Z[:, 0, :, :], w[:, 1, :, :], ALU.mult, [BZ, Bw], [BtC])
            tt(tD[:], Z[:, 1, :, :], w[:, 0, :, :], ALU.mult, [BZ, Bw], [BtD])
            pt(X[:, 1, :, :], tC[:], tD[:], ALU.add, [BtC, BtD], [BX])
            if ci == 0:
                T.dbg(c, "dbg_s5X", X[:], (128, 2, 8, L), BF16, BX)
            stash[ci] = (u_, Bu, X, BX, t0)

        def s5_b(ci):
            u_, Bu, X, BX, t0 = stash.pop(ci)
            for half in range(2):
                yp, Byp = Yps.next()
                for mm_ in range(4):
                    m = half * 4 + mm_
                    c.mm(yp[:, 0:L], Cm[:, 0, m, :], X[:, 0, m, :], mm_ == 0, False, [BCm, BX], [Byp])
                    c.mm(yp[:, 0:L], Cm[:, 1, m, :], X[:, 1, m, :], False, mm_ == 3, [BCm, BX], [Byp])
                dv(lambda: nc.vector.scalar_tensor_tensor(out=zt[:, half, :], in0=u_[:, half, :], scalar=dcl[:, half:half + 1],
                                                          in1=yp[:, 0:L], op0=ALU.mult, op1=ALU.add),
                   [Bu, Bdcl, Byp], [Bzt])
            tt(u2[:], zt[:], zt[:], ALU.mult, [Bzt], [Bu2])
            dv(lambda: nc.vector.tensor_scalar(out=u2[:], in0=u2[:], scalar1=0.044715, scalar2=1.0, op0=ALU.mult,
                                               op1=ALU.add), [Bu2], [Bu2])
            tt(u2[:], u2[:], zt[:], ALU.mult, [Bu2, Bzt], [Bu2])
            c.op("act", lambda: nc.scalar.activation(out=u2[:], in_=u2[:], func=AF.Sigmoid, scale=1.5957691216057308),
                 [Bu2], [Bu2])
            tt(hg[:], u2[:], zt[:], ALU.mult, [Bu2, Bzt], [Bhg])
            c.op("act", lambda: nc.scalar.copy(out=hgb[:], in_=hg[:]), [Bhg], [Bhgb])
            for h2 in range(2):
                gp, Bgp = Yps.next()
                for kt_ in range(2):
                    c.mm(gp[:, 0:L], gw[:, kt_, h2 * 128:(h2 + 1) * 128], hgb[:, kt_, :], kt_ == 0, kt_ == 1, [Bgw, Bhgb], [Bgp])
                c.op("act", lambda: nc.scalar.activation(out=sgt[:, h2, :], in_=gp[:, 0:L], func=AF.Sigmoid,
                                                         bias=gbc[:, h2:h2 + 1]), [Bgp, Bgbc], [Bsgt])
            tt(ot[:], hg[:], sgt[:], ALU.mult, [Bhg, Bsgt], [Bot])
            tp_, Btp = Tpf.next()
            for ts_ in range(L // 128):
                for h2 in range(2):
                    c.op("pe", lambda: nc.tensor.transpose(out=tp_[:, ts_ * 256 + h2 * 128:ts_ * 256 + (h2 + 1) * 128],
                                                           in_=ot[:, h2, ts_ * 128:(ts_ + 1) * 128], identity=identf[:]),
                         [Bot, Bidf], [Btp])
            om, Bom = otm.next()
            c.copy(om[:], tp_[:, 0:(L // 128) * 256].rearrange("p (a e) -> p a e", e=256), [Btp], [Bom])
            c.dma(T["mix"][t0:t0 + L, 768:1024].rearrange("(a p) e -> p a e", p=128), om[:], reads=[Bom],
                  writes=[T.B["mix"]])

        for ci in range(NCH + 1):
            if ci < NCH:
                s5_a(ci)
            if ci >= 1:
                s5_b(ci - 1)


def norm_tile(c, x_t, Bx, gB, BgB, h_, Bh, s_, Bs, jk, Bjk, ngroups=1):
    nc = c.nc
    Wd = D // ngroups
    for g in range(ngroups):
        gs = slice(g * Wd, (g + 1) * Wd)
        c.op("act", lambda: nc.scalar.activation(out=jk[:, gs], in_=x_t[:, gs], func=AF.Square,
                                                 accum_out=s_[:, g:g + 1]), [Bx], [Bjk, Bs])
    c.op("act", lambda: nc.scalar.activation(out=s_[:, 4:4 + ngroups], in_=s_[:, 0:ngroups], func=AF.Sqrt,
                                             bias=gB[:, D:D + 1], scale=1.0 / Wd), [Bs, BgB], [Bs])
    c.op("dve", lambda: nc.vector.reciprocal(out=s_[:, 4:4 + ngroups], in_=s_[:, 4:4 + ngroups]), [Bs], [Bs])
    for g in range(ngroups):
        gs = slice(g * Wd, (g + 1) * Wd)
        c.op("dve", lambda: nc.vector.scalar_tensor_tensor(out=h_[:, gs], in0=x_t[:, gs], scalar=s_[:, 4 + g:5 + g],
                                                           in1=gB[:, gs], op0=ALU.mult, op1=ALU.mult),
             [Bx, Bs, BgB], [Bh])


def phase_out(c, T, l):
    nc = c.nc
    x_d = T["xin%d" % l]
    Bx_d = T.B["xin%d" % l]
    with scope(c) as es:
        W, BW = sb(nc, es, "po_W", [128, 8, D], BF16)
        ident, Bid = sb(nc, es, "po_id", [128, 128], BF16)
        c.dma(ident[:], T["ident"][:, :], reads=[T.B["ident"]], writes=[Bid])
        gB, BgB = load_gB(c, es, T["out_norm_g"][l], T.B["out_norm_g"], "po_g")
        wv = T["w_out"][l].rearrange("(kc p) n -> p kc n", p=128)
        for kc in range(8):
            c.dma(W[:, kc, :], wv[:, kc, :], reads=[T.B["w_out"]], writes=[BW], q="pool")
        mt = Rot(nc, es, "po_mt", [128, D], F32, 4)
        xt = Rot(nc, es, "po_xt", [128, D], F32, 5)
        jkr = Rot(nc, es, "po_jk", [128, D], BF16, 2)
        hb = Rot(nc, es, "po_hb", [128, D], BF16, 2)
        ss = Rot(nc, es, "po_ss", [128, 8], F32, 4)
        tp = Rot(nc, es, "po_tp", [128, 1024], BF16, 2, psum=True)
        hTr = Rot(nc, es, "po_hT", [128, 8, 128], BF16, 3)
        acc = Rot(nc, es, "po_acc", [128, 512], F32, 2, psum=True)
        xo = Rot(nc, es, "po_xo", [128, D], F32, 2)
        PF = 2
        loaded = {}
        staged = {}

        def load(t):
            rows = slice(t * 128, (t + 1) * 128)
            m_, Bm = mt.next()
            c.dma(m_[:], T["mix"][rows, :], reads=[T.B["mix"]], writes=[Bm])
            x_, Bx = xt.next()
            c.dma(x_[:], x_d[rows, :], reads=[Bx_d], writes=[Bx])
            loaded[t] = (m_, Bm, x_, Bx)

        def stage_a(t):
            m_, Bm, x_, Bx = loaded.pop(t)
            jk, Bjk = jkr.next()
            s_, Bs = ss.next()
            h_, Bh = hb.next()
            norm_tile(c, m_, Bm, gB, BgB, h_, Bh, s_, Bs, jk, Bjk, ngroups=4)
            hT, BhT = hTr.next()
            for g in range(2):
                p_, Bp = tp.next()
                for j in range(4):
                    kc = g * 4 + j
                    c.op("pe", lambda: nc.tensor.transpose(out=p_[:, j * 128:(j + 1) * 128],
                                                           in_=h_[:, kc * 128:(kc + 1) * 128],
                                                           identity=ident[:]), [Bh, Bid], [Bp])
                c.copy(hT[:, g * 4:(g + 1) * 4, :], p_[:, 0:512].rearrange("p (j t) -> p j t", j=4), [Bp], [BhT])
            staged[t] = (hT, BhT, x_, Bx)

        def stage_c(t):
            rows = slice(t * 128, (t + 1) * 128)
            hT, BhT, x_, Bx = staged.pop(t)
            o_, Bo = xo.next()
            for dc in range(2):
                ds_ = slice(dc * 512, (dc + 1) * 512)
                a_, Ba = acc.next()
                for k in range(8):
                    c.mm(a_[:, :], hT[:, k, :], W[:, k, ds_], k == 0, k == 7, [BW, BhT], [Ba])
                c.op("dve", lambda: nc.vector.tensor_tensor(out=o_[:, ds_], in0=a_[:, :], in1=x_[:, ds_], op=ALU.add),
                     [Ba, Bx], [Bo])
            c.dma(T["xmid"][rows, :], o_[:], reads=[Bo], writes=[T.B["xmid"]])

        for t in range(min(PF, NT)):
            load(t)
        for t in range(NT + 1):
            if t + PF < NT:
                load(t + PF)
            if t < NT:
                stage_a(t)
            if t >= 1:
                stage_c(t - 1)
    c.barrier()


FFN_TB = 1024


def phase_ffn(c, T, l, out_name):
    nc = c.nc
    moe = (l % 2 == 1)
    i = l // 2
    if moe:
        F = D_FFE
        experts = [(T["moe_w_gate"][i, e], T["moe_w_up"][i, e], T["moe_w_down"][i, e]) for e in range(N_EXP)]
        BWs = (T.B["moe_w_gate"], T.B["moe_w_up"], T.B["moe_w_down"])
    else:
        F = D_FF
        experts = [(T["ffn_w_gate"][i], T["ffn_w_up"][i], T["ffn_w_down"][i])]
        BWs = (T.B["ffn_w_gate"], T.B["ffn_w_up"], T.B["ffn_w_down"])
    TB = FFN_TB
    NTB = TB // 128
    FC = 512
    chunks = [(f0, min(FC, F - f0)) for f0 in range(0, F, FC)]
    with scope(c) as es:
        ident, Bid = sb(nc, es, "ff_id", [128, 128], BF16)
        c.dma(ident[:], T["ident"][:, :], reads=[T.B["ident"]], writes=[Bid])
        gB, BgB = load_gB(c, es, T["norm2_g"][l], T.B["norm2_g"], "ff_g")
        if moe:
            Wr, BWr = sb(nc, es, "ff_Wr", [128, 8, N_EXP], BF16)
            c.dma(Wr[:], T["moe_router_w"][i].rearrange("(kc p) e -> p kc e", p=128), reads=[T.B["moe_router_w"]],
                  writes=[BWr], q="pool")
            rb, Brb = sb(nc, es, "ff_rb", [128, N_EXP], F32)
            c.dma(rb[:], T["moe_router_b"][i].partition_broadcast(128), reads=[T.B["moe_router_b"]], writes=[Brb])
            combs = [sb(nc, es, "ff_comb%d" % i_, [128, NTB, N_EXP], F32) for i_ in range(2)]
            lg = Rot(nc, es, "ff_lg", [128, N_EXP], F32, 2)
            mx = Rot(nc, es, "ff_mx", [128, 8], F32, 2)
            ex = Rot(nc, es, "ff_ex", [128, 2 * N_EXP], F32, 2)
        wg = Rot(nc, es, "ff_wg", [128, 8, FC], BF16, 2)
        wu = Rot(nc, es, "ff_wu", [128, 8, FC], BF16, 2)
        wd = Rot(nc, es, "ff_wd", [128, 4, D], BF16, 3)
        Gp = Rot(nc, es, "ff_G", [128, 512], F32, 2, psum=True)
        Up = Rot(nc, es, "ff_U", [128, 512], F32, 2, psum=True)
        Dp = Rot(nc, es, "ff_D", [128, 512], F32, 2, psum=True)
        sg = Rot(nc, es, "ff_sg", [128, 512], F32, 2)
        act = Rot(nc, es, "ff_act", [128, 4, TB], BF16, 3)
        hTs = [sb(nc, es, "ff_hT%d" % i_, [128, 8, TB], BF16) for i_ in range(2)]
        accs = [sb(nc, es, "ff_acc%d" % i_, [128, NTB, D], F32) for i_ in range(2)]
        npools = norm_pools(nc, es, "ffn_", need_xt=False)
        NB = S // TB

        def prep_steps(tb):
            hT, BhT = hTs[tb % 2]
            acc, Bacc = accs[tb % 2]
            steps = norm_transpose_steps(c, npools, T["xmid"], T.B["xmid"], gB, BgB, ident, Bid, hT, BhT, tb * NTB, NTB,
                                         keep_x=(acc, Bacc))
            if moe:
                comb, Bcomb = combs[tb % 2]
                held = {}

                def make_rm(it):
                    def rm():
                        p_, Bp = Dp.next()
                        for k in range(8):
                            c.mm(p_[:, 0:N_EXP], hT[:, k, it * 128:(it + 1) * 128], Wr[:, k, :], k == 0, k == 7,
                                 [BhT, BWr], [Bp])
                        held[it] = (p_, Bp)
                    return rm

                def make_rp(it):
                    def rp():
                        p_, Bp = held.pop(it)
                        l_, Bl = lg.next()
                        c.op("dve", lambda: nc.vector.tensor_tensor(out=l_[:], in0=p_[:, 0:N_EXP], in1=rb[:], op=ALU.add),
                             [Bp, Brb], [Bl])
                        m_, Bm = mx.next()
                        c.op("dve", lambda: nc.vector.max(out=m_[:, 0:8], in_=l_[:]), [Bl], [Bm])
                        e_, Be = ex.next()
                        c.op("dve", lambda: nc.vector.tensor_scalar(out=e_[:, 0:8], in0=l_[:], scalar1=m_[:, 0:1], scalar2=None,
                                                                    op0=ALU.subtract), [Bl, Bm], [Be])
                        c.op("act", lambda: nc.scalar.activation(out=e_[:, 0:8], in_=e_[:, 0:8], func=AF.Exp), [Be], [Be])
                        c.op("dve", lambda: nc.vector.scalar_tensor_tensor(out=e_[:, 8:16], in0=l_[:], scalar=m_[:, 1:2],
                                                                           in1=e_[:, 0:8], op0=ALU.is_ge, op1=ALU.mult),
                             [Bl, Bm, Be], [Be])
                        c.op("dve", lambda: nc.vector.tensor_reduce(out=m_[:, 2:3], in_=e_[:, 8:16], axis=AX.X, op=ALU.add),
                             [Be], [Bm])
                        c.op("dve", lambda: nc.vector.reciprocal(out=m_[:, 3:4], in_=m_[:, 2:3]), [Bm], [Bm])
                        c.op("dve", lambda: nc.vector.tensor_scalar(out=comb[:, it, :], in0=e_[:, 8:16], scalar1=m_[:, 3:4],
                                                                    scalar2=None, op0=ALU.mult), [Be, Bm], [Bcomb])
                    return rp

                steps.append(lambda: None)
                def make_r(it):
                    rm_, rp_ = make_rm(it), make_rp(it)

                    def r():
                        rm_()
                        rp_()
                    return r

                for it in range(NTB):
                    steps.append(make_r(it))
            return steps

        for st_ in prep_steps(0):
            st_()
        pending = []
        for tb in range(NB):
            hT, BhT = hTs[tb % 2]
            acc, Bacc = accs[tb % 2]
            if moe:
                comb, Bcomb = combs[tb % 2]
            n_chunk = 0

            def stage_down(ctx):
                e, a_, Ba, d_, Bd, nft, fw = ctx
                for it in range(NTB):
                    for dc in range(2):
                        ds_ = slice(dc * 512, (dc + 1) * 512)
                        Dd, BD = Dp.next()
                        for ft in range(nft):
                            M = min(128, fw - ft * 128)
                            c.mm(Dd[:, :], a_[0:M, ft, it * 128:(it + 1) * 128], d_[0:M, ft, ds_], ft == 0, ft == nft - 1,
                                 [Ba, Bd], [BD])
                        if moe:
                            c.op("dve", lambda: nc.vector.scalar_tensor_tensor(
                                out=acc[:, it, ds_], in0=Dd[:, :], scalar=comb[:, it, e:e + 1], in1=acc[:, it, ds_],
                                op0=ALU.mult, op1=ALU.add), [BD, Bcomb, Bacc], [Bacc])
                        else:
                            c.op("dve", lambda: nc.vector.tensor_tensor(out=acc[:, it, ds_], in0=Dd[:, :],
                                                                        in1=acc[:, it, ds_], op=ALU.add),
                                 [BD, Bacc], [Bacc])

            prev_ctx = None
            for e, (wg_d, wu_d, wd_d) in enumerate(experts):
                wgv = wg_d.rearrange("(kc p) f -> p kc f", p=128)
                wuv = wu_d.rearrange("(kc p) f -> p kc f", p=128)
                for (f0, fw) in chunks:
                    n_chunk += 1
                    if n_chunk == 2 and tb + 1 < NB:
                        pending = prep_steps(tb + 1)
                    nfull = fw // 128
                    rem = fw - nfull * 128
                    nft = nfull + (1 if rem else 0)
                    g_, Bg = wg.next()
                    c.dma(g_[:, :, 0:fw], wgv[:, :, f0:f0 + fw], reads=[BWs[0]], writes=[Bg], q="pool")
                    u_, Bu = wu.next()
                    c.dma(u_[:, :, 0:fw], wuv[:, :, f0:f0 + fw], reads=[BWs[1]], writes=[Bu], q="pool")
                    d_, Bd = wd.next()
                    if nfull:
                        c.dma(d_[:, 0:nfull, :], wd_d[f0:f0 + nfull * 128, :].rearrange("(ft p) d -> p ft d", p=128),
                              reads=[BWs[2]], writes=[Bd], q="pool")
                    if rem:
                        c.dma(d_[0:rem, nfull, :], wd_d[f0 + nfull * 128:f0 + fw, :], reads=[BWs[2]], writes=[Bd], q="pool")
                    a_, Ba = act.next()
                    n_inner = 0
                    for ft in range(nft):
                        M = min(128, fw - ft * 128)
                        fs = slice(ft * 128, ft * 128 + M)
                        for tt in range(TB // 512):
                            ts_ = slice(tt * 512, (tt + 1) * 512)
                            if pending:
                                pending.pop(0)()
                            G, BG = Gp.next()
                            for k in range(8):
                                c.mm(G[0:M, :], g_[:, k, fs], hT[:, k, ts_], k == 0, k == 7, [Bg, BhT], [BG])
                            U, BU = Up.next()
                            for k in range(8):
                                c.mm(U[0:M, :], u_[:, k, fs], hT[:, k, ts_], k == 0, k == 7, [Bu, BhT], [BU])
                            s_, Bs = sg.next()
                            c.op("act", lambda: nc.scalar.activation(out=s_[0:M, :], in_=G[0:M, :], func=AF.Silu), [BG], [Bs])
                            c.op("dve", lambda: nc.vector.tensor_tensor(out=a_[0:M, ft, ts_], in0=s_[0:M, :], in1=U[0:M, :],
                                                                        op=ALU.mult), [Bs, BU], [Ba])
                            n_inner += 1
                            if n_inner == 1 and prev_ctx is not None:
                                stage_down(prev_ctx)
                                prev_ctx = None
                    prev_ctx = (e, a_, Ba, d_, Bd, nft, fw)
            stage_down(prev_ctx)
            while pending:
                pending.pop(0)()
            c.dma(T[out_name][tb * TB:(tb + 1) * TB, :].rearrange("(i p) d -> p i d", p=128), acc[:], reads=[Bacc],
                  writes=[T.B[out_name]])
            c.barrier()
    c.barrier()


class Tensors:
    def __init__(self, nc, ext_in=(), ext_out=()):
        self.nc = nc
        self.t = {}
        self.B = {}
        self.ext_in = set(ext_in)
        self.ext_out = set(ext_out)
        self.in_names = []
        self.out_names = []

    def add(self, name, shape, dtype, kind=None):
        if kind is None:
            if name in self.ext_in:
                kind = "ExternalInput"
            elif name in self.ext_out:
                kind = "ExternalOutput"
            else:
                kind = "Internal"
        if kind == "ExternalInput":
            self.in_names.append(name)
        if kind == "ExternalOutput":
            self.out_names.append(name)
        self.t[name] = self.nc.dram_tensor(name, list(shape), dtype, kind=kind).ap()
        self.B[name] = Buf(name)
        return self.t[name]

    def __getitem__(self, k):
        return self.t[k]

    def dbg(self, c, name, ap, shape, dtype, Bsrc):
        if name not in self.ext_out:
            return
        d = self.add(name, shape, dtype, kind="ExternalOutput")
        c.dma(d, ap, reads=[Bsrc], writes=[self.B[name]])


PARAM_SHAPES = {
    "norm1_g": (2, 1024), "w_in": (2, 1024, 2460), "dil_qk_g": (2, 2, 64), "nsa_qk_g": (2, 4, 64),
    "nsa_cmp_pos": (2, 2, 32, 64), "nsa_cmp_w1": (2, 2, 2048, 256), "nsa_cmp_w2": (2, 2, 256, 64),
    "gla_wa2": (2, 16, 128), "gla_ba": (2, 128), "gla_norm_g": (2, 64),
    "s5_a_re": (2, 16, 64), "s5_a_im": (2, 16, 64), "s5_b_re": (2, 16, 64, 16), "s5_b_im": (2, 16, 64, 16),
    "s5_c_re": (2, 16, 16, 64), "s5_c_im": (2, 16, 16, 64), "s5_d": (2, 256), "s5_log_dt": (2, 16),
    "s5_glu_w": (2, 256, 256), "s5_glu_b": (2, 256), "out_norm_g": (2, 1024), "w_out": (2, 1024, 1024),
    "norm2_g": (2, 1024), "ffn_w_gate": (1, 1024, 2752), "ffn_w_up": (1, 1024, 2752),
    "ffn_w_down": (1, 2752, 1024), "moe_router_w": (1, 1024, 8), "moe_router_b": (1, 8),
    "moe_w_gate": (1, 8, 1024, 3584), "moe_w_up": (1, 8, 1024, 3584), "moe_w_down": (1, 8, 3584, 1024),
}


def host_consts():
    cst = {}
    bf = ml_dtypes.bfloat16
    cst["ident"] = np.eye(128, dtype=np.float32).astype(bf)
    p = np.arange(S)
    loc = np.arange(128).astype(np.float32)
    KBd = np.zeros((4, 36, 128), np.float32)
    for di in range(36):
        KBd[0, di] = 128.0 * (di - 32)
        KBd[1, di] = loc
        KBd[2, di] = 1.0
        KBd[3, di] = 1.0
    cst["c_KBd"] = KBd.astype(bf)
    QB = np.zeros((4, 3, 4, 128), np.float32)
    for pi, (win, d) in enumerate(DIL_PATTERNS):
        for h in range(4):
            a = DIL_SLOPES[h] * d
            QB[0, pi, h] = a
            QB[1, pi, h] = a
            QB[2, pi, h] = 0.0
            QB[3, pi, h] = -a * loc
    cst["c_QBdil"] = QB.astype(bf)
    kl = np.arange(128)[:, None]
    ql = np.arange(128)[None, :]
    masks = np.stack([(kl <= ql), (kl >= ql), (kl > ql)], axis=1).astype(np.float32)
    cst["c_masks"] = ((masks - 1.0) * 30000.0).astype(bf)
    cst["c_mask01"] = (kl <= ql).astype(np.float32).astype(bf)
    KBS = np.zeros((68, S), np.float32)
    KBS[p // 64, p] = 32768.0
    KBS[64] = 128.0 * (p // 128)
    KBS[65] = p % 128
    KBS[66] = 1.0
    KBS[67] = 1.0
    cst["c_KBS"] = KBS.astype(bf)
    QBa = np.zeros((4, 4, S), np.float32)
    for h in range(4):
        a = NSA_SLOPES[h]
        QBa[0, h] = a
        QBa[1, h] = a
        QBa[2, h] = -a * 128.0 * (p // 128)
        QBa[3, h] = -a * (p % 128)
    cst["c_QBabs"] = QBa.astype(bf)
    KBc = np.zeros((4, 32, 128), np.float32)
    for m in range(32):
        KBc[0, m] = -128.0 * m
        KBc[1, m] = 16.0 * loc
        KBc[2, m] = 31.0
        KBc[3, m] = 1.0
    cst["c_KBc"] = KBc.astype(bf)
    QBc = np.zeros((4, 4, 128), np.float32)
    for h in range(4):
        a = NSA_SLOPES[h]
        QBc[0, h] = a
        QBc[1, h] = a
        QBc[2, h] = a
        QBc[3, h] = -a * loc
    cst["c_QBc"] = QBc.astype(bf)
    cm = np.zeros((128, 17, 128), np.float32)
    for m in range(17):
        valid = (ql - 16 * kl) >= (31 - 128 * m)
        cm[:, m, :] = np.where(valid, 0.0, -30000.0)
    cst["c_cmask"] = cm.astype(bf)
    cc = np.arange(256)
    jj = np.arange(64)
    cover = ((16 * cc[:, None] < 64 * jj[None, :] + 64) & (16 * cc[:, None] + 32 > 64 * jj[None, :])).astype(np.float32)
    cover[255] = 0.0
    cst["c_cover"] = np.ascontiguousarray(cover.reshape(2, 128, 64).transpose(1, 0, 2)).astype(bf)
    tt = (128 * np.arange(32)[None, :, None] + np.arange(128)[:, None, None])
    cur = tt // 64
    j3 = jj[None, None, :]
    forced = (j3 == 0) | (j3 == cur) | (j3 == cur - 1)
    adjv = np.where(j3 <= cur, np.where(forced, 1.0e4, 0.0), -1.0e30).astype(np.float32)
    cst["c_adj"] = np.ascontiguousarray(adjv)
    cst["identf"] = np.eye(128, dtype=np.float32)
    K2s = np.zeros((64, S), np.float32)
    K2s[p // 64, p] = 1.0
    cst["c_K2sel"] = K2s.astype(bf)
    K2w = np.zeros((64, S), np.float32)
    K2w[p // 128, p] = 1.0
    cst["c_K2win"] = K2w.astype(bf)
    QW = np.zeros((64, 4, S), np.float32)
    for h in range(4):
        QW[0:32, h, :] = NSA_SLOPES[h] * 128.0 * (np.arange(32)[:, None] - (p // 128)[None, :])
    cst["c_QW"] = QW.astype(bf)
    As = np.zeros((128, 32, 4), np.float32)
    for h in range(4):
        As[64:128, :, h] = NSA_SLOPES[h] * 64.0 * (np.arange(64)[:, None] - 2 * np.arange(32)[None, :] - 1) + 30000.0
    cst["c_Asel"] = As
    Ev = np.zeros((128, 2, 4), np.float64)
    for h in range(4):
        Ev[:, 0, h] = np.exp(NSA_SLOPES[h] * (np.arange(128) % 64))
        Ev[:, 1, h] = np.exp(NSA_SLOPES[h] * np.arange(128))
    cst["c_Ev"] = Ev.astype(np.float32)
    pp_ = np.arange(128)[:, None, None]
    mm_ = np.arange(8)[None, :, None]
    ch_ = np.arange(128)[None, None, :]
    cst["c_s5mask"] = ((ch_ // 16) == (2 * (mm_ % 4) + pp_ // 64)).astype(np.float32)
    return cst


CONST_SPECS = {"ident": ((128, 128), BF16), "c_KBd": ((4, 36, 128), BF16), "c_QBdil": ((4, 3, 4, 128), BF16),
               "c_masks": ((128, 3, 128), BF16), "c_KBS": ((68, S), BF16), "c_QBabs": ((4, 4, S), BF16),
               "c_KBc": ((4, 32, 128), BF16), "c_QBc": ((4, 4, 128), BF16), "c_cmask": ((128, 17, 128), BF16),
               "c_cover": ((128, 2, 64), BF16), "c_mask01": ((128, 128), BF16), "c_adj": ((128, 32, 64), F32),
               "identf": ((128, 128), F32), "c_s5mask": ((128, 8, 128), F32),
               "c_K2sel": ((64, S), BF16), "c_K2win": ((64, S), BF16), "c_QW": ((64, 4, S), BF16),
               "c_Asel": ((128, 32, 4), F32), "c_Ev": ((128, 2, 4), F32)}


def build(phases=("proj",), layers=(0, 1), ext_in=(), ext_out=()):
    nc = bass.Bass("TRN2", target_bir_lowering=False)
    _UID[0] = 0
    T = Tensors(nc, ext_in, ext_out)
    T.add("xin0", (S, D), F32, kind="ExternalInput")
    for n, shp in PARAM_SHAPES.items():
        T.add(n, shp, F32, kind="ExternalInput")
    for n, (shp, dt_) in CONST_SPECS.items():
        T.add(n, shp, dt_, kind="ExternalInput")
    T.add("projT", (PW, S), BF16)
    T.add("proj", (S, PW), BF16)
    T.add("xin1", (S, D), F32)
    T.add("dacc", (3, S, 264), F32)
    T.add("mix", (S, D), F32)
    T.add("xmid", (S, D), F32)
    T.add("xin2", (S, D), F32, kind="ExternalOutput")
    c = Ctx(nc)
    for l in layers:
        if "proj" in phases:
            phase_proj(c, T, l)
        if "dil" in phases:
            phase_dil(c, T, l)
        if "nsa" in phases:
            phase_nsa(c, T, l)
        if "gla" in phases:
            phase_gla(c, T, l)
        if "s5" in phases:
            phase_s5(c, T, l)
        if "out" in phases:
            phase_out(c, T, l)
        if "ffn" in phases:
            phase_ffn(c, T, l, "xin%d" % (l + 1))
    c.barrier()
    return nc, T


ALL_PHASES = ("proj", "dil", "nsa", "gla", "s5", "out", "ffn")


def kernel(**inputs):
    nc, T = build(phases=ALL_PHASES, layers=(0, 1))
    cst = host_consts()
    x = np.asarray(inputs["x"], dtype=np.float32)
    shared = {}
    for n in T.in_names:
        if n == "xin0":
            continue
        if n in cst:
            shared[n] = cst[n]
        else:
            shared[n] = np.ascontiguousarray(np.asarray(inputs[n], dtype=np.float32))
    maps = []
    for b in range(8):
        m = dict(shared)
        m["xin0"] = np.ascontiguousarray(x[b])
        maps.append(m)
    res = run_bass_kernel_spmd(nc, maps, core_ids=list(range(8)))
    return np.stack([np.asarray(r["xin2"], dtype=np.float32) for r in res.results], axis=0)
```
